# Optimizing a Trainium2 kernel written in Bass

```python
import math
import jax, jax.numpy as jnp
from jax import lax
import numpy as np

D_MODEL = 1024
BATCH = 8
SEQ = 4096
DEPTH = 2
DEC_BATCH = 2
DEC_SEQ = 16384
PAST_LEN = 128

HEAD_DIM = 64
S5_WIDTH = 384
S5_GROUP = 16
S5_GROUPS = S5_WIDTH // S5_GROUP
S5_STATE = 64
S5_DT_MIN = 1e-3
S5_DT_MAX = 1e-1
CONV_WIDTH = 384
CONV_K = 3
DIL_PATTERNS = ((128, 1), (512, 4), (2048, 16))
DIL_N_GROUPS = 3
DIL_HEADS = 2
DIL_WIDTH = DIL_N_GROUPS * DIL_HEADS * HEAD_DIM
DIL_OUT = DIL_HEADS * HEAD_DIM
DIL_BLOCK = 64
SWA_Q_HEADS = 6
SWA_KV_HEADS = 2
SWA_GROUP = SWA_Q_HEADS // SWA_KV_HEADS
SWA_WINDOW = 128
SWA_BLOCK = 128
SWA_Q_WIDTH = SWA_Q_HEADS * HEAD_DIM
SWA_KV_WIDTH = SWA_KV_HEADS * HEAD_DIM
N_ATTN_HEADS = SWA_Q_HEADS + DIL_N_GROUPS * DIL_HEADS
N_BRANCHES = 4
IN_SIZES = (S5_WIDTH, CONV_WIDTH, CONV_WIDTH, CONV_WIDTH, DIL_WIDTH, DIL_WIDTH, DIL_WIDTH,
            SWA_Q_WIDTH, SWA_KV_WIDTH, SWA_KV_WIDTH, N_BRANCHES * D_MODEL)
IN_COLS = 7424
N_EXPERT_GROUPS = 4
EXPERTS_PER_GROUP = 4
N_EXPERTS = N_EXPERT_GROUPS * EXPERTS_PER_GROUP
TOP_K = 2
D_EXPERT = 256
ALPHA = (2 * DEPTH) ** 0.25
BETA = (8 * DEPTH) ** -0.25
LN_EPS = 1e-5
MASK_VALUE = -1e30

kernel_name = "hybrid_bidir_encoder_s5_conv_dilattn_swa_hmoe"


def layer_norm(x, g, b):
    xf = x.astype(jnp.float32)
    mu = jnp.mean(xf, axis=-1, keepdims=True)
    var = jnp.mean(jnp.square(xf - mu), axis=-1, keepdims=True)
    y = (xf - mu) * lax.rsqrt(var + LN_EPS) * g.astype(jnp.float32) + b.astype(jnp.float32)
    return y.astype(x.dtype)


def alibi_slopes(n):
    return jnp.exp2(-8.0 * jnp.arange(1, n + 1, dtype=jnp.float32) / n)


def _linear_recurrence_combine(e1, e2):
    a1, b1 = e1
    a2, b2 = e2
    return (a1 * a2, a2 * b1 + b2)


def s5_direction(ug, a_re, a_im, log_dt, b_re, b_im, c_re, c_im, reverse):
    f32 = jnp.float32
    lam = lax.complex(a_re.astype(f32), a_im.astype(f32))
    dt = jnp.exp(log_dt.astype(f32))[:, None]
    lam_bar = jnp.exp(lam * dt)
    b_bar = ((lam_bar - 1.0) / lam)[:, :, None] * lax.complex(b_re.astype(f32), b_im.astype(f32))
    bu = jnp.einsum('blgh,gph->blgp', ug, b_bar)
    a = jnp.broadcast_to(lam_bar, (1, ug.shape[1]) + lam_bar.shape)
    _, states = lax.associative_scan(_linear_recurrence_combine, (a, bu), axis=1, reverse=reverse)
    c = lax.complex(c_re.astype(f32), c_im.astype(f32))
    return jnp.einsum('blgp,ghp->blgh', states, c).real


def s5_branch(u, p, l):
    b, seq, _ = u.shape
    ug = u.astype(jnp.float32).reshape(b, seq, S5_GROUPS, S5_GROUP)
    y_fwd = s5_direction(ug, p['s5_a_re'][l, 0], p['s5_a_im'][l, 0], p['s5_log_dt'][l, 0],
                         p['s5_b_re'][l, 0], p['s5_b_im'][l, 0], p['s5_c_re'][l, 0], p['s5_c_im'][l, 0], False)
    y_bwd = s5_direction(ug, p['s5_a_re'][l, 1], p['s5_a_im'][l, 1], p['s5_log_dt'][l, 1],
                         p['s5_b_re'][l, 1], p['s5_b_im'][l, 1], p['s5_c_re'][l, 1], p['s5_c_im'][l, 1], True)
    y = (y_fwd + y_bwd).reshape(b, seq, S5_WIDTH) + p['s5_d'][l].astype(jnp.float32) * u.astype(jnp.float32)
    z = jax.nn.gelu(y)
    gate = jax.nn.sigmoid(jnp.einsum('blc,ce->ble', z, p['s5_glu_w'][l].astype(jnp.float32))
                          + p['s5_glu_b'][l].astype(jnp.float32))
    return (z * gate).astype(u.dtype)


def short_conv(z, w, bias):
    seq = z.shape[1]
    half = CONV_K // 2
    zp = jnp.pad(z, ((0, 0), (half, half), (0, 0)))
    return sum(zp[:, i:i + seq] * w[i] for i in range(CONV_K)) + bias


def band_attention(q, k, v, half_window, block, slopes, dist_unit, sink=None):
    n, seq, hkv, grp, dh = q.shape
    nb = -(-seq // block)
    pad = nb * block - seq
    qb = jnp.pad(q, ((0, 0), (0, pad), (0, 0), (0, 0), (0, 0))).reshape(n, nb, block, hkv, grp, dh)

    def kv_blocks(t):
        tp = jnp.pad(t, ((0, 0), (block, block + pad), (0, 0), (0, 0))).reshape(n, nb + 2, block, hkv, dh)
        return jnp.concatenate([tp[:, :-2], tp[:, 1:-1], tp[:, 2:]], axis=2)

    kb, vb = kv_blocks(k), kv_blocks(v)
    start = jnp.arange(nb)[:, None] * block
    qpos = start + jnp.arange(block)[None, :]
    kpos = start - block + jnp.arange(3 * block)[None, :]
    dist = jnp.abs(qpos[:, :, None] - kpos[:, None, :])
    valid = (dist <= half_window) & (kpos[:, None, :] >= 0) & (kpos[:, None, :] < seq)
    scores = jnp.einsum('nbqhgd,nbkhd->nbhgqk', qb, kb, preferred_element_type=jnp.float32) * (dh ** -0.5)
    bias = -(slopes.astype(jnp.float32)[None, :, :, None, None]
             * (dist * dist_unit).astype(jnp.float32)[:, None, None, :, :])
    scores = jnp.where(valid[:, None, None], scores + bias, MASK_VALUE)
    m = jnp.max(scores, axis=-1, keepdims=True)
    if sink is not None:
        sink_l = sink.astype(jnp.float32)[None, None, :, :, None, None]
        m = jnp.maximum(m, sink_l)
    probs = jnp.exp(scores - m)
    denom = jnp.sum(probs, axis=-1, keepdims=True)
    if sink is not None:
        denom = denom + jnp.exp(sink_l - m)
    lse = (m + jnp.log(denom))[..., 0]
    out = jnp.einsum('nbhgqk,nbkhd->nbqhgd', probs / denom, vb.astype(jnp.float32))
    out = out.reshape(n, nb * block, hkv, grp, dh)[:, :seq].astype(q.dtype)
    lse = jnp.transpose(lse, (0, 1, 4, 2, 3)).reshape(n, nb * block, hkv, grp)[:, :seq]
    return out, lse


def dilated_attention(q, k, v, slopes):
    b, seq = q.shape[:2]
    outs, lses = [], []
    for g, (window, dil) in enumerate(DIL_PATTERNS):
        half = window // (2 * dil)
        sub = seq // dil

        def to_sub(t):
            t = t.reshape((b, sub, dil) + t.shape[2:])
            return jnp.moveaxis(t, 2, 1).reshape((b * dil, sub) + t.shape[3:])

        def from_sub(t):
            t = t.reshape((b, dil, sub) + t.shape[2:])
            return jnp.moveaxis(t, 1, 2).reshape((b, seq) + t.shape[3:])

        o, lse = band_attention(to_sub(q[:, :, g])[:, :, :, None], to_sub(k[:, :, g]), to_sub(v[:, :, g]),
                                half, DIL_BLOCK, slopes[g][:, None], dil)
        outs.append(from_sub(o[:, :, :, 0]))
        lses.append(from_sub(lse[:, :, :, 0]))
    o = jnp.stack(outs).astype(jnp.float32)
    w = jax.nn.softmax(jnp.stack(lses), axis=0)
    return jnp.sum(w[..., None] * o, axis=0).reshape(b, seq, DIL_OUT).astype(q.dtype)


def token_mixer(h, p, l):
    b, seq, _ = h.shape
    z = jnp.einsum('bld,dc->blc', h, p['w_in'][l])
    offsets = np.cumsum(IN_SIZES)[:-1].tolist()
    (u_a, v_b, gate_b, gate_c, q_c, k_c, v_c, q_d, k_d, v_d, g_logits) = jnp.split(z, offsets, axis=-1)
    slopes = alibi_slopes(N_ATTN_HEADS)
    slopes_d = slopes[:SWA_Q_HEADS].reshape(SWA_KV_HEADS, SWA_GROUP)
    slopes_c = slopes[SWA_Q_HEADS:].reshape(DIL_N_GROUPS, DIL_HEADS)
    y_a = jnp.einsum('blc,cd->bld', s5_branch(u_a, p, l), p['w_branch_a'][l])
    y_b = gate_b * short_conv(gate_c * v_b, p['conv_w'][l], p['conv_b'][l])
    y_b = jnp.einsum('blc,cd->bld', y_b, p['w_branch_b'][l])
    dil_shape = (b, seq, DIL_N_GROUPS, DIL_HEADS, HEAD_DIM)
    y_c = dilated_attention(q_c.reshape(dil_shape), k_c.reshape(dil_shape), v_c.reshape(dil_shape), slopes_c)
    y_c = jnp.einsum('blc,cd->bld', y_c, p['w_branch_c'][l])
    o_d, _ = band_attention(q_d.reshape(b, seq, SWA_KV_HEADS, SWA_GROUP, HEAD_DIM),
                            k_d.reshape(b, seq, SWA_KV_HEADS, HEAD_DIM),
                            v_d.reshape(b, seq, SWA_KV_HEADS, HEAD_DIM),
                            SWA_WINDOW, SWA_BLOCK, slopes_d, 1,
                            sink=p['swa_sink'][l].reshape(SWA_KV_HEADS, SWA_GROUP))
    y_d = jnp.einsum('blc,cd->bld', o_d.reshape(b, seq, SWA_Q_WIDTH), p['w_branch_d'][l])
    gates = jax.nn.sigmoid(g_logits.reshape(b, seq, N_BRANCHES, D_MODEL))
    branches = jnp.stack([y_a, y_b, y_c, y_d], axis=2)
    merged = jnp.sum(gates * branches, axis=2)
    return jnp.einsum('bld,de->ble', merged, p['w_o'][l])


def hier_moe(h, p, l):
    b, seq, _ = h.shape
    f32 = jnp.float32
    grp_logits = jnp.einsum('bld,dg->blg', h, p['router_group_w'][l], preferred_element_type=f32) \
        + p['router_group_b'][l].astype(f32)
    p_grp = jax.nn.softmax(grp_logits, axis=-1)
    g_val, g_idx = lax.top_k(p_grp, 1)
    exp_logits = (jnp.einsum('bld,de->ble', h, p['router_expert_w'][l], preferred_element_type=f32)
                  + p['router_expert_b'][l].astype(f32)).reshape(b, seq, N_EXPERT_GROUPS, EXPERTS_PER_GROUP)
    grp_onehot = jax.nn.one_hot(g_idx[..., 0], N_EXPERT_GROUPS, dtype=f32)
    within = jnp.sum(exp_logits * grp_onehot[..., None], axis=2)
    e_val, e_idx = lax.top_k(within, TOP_K)
    e_w = jax.nn.softmax(e_val, axis=-1) * g_val
    global_idx = g_idx * EXPERTS_PER_GROUP + e_idx
    combine = jnp.sum(jax.nn.one_hot(global_idx, N_EXPERTS, dtype=f32) * e_w[..., None], axis=2)
    hg = jnp.einsum('bld,edf->blef', h, p['expert_w_gate'][l])
    hu = jnp.einsum('bld,edf->blef', h, p['expert_w_up'][l])
    act = jax.nn.silu(hg) * hu * combine.astype(h.dtype)[..., None]
    return jnp.einsum('blef,efd->bld', act, p['expert_w_down'][l])


def trunk(x, p):
    h = layer_norm(x, p['ln_in_g'], p['ln_in_b'])
    for l in range(DEPTH):
        h = layer_norm(ALPHA * h + token_mixer(h, p, l), p['ln1_g'][l], p['ln1_b'][l])
        h = layer_norm(ALPHA * h + hier_moe(h, p, l), p['ln2_g'][l], p['ln2_b'][l])
    return h


def setup_inputs(seed: int = 0) -> dict:
    key = jax.random.key(seed)
    ks = jax.random.split(key, 40)
    nrm = lambda i, shape: jax.random.normal(ks[i], shape, jnp.float32)
    P, G, H = S5_STATE, S5_GROUPS, S5_GROUP
    inp = {}
    inp['x_prompt'] = nrm(0, (BATCH, SEQ, D_MODEL))
    inp['x_sample'] = nrm(1, (DEC_BATCH, DEC_SEQ, D_MODEL))
    inp['ln_in_g'] = 1.0 + 0.01 * nrm(2, (D_MODEL,))
    inp['ln_in_b'] = 0.01 * nrm(3, (D_MODEL,))
    inp['w_in'] = nrm(4, (DEPTH, D_MODEL, IN_COLS)) * D_MODEL ** -0.5
    inp['s5_a_re'] = -0.5 + 0.01 * nrm(5, (DEPTH, 2, G, P))
    inp['s5_a_im'] = math.pi * jnp.arange(P, dtype=jnp.float32) + 0.01 * nrm(6, (DEPTH, 2, G, P))
    inp['s5_log_dt'] = jax.random.uniform(ks[7], (DEPTH, 2, G), jnp.float32,
                                          minval=math.log(S5_DT_MIN), maxval=math.log(S5_DT_MAX))
    inp['s5_b_re'] = nrm(8, (DEPTH, 2, G, P, H)) * (2 * H) ** -0.5
    inp['s5_b_im'] = nrm(9, (DEPTH, 2, G, P, H)) * (2 * H) ** -0.5
    inp['s5_c_re'] = nrm(10, (DEPTH, 2, G, H, P)) * P ** -0.5
    inp['s5_c_im'] = nrm(11, (DEPTH, 2, G, H, P)) * P ** -0.5
    inp['s5_d'] = nrm(12, (DEPTH, S5_WIDTH))
    inp['s5_glu_w'] = nrm(13, (DEPTH, S5_WIDTH, S5_WIDTH)) * S5_WIDTH ** -0.5
    inp['s5_glu_b'] = 0.01 * nrm(14, (DEPTH, S5_WIDTH))
    inp['conv_w'] = nrm(15, (DEPTH, CONV_K, CONV_WIDTH)) * CONV_K ** -0.5
    inp['conv_b'] = 0.01 * nrm(16, (DEPTH, CONV_WIDTH))
    inp['swa_sink'] = nrm(17, (DEPTH, SWA_Q_HEADS))
    inp['w_branch_a'] = nrm(18, (DEPTH, S5_WIDTH, D_MODEL)) * S5_WIDTH ** -0.5
    inp['w_branch_b'] = nrm(19, (DEPTH, CONV_WIDTH, D_MODEL)) * CONV_WIDTH ** -0.5
    inp['w_branch_c'] = nrm(20, (DEPTH, DIL_OUT, D_MODEL)) * DIL_OUT ** -0.5
    inp['w_branch_d'] = nrm(21, (DEPTH, SWA_Q_WIDTH, D_MODEL)) * SWA_Q_WIDTH ** -0.5
    inp['w_o'] = nrm(22, (DEPTH, D_MODEL, D_MODEL)) * (D_MODEL ** -0.5 * BETA)
    inp['ln1_g'] = 1.0 + 0.01 * nrm(23, (DEPTH, D_MODEL))
    inp['ln1_b'] = 0.01 * nrm(24, (DEPTH, D_MODEL))
    inp['router_group_w'] = nrm(25, (DEPTH, D_MODEL, N_EXPERT_GROUPS)) * D_MODEL ** -0.5
    inp['router_group_b'] = 0.01 * nrm(26, (DEPTH, N_EXPERT_GROUPS))
    inp['router_expert_w'] = nrm(27, (DEPTH, D_MODEL, N_EXPERTS)) * D_MODEL ** -0.5
    inp['router_expert_b'] = 0.01 * nrm(28, (DEPTH, N_EXPERTS))
    inp['expert_w_gate'] = nrm(29, (DEPTH, N_EXPERTS, D_MODEL, D_EXPERT)) * D_MODEL ** -0.5
    inp['expert_w_up'] = nrm(30, (DEPTH, N_EXPERTS, D_MODEL, D_EXPERT)) * D_MODEL ** -0.5
    inp['expert_w_down'] = nrm(31, (DEPTH, N_EXPERTS, D_EXPERT, D_MODEL)) * (D_EXPERT ** -0.5 * BETA)
    inp['ln2_g'] = 1.0 + 0.01 * nrm(32, (DEPTH, D_MODEL))
    inp['ln2_b'] = 0.01 * nrm(33, (DEPTH, D_MODEL))
    return inp


def reference(x_prompt, x_sample, ln_in_g, ln_in_b, w_in, s5_a_re, s5_a_im, s5_log_dt, s5_b_re, s5_b_im,
              s5_c_re, s5_c_im, s5_d, s5_glu_w, s5_glu_b, conv_w, conv_b, swa_sink, w_branch_a, w_branch_b,
              w_branch_c, w_branch_d, w_o, ln1_g, ln1_b, router_group_w, router_group_b, router_expert_w,
              router_expert_b, expert_w_gate, expert_w_up, expert_w_down, ln2_g, ln2_b):
    p = dict(ln_in_g=ln_in_g, ln_in_b=ln_in_b, w_in=w_in, s5_a_re=s5_a_re, s5_a_im=s5_a_im,
             s5_log_dt=s5_log_dt, s5_b_re=s5_b_re, s5_b_im=s5_b_im, s5_c_re=s5_c_re, s5_c_im=s5_c_im,
             s5_d=s5_d, s5_glu_w=s5_glu_w, s5_glu_b=s5_glu_b, conv_w=conv_w, conv_b=conv_b,
             swa_sink=swa_sink, w_branch_a=w_branch_a, w_branch_b=w_branch_b, w_branch_c=w_branch_c,
             w_branch_d=w_branch_d, w_o=w_o, ln1_g=ln1_g, ln1_b=ln1_b, router_group_w=router_group_w,
             router_group_b=router_group_b, router_expert_w=router_expert_w, router_expert_b=router_expert_b,
             expert_w_gate=expert_w_gate, expert_w_up=expert_w_up, expert_w_down=expert_w_down,
             ln2_g=ln2_g, ln2_b=ln2_b)
    y_prompt = trunk(x_prompt, p)
    y_sample = trunk(x_sample, p)
    return (y_prompt, y_sample)
```

```python
import math
import numpy as np
from contextlib import ExitStack
import concourse.bass as bass
import concourse.mybir as mybir
from concourse.bass_utils import run_bass_kernel_spmd

F32 = mybir.dt.float32
BF16 = mybir.dt.bfloat16
AF = mybir.ActivationFunctionType
ALU = mybir.AluOpType
AX = mybir.AxisListType

D = 1024
NSEG = 4
SEG = 4096
NTOK = NSEG * SEG
PAD = 1024
SEGP = SEG + 2 * PAD
NTOKP = NSEG * SEGP
ST = 2048
NST = NTOK // ST
DEPTH = 2
ALPHA = (2 * DEPTH) ** 0.25
LN_EPS = 1e-5
IN_COLS = 7424
SLOPES = [2.0 ** (-8.0 * (i + 1) / 12) for i in range(12)]
DIL = [(128, 1), (512, 4), (2048, 16)]

WNAMES = ["ln_in_g", "ln_in_b", "w_in", "s5_a_re", "s5_a_im", "s5_log_dt", "s5_b_re", "s5_b_im", "s5_c_re",
          "s5_c_im", "s5_d", "s5_glu_w", "s5_glu_b", "conv_w", "conv_b", "swa_sink", "w_branch_a", "w_branch_b",
          "w_branch_c", "w_branch_d", "w_o", "ln1_g", "ln1_b", "router_group_w", "router_group_b",
          "router_expert_w", "router_expert_b", "expert_w_gate", "expert_w_up", "expert_w_down", "ln2_g", "ln2_b"]
WSHAPES = {
    "ln_in_g": [D], "ln_in_b": [D], "w_in": [2, D, IN_COLS], "s5_a_re": [2, 2, 24, 64], "s5_a_im": [2, 2, 24, 64],
    "s5_log_dt": [2, 2, 24], "s5_b_re": [2, 2, 24, 64, 16], "s5_b_im": [2, 2, 24, 64, 16],
    "s5_c_re": [2, 2, 24, 16, 64], "s5_c_im": [2, 2, 24, 16, 64], "s5_d": [2, 384], "s5_glu_w": [2, 384, 384],
    "s5_glu_b": [2, 384], "conv_w": [2, 3, 384], "conv_b": [2, 384], "swa_sink": [2, 6],
    "w_branch_a": [2, 384, D], "w_branch_b": [2, 384, D], "w_branch_c": [2, 128, D], "w_branch_d": [2, 384, D],
    "w_o": [2, D, D], "ln1_g": [2, D], "ln1_b": [2, D], "router_group_w": [2, D, 4], "router_group_b": [2, 4],
    "router_expert_w": [2, D, 16], "router_expert_b": [2, 16], "expert_w_gate": [2, 16, D, 256],
    "expert_w_up": [2, 16, D, 256], "expert_w_down": [2, 16, 256, D], "ln2_g": [2, D], "ln2_b": [2, D],
}


ENGS = ["sync", "scalar", "vector", "gpsimd", "tensor"]
SEM_ROLL = 30000


class Res:
    __slots__ = ("name", "w", "r")

    def __init__(self, name):
        self.name = name
        self.w = None
        self.r = []


class FW:
    def __init__(self, nc, es):
        self.nc = nc
        self.es = es
        self.q = {e: [] for e in ENGS}
        self.sems = {e: [es.enter_context(nc.semaphore("s_" + e + "0"))] for e in ENGS}
        self.cnt = {e: 0 for e in ENGS}
        self.seen = {e: {} for e in ENGS}
        self.dma_sems = [es.enter_context(nc.semaphore("d%d" % i)) for i in range(24)]
        self.dma_cnt = [0] * 24
        self.dma_i = 0
        self.n_ops = 0
        self.fence = []

    def barrier(self):
        f = []
        for e in ENGS:
            if self.cnt[e] > 0:
                f.append((self.sems[e][-1], self.cnt[e], e))
        for k in range(len(self.dma_sems)):
            if self.dma_cnt[k] > 0:
                f.append((self.dma_sems[k], self.dma_cnt[k], "dma"))
        self.fence = f

    def _ev_new(self, eng):
        if self.cnt[eng] >= SEM_ROLL:
            self.sems[eng].append(self.es.enter_context(self.nc.semaphore("s_%s%d" % (eng, len(self.sems[eng])))))
            self.cnt[eng] = 0
        self.cnt[eng] += 1
        return (self.sems[eng][-1], self.cnt[eng], eng)

    def _need(self, eng, ev, waits, pe_ok=False):
        if ev is None:
            return
        sem, val, src = ev
        if pe_ok and src == "tensor" and eng == "tensor":
            return
        key = id(sem)
        if self.seen[eng].get(key, 0) >= val:
            return
        if key not in waits or waits[key][1] < val:
            waits[key] = (sem, val)

    def op(self, eng, fn, reads=(), writes=(), pe_acc=False):
        waits = {}
        for ev in self.fence:
            self._need(eng, ev, waits)
        for r in reads:
            self._need(eng, r.w, waits)
        for w in writes:
            self._need(eng, w.w, waits, pe_ok=pe_acc)
            for ev in w.r:
                self._need(eng, ev, waits)
        for key, (sem, val) in waits.items():
            self.seen[eng][key] = val
        ev = self._ev_new(eng)
        self.q[eng].append((list(waits.values()), fn, (ev[0], 1)))
        for r in reads:
            r.r.append(ev)
        for w in writes:
            w.w = ev
            w.r = []
        self.n_ops += 1
        return ev

    def dma(self, eng, fn, reads=(), writes=()):
        waits = {}
        for ev in self.fence:
            self._need(eng, ev, waits)
        for r in reads:
            self._need(eng, r.w, waits)
        for w in writes:
            self._need(eng, w.w, waits)
            for ev in w.r:
                self._need(eng, ev, waits)
        k = self.dma_i % len(self.dma_sems)
        self.dma_i += 1
        sem = self.dma_sems[k]
        if self.dma_cnt[k] > 0:
            self._need(eng, (sem, self.dma_cnt[k], "dma"), waits)
        for key, (s, val) in waits.items():
            self.seen[eng][key] = val
        self.dma_cnt[k] += 16
        ev = (sem, self.dma_cnt[k], "dma")
        self.q[eng].append((list(waits.values()), fn, (sem, 16)))
        for r in reads:
            r.r.append(ev)
        for w in writes:
            w.w = ev
            w.r = []
        self.n_ops += 1
        return ev

    def finish(self, final_res):
        waits = {}
        for r in final_res:
            self._need("sync", r.w, waits)
        for k in range(len(self.dma_sems)):
            if self.dma_cnt[k] > 0:
                self._need("sync", (self.dma_sems[k], self.dma_cnt[k], "dma"), waits)
        tail = list(waits.values())
        q = self.q
        with self.nc.Block() as block:
            def replay(e, name):
                for ws, fn, inc in q[name]:
                    for sem, val in ws:
                        e.wait_ge(sem, val)
                    fn(e).then_inc(inc[0], inc[1])
                if name == "sync":
                    for sem, val in tail:
                        e.wait_ge(sem, val)

            @block.sync
            def _(e):
                replay(e, "sync")

            @block.scalar
            def _(e):
                replay(e, "scalar")

            @block.vector
            def _(e):
                replay(e, "vector")

            @block.gpsimd
            def _(e):
                replay(e, "gpsimd")

            @block.tensor
            def _(e):
                replay(e, "tensor")


def build(debug=False, stop_after=None, depth=DEPTH):
    nc = bass.Bass("TRN2", target_bir_lowering=False)
    x = nc.dram_tensor("x", [NTOK, D], F32, kind="ExternalInput").ap()
    flags_d = nc.dram_tensor("flags", [5], F32, kind="ExternalInput").ap()
    W = {n: nc.dram_tensor(n, WSHAPES[n], F32, kind="ExternalInput").ap() for n in WNAMES}
    y = nc.dram_tensor("y", [NTOK, D], F32, kind="ExternalOutput").ap()
    H0 = nc.dram_tensor("H0", [NTOK, D], F32).ap()
    H1 = nc.dram_tensor("H1", [NTOK, D], F32, kind=("ExternalOutput" if debug else "Internal")).ap()
    HT = nc.dram_tensor("HT", [8, 128, NTOK], BF16).ap()
    UAs = nc.dram_tensor("UAs", [3, 128, NTOK], F32).ap()
    CVs = nc.dram_tensor("CVs", [3, 128, NTOKP], BF16).ap()
    KTs = nc.dram_tensor("KTs", [4, 128, NTOKP], BF16).ap()
    VTs = nc.dram_tensor("VTs", [4, 128, NTOKP], BF16).ap()
    MTs = nc.dram_tensor("MTs", [8, 128, NTOK], BF16, kind=("ExternalOutput" if debug else "Internal")).ap()
    YA = nc.dram_tensor("YA", [2, 3, 128, NTOK], F32, kind=("ExternalOutput" if debug else "Internal")).ap()
    dbg = {}
    if debug:
        dbg["h0"] = nc.dram_tensor("dbg_h0", [NTOK, D], F32, kind="ExternalOutput").ap()
        dbg["ua"] = nc.dram_tensor("dbg_ua", [3, 128, NTOK], F32, kind="ExternalOutput").ap()
        dbg["mix"] = nc.dram_tensor("dbg_mix", [NTOK, D], F32, kind="ExternalOutput").ap()
        dbg["st"] = nc.dram_tensor("dbg_st", [NTOK, 8], F32, kind="ExternalOutput").ap()

    es = ExitStack()
    with es:
        fw = FW(nc, es)

        def sb(name, shape, dt=F32):
            return es.enter_context(nc.sbuf_tensor(name, shape, dt))

        def ps(name, shape, dt=F32):
            return es.enter_context(nc.psum_tensor(name, shape, dt))

        ident = sb("ident", [128, 128], BF16)
        r_ident = Res("ident")
        fw.op("gpsimd", lambda e: e.memset(ident[:], 0.0), writes=[r_ident])
        fw.op("gpsimd", lambda e: e.affine_select(out=ident[:], in_=ident[:], pattern=[[-1, 128]],
                                                  compare_op=ALU.not_equal, fill=1.0, base=0, channel_multiplier=1),
              reads=[r_ident], writes=[r_ident])
        flg = sb("flg", [128, 5])
        r_flg = Res("flg")
        fw.dma("sync", lambda e: e.dma_start(out=flg[:], in_=flags_d.partition_broadcast(128)), writes=[r_flg])
        gam = sb("gam", [128, D])
        bet = sb("bet", [128, D])
        r_gb = Res("gb")

        def load_gb(gname, bname, l):
            gsrc = W[gname] if l is None else W[gname][l]
            bsrc = W[bname] if l is None else W[bname][l]
            fw.dma("sync", lambda e: e.dma_start(out=gam[:], in_=gsrc.partition_broadcast(128)), writes=[r_gb])
            fw.dma("sync", lambda e: e.dma_start(out=bet[:], in_=bsrc.partition_broadcast(128)), writes=[r_gb])

        pT = [ps("pT%d" % i, [128, 8, 128], BF16) for i in range(2)]
        r_pT = [Res("pT%d" % i) for i in range(2)]
        pA = [ps("pA%d" % i, [128, 512]) for i in range(4)]
        r_pA = [Res("pA%d" % i) for i in range(4)]
        pX = [ps("pX%d" % i, [128, 512]) for i in range(2)]
        r_pX = [Res("pX%d" % i) for i in range(2)]
        cnt = {"w": 0, "pA": 0, "blk": 0, "uid": 0}

        def uname(n):
            cnt["uid"] += 1
            return "%s_%d" % (n, cnt["uid"])

        def padpos(t):
            return (t // SEG) * SEGP + PAD + (t % SEG)

        def phase_a(l):
            with ExitStack() as pes:
                def sb2(name, shape, dt=F32):
                    return pes.enter_context(nc.sbuf_tensor(uname("a_" + name), shape, dt))
                hT = sb2("hT", [128, 8, ST], BF16)
                r_hT = Res("hT")
                xb = [sb2("xb%d" % i, [128, D]) for i in range(2)]
                r_xb = [Res("xb%d" % i) for i in range(2)]
                tb = [sb2("tb%d" % i, [128, D]) for i in range(2)]
                r_tb = [Res("tb%d" % i) for i in range(2)]
                hb16 = [sb2("hb16_%d" % i, [128, D], BF16) for i in range(2)]
                r_hb16 = [Res("hb16_%d" % i) for i in range(2)]
                st4 = [sb2("st4_%d" % i, [128, 8]) for i in range(2)]
                r_st4 = [Res("st4_%d" % i) for i in range(2)]
                wst = [sb2("wst%d" % i, [128, 8, 128]) for i in range(2)]
                r_wst = [Res("wst%d" % i) for i in range(2)]
                wbf = [sb2("wbf%d" % i, [128, 8, 128], BF16) for i in range(2)]
                r_wbf = [Res("wbf%d" % i) for i in range(2)]
                zt0 = sb2("zt0", [128, ST], BF16)
                r_zt0 = Res("zt0")
                zf = [sb2("zf%d" % i, [128, 512]) for i in range(4)]
                r_zf = [Res("zf%d" % i) for i in range(4)]
                zb = [sb2("zb%d" % i, [128, 512], BF16) for i in range(4)]
                r_zb = [Res("zb%d" % i) for i in range(4)]

                def layer_norm_block(i):
                    ln_rows(fw, xb[i], r_xb[i], tb[i], r_tb[i], st4[i], r_st4[i], gam, bet, r_gb, xb[i], r_xb[i])

                def transpose_block(i, col0):
                    fw.op("scalar", lambda e: e.activation(out=hb16[i][:], in_=xb[i][:], func=AF.Copy),
                          reads=[r_xb[i]], writes=[r_hb16[i]])
                    for k in range(8):
                        fw.op("tensor", lambda e, k=k: e.transpose(pT[i][:, k, :], hb16[i][:, k * 128:(k + 1) * 128],
                                                                   ident[:]),
                              reads=[r_hb16[i], r_ident], writes=[r_pT[i]], pe_acc=True)
                    fw.op("vector", lambda e: e.tensor_copy(out=hT[:, :, col0:col0 + 128], in_=pT[i][:]),
                          reads=[r_pT[i]], writes=[r_hT])

                def load_w_chunk(src_ap):
                    j = cnt["w"] % 2
                    cnt["w"] += 1
                    fw.dma("sync", lambda e: e.dma_start(out=wst[j][:], in_=src_ap.rearrange("(k p) n -> p k n", p=128)),
                           writes=[r_wst[j]])
                    fw.op("gpsimd", lambda e: e.tensor_copy(out=wbf[j][:], in_=wst[j][:]),
                          reads=[r_wst[j]], writes=[r_wbf[j]])
                    return wbf[j], r_wbf[j]

                def proj_fm(wt, r_w, evac):
                    for ts in range(ST // 512):
                        j = cnt["pA"] % 4
                        cnt["pA"] += 1
                        for k in range(8):
                            fw.op("tensor", lambda e, k=k, j=j, ts=ts: e.matmul(
                                pA[j][:], lhsT=wt[:, k, :], rhs=hT[:, k, ts * 512:(ts + 1) * 512],
                                start=(k == 0), stop=(k == 7)),
                                reads=[r_w, r_hT], writes=[r_pA[j]], pe_acc=True)
                        evac(ts, j, pA[j], r_pA[j])

                if l == 0:
                    load_gb("ln_in_g", "ln_in_b", None)
                for st_i in range(NST):
                    t0 = st_i * ST
                    p0 = padpos(t0)
                    for b in range(ST // 128):
                        i = cnt["blk"] % 2
                        cnt["blk"] += 1
                        r0 = t0 + b * 128
                        if l == 0:
                            fw.dma("sync", lambda e, i=i, r0=r0: e.dma_start(out=xb[i][:], in_=x[r0:r0 + 128, :]),
                                   writes=[r_xb[i]])
                            layer_norm_block(i)
                            fw.dma("sync", lambda e, i=i, r0=r0: e.dma_start(out=H0[r0:r0 + 128, :], in_=xb[i][:]),
                                   reads=[r_xb[i]])
                            if debug:
                                fw.dma("sync", lambda e, i=i, r0=r0: e.dma_start(out=dbg["h0"][r0:r0 + 128, :],
                                                                                in_=xb[i][:]), reads=[r_xb[i]])
                        else:
                            fw.dma("sync", lambda e, i=i, r0=r0: e.dma_start(out=xb[i][:], in_=H0[r0:r0 + 128, :]),
                                   writes=[r_xb[i]])
                        transpose_block(i, b * 128)
                    fw.dma("sync", lambda e, t0=t0: e.dma_start(out=HT[:, :, t0:t0 + ST].rearrange("k p n -> p k n"),
                                                                in_=hT[:]), reads=[r_hT])

                    def store_plain(dst, c, t0=t0):
                        def ev(ts, j, pt, r_pt):
                            fw.op("scalar", lambda e: e.activation(out=zf[j][:], in_=pt[:], func=AF.Copy),
                                  reads=[r_pt], writes=[r_zf[j]])
                            fw.dma("sync", lambda e: e.dma_start(out=dst[c, :, t0 + ts * 512:t0 + (ts + 1) * 512],
                                                                 in_=zf[j][:]), reads=[r_zf[j]])
                            if debug and dst is UAs:
                                fw.dma("sync", lambda e: e.dma_start(
                                    out=dbg["ua"][c, :, t0 + ts * 512:t0 + (ts + 1) * 512], in_=zf[j][:]),
                                    reads=[r_zf[j]])
                        return ev

                    def store_pad(dst, c, p0=p0):
                        def ev(ts, j, pt, r_pt):
                            fw.op("scalar", lambda e: e.activation(out=zb[j][:], in_=pt[:], func=AF.Copy),
                                  reads=[r_pt], writes=[r_zb[j]])
                            fw.dma("sync", lambda e: e.dma_start(out=dst[c, :, p0 + ts * 512:p0 + (ts + 1) * 512],
                                                                 in_=zb[j][:]), reads=[r_zb[j]])
                        return ev

                    wi = W["w_in"][l]
                    for c in range(3):
                        wt, r_w = load_w_chunk(wi[:, c * 128:(c + 1) * 128])
                        proj_fm(wt, r_w, store_plain(UAs, c))
                    if stop_after == "ua":
                        continue
                    for c in range(3):
                        wt, r_w = load_w_chunk(wi[:, (15 + c) * 128:(16 + c) * 128])
                        proj_fm(wt, r_w, store_pad(KTs, c))
                    wt, r_w = load_w_chunk(wi[:, 24 * 128:25 * 128])
                    proj_fm(wt, r_w, store_pad(KTs, 3))
                    for c in range(3):
                        wt, r_w = load_w_chunk(wi[:, (18 + c) * 128:(19 + c) * 128])
                        proj_fm(wt, r_w, store_pad(VTs, c))
                    wt, r_w = load_w_chunk(wi[:, 25 * 128:26 * 128])
                    proj_fm(wt, r_w, store_pad(VTs, 3))
                    for c in range(3):
                        wt, r_w = load_w_chunk(wi[:, (3 + c) * 128:(4 + c) * 128])

                        def ev_vb(ts, j, pt, r_pt):
                            fw.op("scalar", lambda e: e.activation(out=zt0[:, ts * 512:(ts + 1) * 512], in_=pt[:],
                                                                   func=AF.Copy), reads=[r_pt], writes=[r_zt0])
                        proj_fm(wt, r_w, ev_vb)
                        wt, r_w = load_w_chunk(wi[:, (9 + c) * 128:(10 + c) * 128])

                        def ev_gc(ts, j, pt, r_pt, c=c, p0=p0):
                            fw.op("vector", lambda e: e.tensor_tensor(out=zb[j][:], in0=pt[:],
                                                                      in1=zt0[:, ts * 512:(ts + 1) * 512], op=ALU.mult),
                                  reads=[r_pt, r_zt0], writes=[r_zb[j]])
                            fw.dma("sync", lambda e: e.dma_start(out=CVs[c, :, p0 + ts * 512:p0 + (ts + 1) * 512],
                                                                 in_=zb[j][:]), reads=[r_zb[j]])
                        proj_fm(wt, r_w, ev_gc)
            fw.barrier()

        def phase_s5(l):
            with ExitStack() as pes:
                def sb2(name, shape, dt=F32):
                    return pes.enter_context(nc.sbuf_tensor(uname("s_" + name), shape, dt))
                r_p = Res("prm")
                names = ["are", "aim", "ldt", "dt", "rho", "th", "c", "s", "t1", "t2", "lr", "li", "nr", "den",
                         "numr", "numi", "kr", "ki", "nki"]
                P = {n: sb2(n, [128, 24]) for n in names}

                def tt(o, a, b, op):
                    fw.op("vector", lambda e: e.tensor_tensor(out=P[o][:], in0=P[a][:], in1=P[b][:], op=op),
                          reads=[r_p], writes=[r_p])

                def ts_(o, a, s1, op0, s2=None, op1=None):
                    if op1 is None:
                        fw.op("vector", lambda e: e.tensor_scalar(out=P[o][:], in0=P[a][:], scalar1=s1, scalar2=None,
                                                                  op0=op0), reads=[r_p], writes=[r_p])
                    else:
                        fw.op("vector", lambda e: e.tensor_scalar(out=P[o][:], in0=P[a][:], scalar1=s1, scalar2=s2,
                                                                  op0=op0, op1=op1), reads=[r_p], writes=[r_p])

                def act(o, a, func, scale=1.0):
                    fw.op("scalar", lambda e: e.activation(out=P[o][:], in_=P[a][:], func=func, scale=scale),
                          reads=[r_p], writes=[r_p])

                for d in range(2):
                    fw.dma("sync", lambda e, d=d: e.dma_start(
                        out=P["are"][:, d * 12:(d + 1) * 12],
                        in_=W["s5_a_re"][l, d].rearrange("(gp g2) p -> (g2 p) gp", g2=2),
                        allow_slow_non_contiguous=True), writes=[r_p])
                    fw.dma("sync", lambda e, d=d: e.dma_start(
                        out=P["aim"][:, d * 12:(d + 1) * 12],
                        in_=W["s5_a_im"][l, d].rearrange("(gp g2) p -> (g2 p) gp", g2=2),
                        allow_slow_non_contiguous=True), writes=[r_p])
                    for g2 in range(2):
                        fw.dma("sync", lambda e, d=d, g2=g2: e.dma_start(
                            out=P["ldt"][64 * g2:64 * g2 + 64, d * 12:(d + 1) * 12],
                            in_=W["s5_log_dt"][l, d].rearrange("(gp g2) -> g2 gp", g2=2)[g2].partition_broadcast(64),
                            allow_slow_non_contiguous=True), writes=[r_p])
                act("dt", "ldt", AF.Exp)
                tt("t1", "are", "dt", ALU.mult)
                act("rho", "t1", AF.Exp)
                tt("th", "aim", "dt", ALU.mult)
                act("t1", "th", AF.Sin, scale=1.0 / 128)
                tt("t2", "t1", "t1", ALU.mult)
                ts_("c", "t2", -2.0, ALU.mult, 1.0, ALU.add)
                act("s", "th", AF.Sin, scale=1.0 / 64)
                for _ in range(6):
                    tt("t1", "c", "c", ALU.mult)
                    tt("t2", "s", "s", ALU.mult)
                    fw.op("vector", lambda e: e.scalar_tensor_tensor(out=P["s"][:], in0=P["c"][:], scalar=2.0,
                                                                     in1=P["s"][:], op0=ALU.mult, op1=ALU.mult),
                          reads=[r_p], writes=[r_p])
                    tt("c", "t1", "t2", ALU.subtract)
                tt("lr", "rho", "c", ALU.mult)
                tt("li", "rho", "s", ALU.mult)
                ts_("nr", "lr", -1.0, ALU.add)
                tt("t1", "are", "are", ALU.mult)
                tt("t2", "aim", "aim", ALU.mult)
                tt("den", "t1", "t2", ALU.add)
                fw.op("vector", lambda e: e.reciprocal(out=P["den"][:], in_=P["den"][:]), reads=[r_p], writes=[r_p])
                tt("t1", "nr", "are", ALU.mult)
                tt("t2", "li", "aim", ALU.mult)
                tt("numr", "t1", "t2", ALU.add)
                tt("t1", "li", "are", ALU.mult)
                tt("t2", "nr", "aim", ALU.mult)
                tt("numi", "t1", "t2", ALU.subtract)
                tt("kr", "numr", "den", ALU.mult)
                tt("ki", "numi", "den", ALU.mult)
                ts_("nki", "ki", -1.0, ALU.mult)
                LRR = sb2("LRR", [128, 2, 24])
                LIS = sb2("LIS", [128, 2, 24])
                for hh in range(2):
                    fw.op("vector", lambda e, hh=hh: e.tensor_copy(out=LRR[:, hh, :], in_=P["lr"][:]),
                          reads=[r_p], writes=[r_p])
                fw.op("vector", lambda e: e.tensor_scalar(out=LIS[:, 0, :], in0=P["li"][:], scalar1=-1.0, scalar2=None,
                                                          op0=ALU.mult), reads=[r_p], writes=[r_p])
                fw.op("vector", lambda e: e.tensor_copy(out=LIS[:, 1, :], in_=P["li"][:]), reads=[r_p], writes=[r_p])

                Bw = sb2("Bw", [128, 48, 128])
                Cw = sb2("Cw", [128, 48, 128])
                r_bw = Res("Bw")
                r_cw = Res("Cw")
                fw.op("gpsimd", lambda e: e.memset(Bw[:], 0.0), writes=[r_bw])
                fw.op("gpsimd", lambda e: e.memset(Cw[:], 0.0), writes=[r_cw])

                def widx(d, gp, ri):
                    return (d * 12 + gp) * 2 + ri
                for d in range(2):
                    for gp in range(12):
                        for g2 in range(2):
                            g = 2 * gp + g2
                            r0 = 16 * (g % 8)
                            for ri, (bn, cn) in enumerate([("s5_b_re", "s5_c_re"), ("s5_b_im", "s5_c_im")]):
                                fw.dma("sync", lambda e, d=d, gp=gp, g2=g2, g=g, r0=r0, ri=ri, bn=bn: e.dma_start(
                                    out=Bw[r0:r0 + 16, widx(d, gp, ri), 64 * g2:64 * g2 + 64],
                                    in_=W[bn][l, d, g].rearrange("p h -> h p"),
                                    allow_slow_non_contiguous=True), writes=[r_bw])
                                fw.dma("sync", lambda e, d=d, gp=gp, g2=g2, g=g, r0=r0, ri=ri, cn=cn: e.dma_start(
                                    out=Cw[64 * g2:64 * g2 + 64, widx(d, gp, ri), r0:r0 + 16],
                                    in_=W[cn][l, d, g].rearrange("h p -> p h"),
                                    allow_slow_non_contiguous=True), writes=[r_cw])
                Cw4 = Cw[:].rearrange("p (a r) n -> p a r n", r=2)
                fw.op("vector", lambda e: e.tensor_scalar(out=Cw4[:, :, 1, :], in0=Cw4[:, :, 1, :], scalar1=-1.0,
                                                          scalar2=None, op0=ALU.mult), reads=[r_cw], writes=[r_cw])

                XS = sb2("XS", [128, 2, 24, 129])
                BU = sb2("BU", [128, 2, 24, 128])
                PQ = sb2("PQ", [128, 2, 2, 24])
                r_xs = Res("XS")
                r_bu = Res("BU")
                r_pq = Res("PQ")
                tmpb = [sb2("tmpb%d" % i, [128, 2, 128]) for i in range(2)]
                r_tmpb = [Res("tmpb%d" % i) for i in range(2)]
                ua = [[sb2("ua%d_%d" % (i, d), [128, 3, 128]) for d in range(2)] for i in range(2)]
                r_ua = [[Res("ua%d_%d" % (i, d)) for d in range(2)] for i in range(2)]
                yo = [sb2("yo%d" % i, [128, 128]) for i in range(2)]
                r_yo = [Res("yo%d" % i) for i in range(2)]
                fw.op("vector", lambda e: e.memset(XS[:], 0.0), writes=[r_xs])
                NT = NTOK // 128
                kcount = 0
                for i in range(NT):
                    tiles = [i, NT - 1 - i]
                    bi = i % 2
                    for d in range(2):
                        tk = tiles[d] * 128
                        fw.dma("sync", lambda e, d=d, tk=tk, bi=bi: e.dma_start(
                            out=ua[bi][d][:], in_=UAs[:, :, tk:tk + 128].rearrange("c p n -> p c n")),
                            writes=[r_ua[bi][d]])
                    for d in range(2):
                        for gp in range(12):
                            col = d * 12 + gp
                            c3 = gp // 4
                            pp = 2 * (col % 2)
                            for ri in range(2):
                                fw.op("tensor", lambda e, d=d, gp=gp, ri=ri, pp=pp, c3=c3, bi=bi: e.matmul(
                                    pA[pp + ri][:, 0:128], lhsT=Bw[:, widx(d, gp, ri), :], rhs=ua[bi][d][:, c3, :],
                                    start=True, stop=True),
                                    reads=[r_bw, r_ua[bi][d]], writes=[r_pA[pp + ri]])
                            tbk = tmpb[col % 2]
                            r_tbk = r_tmpb[col % 2]
                            if d == 0:
                                bre, bim = BU[:, 0, col, :], BU[:, 1, col, :]
                            else:
                                bre, bim = BU[:, 0, col, ::-1], BU[:, 1, col, ::-1]
                            fw.op("vector", lambda e, tbk=tbk, pp=pp, col=col: e.tensor_scalar(
                                out=tbk[:, 0, :], in0=pA[pp][:, 0:128], scalar1=P["kr"][:, col:col + 1], scalar2=None,
                                op0=ALU.mult), reads=[r_pA[pp], r_p], writes=[r_tbk])
                            fw.op("vector", lambda e, tbk=tbk, pp=pp, col=col, bre=bre: e.scalar_tensor_tensor(
                                out=bre, in0=pA[pp + 1][:, 0:128], scalar=P["nki"][:, col:col + 1], in1=tbk[:, 0, :],
                                op0=ALU.mult, op1=ALU.add), reads=[r_pA[pp + 1], r_p, r_tbk], writes=[r_bu])
                            fw.op("vector", lambda e, tbk=tbk, pp=pp, col=col: e.tensor_scalar(
                                out=tbk[:, 1, :], in0=pA[pp + 1][:, 0:128], scalar1=P["kr"][:, col:col + 1],
                                scalar2=None, op0=ALU.mult), reads=[r_pA[pp + 1], r_p], writes=[r_tbk])
                            fw.op("vector", lambda e, tbk=tbk, pp=pp, col=col, bim=bim: e.scalar_tensor_tensor(
                                out=bim, in0=pA[pp][:, 0:128], scalar=P["ki"][:, col:col + 1], in1=tbk[:, 1, :],
                                op0=ALU.mult, op1=ALU.add), reads=[r_pA[pp], r_p, r_tbk], writes=[r_bu])
                    for j in range(128):
                        fw.op("vector", lambda e, j=j: e.tensor_tensor(out=PQ[:, 0], in0=LRR[:], in1=XS[:, :, :, j],
                                                                       op=ALU.mult),
                              reads=[r_xs, r_p], writes=[r_pq])
                        fw.op("vector", lambda e, j=j: e.tensor_tensor(out=PQ[:, 1], in0=LIS[:], in1=XS[:, ::-1, :, j],
                                                                       op=ALU.mult),
                              reads=[r_xs, r_p], writes=[r_pq])
                        fw.op("vector", lambda e: e.tensor_tensor(out=PQ[:, 0], in0=PQ[:, 0], in1=PQ[:, 1], op=ALU.add),
                              reads=[r_pq], writes=[r_pq])
                        fw.op("vector", lambda e, j=j: e.tensor_tensor(out=XS[:, :, :, j + 1], in0=PQ[:, 0],
                                                                       in1=BU[:, :, :, j], op=ALU.add),
                              reads=[r_pq, r_bu], writes=[r_xs])
                    for d in range(2):
                        tk = tiles[d] * 128
                        for c3 in range(3):
                            pj = kcount % 2
                            kcount += 1
                            n = 0
                            for gq in range(4):
                                gp = c3 * 4 + gq
                                col = d * 12 + gp
                                for ri in range(2):
                                    fw.op("tensor", lambda e, d=d, gp=gp, ri=ri, col=col, pj=pj, n=n: e.matmul(
                                        pX[pj][:, 0:128], lhsT=Cw[:, widx(d, gp, ri), :], rhs=XS[:, ri, col, 1:129],
                                        start=(n == 0), stop=(n == 7)),
                                        reads=[r_cw, r_xs], writes=[r_pX[pj]], pe_acc=True)
                                    n += 1
                            ov = yo[pj][:, :] if d == 0 else yo[pj][:, ::-1]
                            fw.op("scalar", lambda e, pj=pj, ov=ov: e.activation(out=ov, in_=pX[pj][:, 0:128],
                                                                                 func=AF.Copy),
                                  reads=[r_pX[pj]], writes=[r_yo[pj]])
                            fw.dma("sync", lambda e, d=d, c3=c3, tk=tk, pj=pj: e.dma_start(
                                out=YA[d, c3, :, tk:tk + 128], in_=yo[pj][:]), reads=[r_yo[pj]])
                    fw.op("vector", lambda e: e.tensor_copy(out=XS[:, :, :, 0], in_=XS[:, :, :, 128]),
                          reads=[r_xs], writes=[r_xs])
                    if (i + 1) % 32 == 0 and i + 1 < NT:
                        sgn = (i + 1) // 32
                        fw.op("vector", lambda e, sgn=sgn: e.tensor_scalar(
                            out=XS[:, :, 0:12, 0], in0=XS[:, :, 0:12, 0], scalar1=flg[:, sgn:sgn + 1], scalar2=None,
                            op0=ALU.mult), reads=[r_xs, r_flg], writes=[r_xs])
                        fw.op("vector", lambda e, sgn=sgn: e.tensor_scalar(
                            out=XS[:, :, 12:24, 0], in0=XS[:, :, 12:24, 0], scalar1=flg[:, 4 - sgn:5 - sgn],
                            scalar2=None, op0=ALU.mult), reads=[r_xs, r_flg], writes=[r_xs])
            fw.barrier()

        def phase_h():
            with ExitStack() as pes:
                hb = [pes.enter_context(nc.sbuf_tensor(uname("h_hb%d" % i), [128, 4, PAD], BF16)) for i in range(2)]
                r_hb = [Res("hb%d" % i) for i in range(2)]
                k = 0
                for (T, nch) in [(KTs, 4), (VTs, 4), (CVs, 3)]:
                    for sg in range(NSEG):
                        jobs = []
                        src = ((sg - 1) * SEGP + SEG) if sg > 0 else (sg * SEGP + PAD)
                        jobs.append((src, sg * SEGP, sg))
                        src = ((sg + 1) * SEGP + PAD) if sg < NSEG - 1 else (sg * SEGP + SEG)
                        jobs.append((src, sg * SEGP + PAD + SEG, sg + 1))
                        for (src, dst, fc) in jobs:
                            b = k % 2
                            k += 1
                            fw.dma("sync", lambda e, T=T, nch=nch, src=src, b=b: e.dma_start(
                                out=hb[b][:, 0:nch, :], in_=T[0:nch, :, src:src + PAD].rearrange("c p n -> p c n")),
                                writes=[r_hb[b]])
                            fw.op("vector", lambda e, nch=nch, b=b, fc=fc: e.tensor_scalar(
                                out=hb[b][:, 0:nch, :], in0=hb[b][:, 0:nch, :], scalar1=flg[:, fc:fc + 1], scalar2=None,
                                op0=ALU.mult), reads=[r_hb[b], r_flg], writes=[r_hb[b]])
                            fw.dma("sync", lambda e, T=T, nch=nch, dst=dst, b=b: e.dma_start(
                                out=T[0:nch, :, dst:dst + PAD].rearrange("c p n -> p c n"), in_=hb[b][:, 0:nch, :]),
                                reads=[r_hb[b]])
            fw.barrier()

        maskD = sb("maskD", [128, 6, 256])
        maskS = sb("maskS", [128, 6, 384])
        r_mask = Res("mask")
        ones_col = sb("ones_col", [128, 1])
        fw.op("vector", lambda e: e.memset(ones_col[:], 1.0), writes=[r_mask])
        with ExitStack() as mes:
            ii = mes.enter_context(nc.sbuf_tensor("m_ii", [128, 128], mybir.dt.int32))
            fi = mes.enter_context(nc.sbuf_tensor("m_fi", [128, 128], F32))
            ta = mes.enter_context(nc.sbuf_tensor("m_ta", [128, 128], F32))
            tv = mes.enter_context(nc.sbuf_tensor("m_tv", [128, 128], F32))
            r_m = Res("m")
            fw.op("gpsimd", lambda e: e.iota(ii[:], pattern=[[-1, 128]], base=0, channel_multiplier=1), writes=[r_m])
            fw.op("vector", lambda e: e.tensor_copy(out=fi[:], in_=ii[:]), reads=[r_m], writes=[r_m])

            def mk_mask(dst, off, half, coef):
                fw.op("vector", lambda e: e.tensor_scalar(out=ta[:], in0=fi[:], scalar1=float(off), scalar2=None,
                                                          op0=ALU.add), reads=[r_m], writes=[r_m])
                fw.op("vector", lambda e: e.tensor_scalar(out=tv[:], in0=ta[:], scalar1=-1.0, scalar2=None,
                                                          op0=ALU.mult), reads=[r_m], writes=[r_m])
                fw.op("vector", lambda e: e.tensor_tensor(out=ta[:], in0=ta[:], in1=tv[:], op=ALU.max),
                      reads=[r_m], writes=[r_m])
                fw.op("vector", lambda e: e.tensor_scalar(out=tv[:], in0=ta[:], scalar1=-1.0, scalar2=float(half) + 0.5,
                                                          op0=ALU.mult, op1=ALU.add), reads=[r_m], writes=[r_m])
                fw.op("vector", lambda e: e.tensor_scalar(out=tv[:], in0=tv[:], scalar1=0.0, scalar2=0.5,
                                                          op0=ALU.max, op1=ALU.min), reads=[r_m], writes=[r_m])
                fw.op("scalar", lambda e: e.activation(out=ta[:], in_=ta[:], func=AF.Exp, scale=-float(coef)),
                      reads=[r_m], writes=[r_m])
                fw.op("vector", lambda e: e.scalar_tensor_tensor(out=dst, in0=ta[:], scalar=2.0, in1=tv[:],
                                                                 op0=ALU.mult, op1=ALU.mult),
                      reads=[r_m], writes=[r_m, r_mask])
            for gi, (win, dil) in enumerate(DIL):
                for h in range(2):
                    sl = SLOPES[6 + 2 * gi + h]
                    for kt in range(2):
                        mk_mask(maskD[:, 2 * gi + h, kt * 128:(kt + 1) * 128], -64 + 128 * kt, 64, sl * dil)
            for h in range(6):
                for kt in range(3):
                    mk_mask(maskS[:, h, kt * 128:(kt + 1) * 128], 128 * (kt - 1), 128, SLOPES[h])
        fw.barrier()

        def phase_b(l, last):
            with ExitStack() as L0:
                def sb0(name, shape, dt=F32):
                    return L0.enter_context(nc.sbuf_tensor(uname("b_" + name), shape, dt))
                hT = sb0("hT", [128, 8, ST], BF16)
                r_hT = Res("hT")
                vcol = sb0("vcol", [128, 2])
                r_vcol = Res("vcol")
                sexp = sb0("sexp", [128, 6])
                r_sexp = Res("sexp")
                fw.dma("sync", lambda e: e.dma_start(out=sexp[:], in_=W["swa_sink"][l].partition_broadcast(128)),
                       writes=[r_sexp])
                fw.op("scalar", lambda e: e.activation(out=sexp[:], in_=sexp[:], func=AF.Exp),
                      reads=[r_sexp], writes=[r_sexp])
                wi = W["w_in"][l]
                for st_i in range(NST):
                    t0 = st_i * ST
                    p0 = padpos(t0)
                    sg = st_i // 2
                    hf = st_i % 2
                    fw.dma("sync", lambda e, t0=t0: e.dma_start(
                        out=hT[:], in_=HT[:, :, t0:t0 + ST].rearrange("k p n -> p k n")), writes=[r_hT])
                    fw.op("vector", lambda e: e.memset(vcol[:], 1.0), writes=[r_vcol])
                    fw.op("vector", lambda e, sg=sg: e.tensor_copy(out=vcol[0:64, 0:1], in_=flg[0:64, sg:sg + 1]),
                          reads=[r_flg], writes=[r_vcol])
                    fw.op("vector", lambda e, sg=sg: e.tensor_copy(out=vcol[64:128, 1:2], in_=flg[64:128, sg + 1:sg + 2]),
                          reads=[r_flg], writes=[r_vcol])
                    with ExitStack() as L1:
                        def sb1(name, shape, dt=F32):
                            return L1.enter_context(nc.sbuf_tensor(uname("b1_" + name), shape, dt))
                        brT = sb1("brT", [128, 10, ST], BF16)
                        r_br = Res("brT")
                        wst = [sb1("wst%d" % i, [128, 8, 128]) for i in range(2)]
                        r_wst = [Res("wst%d" % i) for i in range(2)]
                        wbf = [sb1("wbf%d" % i, [128, 8, 128], BF16) for i in range(2)]
                        r_wbf = [Res("wbf%d" % i) for i in range(2)]

                        def load_w_chunk(parts):
                            j = cnt["w"] % 2
                            cnt["w"] += 1
                            for (src_ap, c0, n) in parts:
                                fw.dma("sync", lambda e, src_ap=src_ap, c0=c0, n=n, j=j: e.dma_start(
                                    out=wst[j][:, :, c0:c0 + n], in_=src_ap.rearrange("(k p) n -> p k n", p=128)),
                                    writes=[r_wst[j]])
                            fw.op("gpsimd", lambda e, j=j: e.tensor_copy(out=wbf[j][:], in_=wst[j][:]),
                                  reads=[r_wst[j]], writes=[r_wbf[j]])
                            return wbf[j], r_wbf[j]

                        def proj_fm(wt, r_w, evac):
                            for ts in range(ST // 512):
                                j = cnt["pA"] % 4
                                cnt["pA"] += 1
                                for k in range(8):
                                    fw.op("tensor", lambda e, k=k, j=j, ts=ts: e.matmul(
                                        pA[j][:], lhsT=wt[:, k, :], rhs=hT[:, k, ts * 512:(ts + 1) * 512],
                                        start=(k == 0), stop=(k == 7)),
                                        reads=[r_w, r_hT], writes=[r_pA[j]], pe_acc=True)
                                evac(ts, j, pA[j], r_pA[j])

                        with ExitStack() as S1:
                            def sbs(name, shape, dt=F32):
                                return S1.enter_context(nc.sbuf_tensor(uname("b2_" + name), shape, dt))
                            KT1 = sbs("KT1", [128, 2 * ST], BF16)
                            VT1 = sbs("VT1", [128, 2 * ST], BF16)
                            r_kv = Res("kv")
                            QT1 = sbs("QT1", [128, ST], BF16)
                            r_q = Res("q")
                            UACC = sbs("UACC", [128, 2, ST])
                            r_ua = Res("uacc")
                            RC = sbs("RC", [128, ST])
                            r_rc = Res("rc")
                            Et = [sbs("E%d" % i, [128, 384]) for i in range(2)]
                            r_E = [Res("E%d" % i) for i in range(2)]
                            Pt = [sbs("P%d" % i, [128, 384], BF16) for i in range(2)]
                            r_P = [Res("P%d" % i) for i in range(2)]
                            VE = [sbs("VE%d" % i, [128, 3, 2, 192], BF16) for i in range(2)]
                            r_VE = [Res("VE%d" % i) for i in range(2)]
                            sm = [sbs("sm%d" % i, [128, 128]) for i in range(2)]
                            r_sm = [Res("sm%d" % i) for i in range(2)]
                            for i in range(2):
                                fw.op("vector", lambda e, i=i: e.memset(VE[i][:], 1.0), writes=[r_VE[i]])
                            ac = {"u": 0}

                            def q_evac(ts, j, pt, r_pt):
                                fw.op("scalar", lambda e: e.activation(out=QT1[:, ts * 512:(ts + 1) * 512], in_=pt[:],
                                                                       func=AF.Copy), reads=[r_pt], writes=[r_q])

                            def load_kv(c, p0=p0):
                                fw.dma("sync", lambda e, c=c, p0=p0: e.dma_start(
                                    out=KT1[:], in_=KTs[c, :, p0 - PAD:p0 - PAD + 2 * ST]), writes=[r_kv])
                                fw.dma("sync", lambda e, c=c, p0=p0: e.dma_start(
                                    out=VT1[:], in_=VTs[c, :, p0 - PAD:p0 - PAD + 2 * ST]), writes=[r_kv])

                            def unit(nkt, kcols, qcols, heads, mask_of, valid_of, lhs_of, sink_dst):
                                u = ac["u"] % 2
                                ac["u"] += 1
                                for kt in range(nkt):
                                    fw.op("tensor", lambda e, kt=kt, u=u: e.transpose(
                                        pT[u][:, kt, :], VT1[:, kcols(kt)], ident[:]),
                                        reads=[r_kv, r_ident], writes=[r_pT[u]], pe_acc=True)
                                fw.op("vector", lambda e, u=u: e.tensor_copy(
                                    out=VE[u][:, 0:nkt, :, 64:128],
                                    in_=pT[u][:, 0:nkt, :].rearrange("p k (h d) -> p k h d", h=2)),
                                    reads=[r_pT[u]], writes=[r_VE[u]])
                                for (hrow, vslot, tag) in heads:
                                    j = cnt["pA"] % 4
                                    cnt["pA"] += 1
                                    j2 = cnt["pA"] % 4
                                    cnt["pA"] += 1
                                    ei = ac["u"] % 2
                                    for kt in range(nkt):
                                        fw.op("tensor", lambda e, kt=kt, j=j, hrow=hrow: e.matmul(
                                            pA[j][:, kt * 128:(kt + 1) * 128], lhsT=KT1[hrow:hrow + 64, kcols(kt)],
                                            rhs=QT1[hrow:hrow + 64, qcols], start=True, stop=True),
                                            reads=[r_kv, r_q], writes=[r_pA[j]], pe_acc=True)
                                    fw.op("scalar", lambda e, j=j, ei=ei: e.activation(
                                        out=Et[ei][:, 0:nkt * 128], in_=pA[j][:, 0:nkt * 128], func=AF.Exp, scale=0.125),
                                        reads=[r_pA[j]], writes=[r_E[ei]])
                                    for kt in range(nkt):
                                        vc = valid_of(kt)
                                        fw.op("vector", lambda e, kt=kt, ei=ei, vc=vc, tag=tag: e.scalar_tensor_tensor(
                                            out=Pt[ei][:, kt * 128:(kt + 1) * 128], in0=Et[ei][:, kt * 128:(kt + 1) * 128],
                                            scalar=vc, in1=mask_of(tag)[:, kt * 128:(kt + 1) * 128],
                                            op0=ALU.mult, op1=ALU.mult),
                                            reads=[r_E[ei], r_mask, r_vcol, r_flg], writes=[r_P[ei]])
                                    for kt in range(nkt):
                                        fw.op("tensor", lambda e, kt=kt, j2=j2, ei=ei, u=u, vslot=vslot, tag=tag: e.matmul(
                                            pA[j2][:, 0:128], lhsT=lhs_of(VE[u], kt, vslot, tag),
                                            rhs=Pt[ei][:, kt * 128:(kt + 1) * 128], start=(kt == 0), stop=(kt == nkt - 1)),
                                            reads=[r_VE[u], r_P[ei]], writes=[r_pA[j2]], pe_acc=True)
                                    sink_dst(tag, pA[j2], r_pA[j2])

                            for gi, (win, dil) in enumerate(DIL):
                                wt, r_w = load_w_chunk([(wi[:, (12 + gi) * 128:(13 + gi) * 128], 0, 128)])
                                proj_fm(wt, r_w, q_evac)
                                load_kv(gi)
                                nsub = ST // dil
                                for r in range(dil):
                                    for qb in range(nsub // 128):
                                        q0 = qb * 128
                                        c_lo = r + dil * q0

                                        def kcols(kt, c_lo=c_lo, dil=dil):
                                            b = PAD + c_lo + dil * (-64 + 128 * kt)
                                            return slice(b, b + 127 * dil + 1, dil)
                                        qcols = slice(c_lo, c_lo + 127 * dil + 1, dil)

                                        def valid_of(kt, qb=qb, nsub=nsub):
                                            if hf == 0 and qb == 0 and kt == 0:
                                                return vcol[:, 0:1]
                                            if hf == 1 and qb == nsub // 128 - 1 and kt == 1:
                                                return vcol[:, 1:2]
                                            return ones_col[:, 0:1]

                                        def mask_of(tag, gi=gi):
                                            return maskD[:, 2 * gi + tag, :]

                                        def lhs_of(ve, kt, vslot, tag):
                                            return ve[:, kt, tag, 64:192] if tag == 0 else ve[:, kt, tag, 0:128]

                                        def sink_dst(tag, pu, r_pu, gi=gi, qcols=qcols):
                                            dstv = UACC[:, tag, qcols]
                                            if gi == 0:
                                                fw.op("vector", lambda e: e.tensor_copy(out=dstv, in_=pu[:, 0:128]),
                                                      reads=[r_pu], writes=[r_ua])
                                            else:
                                                fw.op("vector", lambda e: e.tensor_tensor(out=dstv, in0=dstv,
                                                                                          in1=pu[:, 0:128], op=ALU.add),
                                                      reads=[r_pu, r_ua], writes=[r_ua])
                                        unit(2, kcols, qcols, [(0, 0, 0), (64, 1, 1)], mask_of, valid_of, lhs_of, sink_dst)
                            fw.op("vector", lambda e: e.reciprocal(out=RC[0:64, :], in_=UACC[64:128, 0, :]),
                                  reads=[r_ua], writes=[r_rc])
                            fw.op("vector", lambda e: e.reciprocal(out=RC[64:128, :], in_=UACC[0:64, 1, :]),
                                  reads=[r_ua], writes=[r_rc])
                            fw.op("vector", lambda e: e.tensor_tensor(out=brT[0:64, 6, :], in0=UACC[0:64, 0, :],
                                                                      in1=RC[0:64, :], op=ALU.mult),
                                  reads=[r_ua, r_rc], writes=[r_br])
                            fw.op("vector", lambda e: e.tensor_tensor(out=brT[64:128, 6, :], in0=UACC[64:128, 1, :],
                                                                      in1=RC[64:128, :], op=ALU.mult),
                                  reads=[r_ua, r_rc], writes=[r_br])
                            load_kv(3)
                            for jq in range(3):
                                wt, r_w = load_w_chunk([(wi[:, 2688 + 64 * jq:2688 + 64 * jq + 64], 0, 64),
                                                        (wi[:, 2688 + 64 * (jq + 3):2688 + 64 * (jq + 3) + 64], 64, 64)])
                                proj_fm(wt, r_w, q_evac)
                                for qb in range(ST // 128):
                                    q0 = qb * 128

                                    def kcols(kt, q0=q0):
                                        b = PAD + q0 - 128 + 128 * kt
                                        return slice(b, b + 128)
                                    qcols = slice(q0, q0 + 128)

                                    def valid_of(kt, qb=qb):
                                        if hf == 0 and qb == 0 and kt == 0:
                                            return flg[:, sg:sg + 1]
                                        if hf == 1 and qb == ST // 128 - 1 and kt == 2:
                                            return flg[:, sg + 1:sg + 2]
                                        return ones_col[:, 0:1]

                                    def mask_of(tag):
                                        return maskS[:, tag, :]

                                    def lhs_of(ve, kt, vslot, tag):
                                        return ve[:, kt, vslot, 64:192] if tag % 2 == 0 else ve[:, kt, vslot, 0:128]

                                    def sink_dst(tag, pu, r_pu, qcols=qcols):
                                        h = tag
                                        ch, half = 7 + h // 2, h % 2
                                        si = ac["u"] % 2
                                        if half == 0:
                                            urows, drows = slice(0, 64), slice(64, 128)
                                        else:
                                            urows, drows = slice(64, 128), slice(0, 64)
                                        fw.op("vector", lambda e: e.tensor_scalar(
                                            out=sm[si][drows, :], in0=pu[drows, 0:128], scalar1=sexp[drows, h:h + 1],
                                            scalar2=None, op0=ALU.add), reads=[r_pu, r_sexp], writes=[r_sm[si]])
                                        fw.op("vector", lambda e: e.reciprocal(out=sm[si][drows, :], in_=sm[si][drows, :]),
                                              reads=[r_sm[si]], writes=[r_sm[si]])
                                        fw.op("vector", lambda e: e.tensor_tensor(
                                            out=brT[urows, ch, qcols], in0=pu[urows, 0:128], in1=sm[si][drows, :],
                                            op=ALU.mult), reads=[r_pu, r_sm[si]], writes=[r_br])
                                    unit(3, kcols, qcols, [(0, 0, jq), (64, 1, jq + 3)], mask_of, valid_of, lhs_of, sink_dst)
                        fw.barrier()
                        if stop_after == "attn":
                            fw.dma("sync", lambda e, t0=t0: e.dma_start(
                                out=dbg["br"][:, :, t0:t0 + ST].rearrange("c p n -> p c n"), in_=brT[:]), reads=[r_br])
                            fw.barrier()
                            continue
                        with ExitStack() as S2:
                            def sbt(name, shape, dt=F32):
                                return S2.enter_context(nc.sbuf_tensor(uname("b3_" + name), shape, dt))
                            dvec = sbt("dvec", [128, 3])
                            glub = sbt("glub", [128, 3])
                            cwt = sbt("cwt", [128, 3, 3])
                            cbt = sbt("cbt", [128, 3])
                            r_sv = Res("sv")
                            fw.dma("sync", lambda e: e.dma_start(out=dvec[:], in_=W["s5_d"][l].rearrange("(c p) -> p c", p=128),
                                                                 allow_slow_non_contiguous=True), writes=[r_sv])
                            fw.dma("sync", lambda e: e.dma_start(out=glub[:], in_=W["s5_glu_b"][l].rearrange("(c p) -> p c", p=128),
                                                                 allow_slow_non_contiguous=True), writes=[r_sv])
                            fw.dma("sync", lambda e: e.dma_start(out=cwt[:], in_=W["conv_w"][l].rearrange("t (c p) -> p t c", p=128),
                                                                 allow_slow_non_contiguous=True), writes=[r_sv])
                            fw.dma("sync", lambda e: e.dma_start(out=cbt[:], in_=W["conv_b"][l].rearrange("(c p) -> p c", p=128),
                                                                 allow_slow_non_contiguous=True), writes=[r_sv])
                            gluw32 = sbt("gluw32", [128, 3, 384])
                            gluw = sbt("gluw", [128, 3, 384], BF16)
                            r_gluw = Res("gluw")
                            fw.dma("sync", lambda e: e.dma_start(out=gluw32[:], in_=W["s5_glu_w"][l].rearrange("(c p) n -> p c n", p=128)),
                                   writes=[r_gluw])
                            fw.op("gpsimd", lambda e: e.tensor_copy(out=gluw[:], in_=gluw32[:]), reads=[r_gluw], writes=[r_gluw])
                            yf = [sbt("yf%d" % i, [128, 512]) for i in range(2)]
                            yb_ = [sbt("yb%d" % i, [128, 512]) for i in range(2)]
                            uu = [sbt("uu%d" % i, [128, 512]) for i in range(2)]
                            r_y3 = [Res("y3_%d" % i) for i in range(2)]
                            zf32 = sbt("zf32", [128, 3, 512])
                            zb16 = sbt("zb16", [128, 3, 512], BF16)
                            r_z = Res("z")
                            gt = [sbt("gt%d" % i, [128, 512]) for i in range(2)]
                            r_gt = [Res("gt%d" % i) for i in range(2)]
                            kk = 0
                            for ts in range(ST // 512):
                                tk = t0 + ts * 512
                                for c in range(3):
                                    b = kk % 2
                                    kk += 1
                                    fw.dma("sync", lambda e, c=c, tk=tk, b=b: e.dma_start(out=yf[b][:], in_=YA[0, c, :, tk:tk + 512]),
                                           writes=[r_y3[b]])
                                    fw.dma("sync", lambda e, c=c, tk=tk, b=b: e.dma_start(out=yb_[b][:], in_=YA[1, c, :, tk:tk + 512]),
                                           writes=[r_y3[b]])
                                    fw.dma("sync", lambda e, c=c, tk=tk, b=b: e.dma_start(out=uu[b][:], in_=UAs[c, :, tk:tk + 512]),
                                           writes=[r_y3[b]])
                                    fw.op("vector", lambda e, b=b: e.tensor_tensor(out=yf[b][:], in0=yf[b][:], in1=yb_[b][:], op=ALU.add),
                                          reads=[r_y3[b]], writes=[r_y3[b]])
                                    fw.op("vector", lambda e, b=b, c=c: e.scalar_tensor_tensor(
                                        out=yf[b][:], in0=uu[b][:], scalar=dvec[:, c:c + 1], in1=yf[b][:], op0=ALU.mult, op1=ALU.add),
                                        reads=[r_y3[b], r_sv], writes=[r_y3[b]])
                                    fw.op("scalar", lambda e, b=b, c=c: e.activation(out=zf32[:, c, :], in_=yf[b][:], func=AF.Gelu),
                                          reads=[r_y3[b]], writes=[r_z])
                                    fw.op("vector", lambda e, c=c: e.tensor_copy(out=zb16[:, c, :], in_=zf32[:, c, :]),
                                          reads=[r_z], writes=[r_z])
                                for co in range(3):
                                    j = cnt["pA"] % 4
                                    cnt["pA"] += 1
                                    for ci in range(3):
                                        fw.op("tensor", lambda e, ci=ci, co=co, j=j: e.matmul(
                                            pA[j][:], lhsT=gluw[:, ci, co * 128:(co + 1) * 128], rhs=zb16[:, ci, :],
                                            start=(ci == 0), stop=(ci == 2)), reads=[r_gluw, r_z], writes=[r_pA[j]], pe_acc=True)
                                    g2 = co % 2
                                    fw.op("vector", lambda e, j=j, g2=g2, co=co: e.tensor_scalar(
                                        out=gt[g2][:], in0=pA[j][:], scalar1=glub[:, co:co + 1], scalar2=None, op0=ALU.add),
                                        reads=[r_pA[j], r_sv], writes=[r_gt[g2]])
                                    fw.op("scalar", lambda e, g2=g2: e.activation(out=gt[g2][:], in_=gt[g2][:], func=AF.Sigmoid),
                                          reads=[r_gt[g2]], writes=[r_gt[g2]])
                                    fw.op("vector", lambda e, g2=g2, co=co, ts=ts: e.tensor_tensor(
                                        out=brT[:, co, ts * 512:(ts + 1) * 512], in0=zf32[:, co, :], in1=gt[g2][:], op=ALU.mult),
                                        reads=[r_z, r_gt[g2]], writes=[r_br])
                            cvt = sbt("cvt", [128, ST + 2], BF16)
                            r_cvt = Res("cvt")
                            accf = [sbt("accf%d" % i, [128, 512]) for i in range(2)]
                            r_accf = [Res("accf%d" % i) for i in range(2)]
                            for c in range(3):
                                wt, r_w = load_w_chunk([(wi[:, (6 + c) * 128:(7 + c) * 128], 0, 128)])
                                fw.dma("sync", lambda e, c=c, p0=p0: e.dma_start(out=cvt[:], in_=CVs[c, :, p0 - 1:p0 + ST + 1]),
                                       writes=[r_cvt])

                                def ev_gb(ts, j, pt, r_pt, c=c):
                                    a = j % 2
                                    o = ts * 512
                                    fw.op("vector", lambda e: e.tensor_scalar(out=accf[a][:], in0=cvt[:, o:o + 512],
                                                                              scalar1=cwt[:, 0, c:c + 1], scalar2=None, op0=ALU.mult),
                                          reads=[r_cvt, r_sv], writes=[r_accf[a]])
                                    for tap in (1, 2):
                                        fw.op("vector", lambda e, tap=tap: e.scalar_tensor_tensor(
                                            out=accf[a][:], in0=cvt[:, o + tap:o + tap + 512], scalar=cwt[:, tap, c:c + 1],
                                            in1=accf[a][:], op0=ALU.mult, op1=ALU.add),
                                            reads=[r_cvt, r_sv, r_accf[a]], writes=[r_accf[a]])
                                    fw.op("vector", lambda e: e.scalar_tensor_tensor(
                                        out=brT[:, 3 + c, o:o + 512], in0=accf[a][:], scalar=cbt[:, c:c + 1], in1=pt[:],
                                        op0=ALU.add, op1=ALU.mult), reads=[r_accf[a], r_sv, r_pt], writes=[r_br])
                                proj_fm(wt, r_w, ev_gb)
                            if stop_after == "branches":
                                fw.dma("sync", lambda e, t0=t0: e.dma_start(
                                    out=dbg["br"][:, :, t0:t0 + ST].rearrange("c p n -> p c n"), in_=brT[:]), reads=[r_br])
                                fw.barrier()
                                continue
                            wbr32 = [sbt("wbr32_%d" % i, [128, 3, 128]) for i in range(2)]
                            wbr = [sbt("wbr%d" % i, [128, 3, 128], BF16) for i in range(2)]
                            r_wbr = [Res("wbr%d" % i) for i in range(2)]
                            mac = sbt("mac", [128, ST])
                            r_mac = Res("mac")
                            mbf = [sbt("mbf%d" % i, [128, ST], BF16) for i in range(2)]
                            r_mbf = [Res("mbf%d" % i) for i in range(2)]
                            sgt = [sbt("sgt%d" % i, [128, 512]) for i in range(2)]
                            r_sgt = [Res("sgt%d" % i) for i in range(2)]
                            BRS = [("w_branch_a", 3, 0), ("w_branch_b", 3, 3), ("w_branch_c", 1, 6), ("w_branch_d", 3, 7)]
                            q = 0
                            for jo in range(8):
                                for br, (wn, nch, ch0) in enumerate(BRS):
                                    wg, r_wg = load_w_chunk([(wi[:, 3328 + br * 1024 + jo * 128:3328 + br * 1024 + (jo + 1) * 128], 0, 128)])
                                    wb = q % 2
                                    q += 1
                                    fw.dma("sync", lambda e, wn=wn, nch=nch, jo=jo, wb=wb: e.dma_start(
                                        out=wbr32[wb][:, 0:nch, :],
                                        in_=W[wn][l][:, jo * 128:(jo + 1) * 128].rearrange("(c p) n -> p c n", p=128)),
                                        writes=[r_wbr[wb]])
                                    fw.op("gpsimd", lambda e, nch=nch, wb=wb: e.tensor_copy(out=wbr[wb][:, 0:nch, :],
                                                                                          in_=wbr32[wb][:, 0:nch, :]),
                                          reads=[r_wbr[wb]], writes=[r_wbr[wb]])
                                    for ts in range(ST // 512):
                                        j = cnt["pA"] % 4
                                        cnt["pA"] += 1
                                        xk = (q + ts) % 2
                                        o = ts * 512
                                        for k in range(8):
                                            fw.op("tensor", lambda e, k=k, j=j, o=o, wg=wg: e.matmul(
                                                pA[j][:], lhsT=wg[:, k, :], rhs=hT[:, k, o:o + 512], start=(k == 0), stop=(k == 7)),
                                                reads=[r_wg, r_hT], writes=[r_pA[j]], pe_acc=True)
                                        for c in range(nch):
                                            fw.op("tensor", lambda e, c=c, xk=xk, o=o, wb=wb, ch0=ch0, nch=nch: e.matmul(
                                                pX[xk][:], lhsT=wbr[wb][:, c, :], rhs=brT[:, ch0 + c, o:o + 512],
                                                start=(c == 0), stop=(c == nch - 1)),
                                                reads=[r_wbr[wb], r_br], writes=[r_pX[xk]], pe_acc=True)
                                        fw.op("scalar", lambda e, j=j, xk=xk: e.activation(out=sgt[xk][:], in_=pA[j][:], func=AF.Sigmoid),
                                              reads=[r_pA[j]], writes=[r_sgt[xk]])
                                        if br == 0:
                                            fw.op("vector", lambda e, xk=xk, o=o: e.tensor_tensor(
                                                out=mac[:, o:o + 512], in0=sgt[xk][:], in1=pX[xk][:], op=ALU.mult),
                                                reads=[r_sgt[xk], r_pX[xk]], writes=[r_mac])
                                        else:
                                            fw.op("vector", lambda e, xk=xk: e.tensor_tensor(
                                                out=sgt[xk][:], in0=sgt[xk][:], in1=pX[xk][:], op=ALU.mult),
                                                reads=[r_sgt[xk], r_pX[xk]], writes=[r_sgt[xk]])
                                            fw.op("vector", lambda e, xk=xk, o=o: e.tensor_tensor(
                                                out=mac[:, o:o + 512], in0=mac[:, o:o + 512], in1=sgt[xk][:], op=ALU.add),
                                                reads=[r_sgt[xk], r_mac], writes=[r_mac])
                                mi = jo % 2
                                fw.op("scalar", lambda e, mi=mi: e.activation(out=mbf[mi][:], in_=mac[:], func=AF.Copy),
                                      reads=[r_mac], writes=[r_mbf[mi]])
                                fw.dma("sync", lambda e, jo=jo, t0=t0, mi=mi: e.dma_start(out=MTs[jo, :, t0:t0 + ST], in_=mbf[mi][:]),
                                       reads=[r_mbf[mi]])
                    fw.barrier()
                    if stop_after in ("attn", "branches"):
                        continue
                    with ExitStack() as S3:
                        def sbu(name, shape, dt=F32):
                            return S3.enter_context(nc.sbuf_tensor(uname("b4_" + name), shape, dt))
                        fw.dma("sync", lambda e, t0=t0: e.dma_start(
                            out=hT[:], in_=MTs[:, :, t0:t0 + ST].rearrange("k p n -> p k n")), writes=[r_hT])
                        wo32 = [sbu("wo32_%d" % i, [128, 8, 256]) for i in range(2)]
                        r_wo32 = [Res("wo32_%d" % i) for i in range(2)]
                        wo = sbu("wo", [128, 8, D], BF16)
                        r_wo = Res("wo")
                        for pc in range(4):
                            a = pc % 2
                            fw.dma("sync", lambda e, pc=pc, a=a: e.dma_start(
                                out=wo32[a][:], in_=W["w_o"][l][:, pc * 256:(pc + 1) * 256].rearrange("(k p) n -> p k n", p=128)),
                                writes=[r_wo32[a]])
                            fw.op("gpsimd", lambda e, pc=pc, a=a: e.tensor_copy(out=wo[:, :, pc * 256:(pc + 1) * 256], in_=wo32[a][:]),
                                  reads=[r_wo32[a]], writes=[r_wo])
                        load_gb("ln1_g", "ln1_b", l)
                        xb = [sbu("xb%d" % i, [128, D]) for i in range(2)]
                        r_xb = [Res("xb%d" % i) for i in range(2)]
                        tb = [sbu("tb%d" % i, [128, D]) for i in range(2)]
                        r_tb = [Res("tb%d" % i) for i in range(2)]
                        hb16 = [sbu("hb16_%d" % i, [128, D], BF16) for i in range(2)]
                        r_hb16 = [Res("hb16_%d" % i) for i in range(2)]
                        st4 = [sbu("st4_%d" % i, [128, 8]) for i in range(2)]
                        r_st4 = [Res("st4_%d" % i) for i in range(2)]
                        mo = [sbu("mo%d" % i, [128, D]) for i in range(2)]
                        r_mo = [Res("mo%d" % i) for i in range(2)]
                        tkb = [sbu("tkb%d" % i, [128, 8, 128], BF16) for i in range(2)]
                        r_tkb = [Res("tkb%d" % i) for i in range(2)]
                        for blk in range(ST // 128):
                            i = blk % 2
                            r0 = t0 + blk * 128
                            fw.dma("sync", lambda e, i=i, r0=r0: e.dma_start(out=xb[i][:], in_=H0[r0:r0 + 128, :]), writes=[r_xb[i]])
                            for nh in range(2):
                                j = cnt["pA"] % 4
                                cnt["pA"] += 1
                                for k in range(8):
                                    fw.op("tensor", lambda e, k=k, j=j, nh=nh, blk=blk: e.matmul(
                                        pA[j][:], lhsT=hT[:, k, blk * 128:(blk + 1) * 128], rhs=wo[:, k, nh * 512:(nh + 1) * 512],
                                        start=(k == 0), stop=(k == 7)), reads=[r_hT, r_wo], writes=[r_pA[j]], pe_acc=True)
                                fw.op("scalar", lambda e, i=i, j=j, nh=nh: e.activation(
                                    out=mo[i][:, nh * 512:(nh + 1) * 512], in_=pA[j][:], func=AF.Copy),
                                    reads=[r_pA[j]], writes=[r_mo[i]])
                            if debug:
                                fw.dma("sync", lambda e, i=i, r0=r0: e.dma_start(out=dbg["mix"][r0:r0 + 128, :], in_=mo[i][:]),
                                       reads=[r_mo[i]])
                            fw.op("vector", lambda e, i=i: e.scalar_tensor_tensor(
                                out=xb[i][:], in0=xb[i][:], scalar=ALPHA, in1=mo[i][:], op0=ALU.mult, op1=ALU.add),
                                reads=[r_xb[i], r_mo[i]], writes=[r_xb[i]])
                            fw.dma("sync", lambda e, i=i, r0=r0: e.dma_start(out=H1[r0:r0 + 128, :], in_=xb[i][:]), reads=[r_xb[i]])
                        fw.barrier()
                        for blk in range(ST // 128):
                            i = blk % 2
                            r0 = t0 + blk * 128
                            fw.dma("sync", lambda e, i=i, r0=r0: e.dma_start(out=xb[i][:], in_=H1[r0:r0 + 128, :]), writes=[r_xb[i]])
                            ln_rows(fw, xb[i], r_xb[i], tb[i], r_tb[i], st4[i], r_st4[i], gam, bet, r_gb, xb[i], r_xb[i])
                            if debug:
                                fw.dma("sync", lambda e, i=i, r0=r0: e.dma_start(out=dbg["st"][r0:r0 + 128, :], in_=st4[i][:]),
                                       reads=[r_st4[i]])
                            fw.dma("sync", lambda e, i=i, r0=r0: e.dma_start(out=H1[r0:r0 + 128, :], in_=xb[i][:]), reads=[r_xb[i]])
                            fw.op("scalar", lambda e, i=i: e.activation(out=hb16[i][:], in_=xb[i][:], func=AF.Copy),
                                  reads=[r_xb[i]], writes=[r_hb16[i]])
                            for k in range(8):
                                fw.op("tensor", lambda e, k=k, i=i: e.transpose(pT[i][:, k, :], hb16[i][:, k * 128:(k + 1) * 128], ident[:]),
                                      reads=[r_hb16[i], r_ident], writes=[r_pT[i]], pe_acc=True)
                            fw.op("vector", lambda e, i=i: e.tensor_copy(out=tkb[i][:], in_=pT[i][:]), reads=[r_pT[i]], writes=[r_tkb[i]])
                            fw.dma("sync", lambda e, i=i, r0=r0: e.dma_start(
                                out=HT[:, :, r0:r0 + 128].rearrange("k p n -> p k n"), in_=tkb[i][:]), reads=[r_tkb[i]])
                    fw.barrier()
                    if stop_after == "ln1":
                        continue
                    with ExitStack() as S4:
                        def sbv(name, shape, dt=F32):
                            return S4.enter_context(nc.sbuf_tensor(uname("b5_" + name), shape, dt))
                        fw.dma("sync", lambda e, t0=t0: e.dma_start(
                            out=hT[:], in_=HT[:, :, t0:t0 + ST].rearrange("k p n -> p k n")), writes=[r_hT])
                        wr32 = sbv("wr32", [128, 8, 20])
                        wr = sbv("wr", [128, 8, 20], BF16)
                        r_wr = Res("wr")
                        fw.dma("sync", lambda e: e.dma_start(out=wr32[:, :, 0:4], in_=W["router_group_w"][l].rearrange("(k p) n -> p k n", p=128),
                                                             allow_slow_non_contiguous=True), writes=[r_wr])
                        fw.dma("sync", lambda e: e.dma_start(out=wr32[:, :, 4:20], in_=W["router_expert_w"][l].rearrange("(k p) n -> p k n", p=128),
                                                             allow_slow_non_contiguous=True), writes=[r_wr])
                        fw.op("vector", lambda e: e.tensor_copy(out=wr[:], in_=wr32[:]), reads=[r_wr], writes=[r_wr])
                        rb = sbv("rb", [128, 20])
                        r_rb = Res("rb")
                        fw.dma("sync", lambda e: e.dma_start(out=rb[:, 0:4], in_=W["router_group_b"][l].partition_broadcast(128)), writes=[r_rb])
                        fw.dma("sync", lambda e: e.dma_start(out=rb[:, 4:20], in_=W["router_expert_b"][l].partition_broadcast(128)), writes=[r_rb])
                        comb = sbv("comb", [128, ST // 128, 16])
                        r_comb = Res("comb")
                        rt = sbv("rt", [128, 64])
                        r_rt = Res("rt")
                        for blk in range(ST // 128):
                            xk = blk % 2
                            for k in range(8):
                                fw.op("tensor", lambda e, k=k, xk=xk, blk=blk: e.matmul(
                                    pX[xk][:, 0:20], lhsT=hT[:, k, blk * 128:(blk + 1) * 128], rhs=wr[:, k, :],
                                    start=(k == 0), stop=(k == 7)), reads=[r_hT, r_wr], writes=[r_pX[xk]], pe_acc=True)
                            lg = rt[:, 0:20]

                            def V(fn, extra_r=()):
                                fw.op("vector", fn, reads=[r_rt] + list(extra_r), writes=[r_rt])
                            fw.op("vector", lambda e, xk=xk: e.tensor_tensor(out=rt[:, 0:20], in0=pX[xk][:, 0:20], in1=rb[:], op=ALU.add),
                                  reads=[r_pX[xk], r_rb], writes=[r_rt])
                            V(lambda e: e.reduce_max(out=rt[:, 20:21], in_=rt[:, 0:4], axis=AX.X))
                            V(lambda e: e.tensor_scalar(out=rt[:, 24:28], in0=rt[:, 0:4], scalar1=rt[:, 20:21], scalar2=None, op0=ALU.subtract))
                            fw.op("scalar", lambda e: e.activation(out=rt[:, 28:32], in_=rt[:, 24:28], func=AF.Exp), reads=[r_rt], writes=[r_rt])
                            V(lambda e: e.reduce_sum(out=rt[:, 21:22], in_=rt[:, 28:32], axis=AX.X))
                            V(lambda e: e.reciprocal(out=rt[:, 21:22], in_=rt[:, 21:22]))
                            V(lambda e: e.tensor_scalar(out=rt[:, 24:28], in0=rt[:, 24:28], scalar1=-1e30, scalar2=1.0, op0=ALU.mult, op1=ALU.min))
                            V(lambda e: e.tensor_scalar(out=rt[:, 24:28], in0=rt[:, 24:28], scalar1=-1.0, scalar2=1.0, op0=ALU.mult, op1=ALU.add))
                            V(lambda e: e.tensor_scalar(out=rt[:, 32:36], in0=rt[:, 4:8], scalar1=rt[:, 24:25], scalar2=None, op0=ALU.mult))
                            for gq in range(1, 4):
                                V(lambda e, gq=gq: e.scalar_tensor_tensor(out=rt[:, 32:36], in0=rt[:, 4 + 4 * gq:8 + 4 * gq],
                                                                          scalar=rt[:, 24 + gq:25 + gq], in1=rt[:, 32:36],
                                                                          op0=ALU.mult, op1=ALU.add))
                            V(lambda e: e.reduce_max(out=rt[:, 22:23], in_=rt[:, 32:36], axis=AX.X))
                            V(lambda e: e.tensor_scalar(out=rt[:, 36:40], in0=rt[:, 32:36], scalar1=rt[:, 22:23], scalar2=None, op0=ALU.subtract))
                            V(lambda e: e.tensor_scalar(out=rt[:, 36:40], in0=rt[:, 36:40], scalar1=-1e30, scalar2=1.0, op0=ALU.mult, op1=ALU.min))
                            V(lambda e: e.tensor_scalar(out=rt[:, 36:40], in0=rt[:, 36:40], scalar1=-1.0, scalar2=1.0, op0=ALU.mult, op1=ALU.add))
                            V(lambda e: e.scalar_tensor_tensor(out=rt[:, 40:44], in0=rt[:, 36:40], scalar=-1e4, in1=rt[:, 32:36],
                                                               op0=ALU.mult, op1=ALU.add))
                            V(lambda e: e.reduce_max(out=rt[:, 23:24], in_=rt[:, 40:44], axis=AX.X))
                            V(lambda e: e.tensor_scalar(out=rt[:, 44:48], in0=rt[:, 40:44], scalar1=rt[:, 23:24], scalar2=None, op0=ALU.subtract))
                            V(lambda e: e.tensor_scalar(out=rt[:, 44:48], in0=rt[:, 44:48], scalar1=-1e30, scalar2=1.0, op0=ALU.mult, op1=ALU.min))
                            V(lambda e: e.tensor_scalar(out=rt[:, 44:48], in0=rt[:, 44:48], scalar1=-1.0, scalar2=1.0, op0=ALU.mult, op1=ALU.add))
                            V(lambda e: e.tensor_tensor(out=rt[:, 48:49], in0=rt[:, 23:24], in1=rt[:, 22:23], op=ALU.subtract))
                            fw.op("scalar", lambda e: e.activation(out=rt[:, 49:50], in_=rt[:, 48:49], func=AF.Exp), reads=[r_rt], writes=[r_rt])
                            V(lambda e: e.tensor_scalar(out=rt[:, 50:51], in0=rt[:, 49:50], scalar1=1.0, scalar2=None, op0=ALU.add))
                            V(lambda e: e.reciprocal(out=rt[:, 50:51], in_=rt[:, 50:51]))
                            V(lambda e: e.tensor_tensor(out=rt[:, 51:52], in0=rt[:, 49:50], in1=rt[:, 50:51], op=ALU.mult))
                            V(lambda e: e.tensor_tensor(out=rt[:, 50:51], in0=rt[:, 50:51], in1=rt[:, 21:22], op=ALU.mult))
                            V(lambda e: e.tensor_tensor(out=rt[:, 51:52], in0=rt[:, 51:52], in1=rt[:, 21:22], op=ALU.mult))
                            V(lambda e: e.tensor_scalar(out=rt[:, 52:56], in0=rt[:, 36:40], scalar1=rt[:, 50:51], scalar2=None, op0=ALU.mult))
                            V(lambda e: e.scalar_tensor_tensor(out=rt[:, 52:56], in0=rt[:, 44:48], scalar=rt[:, 51:52], in1=rt[:, 52:56],
                                                               op0=ALU.mult, op1=ALU.add))
                            for gq in range(4):
                                fw.op("vector", lambda e, gq=gq, blk=blk: e.tensor_scalar(
                                    out=comb[:, blk, 4 * gq:4 * gq + 4], in0=rt[:, 52:56], scalar1=rt[:, 24 + gq:25 + gq], scalar2=None,
                                    op0=ALU.mult), reads=[r_rt], writes=[r_comb])
                        wg32 = sbv("wg32", [128, 8, 256])
                        wu32 = sbv("wu32", [128, 8, 256])
                        wgb = sbv("wgb", [128, 8, 256], BF16)
                        wub = sbv("wub", [128, 8, 256], BF16)
                        wd32 = sbv("wd32", [128, 2, D])
                        wdb = sbv("wdb", [128, 2, D], BF16)
                        r_wg32, r_wu32, r_wd32 = Res("wg32"), Res("wu32"), Res("wd32")
                        r_wgb, r_wub, r_wdb = Res("wgb"), Res("wub"), Res("wdb")
                        HB = ST // 256
                        macc = sbv("macc", [128, HB, D])
                        r_macc = Res("macc")
                        actT = sbv("actT", [128, 2, ST // 2], BF16)
                        r_act = Res("actT")
                        sgl = [sbv("sgl%d" % i, [128, 512]) for i in range(2)]
                        r_sgl = [Res("sgl%d" % i) for i in range(2)]
                        xq = [sbv("xq%d" % i, [128, D]) for i in range(2)]
                        r_xq = [Res("xq%d" % i) for i in range(2)]
                        tq = [sbv("tq%d" % i, [128, D]) for i in range(2)]
                        r_tq = [Res("tq%d" % i) for i in range(2)]
                        sq4 = [sbv("sq4_%d" % i, [128, 8]) for i in range(2)]
                        r_sq4 = [Res("sq4_%d" % i) for i in range(2)]
                        load_gb("ln2_g", "ln2_b", l)
                        for hv in range(2):
                            c0 = hv * (ST // 2)
                            for ex in range(16):
                                fw.dma("sync", lambda e, ex=ex: e.dma_start(
                                    out=wg32[:], in_=W["expert_w_gate"][l, ex].rearrange("(k p) n -> p k n", p=128)), writes=[r_wg32])
                                fw.op("gpsimd", lambda e: e.tensor_copy(out=wgb[:], in_=wg32[:]), reads=[r_wg32], writes=[r_wgb])
                                fw.dma("sync", lambda e, ex=ex: e.dma_start(
                                    out=wu32[:], in_=W["expert_w_up"][l, ex].rearrange("(k p) n -> p k n", p=128)), writes=[r_wu32])
                                fw.op("gpsimd", lambda e: e.tensor_copy(out=wub[:], in_=wu32[:]), reads=[r_wu32], writes=[r_wub])
                                fw.dma("sync", lambda e, ex=ex: e.dma_start(
                                    out=wd32[:], in_=W["expert_w_down"][l, ex].rearrange("(c p) n -> p c n", p=128)), writes=[r_wd32])
                                fw.op("gpsimd", lambda e: e.tensor_copy(out=wdb[:], in_=wd32[:]), reads=[r_wd32], writes=[r_wdb])
                                for c in range(2):
                                    for ts in range(ST // 1024):
                                        o = c0 + ts * 512
                                        j = cnt["pA"] % 4
                                        cnt["pA"] += 1
                                        j2 = cnt["pA"] % 4
                                        cnt["pA"] += 1
                                        for k in range(8):
                                            fw.op("tensor", lambda e, k=k, j=j, c=c, o=o: e.matmul(
                                                pA[j][:], lhsT=wgb[:, k, c * 128:(c + 1) * 128], rhs=hT[:, k, o:o + 512],
                                                start=(k == 0), stop=(k == 7)), reads=[r_wgb, r_hT], writes=[r_pA[j]], pe_acc=True)
                                        for k in range(8):
                                            fw.op("tensor", lambda e, k=k, j2=j2, c=c, o=o: e.matmul(
                                                pA[j2][:], lhsT=wub[:, k, c * 128:(c + 1) * 128], rhs=hT[:, k, o:o + 512],
                                                start=(k == 0), stop=(k == 7)), reads=[r_wub, r_hT], writes=[r_pA[j2]], pe_acc=True)
                                        si = (c + ts) % 2
                                        fw.op("scalar", lambda e, j=j, si=si: e.activation(out=sgl[si][:], in_=pA[j][:], func=AF.Silu),
                                              reads=[r_pA[j]], writes=[r_sgl[si]])
                                        fw.op("vector", lambda e, j2=j2, si=si, c=c, ts=ts: e.tensor_tensor(
                                            out=actT[:, c, ts * 512:(ts + 1) * 512], in0=sgl[si][:], in1=pA[j2][:], op=ALU.mult),
                                            reads=[r_sgl[si], r_pA[j2]], writes=[r_act])
                                for bl in range(HB):
                                    blk = hv * HB + bl
                                    for nh in range(2):
                                        xk = (bl * 2 + nh) % 2
                                        for c in range(2):
                                            fw.op("tensor", lambda e, c=c, xk=xk, bl=bl, nh=nh: e.matmul(
                                                pX[xk][:], lhsT=actT[:, c, bl * 128:(bl + 1) * 128], rhs=wdb[:, c, nh * 512:(nh + 1) * 512],
                                                start=(c == 0), stop=(c == 1)), reads=[r_act, r_wdb], writes=[r_pX[xk]], pe_acc=True)
                                        if ex == 0:
                                            fw.op("vector", lambda e, xk=xk, bl=bl, nh=nh, blk=blk, ex=ex: e.tensor_scalar(
                                                out=macc[:, bl, nh * 512:(nh + 1) * 512], in0=pX[xk][:], scalar1=comb[:, blk, ex:ex + 1],
                                                scalar2=None, op0=ALU.mult), reads=[r_pX[xk], r_comb], writes=[r_macc])
                                        else:
                                            fw.op("vector", lambda e, xk=xk, bl=bl, nh=nh, blk=blk, ex=ex: e.scalar_tensor_tensor(
                                                out=macc[:, bl, nh * 512:(nh + 1) * 512], in0=pX[xk][:], scalar=comb[:, blk, ex:ex + 1],
                                                in1=macc[:, bl, nh * 512:(nh + 1) * 512], op0=ALU.mult, op1=ALU.add),
                                                reads=[r_pX[xk], r_comb, r_macc], writes=[r_macc])
                            dst = y if last else H0
                            for bl in range(HB):
                                i = bl % 2
                                r0 = t0 + (hv * HB + bl) * 128
                                fw.dma("sync", lambda e, i=i, r0=r0: e.dma_start(out=xq[i][:], in_=H1[r0:r0 + 128, :]), writes=[r_xq[i]])
                                fw.op("vector", lambda e, i=i, bl=bl: e.scalar_tensor_tensor(
                                    out=xq[i][:], in0=xq[i][:], scalar=ALPHA, in1=macc[:, bl, :], op0=ALU.mult, op1=ALU.add),
                                    reads=[r_xq[i], r_macc], writes=[r_xq[i]])
                                ln_rows(fw, xq[i], r_xq[i], tq[i], r_tq[i], sq4[i], r_sq4[i], gam, bet, r_gb, xq[i], r_xq[i])
                                fw.dma("sync", lambda e, i=i, r0=r0, dst=dst: e.dma_start(out=dst[r0:r0 + 128, :], in_=xq[i][:]),
                                       reads=[r_xq[i]])
                    fw.barrier()
            fw.barrier()

        if debug:
            dbg["br"] = nc.dram_tensor("dbg_br", [10, 128, NTOK], BF16, kind="ExternalOutput").ap()
        for l in range(depth):
            phase_a(l)
            if stop_after in ("a", "ua"):
                break
            phase_s5(l)
            if stop_after == "s5":
                break
            phase_h()
            phase_b(l, l == depth - 1)
            if stop_after in ("attn", "branches", "ln1"):
                break
    return nc, fw


def ln_rows(fw, xin, r_xin, tmp, r_tmp, s, r_s, gam, bet, r_gb, out_tile, r_out):
    fw.op("vector", lambda e: e.reduce_sum(out=s[:, 0:1], in_=xin[:], axis=AX.X), reads=[r_xin], writes=[r_s])
    fw.op("scalar", lambda e: e.activation(out=tmp[:], in_=xin[:], func=AF.Square), reads=[r_xin, r_s], writes=[r_tmp])
    fw.op("vector", lambda e: e.reduce_sum(out=s[:, 1:2], in_=tmp[:], axis=AX.X), reads=[r_tmp], writes=[r_s])
    fw.op("vector", lambda e: e.tensor_scalar(out=s[:, 2:3], in0=s[:, 0:1], scalar1=1.0 / D, scalar2=None,
                                              op0=ALU.mult), reads=[r_s], writes=[r_s])
    fw.op("vector", lambda e: e.tensor_tensor(out=s[:, 3:4], in0=s[:, 2:3], in1=s[:, 2:3], op=ALU.mult),
          reads=[r_s], writes=[r_s])
    fw.op("vector", lambda e: e.scalar_tensor_tensor(out=s[:, 4:5], in0=s[:, 1:2], scalar=1.0 / D, in1=s[:, 3:4],
                                                     op0=ALU.mult, op1=ALU.subtract), reads=[r_s], writes=[r_s])
    fw.op("vector", lambda e: e.tensor_scalar(out=s[:, 4:5], in0=s[:, 4:5], scalar1=LN_EPS, scalar2=None,
                                              op0=ALU.add), reads=[r_s], writes=[r_s])
    fw.op("scalar", lambda e: e.activation(out=s[:, 5:6], in_=s[:, 4:5], func=AF.Sqrt), reads=[r_s], writes=[r_s])
    fw.op("vector", lambda e: e.reciprocal(out=s[:, 6:7], in_=s[:, 5:6]), reads=[r_s], writes=[r_s])
    fw.op("vector", lambda e: e.scalar_tensor_tensor(out=s[:, 7:8], in0=s[:, 2:3], scalar=-1.0, in1=s[:, 6:7],
                                                     op0=ALU.mult, op1=ALU.mult), reads=[r_s], writes=[r_s])
    fw.op("vector", lambda e: e.tensor_scalar(out=tmp[:], in0=xin[:], scalar1=s[:, 2:3], scalar2=s[:, 6:7],
                                              op0=ALU.subtract, op1=ALU.mult), reads=[r_xin, r_s], writes=[r_tmp])
    fw.op("vector", lambda e: e.tensor_tensor(out=tmp[:], in0=tmp[:], in1=gam[:], op=ALU.mult),
          reads=[r_tmp, r_gb], writes=[r_tmp])
    fw.op("vector", lambda e: e.tensor_tensor(out=out_tile[:], in0=tmp[:], in1=bet[:], op=ALU.add),
          reads=[r_tmp, r_gb], writes=[r_out])


_CACHE = {}


def kernel(**inputs):
    xp = np.ascontiguousarray(inputs["x_prompt"], dtype=np.float32)
    xs = np.ascontiguousarray(inputs["x_sample"], dtype=np.float32)
    slots = {0: [("s", 0)], 1: [("s", 1)], 2: [("p", 0), ("p", 1)], 3: [("p", 2), ("p", 3)],
             4: [("p", 4)], 5: [("p", 5)], 6: [("p", 6)], 7: [("p", 7)]}
    in_maps = []
    for c in range(8):
        xc = np.zeros((NTOK, D), np.float32)
        fl = np.zeros(5, np.float32)
        if slots[c][0][0] == "s":
            xc[:] = xs[slots[c][0][1]]
            fl[1:4] = 1.0
        else:
            for j, (_, pi) in enumerate(slots[c]):
                xc[j * SEG:(j + 1) * SEG] = xp[pi]
        m = {"x": xc, "flags": fl}
        for n in WNAMES:
            m[n] = np.ascontiguousarray(inputs[n], dtype=np.float32)
        in_maps.append(m)
    if "nc" not in _CACHE:
        nc, fw = build()
        fw.finish(_CACHE.get("final", []))
        _CACHE["nc"] = nc
    res = run_bass_kernel_spmd(_CACHE["nc"], in_maps, core_ids=list(range(8)))
    yp = np.zeros_like(xp)
    ys = np.zeros_like(xs)
    for c in range(8):
        yc = np.asarray(res.results[c]["y"], dtype=np.float32)
        if slots[c][0][0] == "s":
            ys[slots[c][0][1]] = yc
        else:
            for j, (_, pi) in enumerate(slots[c]):
                yp[pi] = yc[j * SEG:(j + 1) * SEG]
    return (yp, ys)
```

```python
import math
import numpy as np
from contextlib import ExitStack
import concourse.bass as bass
import concourse.mybir as mybir
from concourse.bass_utils import run_bass_kernel_spmd

F32 = mybir.dt.float32
BF16 = mybir.dt.bfloat16
AF = mybir.ActivationFunctionType
ALU = mybir.AluOpType
AX = mybir.AxisListType

D = 1024
NSEG = 4
SEG = 4096
NTOK = NSEG * SEG
PAD = 1024
SEGP = SEG + 2 * PAD
NTOKP = NSEG * SEGP
ST = 2048
NST = NTOK // ST
DEPTH = 2
ALPHA = (2 * DEPTH) ** 0.25
LN_EPS = 1e-5
IN_COLS = 7424
SLOPES = [2.0 ** (-8.0 * (i + 1) / 12) for i in range(12)]
DIL = [(128, 1), (512, 4), (2048, 16)]

WNAMES = ["ln_in_g", "ln_in_b", "w_in", "s5_a_re", "s5_a_im", "s5_log_dt", "s5_b_re", "s5_b_im", "s5_c_re",
          "s5_c_im", "s5_d", "s5_glu_w", "s5_glu_b", "conv_w", "conv_b", "swa_sink", "w_branch_a", "w_branch_b",
          "w_branch_c", "w_branch_d", "w_o", "ln1_g", "ln1_b", "router_group_w", "router_group_b",
          "router_expert_w", "router_expert_b", "expert_w_gate", "expert_w_up", "expert_w_down", "ln2_g", "ln2_b"]
WSHAPES = {
    "ln_in_g": [D], "ln_in_b": [D], "w_in": [2, D, IN_COLS], "s5_a_re": [2, 2, 24, 64], "s5_a_im": [2, 2, 24, 64],
    "s5_log_dt": [2, 2, 24], "s5_b_re": [2, 2, 24, 64, 16], "s5_b_im": [2, 2, 24, 64, 16],
    "s5_c_re": [2, 2, 24, 16, 64], "s5_c_im": [2, 2, 24, 16, 64], "s5_d": [2, 384], "s5_glu_w": [2, 384, 384],
    "s5_glu_b": [2, 384], "conv_w": [2, 3, 384], "conv_b": [2, 384], "swa_sink": [2, 6],
    "w_branch_a": [2, 384, D], "w_branch_b": [2, 384, D], "w_branch_c": [2, 128, D], "w_branch_d": [2, 384, D],
    "w_o": [2, D, D], "ln1_g": [2, D], "ln1_b": [2, D], "router_group_w": [2, D, 4], "router_group_b": [2, 4],
    "router_expert_w": [2, D, 16], "router_expert_b": [2, 16], "expert_w_gate": [2, 16, D, 256],
    "expert_w_up": [2, 16, D, 256], "expert_w_down": [2, 16, 256, D], "ln2_g": [2, D], "ln2_b": [2, D],
}


ENGS = ["sync", "scalar", "vector", "gpsimd", "tensor"]
SEM_ROLL = 30000


class Res:
    __slots__ = ("name", "w", "r")

    def __init__(self, name):
        self.name = name
        self.w = None
        self.r = []


class FW:
    def __init__(self, nc, es):
        self.nc = nc
        self.es = es
        self.q = {e: [] for e in ENGS}
        self.sems = {e: [es.enter_context(nc.semaphore("s_" + e + "0"))] for e in ENGS}
        self.cnt = {e: 0 for e in ENGS}
        self.seen = {e: {} for e in ENGS}
        self.dma_sems = [es.enter_context(nc.semaphore("d%d" % i)) for i in range(24)]
        self.dma_cnt = [0] * 24
        self.dma_i = 0
        self.n_ops = 0
        self.fence = []

    def barrier(self):
        f = []
        for e in ENGS:
            if self.cnt[e] > 0:
                f.append((self.sems[e][-1], self.cnt[e], e))
        for k in range(len(self.dma_sems)):
            if self.dma_cnt[k] > 0:
                f.append((self.dma_sems[k], self.dma_cnt[k], "dma"))
        self.fence = f

    def _ev_new(self, eng):
        if self.cnt[eng] >= SEM_ROLL:
            self.sems[eng].append(self.es.enter_context(self.nc.semaphore("s_%s%d" % (eng, len(self.sems[eng])))))
            self.cnt[eng] = 0
        self.cnt[eng] += 1
        return (self.sems[eng][-1], self.cnt[eng], eng)

    def _need(self, eng, ev, waits, pe_ok=False):
        if ev is None:
            return
        sem, val, src = ev
        if pe_ok and src == "tensor" and eng == "tensor":
            return
        key = id(sem)
        if self.seen[eng].get(key, 0) >= val:
            return
        if key not in waits or waits[key][1] < val:
            waits[key] = (sem, val)

    def op(self, eng, fn, reads=(), writes=(), pe_acc=False):
        waits = {}
        for ev in self.fence:
            self._need(eng, ev, waits)
        for r in reads:
            self._need(eng, r.w, waits)
        for w in writes:
            self._need(eng, w.w, waits, pe_ok=pe_acc)
            for ev in w.r:
                self._need(eng, ev, waits)
        for key, (sem, val) in waits.items():
            self.seen[eng][key] = val
        ev = self._ev_new(eng)
        self.q[eng].append((list(waits.values()), fn, (ev[0], 1)))
        for r in reads:
            r.r.append(ev)
        for w in writes:
            w.w = ev
            w.r = []
        self.n_ops += 1
        return ev

    def dma(self, eng, fn, reads=(), writes=()):
        if len(writes) == 0 and eng == "sync":
            eng = "scalar"
        waits = {}
        for ev in self.fence:
            self._need(eng, ev, waits)
        for r in reads:
            self._need(eng, r.w, waits)
        for w in writes:
            self._need(eng, w.w, waits)
            for ev in w.r:
                self._need(eng, ev, waits)
        k = self.dma_i % len(self.dma_sems)
        self.dma_i += 1
        sem = self.dma_sems[k]
        if self.dma_cnt[k] > 0:
            self._need(eng, (sem, self.dma_cnt[k], "dma"), waits)
        for key, (s, val) in waits.items():
            self.seen[eng][key] = val
        self.dma_cnt[k] += 16
        ev = (sem, self.dma_cnt[k], "dma")
        self.q[eng].append((list(waits.values()), fn, (sem, 16)))
        for r in reads:
            r.r.append(ev)
        for w in writes:
            w.w = ev
            w.r = []
        self.n_ops += 1
        return ev

    def finish(self, final_res):
        waits = {}
        for r in final_res:
            self._need("sync", r.w, waits)
        for k in range(len(self.dma_sems)):
            if self.dma_cnt[k] > 0:
                self._need("sync", (self.dma_sems[k], self.dma_cnt[k], "dma"), waits)
        tail = list(waits.values())
        q = self.q
        with self.nc.Block() as block:
            def replay(e, name):
                for ws, fn, inc in q[name]:
                    for sem, val in ws:
                        e.wait_ge(sem, val)
                    fn(e).then_inc(inc[0], inc[1])
                if name == "sync":
                    for sem, val in tail:
                        e.wait_ge(sem, val)

            @block.sync
            def _(e):
                replay(e, "sync")

            @block.scalar
            def _(e):
                replay(e, "scalar")

            @block.vector
            def _(e):
                replay(e, "vector")

            @block.gpsimd
            def _(e):
                replay(e, "gpsimd")

            @block.tensor
            def _(e):
                replay(e, "tensor")


def build(debug=False, stop_after=None, depth=DEPTH):
    nc = bass.Bass("TRN2", target_bir_lowering=False)
    x = nc.dram_tensor("x", [NTOK, D], F32, kind="ExternalInput").ap()
    flags_d = nc.dram_tensor("flags", [5], F32, kind="ExternalInput").ap()
    W = {n: nc.dram_tensor(n, WSHAPES[n], F32, kind="ExternalInput").ap() for n in WNAMES}
    y = nc.dram_tensor("y", [NTOK, D], F32, kind="ExternalOutput").ap()
    H0 = nc.dram_tensor("H0", [NTOK, D], F32).ap()
    H1 = nc.dram_tensor("H1", [NTOK, D], F32, kind=("ExternalOutput" if debug else "Internal")).ap()
    HT = nc.dram_tensor("HT", [8, 128, NTOK], BF16).ap()
    UAs = nc.dram_tensor("UAs", [3, 128, NTOK], F32).ap()
    CVs = nc.dram_tensor("CVs", [3, 128, NTOKP], BF16).ap()
    KTs = nc.dram_tensor("KTs", [4, 128, NTOKP], BF16).ap()
    VTs = nc.dram_tensor("VTs", [4, 128, NTOKP], BF16).ap()
    MTs = nc.dram_tensor("MTs", [8, 128, NTOK], BF16, kind=("ExternalOutput" if debug else "Internal")).ap()
    YA = nc.dram_tensor("YA", [2, 3, 128, NTOK], F32, kind=("ExternalOutput" if debug else "Internal")).ap()
    dbg = {}
    if debug:
        dbg["h0"] = nc.dram_tensor("dbg_h0", [NTOK, D], F32, kind="ExternalOutput").ap()
        dbg["ua"] = nc.dram_tensor("dbg_ua", [3, 128, NTOK], F32, kind="ExternalOutput").ap()
        dbg["mix"] = nc.dram_tensor("dbg_mix", [NTOK, D], F32, kind="ExternalOutput").ap()
        dbg["st"] = nc.dram_tensor("dbg_st", [NTOK, 8], F32, kind="ExternalOutput").ap()

    es = ExitStack()
    with es:
        fw = FW(nc, es)

        def sb(name, shape, dt=F32):
            return es.enter_context(nc.sbuf_tensor(name, shape, dt))

        def ps(name, shape, dt=F32):
            return es.enter_context(nc.psum_tensor(name, shape, dt))

        ident = sb("ident", [128, 128], BF16)
        r_ident = Res("ident")
        fw.op("gpsimd", lambda e: e.memset(ident[:], 0.0), writes=[r_ident])
        fw.op("gpsimd", lambda e: e.affine_select(out=ident[:], in_=ident[:], pattern=[[-1, 128]],
                                                  compare_op=ALU.not_equal, fill=1.0, base=0, channel_multiplier=1),
              reads=[r_ident], writes=[r_ident])
        flg = sb("flg", [128, 5])
        r_flg = Res("flg")
        fw.dma("sync", lambda e: e.dma_start(out=flg[:], in_=flags_d.partition_broadcast(128)), writes=[r_flg])
        gam = sb("gam", [128, D])
        bet = sb("bet", [128, D])
        r_gb = Res("gb")

        def load_gb(gname, bname, l):
            gsrc = W[gname] if l is None else W[gname][l]
            bsrc = W[bname] if l is None else W[bname][l]
            fw.dma("sync", lambda e: e.dma_start(out=gam[:], in_=gsrc.partition_broadcast(128)), writes=[r_gb])
            fw.dma("sync", lambda e: e.dma_start(out=bet[:], in_=bsrc.partition_broadcast(128)), writes=[r_gb])

        pT = [ps("pT%d" % i, [128, 8, 128], BF16) for i in range(2)]
        r_pT = [Res("pT%d" % i) for i in range(2)]
        pA = [ps("pA%d" % i, [128, 512]) for i in range(4)]
        r_pA = [Res("pA%d" % i) for i in range(4)]
        pX = [ps("pX%d" % i, [128, 512]) for i in range(2)]
        r_pX = [Res("pX%d" % i) for i in range(2)]
        cnt = {"w": 0, "pA": 0, "blk": 0, "uid": 0}

        def uname(n):
            cnt["uid"] += 1
            return "%s_%d" % (n, cnt["uid"])

        def padpos(t):
            return (t // SEG) * SEGP + PAD + (t % SEG)

        def phase_a(l):
            with ExitStack() as pes:
                def sb2(name, shape, dt=F32):
                    return pes.enter_context(nc.sbuf_tensor(uname("a_" + name), shape, dt))
                hT = sb2("hT", [128, 8, ST], BF16)
                r_hT = Res("hT")
                xb = [sb2("xb%d" % i, [128, D]) for i in range(2)]
                r_xb = [Res("xb%d" % i) for i in range(2)]
                tb = [sb2("tb%d" % i, [128, D]) for i in range(2)]
                r_tb = [Res("tb%d" % i) for i in range(2)]
                hb16 = [sb2("hb16_%d" % i, [128, D], BF16) for i in range(2)]
                r_hb16 = [Res("hb16_%d" % i) for i in range(2)]
                st4 = [sb2("st4_%d" % i, [128, 8]) for i in range(2)]
                r_st4 = [Res("st4_%d" % i) for i in range(2)]
                wst = [sb2("wst%d" % i, [128, 8, 128]) for i in range(2)]
                r_wst = [Res("wst%d" % i) for i in range(2)]
                wbf = [sb2("wbf%d" % i, [128, 8, 128], BF16) for i in range(2)]
                r_wbf = [Res("wbf%d" % i) for i in range(2)]
                zt0 = sb2("zt0", [128, ST], BF16)
                r_zt0 = Res("zt0")
                zf = [sb2("zf%d" % i, [128, 512]) for i in range(4)]
                r_zf = [Res("zf%d" % i) for i in range(4)]
                zb = [sb2("zb%d" % i, [128, 512], BF16) for i in range(4)]
                r_zb = [Res("zb%d" % i) for i in range(4)]

                def layer_norm_block(i):
                    ln_rows(fw, xb[i], r_xb[i], tb[i], r_tb[i], st4[i], r_st4[i], gam, bet, r_gb, xb[i], r_xb[i])

                def transpose_block(i, col0):
                    fw.op("scalar", lambda e: e.activation(out=hb16[i][:], in_=xb[i][:], func=AF.Copy),
                          reads=[r_xb[i]], writes=[r_hb16[i]])
                    for k in range(8):
                        fw.op("tensor", lambda e, k=k: e.transpose(pT[i][:, k, :], hb16[i][:, k * 128:(k + 1) * 128],
                                                                   ident[:]),
                              reads=[r_hb16[i], r_ident], writes=[r_pT[i]], pe_acc=True)
                    fw.op("vector", lambda e: e.tensor_copy(out=hT[:, :, col0:col0 + 128], in_=pT[i][:]),
                          reads=[r_pT[i]], writes=[r_hT])

                def load_w_chunk(src_ap):
                    j = cnt["w"] % 2
                    cnt["w"] += 1
                    fw.dma("sync", lambda e: e.dma_start(out=wst[j][:], in_=src_ap.rearrange("(k p) n -> p k n", p=128)),
                           writes=[r_wst[j]])
                    fw.op("gpsimd", lambda e: e.tensor_copy(out=wbf[j][:], in_=wst[j][:]),
                          reads=[r_wst[j]], writes=[r_wbf[j]])
                    return wbf[j], r_wbf[j]

                def proj_fm(wt, r_w, evac):
                    for ts in range(ST // 512):
                        j = cnt["pA"] % 4
                        cnt["pA"] += 1
                        for k in range(8):
                            fw.op("tensor", lambda e, k=k, j=j, ts=ts: e.matmul(
                                pA[j][:], lhsT=wt[:, k, :], rhs=hT[:, k, ts * 512:(ts + 1) * 512],
                                start=(k == 0), stop=(k == 7)),
                                reads=[r_w, r_hT], writes=[r_pA[j]], pe_acc=True)
                        evac(ts, j, pA[j], r_pA[j])

                if l == 0:
                    load_gb("ln_in_g", "ln_in_b", None)
                for st_i in range(NST):
                    t0 = st_i * ST
                    p0 = padpos(t0)
                    for b in range(ST // 128):
                        i = cnt["blk"] % 2
                        cnt["blk"] += 1
                        r0 = t0 + b * 128
                        if l == 0:
                            fw.dma("sync", lambda e, i=i, r0=r0: e.dma_start(out=xb[i][:], in_=x[r0:r0 + 128, :]),
                                   writes=[r_xb[i]])
                            layer_norm_block(i)
                            fw.dma("sync", lambda e, i=i, r0=r0: e.dma_start(out=H0[r0:r0 + 128, :], in_=xb[i][:]),
                                   reads=[r_xb[i]])
                            if debug:
                                fw.dma("sync", lambda e, i=i, r0=r0: e.dma_start(out=dbg["h0"][r0:r0 + 128, :],
                                                                                in_=xb[i][:]), reads=[r_xb[i]])
                        else:
                            fw.dma("sync", lambda e, i=i, r0=r0: e.dma_start(out=xb[i][:], in_=H0[r0:r0 + 128, :]),
                                   writes=[r_xb[i]])
                        transpose_block(i, b * 128)
                    fw.dma("sync", lambda e, t0=t0: e.dma_start(out=HT[:, :, t0:t0 + ST].rearrange("k p n -> p k n"),
                                                                in_=hT[:]), reads=[r_hT])

                    def store_plain(dst, c, t0=t0):
                        def ev(ts, j, pt, r_pt):
                            fw.op("scalar", lambda e: e.activation(out=zf[j][:], in_=pt[:], func=AF.Copy),
                                  reads=[r_pt], writes=[r_zf[j]])
                            fw.dma("sync", lambda e: e.dma_start(out=dst[c, :, t0 + ts * 512:t0 + (ts + 1) * 512],
                                                                 in_=zf[j][:]), reads=[r_zf[j]])
                            if debug and dst is UAs:
                                fw.dma("sync", lambda e: e.dma_start(
                                    out=dbg["ua"][c, :, t0 + ts * 512:t0 + (ts + 1) * 512], in_=zf[j][:]),
                                    reads=[r_zf[j]])
                        return ev

                    def store_pad(dst, c, p0=p0):
                        def ev(ts, j, pt, r_pt):
                            fw.op("scalar", lambda e: e.activation(out=zb[j][:], in_=pt[:], func=AF.Copy),
                                  reads=[r_pt], writes=[r_zb[j]])
                            fw.dma("sync", lambda e: e.dma_start(out=dst[c, :, p0 + ts * 512:p0 + (ts + 1) * 512],
                                                                 in_=zb[j][:]), reads=[r_zb[j]])
                        return ev

                    wi = W["w_in"][l]
                    for c in range(3):
                        wt, r_w = load_w_chunk(wi[:, c * 128:(c + 1) * 128])
                        proj_fm(wt, r_w, store_plain(UAs, c))
                    if stop_after == "ua":
                        continue
                    for c in range(3):
                        wt, r_w = load_w_chunk(wi[:, (15 + c) * 128:(16 + c) * 128])
                        proj_fm(wt, r_w, store_pad(KTs, c))
                    wt, r_w = load_w_chunk(wi[:, 24 * 128:25 * 128])
                    proj_fm(wt, r_w, store_pad(KTs, 3))
                    for c in range(3):
                        wt, r_w = load_w_chunk(wi[:, (18 + c) * 128:(19 + c) * 128])
                        proj_fm(wt, r_w, store_pad(VTs, c))
                    wt, r_w = load_w_chunk(wi[:, 25 * 128:26 * 128])
                    proj_fm(wt, r_w, store_pad(VTs, 3))
                    for c in range(3):
                        wt, r_w = load_w_chunk(wi[:, (3 + c) * 128:(4 + c) * 128])

                        def ev_vb(ts, j, pt, r_pt):
                            fw.op("scalar", lambda e: e.activation(out=zt0[:, ts * 512:(ts + 1) * 512], in_=pt[:],
                                                                   func=AF.Copy), reads=[r_pt], writes=[r_zt0])
                        proj_fm(wt, r_w, ev_vb)
                        wt, r_w = load_w_chunk(wi[:, (9 + c) * 128:(10 + c) * 128])

                        def ev_gc(ts, j, pt, r_pt, c=c, p0=p0):
                            fw.op("vector", lambda e: e.tensor_tensor(out=zb[j][:], in0=pt[:],
                                                                      in1=zt0[:, ts * 512:(ts + 1) * 512], op=ALU.mult),
                                  reads=[r_pt, r_zt0], writes=[r_zb[j]])
                            fw.dma("sync", lambda e: e.dma_start(out=CVs[c, :, p0 + ts * 512:p0 + (ts + 1) * 512],
                                                                 in_=zb[j][:]), reads=[r_zb[j]])
                        proj_fm(wt, r_w, ev_gc)
            fw.barrier()

        def phase_s5(l):
            with ExitStack() as pes:
                def sb2(name, shape, dt=F32):
                    return pes.enter_context(nc.sbuf_tensor(uname("s_" + name), shape, dt))
                r_p = Res("prm")
                names = ["are", "aim", "ldt", "dt", "rho", "th", "c", "s", "t1", "t2", "lr", "li", "nr", "den",
                         "numr", "numi", "kr", "ki", "nki"]
                P = {n: sb2(n, [128, 24]) for n in names}

                def tt(o, a, b, op):
                    fw.op("vector", lambda e: e.tensor_tensor(out=P[o][:], in0=P[a][:], in1=P[b][:], op=op),
                          reads=[r_p], writes=[r_p])

                def ts_(o, a, s1, op0, s2=None, op1=None):
                    if op1 is None:
                        fw.op("vector", lambda e: e.tensor_scalar(out=P[o][:], in0=P[a][:], scalar1=s1, scalar2=None,
                                                                  op0=op0), reads=[r_p], writes=[r_p])
                    else:
                        fw.op("vector", lambda e: e.tensor_scalar(out=P[o][:], in0=P[a][:], scalar1=s1, scalar2=s2,
                                                                  op0=op0, op1=op1), reads=[r_p], writes=[r_p])

                def act(o, a, func, scale=1.0):
                    fw.op("scalar", lambda e: e.activation(out=P[o][:], in_=P[a][:], func=func, scale=scale),
                          reads=[r_p], writes=[r_p])

                for d in range(2):
                    fw.dma("sync", lambda e, d=d: e.dma_start(
                        out=P["are"][:, d * 12:(d + 1) * 12],
                        in_=W["s5_a_re"][l, d].rearrange("(gp g2) p -> (g2 p) gp", g2=2),
                        allow_slow_non_contiguous=True), writes=[r_p])
                    fw.dma("sync", lambda e, d=d: e.dma_start(
                        out=P["aim"][:, d * 12:(d + 1) * 12],
                        in_=W["s5_a_im"][l, d].rearrange("(gp g2) p -> (g2 p) gp", g2=2),
                        allow_slow_non_contiguous=True), writes=[r_p])
                    for g2 in range(2):
                        fw.dma("sync", lambda e, d=d, g2=g2: e.dma_start(
                            out=P["ldt"][64 * g2:64 * g2 + 64, d * 12:(d + 1) * 12],
                            in_=W["s5_log_dt"][l, d].rearrange("(gp g2) -> g2 gp", g2=2)[g2].partition_broadcast(64),
                            allow_slow_non_contiguous=True), writes=[r_p])
                act("dt", "ldt", AF.Exp)
                tt("t1", "are", "dt", ALU.mult)
                act("rho", "t1", AF.Exp)
                tt("th", "aim", "dt", ALU.mult)
                act("t1", "th", AF.Sin, scale=1.0 / 128)
                tt("t2", "t1", "t1", ALU.mult)
                ts_("c", "t2", -2.0, ALU.mult, 1.0, ALU.add)
                act("s", "th", AF.Sin, scale=1.0 / 64)
                for _ in range(6):
                    tt("t1", "c", "c", ALU.mult)
                    tt("t2", "s", "s", ALU.mult)
                    fw.op("vector", lambda e: e.scalar_tensor_tensor(out=P["s"][:], in0=P["c"][:], scalar=2.0,
                                                                     in1=P["s"][:], op0=ALU.mult, op1=ALU.mult),
                          reads=[r_p], writes=[r_p])
                    tt("c", "t1", "t2", ALU.subtract)
                tt("lr", "rho", "c", ALU.mult)
                tt("li", "rho", "s", ALU.mult)
                ts_("nr", "lr", -1.0, ALU.add)
                tt("t1", "are", "are", ALU.mult)
                tt("t2", "aim", "aim", ALU.mult)
                tt("den", "t1", "t2", ALU.add)
                fw.op("vector", lambda e: e.reciprocal(out=P["den"][:], in_=P["den"][:]), reads=[r_p], writes=[r_p])
                tt("t1", "nr", "are", ALU.mult)
                tt("t2", "li", "aim", ALU.mult)
                tt("numr", "t1", "t2", ALU.add)
                tt("t1", "li", "are", ALU.mult)
                tt("t2", "nr", "aim", ALU.mult)
                tt("numi", "t1", "t2", ALU.subtract)
                tt("kr", "numr", "den", ALU.mult)
                tt("ki", "numi", "den", ALU.mult)
                ts_("nki", "ki", -1.0, ALU.mult)
                LRR = sb2("LRR", [128, 2, 24])
                LIS = sb2("LIS", [128, 2, 24])
                for hh in range(2):
                    fw.op("vector", lambda e, hh=hh: e.tensor_copy(out=LRR[:, hh, :], in_=P["lr"][:]),
                          reads=[r_p], writes=[r_p])
                fw.op("vector", lambda e: e.tensor_scalar(out=LIS[:, 0, :], in0=P["li"][:], scalar1=-1.0, scalar2=None,
                                                          op0=ALU.mult), reads=[r_p], writes=[r_p])
                fw.op("vector", lambda e: e.tensor_copy(out=LIS[:, 1, :], in_=P["li"][:]), reads=[r_p], writes=[r_p])

                Bw = sb2("Bw", [128, 48, 128])
                Cw = sb2("Cw", [128, 48, 128])
                r_bw = Res("Bw")
                r_cw = Res("Cw")
                fw.op("gpsimd", lambda e: e.memset(Bw[:], 0.0), writes=[r_bw])
                fw.op("gpsimd", lambda e: e.memset(Cw[:], 0.0), writes=[r_cw])

                def widx(d, gp, ri):
                    return (d * 12 + gp) * 2 + ri
                for d in range(2):
                    for gp in range(12):
                        for g2 in range(2):
                            g = 2 * gp + g2
                            r0 = 16 * (g % 8)
                            for ri, (bn, cn) in enumerate([("s5_b_re", "s5_c_re"), ("s5_b_im", "s5_c_im")]):
                                fw.dma("sync", lambda e, d=d, gp=gp, g2=g2, g=g, r0=r0, ri=ri, bn=bn: e.dma_start(
                                    out=Bw[r0:r0 + 16, widx(d, gp, ri), 64 * g2:64 * g2 + 64],
                                    in_=W[bn][l, d, g].rearrange("p h -> h p"),
                                    allow_slow_non_contiguous=True), writes=[r_bw])
                                fw.dma("sync", lambda e, d=d, gp=gp, g2=g2, g=g, r0=r0, ri=ri, cn=cn: e.dma_start(
                                    out=Cw[64 * g2:64 * g2 + 64, widx(d, gp, ri), r0:r0 + 16],
                                    in_=W[cn][l, d, g].rearrange("h p -> p h"),
                                    allow_slow_non_contiguous=True), writes=[r_cw])
                Cw4 = Cw[:].rearrange("p (a r) n -> p a r n", r=2)
                fw.op("vector", lambda e: e.tensor_scalar(out=Cw4[:, :, 1, :], in0=Cw4[:, :, 1, :], scalar1=-1.0,
                                                          scalar2=None, op0=ALU.mult), reads=[r_cw], writes=[r_cw])

                XS = sb2("XS", [128, 2, 24, 129])
                BU = sb2("BU", [128, 2, 24, 128])
                PQ = sb2("PQ", [128, 2, 2, 24])
                r_xs = Res("XS")
                r_bu = Res("BU")
                r_pq = Res("PQ")
                r_pq1 = Res("PQ1")
                tmpb = [sb2("tmpb%d" % i, [128, 2, 128]) for i in range(2)]
                r_tmpb = [Res("tmpb%d" % i) for i in range(2)]
                ua = [[sb2("ua%d_%d" % (i, d), [128, 3, 128]) for d in range(2)] for i in range(2)]
                r_ua = [[Res("ua%d_%d" % (i, d)) for d in range(2)] for i in range(2)]
                yo = [sb2("yo%d" % i, [128, 128]) for i in range(2)]
                r_yo = [Res("yo%d" % i) for i in range(2)]
                fw.op("vector", lambda e: e.memset(XS[:], 0.0), writes=[r_xs])
                NT = NTOK // 128
                kcount = 0
                for i in range(NT):
                    tiles = [i, NT - 1 - i]
                    bi = i % 2
                    for d in range(2):
                        tk = tiles[d] * 128
                        fw.dma("sync", lambda e, d=d, tk=tk, bi=bi: e.dma_start(
                            out=ua[bi][d][:], in_=UAs[:, :, tk:tk + 128].rearrange("c p n -> p c n")),
                            writes=[r_ua[bi][d]])
                    for d in range(2):
                        for gp in range(12):
                            col = d * 12 + gp
                            c3 = gp // 4
                            pp = 2 * (col % 2)
                            for ri in range(2):
                                fw.op("tensor", lambda e, d=d, gp=gp, ri=ri, pp=pp, c3=c3, bi=bi: e.matmul(
                                    pA[pp + ri][:, 0:128], lhsT=Bw[:, widx(d, gp, ri), :], rhs=ua[bi][d][:, c3, :],
                                    start=True, stop=True),
                                    reads=[r_bw, r_ua[bi][d]], writes=[r_pA[pp + ri]])
                            tbk = tmpb[col % 2]
                            r_tbk = r_tmpb[col % 2]
                            if d == 0:
                                bre, bim = BU[:, 0, col, :], BU[:, 1, col, :]
                            else:
                                bre, bim = BU[:, 0, col, ::-1], BU[:, 1, col, ::-1]
                            fw.op("vector", lambda e, tbk=tbk, pp=pp, col=col: e.tensor_scalar(
                                out=tbk[:, 0, :], in0=pA[pp][:, 0:128], scalar1=P["kr"][:, col:col + 1], scalar2=None,
                                op0=ALU.mult), reads=[r_pA[pp], r_p], writes=[r_tbk])
                            fw.op("vector", lambda e, tbk=tbk, pp=pp, col=col, bre=bre: e.scalar_tensor_tensor(
                                out=bre, in0=pA[pp + 1][:, 0:128], scalar=P["nki"][:, col:col + 1], in1=tbk[:, 0, :],
                                op0=ALU.mult, op1=ALU.add), reads=[r_pA[pp + 1], r_p, r_tbk], writes=[r_bu])
                            fw.op("vector", lambda e, tbk=tbk, pp=pp, col=col: e.tensor_scalar(
                                out=tbk[:, 1, :], in0=pA[pp + 1][:, 0:128], scalar1=P["kr"][:, col:col + 1],
                                scalar2=None, op0=ALU.mult), reads=[r_pA[pp + 1], r_p], writes=[r_tbk])
                            fw.op("vector", lambda e, tbk=tbk, pp=pp, col=col, bim=bim: e.scalar_tensor_tensor(
                                out=bim, in0=pA[pp][:, 0:128], scalar=P["ki"][:, col:col + 1], in1=tbk[:, 1, :],
                                op0=ALU.mult, op1=ALU.add), reads=[r_pA[pp], r_p, r_tbk], writes=[r_bu])
                    for j in range(128):
                        fw.op("vector", lambda e, j=j: e.tensor_tensor(out=PQ[:, 0], in0=LRR[:], in1=XS[:, :, :, j],
                                                                       op=ALU.mult),
                              reads=[r_xs, r_p], writes=[r_pq])
                        fw.op("vector", lambda e, j=j: e.tensor_tensor(out=PQ[:, 1], in0=LIS[:], in1=XS[:, ::-1, :, j],
                                                                       op=ALU.mult),
                              reads=[r_xs, r_p], writes=[r_pq1])
                        fw.op("vector", lambda e: e.tensor_tensor(out=PQ[:, 0], in0=PQ[:, 0], in1=PQ[:, 1], op=ALU.add),
                              reads=[r_pq, r_pq1], writes=[r_pq])
                        fw.op("vector", lambda e, j=j: e.tensor_tensor(out=XS[:, :, :, j + 1], in0=PQ[:, 0],
                                                                       in1=BU[:, :, :, j], op=ALU.add),
                              reads=[r_pq, r_bu], writes=[r_xs])
                    for d in range(2):
                        tk = tiles[d] * 128
                        for c3 in range(3):
                            pj = kcount % 2
                            kcount += 1
                            n = 0
                            for gq in range(4):
                                gp = c3 * 4 + gq
                                col = d * 12 + gp
                                for ri in range(2):
                                    fw.op("tensor", lambda e, d=d, gp=gp, ri=ri, col=col, pj=pj, n=n: e.matmul(
                                        pX[pj][:, 0:128], lhsT=Cw[:, widx(d, gp, ri), :], rhs=XS[:, ri, col, 1:129],
                                        start=(n == 0), stop=(n == 7)),
                                        reads=[r_cw, r_xs], writes=[r_pX[pj]], pe_acc=True)
                                    n += 1
                            ov = yo[pj][:, :] if d == 0 else yo[pj][:, ::-1]
                            fw.op("scalar", lambda e, pj=pj, ov=ov: e.activation(out=ov, in_=pX[pj][:, 0:128],
                                                                                 func=AF.Copy),
                                  reads=[r_pX[pj]], writes=[r_yo[pj]])
                            fw.dma("sync", lambda e, d=d, c3=c3, tk=tk, pj=pj: e.dma_start(
                                out=YA[d, c3, :, tk:tk + 128], in_=yo[pj][:]), reads=[r_yo[pj]])
                    fw.op("vector", lambda e: e.tensor_copy(out=XS[:, :, :, 0], in_=XS[:, :, :, 128]),
                          reads=[r_xs], writes=[r_xs])
                    if (i + 1) % 32 == 0 and i + 1 < NT:
                        sgn = (i + 1) // 32
                        fw.op("vector", lambda e, sgn=sgn: e.tensor_scalar(
                            out=XS[:, :, 0:12, 0], in0=XS[:, :, 0:12, 0], scalar1=flg[:, sgn:sgn + 1], scalar2=None,
                            op0=ALU.mult), reads=[r_xs, r_flg], writes=[r_xs])
                        fw.op("vector", lambda e, sgn=sgn: e.tensor_scalar(
                            out=XS[:, :, 12:24, 0], in0=XS[:, :, 12:24, 0], scalar1=flg[:, 4 - sgn:5 - sgn],
                            scalar2=None, op0=ALU.mult), reads=[r_xs, r_flg], writes=[r_xs])
            fw.barrier()

        def phase_h():
            with ExitStack() as pes:
                hb = [pes.enter_context(nc.sbuf_tensor(uname("h_hb%d" % i), [128, 4, PAD], BF16)) for i in range(2)]
                r_hb = [Res("hb%d" % i) for i in range(2)]
                k = 0
                for (T, nch) in [(KTs, 4), (VTs, 4), (CVs, 3)]:
                    for sg in range(NSEG):
                        jobs = []
                        src = ((sg - 1) * SEGP + SEG) if sg > 0 else (sg * SEGP + PAD)
                        jobs.append((src, sg * SEGP, sg))
                        src = ((sg + 1) * SEGP + PAD) if sg < NSEG - 1 else (sg * SEGP + SEG)
                        jobs.append((src, sg * SEGP + PAD + SEG, sg + 1))
                        for (src, dst, fc) in jobs:
                            b = k % 2
                            k += 1
                            fw.dma("sync", lambda e, T=T, nch=nch, src=src, b=b: e.dma_start(
                                out=hb[b][:, 0:nch, :], in_=T[0:nch, :, src:src + PAD].rearrange("c p n -> p c n")),
                                writes=[r_hb[b]])
                            fw.op("vector", lambda e, nch=nch, b=b, fc=fc: e.tensor_scalar(
                                out=hb[b][:, 0:nch, :], in0=hb[b][:, 0:nch, :], scalar1=flg[:, fc:fc + 1], scalar2=None,
                                op0=ALU.mult), reads=[r_hb[b], r_flg], writes=[r_hb[b]])
                            fw.dma("sync", lambda e, T=T, nch=nch, dst=dst, b=b: e.dma_start(
                                out=T[0:nch, :, dst:dst + PAD].rearrange("c p n -> p c n"), in_=hb[b][:, 0:nch, :]),
                                reads=[r_hb[b]])
            fw.barrier()

        maskD = sb("maskD", [128, 6, 256])
        maskS = sb("maskS", [128, 6, 384])
        r_mask = Res("mask")
        ones_col = sb("ones_col", [128, 1])
        fw.op("vector", lambda e: e.memset(ones_col[:], 1.0), writes=[r_mask])
        with ExitStack() as mes:
            ii = mes.enter_context(nc.sbuf_tensor("m_ii", [128, 128], mybir.dt.int32))
            fi = mes.enter_context(nc.sbuf_tensor("m_fi", [128, 128], F32))
            ta = mes.enter_context(nc.sbuf_tensor("m_ta", [128, 128], F32))
            tv = mes.enter_context(nc.sbuf_tensor("m_tv", [128, 128], F32))
            r_m = Res("m")
            fw.op("gpsimd", lambda e: e.iota(ii[:], pattern=[[-1, 128]], base=0, channel_multiplier=1), writes=[r_m])
            fw.op("vector", lambda e: e.tensor_copy(out=fi[:], in_=ii[:]), reads=[r_m], writes=[r_m])

            def mk_mask(dst, off, half, coef):
                fw.op("vector", lambda e: e.tensor_scalar(out=ta[:], in0=fi[:], scalar1=float(off), scalar2=None,
                                                          op0=ALU.add), reads=[r_m], writes=[r_m])
                fw.op("vector", lambda e: e.tensor_scalar(out=tv[:], in0=ta[:], scalar1=-1.0, scalar2=None,
                                                          op0=ALU.mult), reads=[r_m], writes=[r_m])
                fw.op("vector", lambda e: e.tensor_tensor(out=ta[:], in0=ta[:], in1=tv[:], op=ALU.max),
                      reads=[r_m], writes=[r_m])
                fw.op("vector", lambda e: e.tensor_scalar(out=tv[:], in0=ta[:], scalar1=-1.0, scalar2=float(half) + 0.5,
                                                          op0=ALU.mult, op1=ALU.add), reads=[r_m], writes=[r_m])
                fw.op("vector", lambda e: e.tensor_scalar(out=tv[:], in0=tv[:], scalar1=0.0, scalar2=0.5,
                                                          op0=ALU.max, op1=ALU.min), reads=[r_m], writes=[r_m])
                fw.op("scalar", lambda e: e.activation(out=ta[:], in_=ta[:], func=AF.Exp, scale=-float(coef)),
                      reads=[r_m], writes=[r_m])
                fw.op("vector", lambda e: e.scalar_tensor_tensor(out=dst, in0=ta[:], scalar=2.0, in1=tv[:],
                                                                 op0=ALU.mult, op1=ALU.mult),
                      reads=[r_m], writes=[r_m, r_mask])
            for gi, (win, dil) in enumerate(DIL):
                for h in range(2):
                    sl = SLOPES[6 + 2 * gi + h]
                    for kt in range(2):
                        mk_mask(maskD[:, 2 * gi + h, kt * 128:(kt + 1) * 128], -64 + 128 * kt, 64, sl * dil)
            for h in range(6):
                for kt in range(3):
                    mk_mask(maskS[:, h, kt * 128:(kt + 1) * 128], 128 * (kt - 1), 128, SLOPES[h])
        fw.barrier()

        def phase_b(l, last):
            with ExitStack() as L0:
                def sb0(name, shape, dt=F32):
                    return L0.enter_context(nc.sbuf_tensor(uname("b_" + name), shape, dt))
                hT = sb0("hT", [128, 8, ST], BF16)
                r_hT = Res("hT")
                vcol = sb0("vcol", [128, 2])
                r_vcol = Res("vcol")
                sexp = sb0("sexp", [128, 6])
                r_sexp = Res("sexp")
                fw.dma("sync", lambda e: e.dma_start(out=sexp[:], in_=W["swa_sink"][l].partition_broadcast(128)),
                       writes=[r_sexp])
                fw.op("scalar", lambda e: e.activation(out=sexp[:], in_=sexp[:], func=AF.Exp),
                      reads=[r_sexp], writes=[r_sexp])
                wi = W["w_in"][l]
                for st_i in range(NST):
                    t0 = st_i * ST
                    p0 = padpos(t0)
                    sg = st_i // 2
                    hf = st_i % 2
                    fw.dma("sync", lambda e, t0=t0: e.dma_start(
                        out=hT[:], in_=HT[:, :, t0:t0 + ST].rearrange("k p n -> p k n")), writes=[r_hT])
                    fw.op("vector", lambda e: e.memset(vcol[:], 1.0), writes=[r_vcol])
                    fw.op("vector", lambda e, sg=sg: e.tensor_copy(out=vcol[0:64, 0:1], in_=flg[0:64, sg:sg + 1]),
                          reads=[r_flg], writes=[r_vcol])
                    fw.op("vector", lambda e, sg=sg: e.tensor_copy(out=vcol[64:128, 1:2], in_=flg[64:128, sg + 1:sg + 2]),
                          reads=[r_flg], writes=[r_vcol])
                    with ExitStack() as L1:
                        def sb1(name, shape, dt=F32):
                            return L1.enter_context(nc.sbuf_tensor(uname("b1_" + name), shape, dt))
                        brT = sb1("brT", [128, 10, ST], BF16)
                        r_br = Res("brT")
                        wst = [sb1("wst%d" % i, [128, 8, 128]) for i in range(2)]
                        r_wst = [Res("wst%d" % i) for i in range(2)]
                        wbf = [sb1("wbf%d" % i, [128, 8, 128], BF16) for i in range(2)]
                        r_wbf = [Res("wbf%d" % i) for i in range(2)]

                        def load_w_chunk(parts):
                            j = cnt["w"] % 2
                            cnt["w"] += 1
                            for (src_ap, c0, n) in parts:
                                fw.dma("sync", lambda e, src_ap=src_ap, c0=c0, n=n, j=j: e.dma_start(
                                    out=wst[j][:, :, c0:c0 + n], in_=src_ap.rearrange("(k p) n -> p k n", p=128)),
                                    writes=[r_wst[j]])
                            fw.op("gpsimd", lambda e, j=j: e.tensor_copy(out=wbf[j][:], in_=wst[j][:]),
                                  reads=[r_wst[j]], writes=[r_wbf[j]])
                            return wbf[j], r_wbf[j]

                        def proj_fm(wt, r_w, evac):
                            for ts in range(ST // 512):
                                j = cnt["pA"] % 4
                                cnt["pA"] += 1
                                for k in range(8):
                                    fw.op("tensor", lambda e, k=k, j=j, ts=ts: e.matmul(
                                        pA[j][:], lhsT=wt[:, k, :], rhs=hT[:, k, ts * 512:(ts + 1) * 512],
                                        start=(k == 0), stop=(k == 7)),
                                        reads=[r_w, r_hT], writes=[r_pA[j]], pe_acc=True)
                                evac(ts, j, pA[j], r_pA[j])

                        with ExitStack() as S1:
                            def sbs(name, shape, dt=F32):
                                return S1.enter_context(nc.sbuf_tensor(uname("b2_" + name), shape, dt))
                            KT1 = sbs("KT1", [128, 2 * ST], BF16)
                            VT1 = sbs("VT1", [128, 2 * ST], BF16)
                            r_kv = Res("kv")
                            QT1 = sbs("QT1", [128, ST], BF16)
                            r_q = Res("q")
                            UACC = sbs("UACC", [128, 2, ST])
                            r_ua = Res("uacc")
                            RC = sbs("RC", [128, ST])
                            r_rc = Res("rc")
                            Et = [sbs("E%d" % i, [128, 384]) for i in range(2)]
                            r_E = [Res("E%d" % i) for i in range(2)]
                            Pt = [sbs("P%d" % i, [128, 384], BF16) for i in range(2)]
                            r_P = [Res("P%d" % i) for i in range(2)]
                            VE = [sbs("VE%d" % i, [128, 3, 2, 192], BF16) for i in range(2)]
                            r_VE = [Res("VE%d" % i) for i in range(2)]
                            sm = [sbs("sm%d" % i, [128, 128]) for i in range(2)]
                            r_sm = [Res("sm%d" % i) for i in range(2)]
                            for i in range(2):
                                fw.op("vector", lambda e, i=i: e.memset(VE[i][:], 1.0), writes=[r_VE[i]])
                            ac = {"u": 0}

                            def q_evac(ts, j, pt, r_pt):
                                fw.op("scalar", lambda e: e.activation(out=QT1[:, ts * 512:(ts + 1) * 512], in_=pt[:],
                                                                       func=AF.Copy), reads=[r_pt], writes=[r_q])

                            def load_kv(c, p0=p0):
                                fw.dma("sync", lambda e, c=c, p0=p0: e.dma_start(
                                    out=KT1[:], in_=KTs[c, :, p0 - PAD:p0 - PAD + 2 * ST]), writes=[r_kv])
                                fw.dma("sync", lambda e, c=c, p0=p0: e.dma_start(
                                    out=VT1[:], in_=VTs[c, :, p0 - PAD:p0 - PAD + 2 * ST]), writes=[r_kv])

                            def unit(nkt, kcols, qcols, heads, mask_of, valid_of, lhs_of, sink_dst):
                                u = ac["u"] % 2
                                ac["u"] += 1
                                for kt in range(nkt):
                                    fw.op("tensor", lambda e, kt=kt, u=u: e.transpose(
                                        pT[u][:, kt, :], VT1[:, kcols(kt)], ident[:]),
                                        reads=[r_kv, r_ident], writes=[r_pT[u]], pe_acc=True)
                                fw.op("vector", lambda e, u=u: e.tensor_copy(
                                    out=VE[u][:, 0:nkt, :, 64:128],
                                    in_=pT[u][:, 0:nkt, :].rearrange("p k (h d) -> p k h d", h=2)),
                                    reads=[r_pT[u]], writes=[r_VE[u]])
                                for (hrow, vslot, tag) in heads:
                                    j = cnt["pA"] % 4
                                    cnt["pA"] += 1
                                    j2 = cnt["pA"] % 4
                                    cnt["pA"] += 1
                                    ei = ac["u"] % 2
                                    for kt in range(nkt):
                                        fw.op("tensor", lambda e, kt=kt, j=j, hrow=hrow: e.matmul(
                                            pA[j][:, kt * 128:(kt + 1) * 128], lhsT=KT1[hrow:hrow + 64, kcols(kt)],
                                            rhs=QT1[hrow:hrow + 64, qcols], start=True, stop=True),
                                            reads=[r_kv, r_q], writes=[r_pA[j]], pe_acc=True)
                                    fw.op("scalar", lambda e, j=j, ei=ei: e.activation(
                                        out=Et[ei][:, 0:nkt * 128], in_=pA[j][:, 0:nkt * 128], func=AF.Exp, scale=0.125),
                                        reads=[r_pA[j]], writes=[r_E[ei]])
                                    for kt in range(nkt):
                                        vc = valid_of(kt)
                                        fw.op("vector", lambda e, kt=kt, ei=ei, vc=vc, tag=tag: e.scalar_tensor_tensor(
                                            out=Pt[ei][:, kt * 128:(kt + 1) * 128], in0=Et[ei][:, kt * 128:(kt + 1) * 128],
                                            scalar=vc, in1=mask_of(tag)[:, kt * 128:(kt + 1) * 128],
                                            op0=ALU.mult, op1=ALU.mult),
                                            reads=[r_E[ei], r_mask, r_vcol, r_flg], writes=[r_P[ei]])
                                    for kt in range(nkt):
                                        fw.op("tensor", lambda e, kt=kt, j2=j2, ei=ei, u=u, vslot=vslot, tag=tag: e.matmul(
                                            pA[j2][:, 0:128], lhsT=lhs_of(VE[u], kt, vslot, tag),
                                            rhs=Pt[ei][:, kt * 128:(kt + 1) * 128], start=(kt == 0), stop=(kt == nkt - 1)),
                                            reads=[r_VE[u], r_P[ei]], writes=[r_pA[j2]], pe_acc=True)
                                    sink_dst(tag, pA[j2], r_pA[j2])

                            for gi, (win, dil) in enumerate(DIL):
                                wt, r_w = load_w_chunk([(wi[:, (12 + gi) * 128:(13 + gi) * 128], 0, 128)])
                                proj_fm(wt, r_w, q_evac)
                                load_kv(gi)
                                nsub = ST // dil
                                for r in range(dil):
                                    for qb in range(nsub // 128):
                                        q0 = qb * 128
                                        c_lo = r + dil * q0

                                        def kcols(kt, c_lo=c_lo, dil=dil):
                                            b = PAD + c_lo + dil * (-64 + 128 * kt)
                                            return slice(b, b + 127 * dil + 1, dil)
                                        qcols = slice(c_lo, c_lo + 127 * dil + 1, dil)

                                        def valid_of(kt, qb=qb, nsub=nsub):
                                            if hf == 0 and qb == 0 and kt == 0:
                                                return vcol[:, 0:1]
                                            if hf == 1 and qb == nsub // 128 - 1 and kt == 1:
                                                return vcol[:, 1:2]
                                            return ones_col[:, 0:1]

                                        def mask_of(tag, gi=gi):
                                            return maskD[:, 2 * gi + tag, :]

                                        def lhs_of(ve, kt, vslot, tag):
                                            return ve[:, kt, tag, 64:192] if tag == 0 else ve[:, kt, tag, 0:128]

                                        def sink_dst(tag, pu, r_pu, gi=gi, qcols=qcols):
                                            dstv = UACC[:, tag, qcols]
                                            if gi == 0:
                                                fw.op("vector", lambda e: e.tensor_copy(out=dstv, in_=pu[:, 0:128]),
                                                      reads=[r_pu], writes=[r_ua])
                                            else:
                                                fw.op("vector", lambda e: e.tensor_tensor(out=dstv, in0=dstv,
                                                                                          in1=pu[:, 0:128], op=ALU.add),
                                                      reads=[r_pu, r_ua], writes=[r_ua])
                                        unit(2, kcols, qcols, [(0, 0, 0), (64, 1, 1)], mask_of, valid_of, lhs_of, sink_dst)
                            fw.op("vector", lambda e: e.reciprocal(out=RC[0:64, :], in_=UACC[64:128, 0, :]),
                                  reads=[r_ua], writes=[r_rc])
                            fw.op("vector", lambda e: e.reciprocal(out=RC[64:128, :], in_=UACC[0:64, 1, :]),
                                  reads=[r_ua], writes=[r_rc])
                            fw.op("vector", lambda e: e.tensor_tensor(out=brT[0:64, 6, :], in0=UACC[0:64, 0, :],
                                                                      in1=RC[0:64, :], op=ALU.mult),
                                  reads=[r_ua, r_rc], writes=[r_br])
                            fw.op("vector", lambda e: e.tensor_tensor(out=brT[64:128, 6, :], in0=UACC[64:128, 1, :],
                                                                      in1=RC[64:128, :], op=ALU.mult),
                                  reads=[r_ua, r_rc], writes=[r_br])
                            load_kv(3)
                            for jq in range(3):
                                wt, r_w = load_w_chunk([(wi[:, 2688 + 64 * jq:2688 + 64 * jq + 64], 0, 64),
                                                        (wi[:, 2688 + 64 * (jq + 3):2688 + 64 * (jq + 3) + 64], 64, 64)])
                                proj_fm(wt, r_w, q_evac)
                                for qb in range(ST // 128):
                                    q0 = qb * 128

                                    def kcols(kt, q0=q0):
                                        b = PAD + q0 - 128 + 128 * kt
                                        return slice(b, b + 128)
                                    qcols = slice(q0, q0 + 128)

                                    def valid_of(kt, qb=qb):
                                        if hf == 0 and qb == 0 and kt == 0:
                                            return flg[:, sg:sg + 1]
                                        if hf == 1 and qb == ST // 128 - 1 and kt == 2:
                                            return flg[:, sg + 1:sg + 2]
                                        return ones_col[:, 0:1]

                                    def mask_of(tag):
                                        return maskS[:, tag, :]

                                    def lhs_of(ve, kt, vslot, tag):
                                        return ve[:, kt, vslot, 64:192] if tag % 2 == 0 else ve[:, kt, vslot, 0:128]

                                    def sink_dst(tag, pu, r_pu, qcols=qcols):
                                        h = tag
                                        ch, half = 7 + h // 2, h % 2
                                        si = ac["u"] % 2
                                        if half == 0:
                                            urows, drows = slice(0, 64), slice(64, 128)
                                        else:
                                            urows, drows = slice(64, 128), slice(0, 64)
                                        fw.op("vector", lambda e: e.tensor_scalar(
                                            out=sm[si][drows, :], in0=pu[drows, 0:128], scalar1=sexp[drows, h:h + 1],
                                            scalar2=None, op0=ALU.add), reads=[r_pu, r_sexp], writes=[r_sm[si]])
                                        fw.op("vector", lambda e: e.reciprocal(out=sm[si][drows, :], in_=sm[si][drows, :]),
                                              reads=[r_sm[si]], writes=[r_sm[si]])
                                        fw.op("vector", lambda e: e.tensor_tensor(
                                            out=brT[urows, ch, qcols], in0=pu[urows, 0:128], in1=sm[si][drows, :],
                                            op=ALU.mult), reads=[r_pu, r_sm[si]], writes=[r_br])
                                    unit(3, kcols, qcols, [(0, 0, jq), (64, 1, jq + 3)], mask_of, valid_of, lhs_of, sink_dst)
                        fw.barrier()
                        if stop_after == "attn":
                            fw.dma("sync", lambda e, t0=t0: e.dma_start(
                                out=dbg["br"][:, :, t0:t0 + ST].rearrange("c p n -> p c n"), in_=brT[:]), reads=[r_br])
                            fw.barrier()
                            continue
                        with ExitStack() as S2:
                            def sbt(name, shape, dt=F32):
                                return S2.enter_context(nc.sbuf_tensor(uname("b3_" + name), shape, dt))
                            dvec = sbt("dvec", [128, 3])
                            glub = sbt("glub", [128, 3])
                            cwt = sbt("cwt", [128, 3, 3])
                            cbt = sbt("cbt", [128, 3])
                            r_sv = Res("sv")
                            fw.dma("sync", lambda e: e.dma_start(out=dvec[:], in_=W["s5_d"][l].rearrange("(c p) -> p c", p=128),
                                                                 allow_slow_non_contiguous=True), writes=[r_sv])
                            fw.dma("sync", lambda e: e.dma_start(out=glub[:], in_=W["s5_glu_b"][l].rearrange("(c p) -> p c", p=128),
                                                                 allow_slow_non_contiguous=True), writes=[r_sv])
                            fw.dma("sync", lambda e: e.dma_start(out=cwt[:], in_=W["conv_w"][l].rearrange("t (c p) -> p t c", p=128),
                                                                 allow_slow_non_contiguous=True), writes=[r_sv])
                            fw.dma("sync", lambda e: e.dma_start(out=cbt[:], in_=W["conv_b"][l].rearrange("(c p) -> p c", p=128),
                                                                 allow_slow_non_contiguous=True), writes=[r_sv])
                            gluw32 = sbt("gluw32", [128, 3, 384])
                            gluw = sbt("gluw", [128, 3, 384], BF16)
                            r_gluw = Res("gluw")
                            fw.dma("sync", lambda e: e.dma_start(out=gluw32[:], in_=W["s5_glu_w"][l].rearrange("(c p) n -> p c n", p=128)),
                                   writes=[r_gluw])
                            fw.op("gpsimd", lambda e: e.tensor_copy(out=gluw[:], in_=gluw32[:]), reads=[r_gluw], writes=[r_gluw])
                            yf = [sbt("yf%d" % i, [128, 512]) for i in range(2)]
                            yb_ = [sbt("yb%d" % i, [128, 512]) for i in range(2)]
                            uu = [sbt("uu%d" % i, [128, 512]) for i in range(2)]
                            r_y3 = [Res("y3_%d" % i) for i in range(2)]
                            zf32 = sbt("zf32", [128, 3, 512])
                            zb16 = sbt("zb16", [128, 3, 512], BF16)
                            r_z = Res("z")
                            gt = [sbt("gt%d" % i, [128, 512]) for i in range(2)]
                            r_gt = [Res("gt%d" % i) for i in range(2)]
                            kk = 0
                            for ts in range(ST // 512):
                                tk = t0 + ts * 512
                                for c in range(3):
                                    b = kk % 2
                                    kk += 1
                                    fw.dma("sync", lambda e, c=c, tk=tk, b=b: e.dma_start(out=yf[b][:], in_=YA[0, c, :, tk:tk + 512]),
                                           writes=[r_y3[b]])
                                    fw.dma("sync", lambda e, c=c, tk=tk, b=b: e.dma_start(out=yb_[b][:], in_=YA[1, c, :, tk:tk + 512]),
                                           writes=[r_y3[b]])
                                    fw.dma("sync", lambda e, c=c, tk=tk, b=b: e.dma_start(out=uu[b][:], in_=UAs[c, :, tk:tk + 512]),
                                           writes=[r_y3[b]])
                                    fw.op("vector", lambda e, b=b: e.tensor_tensor(out=yf[b][:], in0=yf[b][:], in1=yb_[b][:], op=ALU.add),
                                          reads=[r_y3[b]], writes=[r_y3[b]])
                                    fw.op("vector", lambda e, b=b, c=c: e.scalar_tensor_tensor(
                                        out=yf[b][:], in0=uu[b][:], scalar=dvec[:, c:c + 1], in1=yf[b][:], op0=ALU.mult, op1=ALU.add),
                                        reads=[r_y3[b], r_sv], writes=[r_y3[b]])
                                    fw.op("scalar", lambda e, b=b, c=c: e.activation(out=zf32[:, c, :], in_=yf[b][:], func=AF.Gelu),
                                          reads=[r_y3[b]], writes=[r_z])
                                    fw.op("vector", lambda e, c=c: e.tensor_copy(out=zb16[:, c, :], in_=zf32[:, c, :]),
                                          reads=[r_z], writes=[r_z])
                                for co in range(3):
                                    j = cnt["pA"] % 4
                                    cnt["pA"] += 1
                                    for ci in range(3):
                                        fw.op("tensor", lambda e, ci=ci, co=co, j=j: e.matmul(
                                            pA[j][:], lhsT=gluw[:, ci, co * 128:(co + 1) * 128], rhs=zb16[:, ci, :],
                                            start=(ci == 0), stop=(ci == 2)), reads=[r_gluw, r_z], writes=[r_pA[j]], pe_acc=True)
                                    g2 = co % 2
                                    fw.op("vector", lambda e, j=j, g2=g2, co=co: e.tensor_scalar(
                                        out=gt[g2][:], in0=pA[j][:], scalar1=glub[:, co:co + 1], scalar2=None, op0=ALU.add),
                                        reads=[r_pA[j], r_sv], writes=[r_gt[g2]])
                                    fw.op("scalar", lambda e, g2=g2: e.activation(out=gt[g2][:], in_=gt[g2][:], func=AF.Sigmoid),
                                          reads=[r_gt[g2]], writes=[r_gt[g2]])
                                    fw.op("vector", lambda e, g2=g2, co=co, ts=ts: e.tensor_tensor(
                                        out=brT[:, co, ts * 512:(ts + 1) * 512], in0=zf32[:, co, :], in1=gt[g2][:], op=ALU.mult),
                                        reads=[r_z, r_gt[g2]], writes=[r_br])
                            cvt = sbt("cvt", [128, ST + 2], BF16)
                            r_cvt = Res("cvt")
                            accf = [sbt("accf%d" % i, [128, 512]) for i in range(2)]
                            r_accf = [Res("accf%d" % i) for i in range(2)]
                            for c in range(3):
                                wt, r_w = load_w_chunk([(wi[:, (6 + c) * 128:(7 + c) * 128], 0, 128)])
                                fw.dma("sync", lambda e, c=c, p0=p0: e.dma_start(out=cvt[:], in_=CVs[c, :, p0 - 1:p0 + ST + 1]),
                                       writes=[r_cvt])

                                def ev_gb(ts, j, pt, r_pt, c=c):
                                    a = j % 2
                                    o = ts * 512
                                    fw.op("vector", lambda e: e.tensor_scalar(out=accf[a][:], in0=cvt[:, o:o + 512],
                                                                              scalar1=cwt[:, 0, c:c + 1], scalar2=None, op0=ALU.mult),
                                          reads=[r_cvt, r_sv], writes=[r_accf[a]])
                                    for tap in (1, 2):
                                        fw.op("vector", lambda e, tap=tap: e.scalar_tensor_tensor(
                                            out=accf[a][:], in0=cvt[:, o + tap:o + tap + 512], scalar=cwt[:, tap, c:c + 1],
                                            in1=accf[a][:], op0=ALU.mult, op1=ALU.add),
                                            reads=[r_cvt, r_sv, r_accf[a]], writes=[r_accf[a]])
                                    fw.op("vector", lambda e: e.scalar_tensor_tensor(
                                        out=brT[:, 3 + c, o:o + 512], in0=accf[a][:], scalar=cbt[:, c:c + 1], in1=pt[:],
                                        op0=ALU.add, op1=ALU.mult), reads=[r_accf[a], r_sv, r_pt], writes=[r_br])
                                proj_fm(wt, r_w, ev_gb)
                            if stop_after == "branches":
                                fw.dma("sync", lambda e, t0=t0: e.dma_start(
                                    out=dbg["br"][:, :, t0:t0 + ST].rearrange("c p n -> p c n"), in_=brT[:]), reads=[r_br])
                                fw.barrier()
                                continue
                            wbr32 = [sbt("wbr32_%d" % i, [128, 3, 128]) for i in range(2)]
                            wbr = [sbt("wbr%d" % i, [128, 3, 128], BF16) for i in range(2)]
                            r_wbr = [Res("wbr%d" % i) for i in range(2)]
                            mac = sbt("mac", [128, ST])
                            r_mac = Res("mac")
                            mbf = [sbt("mbf%d" % i, [128, ST], BF16) for i in range(2)]
                            r_mbf = [Res("mbf%d" % i) for i in range(2)]
                            sgt = [sbt("sgt%d" % i, [128, 512]) for i in range(2)]
                            r_sgt = [Res("sgt%d" % i) for i in range(2)]
                            BRS = [("w_branch_a", 3, 0), ("w_branch_b", 3, 3), ("w_branch_c", 1, 6), ("w_branch_d", 3, 7)]
                            q = 0
                            for jo in range(8):
                                for br, (wn, nch, ch0) in enumerate(BRS):
                                    wg, r_wg = load_w_chunk([(wi[:, 3328 + br * 1024 + jo * 128:3328 + br * 1024 + (jo + 1) * 128], 0, 128)])
                                    wb = q % 2
                                    q += 1
                                    fw.dma("sync", lambda e, wn=wn, nch=nch, jo=jo, wb=wb: e.dma_start(
                                        out=wbr32[wb][:, 0:nch, :],
                                        in_=W[wn][l][:, jo * 128:(jo + 1) * 128].rearrange("(c p) n -> p c n", p=128)),
                                        writes=[r_wbr[wb]])
                                    fw.op("gpsimd", lambda e, nch=nch, wb=wb: e.tensor_copy(out=wbr[wb][:, 0:nch, :],
                                                                                          in_=wbr32[wb][:, 0:nch, :]),
                                          reads=[r_wbr[wb]], writes=[r_wbr[wb]])
                                    for ts in range(ST // 512):
                                        j = cnt["pA"] % 4
                                        cnt["pA"] += 1
                                        xk = (q + ts) % 2
                                        o = ts * 512
                                        for k in range(8):
                                            fw.op("tensor", lambda e, k=k, j=j, o=o, wg=wg: e.matmul(
                                                pA[j][:], lhsT=wg[:, k, :], rhs=hT[:, k, o:o + 512], start=(k == 0), stop=(k == 7)),
                                                reads=[r_wg, r_hT], writes=[r_pA[j]], pe_acc=True)
                                        for c in range(nch):
                                            fw.op("tensor", lambda e, c=c, xk=xk, o=o, wb=wb, ch0=ch0, nch=nch: e.matmul(
                                                pX[xk][:], lhsT=wbr[wb][:, c, :], rhs=brT[:, ch0 + c, o:o + 512],
                                                start=(c == 0), stop=(c == nch - 1)),
                                                reads=[r_wbr[wb], r_br], writes=[r_pX[xk]], pe_acc=True)
                                        fw.op("scalar", lambda e, j=j, xk=xk: e.activation(out=sgt[xk][:], in_=pA[j][:], func=AF.Sigmoid),
                                              reads=[r_pA[j]], writes=[r_sgt[xk]])
                                        if br == 0:
                                            fw.op("vector", lambda e, xk=xk, o=o: e.tensor_tensor(
                                                out=mac[:, o:o + 512], in0=sgt[xk][:], in1=pX[xk][:], op=ALU.mult),
                                                reads=[r_sgt[xk], r_pX[xk]], writes=[r_mac])
                                        else:
                                            fw.op("vector", lambda e, xk=xk: e.tensor_tensor(
                                                out=sgt[xk][:], in0=sgt[xk][:], in1=pX[xk][:], op=ALU.mult),
                                                reads=[r_sgt[xk], r_pX[xk]], writes=[r_sgt[xk]])
                                            fw.op("vector", lambda e, xk=xk, o=o: e.tensor_tensor(
                                                out=mac[:, o:o + 512], in0=mac[:, o:o + 512], in1=sgt[xk][:], op=ALU.add),
                                                reads=[r_sgt[xk], r_mac], writes=[r_mac])
                                mi = jo % 2
                                fw.op("scalar", lambda e, mi=mi: e.activation(out=mbf[mi][:], in_=mac[:], func=AF.Copy),
                                      reads=[r_mac], writes=[r_mbf[mi]])
                                fw.dma("sync", lambda e, jo=jo, t0=t0, mi=mi: e.dma_start(out=MTs[jo, :, t0:t0 + ST], in_=mbf[mi][:]),
                                       reads=[r_mbf[mi]])
                    fw.barrier()
                    if stop_after in ("attn", "branches"):
                        continue
                    with ExitStack() as S3:
                        def sbu(name, shape, dt=F32):
                            return S3.enter_context(nc.sbuf_tensor(uname("b4_" + name), shape, dt))
                        fw.dma("sync", lambda e, t0=t0: e.dma_start(
                            out=hT[:], in_=MTs[:, :, t0:t0 + ST].rearrange("k p n -> p k n")), writes=[r_hT])
                        wo32 = [sbu("wo32_%d" % i, [128, 8, 256]) for i in range(2)]
                        r_wo32 = [Res("wo32_%d" % i) for i in range(2)]
                        wo = sbu("wo", [128, 8, D], BF16)
                        r_wo = Res("wo")
                        for pc in range(4):
                            a = pc % 2
                            fw.dma("sync", lambda e, pc=pc, a=a: e.dma_start(
                                out=wo32[a][:], in_=W["w_o"][l][:, pc * 256:(pc + 1) * 256].rearrange("(k p) n -> p k n", p=128)),
                                writes=[r_wo32[a]])
                            fw.op("gpsimd", lambda e, pc=pc, a=a: e.tensor_copy(out=wo[:, :, pc * 256:(pc + 1) * 256], in_=wo32[a][:]),
                                  reads=[r_wo32[a]], writes=[r_wo])
                        load_gb("ln1_g", "ln1_b", l)
                        xb = [sbu("xb%d" % i, [128, D]) for i in range(2)]
                        r_xb = [Res("xb%d" % i) for i in range(2)]
                        tb = [sbu("tb%d" % i, [128, D]) for i in range(2)]
                        r_tb = [Res("tb%d" % i) for i in range(2)]
                        hb16 = [sbu("hb16_%d" % i, [128, D], BF16) for i in range(2)]
                        r_hb16 = [Res("hb16_%d" % i) for i in range(2)]
                        st4 = [sbu("st4_%d" % i, [128, 8]) for i in range(2)]
                        r_st4 = [Res("st4_%d" % i) for i in range(2)]
                        mo = [sbu("mo%d" % i, [128, D]) for i in range(2)]
                        r_mo = [Res("mo%d" % i) for i in range(2)]
                        tkb = [sbu("tkb%d" % i, [128, 8, 128], BF16) for i in range(2)]
                        r_tkb = [Res("tkb%d" % i) for i in range(2)]
                        for blk in range(ST // 128):
                            i = blk % 2
                            r0 = t0 + blk * 128
                            fw.dma("sync", lambda e, i=i, r0=r0: e.dma_start(out=xb[i][:], in_=H0[r0:r0 + 128, :]), writes=[r_xb[i]])
                            for nh in range(2):
                                j = cnt["pA"] % 4
                                cnt["pA"] += 1
                                for k in range(8):
                                    fw.op("tensor", lambda e, k=k, j=j, nh=nh, blk=blk: e.matmul(
                                        pA[j][:], lhsT=hT[:, k, blk * 128:(blk + 1) * 128], rhs=wo[:, k, nh * 512:(nh + 1) * 512],
                                        start=(k == 0), stop=(k == 7)), reads=[r_hT, r_wo], writes=[r_pA[j]], pe_acc=True)
                                fw.op("scalar", lambda e, i=i, j=j, nh=nh: e.activation(
                                    out=mo[i][:, nh * 512:(nh + 1) * 512], in_=pA[j][:], func=AF.Copy),
                                    reads=[r_pA[j]], writes=[r_mo[i]])
                            if debug:
                                fw.dma("sync", lambda e, i=i, r0=r0: e.dma_start(out=dbg["mix"][r0:r0 + 128, :], in_=mo[i][:]),
                                       reads=[r_mo[i]])
                            fw.op("vector", lambda e, i=i: e.scalar_tensor_tensor(
                                out=xb[i][:], in0=xb[i][:], scalar=ALPHA, in1=mo[i][:], op0=ALU.mult, op1=ALU.add),
                                reads=[r_xb[i], r_mo[i]], writes=[r_xb[i]])
                            fw.dma("sync", lambda e, i=i, r0=r0: e.dma_start(out=H1[r0:r0 + 128, :], in_=xb[i][:]), reads=[r_xb[i]])
                        fw.barrier()
                        for blk in range(ST // 128):
                            i = blk % 2
                            r0 = t0 + blk * 128
                            fw.dma("sync", lambda e, i=i, r0=r0: e.dma_start(out=xb[i][:], in_=H1[r0:r0 + 128, :]), writes=[r_xb[i]])
                            ln_rows(fw, xb[i], r_xb[i], tb[i], r_tb[i], st4[i], r_st4[i], gam, bet, r_gb, xb[i], r_xb[i])
                            if debug:
                                fw.dma("sync", lambda e, i=i, r0=r0: e.dma_start(out=dbg["st"][r0:r0 + 128, :], in_=st4[i][:]),
                                       reads=[r_st4[i]])
                            fw.dma("sync", lambda e, i=i, r0=r0: e.dma_start(out=H1[r0:r0 + 128, :], in_=xb[i][:]), reads=[r_xb[i]])
                            fw.op("scalar", lambda e, i=i: e.activation(out=hb16[i][:], in_=xb[i][:], func=AF.Copy),
                                  reads=[r_xb[i]], writes=[r_hb16[i]])
                            for k in range(8):
                                fw.op("tensor", lambda e, k=k, i=i: e.transpose(pT[i][:, k, :], hb16[i][:, k * 128:(k + 1) * 128], ident[:]),
                                      reads=[r_hb16[i], r_ident], writes=[r_pT[i]], pe_acc=True)
                            fw.op("vector", lambda e, i=i: e.tensor_copy(out=tkb[i][:], in_=pT[i][:]), reads=[r_pT[i]], writes=[r_tkb[i]])
                            fw.dma("sync", lambda e, i=i, r0=r0: e.dma_start(
                                out=HT[:, :, r0:r0 + 128].rearrange("k p n -> p k n"), in_=tkb[i][:]), reads=[r_tkb[i]])
                    fw.barrier()
                    if stop_after == "ln1":
                        continue
                    with ExitStack() as S4:
                        def sbv(name, shape, dt=F32):
                            return S4.enter_context(nc.sbuf_tensor(uname("b5_" + name), shape, dt))
                        fw.dma("sync", lambda e, t0=t0: e.dma_start(
                            out=hT[:], in_=HT[:, :, t0:t0 + ST].rearrange("k p n -> p k n")), writes=[r_hT])
                        wr32 = sbv("wr32", [128, 8, 20])
                        wr = sbv("wr", [128, 8, 20], BF16)
                        r_wr = Res("wr")
                        fw.dma("sync", lambda e: e.dma_start(out=wr32[:, :, 0:4], in_=W["router_group_w"][l].rearrange("(k p) n -> p k n", p=128),
                                                             allow_slow_non_contiguous=True), writes=[r_wr])
                        fw.dma("sync", lambda e: e.dma_start(out=wr32[:, :, 4:20], in_=W["router_expert_w"][l].rearrange("(k p) n -> p k n", p=128),
                                                             allow_slow_non_contiguous=True), writes=[r_wr])
                        fw.op("vector", lambda e: e.tensor_copy(out=wr[:], in_=wr32[:]), reads=[r_wr], writes=[r_wr])
                        rb = sbv("rb", [128, 20])
                        r_rb = Res("rb")
                        fw.dma("sync", lambda e: e.dma_start(out=rb[:, 0:4], in_=W["router_group_b"][l].partition_broadcast(128)), writes=[r_rb])
                        fw.dma("sync", lambda e: e.dma_start(out=rb[:, 4:20], in_=W["router_expert_b"][l].partition_broadcast(128)), writes=[r_rb])
                        comb = sbv("comb", [128, ST // 128, 16])
                        r_comb = Res("comb")
                        rt = sbv("rt", [128, 64])
                        r_rt = Res("rt")
                        for blk in range(ST // 128):
                            xk = blk % 2
                            for k in range(8):
                                fw.op("tensor", lambda e, k=k, xk=xk, blk=blk: e.matmul(
                                    pX[xk][:, 0:20], lhsT=hT[:, k, blk * 128:(blk + 1) * 128], rhs=wr[:, k, :],
                                    start=(k == 0), stop=(k == 7)), reads=[r_hT, r_wr], writes=[r_pX[xk]], pe_acc=True)
                            lg = rt[:, 0:20]

                            def V(fn, extra_r=()):
                                fw.op("vector", fn, reads=[r_rt] + list(extra_r), writes=[r_rt])
                            fw.op("vector", lambda e, xk=xk: e.tensor_tensor(out=rt[:, 0:20], in0=pX[xk][:, 0:20], in1=rb[:], op=ALU.add),
                                  reads=[r_pX[xk], r_rb], writes=[r_rt])
                            V(lambda e: e.reduce_max(out=rt[:, 20:21], in_=rt[:, 0:4], axis=AX.X))
                            V(lambda e: e.tensor_scalar(out=rt[:, 24:28], in0=rt[:, 0:4], scalar1=rt[:, 20:21], scalar2=None, op0=ALU.subtract))
                            fw.op("scalar", lambda e: e.activation(out=rt[:, 28:32], in_=rt[:, 24:28], func=AF.Exp), reads=[r_rt], writes=[r_rt])
                            V(lambda e: e.reduce_sum(out=rt[:, 21:22], in_=rt[:, 28:32], axis=AX.X))
                            V(lambda e: e.reciprocal(out=rt[:, 21:22], in_=rt[:, 21:22]))
                            V(lambda e: e.tensor_scalar(out=rt[:, 24:28], in0=rt[:, 24:28], scalar1=-1e30, scalar2=1.0, op0=ALU.mult, op1=ALU.min))
                            V(lambda e: e.tensor_scalar(out=rt[:, 24:28], in0=rt[:, 24:28], scalar1=-1.0, scalar2=1.0, op0=ALU.mult, op1=ALU.add))
                            V(lambda e: e.tensor_scalar(out=rt[:, 32:36], in0=rt[:, 4:8], scalar1=rt[:, 24:25], scalar2=None, op0=ALU.mult))
                            for gq in range(1, 4):
                                V(lambda e, gq=gq: e.scalar_tensor_tensor(out=rt[:, 32:36], in0=rt[:, 4 + 4 * gq:8 + 4 * gq],
                                                                          scalar=rt[:, 24 + gq:25 + gq], in1=rt[:, 32:36],
                                                                          op0=ALU.mult, op1=ALU.add))
                            V(lambda e: e.reduce_max(out=rt[:, 22:23], in_=rt[:, 32:36], axis=AX.X))
                            V(lambda e: e.tensor_scalar(out=rt[:, 36:40], in0=rt[:, 32:36], scalar1=rt[:, 22:23], scalar2=None, op0=ALU.subtract))
                            V(lambda e: e.tensor_scalar(out=rt[:, 36:40], in0=rt[:, 36:40], scalar1=-1e30, scalar2=1.0, op0=ALU.mult, op1=ALU.min))
                            V(lambda e: e.tensor_scalar(out=rt[:, 36:40], in0=rt[:, 36:40], scalar1=-1.0, scalar2=1.0, op0=ALU.mult, op1=ALU.add))
                            V(lambda e: e.scalar_tensor_tensor(out=rt[:, 40:44], in0=rt[:, 36:40], scalar=-1e4, in1=rt[:, 32:36],
                                                               op0=ALU.mult, op1=ALU.add))
                            V(lambda e: e.reduce_max(out=rt[:, 23:24], in_=rt[:, 40:44], axis=AX.X))
                            V(lambda e: e.tensor_scalar(out=rt[:, 44:48], in0=rt[:, 40:44], scalar1=rt[:, 23:24], scalar2=None, op0=ALU.subtract))
                            V(lambda e: e.tensor_scalar(out=rt[:, 44:48], in0=rt[:, 44:48], scalar1=-1e30, scalar2=1.0, op0=ALU.mult, op1=ALU.min))
                            V(lambda e: e.tensor_scalar(out=rt[:, 44:48], in0=rt[:, 44:48], scalar1=-1.0, scalar2=1.0, op0=ALU.mult, op1=ALU.add))
                            V(lambda e: e.tensor_tensor(out=rt[:, 48:49], in0=rt[:, 23:24], in1=rt[:, 22:23], op=ALU.subtract))
                            fw.op("scalar", lambda e: e.activation(out=rt[:, 49:50], in_=rt[:, 48:49], func=AF.Exp), reads=[r_rt], writes=[r_rt])
                            V(lambda e: e.tensor_scalar(out=rt[:, 50:51], in0=rt[:, 49:50], scalar1=1.0, scalar2=None, op0=ALU.add))
                            V(lambda e: e.reciprocal(out=rt[:, 50:51], in_=rt[:, 50:51]))
                            V(lambda e: e.tensor_tensor(out=rt[:, 51:52], in0=rt[:, 49:50], in1=rt[:, 50:51], op=ALU.mult))
                            V(lambda e: e.tensor_tensor(out=rt[:, 50:51], in0=rt[:, 50:51], in1=rt[:, 21:22], op=ALU.mult))
                            V(lambda e: e.tensor_tensor(out=rt[:, 51:52], in0=rt[:, 51:52], in1=rt[:, 21:22], op=ALU.mult))
                            V(lambda e: e.tensor_scalar(out=rt[:, 52:56], in0=rt[:, 36:40], scalar1=rt[:, 50:51], scalar2=None, op0=ALU.mult))
                            V(lambda e: e.scalar_tensor_tensor(out=rt[:, 52:56], in0=rt[:, 44:48], scalar=rt[:, 51:52], in1=rt[:, 52:56],
                                                               op0=ALU.mult, op1=ALU.add))
                            for gq in range(4):
                                fw.op("vector", lambda e, gq=gq, blk=blk: e.tensor_scalar(
                                    out=comb[:, blk, 4 * gq:4 * gq + 4], in0=rt[:, 52:56], scalar1=rt[:, 24 + gq:25 + gq], scalar2=None,
                                    op0=ALU.mult), reads=[r_rt], writes=[r_comb])
                        wg32 = sbv("wg32", [128, 8, 256])
                        wu32 = sbv("wu32", [128, 8, 256])
                        wgb = sbv("wgb", [128, 8, 256], BF16)
                        wub = sbv("wub", [128, 8, 256], BF16)
                        wd32 = sbv("wd32", [128, 2, D])
                        wdb = sbv("wdb", [128, 2, D], BF16)
                        r_wg32, r_wu32, r_wd32 = Res("wg32"), Res("wu32"), Res("wd32")
                        r_wgb, r_wub, r_wdb = Res("wgb"), Res("wub"), Res("wdb")
                        HB = ST // 256
                        macc = sbv("macc", [128, HB, D])
                        r_macc = Res("macc")
                        actT = sbv("actT", [128, 2, ST // 2], BF16)
                        r_act = Res("actT")
                        sgl = [sbv("sgl%d" % i, [128, 512]) for i in range(2)]
                        r_sgl = [Res("sgl%d" % i) for i in range(2)]
                        xq = [sbv("xq%d" % i, [128, D]) for i in range(2)]
                        r_xq = [Res("xq%d" % i) for i in range(2)]
                        tq = [sbv("tq%d" % i, [128, D]) for i in range(2)]
                        r_tq = [Res("tq%d" % i) for i in range(2)]
                        sq4 = [sbv("sq4_%d" % i, [128, 8]) for i in range(2)]
                        r_sq4 = [Res("sq4_%d" % i) for i in range(2)]
                        load_gb("ln2_g", "ln2_b", l)
                        for hv in range(2):
                            c0 = hv * (ST // 2)
                            for ex in range(16):
                                fw.dma("sync", lambda e, ex=ex: e.dma_start(
                                    out=wg32[:], in_=W["expert_w_gate"][l, ex].rearrange("(k p) n -> p k n", p=128)), writes=[r_wg32])
                                fw.op("gpsimd", lambda e: e.tensor_copy(out=wgb[:], in_=wg32[:]), reads=[r_wg32], writes=[r_wgb])
                                fw.dma("sync", lambda e, ex=ex: e.dma_start(
                                    out=wu32[:], in_=W["expert_w_up"][l, ex].rearrange("(k p) n -> p k n", p=128)), writes=[r_wu32])
                                fw.op("gpsimd", lambda e: e.tensor_copy(out=wub[:], in_=wu32[:]), reads=[r_wu32], writes=[r_wub])
                                fw.dma("sync", lambda e, ex=ex: e.dma_start(
                                    out=wd32[:], in_=W["expert_w_down"][l, ex].rearrange("(c p) n -> p c n", p=128)), writes=[r_wd32])
                                fw.op("gpsimd", lambda e: e.tensor_copy(out=wdb[:], in_=wd32[:]), reads=[r_wd32], writes=[r_wdb])
                                for c in range(2):
                                    for ts in range(ST // 1024):
                                        o = c0 + ts * 512
                                        j = cnt["pA"] % 4
                                        cnt["pA"] += 1
                                        j2 = cnt["pA"] % 4
                                        cnt["pA"] += 1
                                        for k in range(8):
                                            fw.op("tensor", lambda e, k=k, j=j, c=c, o=o: e.matmul(
                                                pA[j][:], lhsT=wgb[:, k, c * 128:(c + 1) * 128], rhs=hT[:, k, o:o + 512],
                                                start=(k == 0), stop=(k == 7)), reads=[r_wgb, r_hT], writes=[r_pA[j]], pe_acc=True)
                                        for k in range(8):
                                            fw.op("tensor", lambda e, k=k, j2=j2, c=c, o=o: e.matmul(
                                                pA[j2][:], lhsT=wub[:, k, c * 128:(c + 1) * 128], rhs=hT[:, k, o:o + 512],
                                                start=(k == 0), stop=(k == 7)), reads=[r_wub, r_hT], writes=[r_pA[j2]], pe_acc=True)
                                        si = (c + ts) % 2
                                        fw.op("scalar", lambda e, j=j, si=si: e.activation(out=sgl[si][:], in_=pA[j][:], func=AF.Silu),
                                              reads=[r_pA[j]], writes=[r_sgl[si]])
                                        fw.op("vector", lambda e, j2=j2, si=si, c=c, ts=ts: e.tensor_tensor(
                                            out=actT[:, c, ts * 512:(ts + 1) * 512], in0=sgl[si][:], in1=pA[j2][:], op=ALU.mult),
                                            reads=[r_sgl[si], r_pA[j2]], writes=[r_act])
                                for bl in range(HB):
                                    blk = hv * HB + bl
                                    for nh in range(2):
                                        xk = (bl * 2 + nh) % 2
                                        for c in range(2):
                                            fw.op("tensor", lambda e, c=c, xk=xk, bl=bl, nh=nh: e.matmul(
                                                pX[xk][:], lhsT=actT[:, c, bl * 128:(bl + 1) * 128], rhs=wdb[:, c, nh * 512:(nh + 1) * 512],
                                                start=(c == 0), stop=(c == 1)), reads=[r_act, r_wdb], writes=[r_pX[xk]], pe_acc=True)
                                        if ex == 0:
                                            fw.op("vector", lambda e, xk=xk, bl=bl, nh=nh, blk=blk, ex=ex: e.tensor_scalar(
                                                out=macc[:, bl, nh * 512:(nh + 1) * 512], in0=pX[xk][:], scalar1=comb[:, blk, ex:ex + 1],
                                                scalar2=None, op0=ALU.mult), reads=[r_pX[xk], r_comb], writes=[r_macc])
                                        else:
                                            fw.op("vector", lambda e, xk=xk, bl=bl, nh=nh, blk=blk, ex=ex: e.scalar_tensor_tensor(
                                                out=macc[:, bl, nh * 512:(nh + 1) * 512], in0=pX[xk][:], scalar=comb[:, blk, ex:ex + 1],
                                                in1=macc[:, bl, nh * 512:(nh + 1) * 512], op0=ALU.mult, op1=ALU.add),
                                                reads=[r_pX[xk], r_comb, r_macc], writes=[r_macc])
                            dst = y if last else H0
                            for bl in range(HB):
                                i = bl % 2
                                r0 = t0 + (hv * HB + bl) * 128
                                fw.dma("sync", lambda e, i=i, r0=r0: e.dma_start(out=xq[i][:], in_=H1[r0:r0 + 128, :]), writes=[r_xq[i]])
                                fw.op("vector", lambda e, i=i, bl=bl: e.scalar_tensor_tensor(
                                    out=xq[i][:], in0=xq[i][:], scalar=ALPHA, in1=macc[:, bl, :], op0=ALU.mult, op1=ALU.add),
                                    reads=[r_xq[i], r_macc], writes=[r_xq[i]])
                                ln_rows(fw, xq[i], r_xq[i], tq[i], r_tq[i], sq4[i], r_sq4[i], gam, bet, r_gb, xq[i], r_xq[i])
                                fw.dma("sync", lambda e, i=i, r0=r0, dst=dst: e.dma_start(out=dst[r0:r0 + 128, :], in_=xq[i][:]),
                                       reads=[r_xq[i]])
                    fw.barrier()
            fw.barrier()

        if debug:
            dbg["br"] = nc.dram_tensor("dbg_br", [10, 128, NTOK], BF16, kind="ExternalOutput").ap()
        for l in range(depth):
            phase_a(l)
            if stop_after in ("a", "ua"):
                break
            phase_s5(l)
            if stop_after == "s5":
                break
            phase_h()
            phase_b(l, l == depth - 1)
            if stop_after in ("attn", "branches", "ln1"):
                break
    return nc, fw


def ln_rows(fw, xin, r_xin, tmp, r_tmp, s, r_s, gam, bet, r_gb, out_tile, r_out):
    fw.op("vector", lambda e: e.reduce_sum(out=s[:, 0:1], in_=xin[:], axis=AX.X), reads=[r_xin], writes=[r_s])
    fw.op("scalar", lambda e: e.activation(out=tmp[:], in_=xin[:], func=AF.Square), reads=[r_xin, r_s], writes=[r_tmp])
    fw.op("vector", lambda e: e.reduce_sum(out=s[:, 1:2], in_=tmp[:], axis=AX.X), reads=[r_tmp], writes=[r_s])
    fw.op("vector", lambda e: e.tensor_scalar(out=s[:, 2:3], in0=s[:, 0:1], scalar1=1.0 / D, scalar2=None,
                                              op0=ALU.mult), reads=[r_s], writes=[r_s])
    fw.op("vector", lambda e: e.tensor_tensor(out=s[:, 3:4], in0=s[:, 2:3], in1=s[:, 2:3], op=ALU.mult),
          reads=[r_s], writes=[r_s])
    fw.op("vector", lambda e: e.scalar_tensor_tensor(out=s[:, 4:5], in0=s[:, 1:2], scalar=1.0 / D, in1=s[:, 3:4],
                                                     op0=ALU.mult, op1=ALU.subtract), reads=[r_s], writes=[r_s])
    fw.op("vector", lambda e: e.tensor_scalar(out=s[:, 4:5], in0=s[:, 4:5], scalar1=LN_EPS, scalar2=None,
                                              op0=ALU.add), reads=[r_s], writes=[r_s])
    fw.op("scalar", lambda e: e.activation(out=s[:, 5:6], in_=s[:, 4:5], func=AF.Sqrt), reads=[r_s], writes=[r_s])
    fw.op("vector", lambda e: e.reciprocal(out=s[:, 6:7], in_=s[:, 5:6]), reads=[r_s], writes=[r_s])
    fw.op("vector", lambda e: e.scalar_tensor_tensor(out=s[:, 7:8], in0=s[:, 2:3], scalar=-1.0, in1=s[:, 6:7],
                                                     op0=ALU.mult, op1=ALU.mult), reads=[r_s], writes=[r_s])
    fw.op("vector", lambda e: e.tensor_scalar(out=tmp[:], in0=xin[:], scalar1=s[:, 2:3], scalar2=s[:, 6:7],
                                              op0=ALU.subtract, op1=ALU.mult), reads=[r_xin, r_s], writes=[r_tmp])
    fw.op("vector", lambda e: e.tensor_tensor(out=tmp[:], in0=tmp[:], in1=gam[:], op=ALU.mult),
          reads=[r_tmp, r_gb], writes=[r_tmp])
    fw.op("vector", lambda e: e.tensor_tensor(out=out_tile[:], in0=tmp[:], in1=bet[:], op=ALU.add),
          reads=[r_tmp, r_gb], writes=[r_out])


_CACHE = {}


def kernel(**inputs):
    xp = np.ascontiguousarray(inputs["x_prompt"], dtype=np.float32)
    xs = np.ascontiguousarray(inputs["x_sample"], dtype=np.float32)
    slots = {0: [("s", 0)], 1: [("s", 1)], 2: [("p", 0), ("p", 1)], 3: [("p", 2), ("p", 3)],
             4: [("p", 4)], 5: [("p", 5)], 6: [("p", 6)], 7: [("p", 7)]}
    in_maps = []
    for c in range(8):
        xc = np.zeros((NTOK, D), np.float32)
        fl = np.zeros(5, np.float32)
        if slots[c][0][0] == "s":
            xc[:] = xs[slots[c][0][1]]
            fl[1:4] = 1.0
        else:
            for j, (_, pi) in enumerate(slots[c]):
                xc[j * SEG:(j + 1) * SEG] = xp[pi]
        m = {"x": xc, "flags": fl}
        for n in WNAMES:
            m[n] = np.ascontiguousarray(inputs[n], dtype=np.float32)
        in_maps.append(m)
    if "nc" not in _CACHE:
        nc, fw = build()
        fw.finish(_CACHE.get("final", []))
        _CACHE["nc"] = nc
    res = run_bass_kernel_spmd(_CACHE["nc"], in_maps, core_ids=list(range(8)))
    yp = np.zeros_like(xp)
    ys = np.zeros_like(xs)
    for c in range(8):
        yc = np.asarray(res.results[c]["y"], dtype=np.float32)
        if slots[c][0][0] == "s":
            ys[slots[c][0][1]] = yc
        else:
            for j, (_, pi) in enumerate(slots[c]):
                yp[pi] = yc[j * SEG:(j + 1) * SEG]
    return (yp, ys)
```

```python
import math
import numpy as np
from contextlib import ExitStack
import concourse.bass as bass
import concourse.mybir as mybir
from concourse.bass_utils import run_bass_kernel_spmd

F32 = mybir.dt.float32
BF16 = mybir.dt.bfloat16
AF = mybir.ActivationFunctionType
ALU = mybir.AluOpType
AX = mybir.AxisListType

D = 1024
NSEG = 4
SEG = 4096
NTOK = NSEG * SEG
PAD = 1024
SEGP = SEG + 2 * PAD
NTOKP = NSEG * SEGP
ST = 2048
NST = NTOK // ST
DEPTH = 2
ALPHA = (2 * DEPTH) ** 0.25
LN_EPS = 1e-5
IN_COLS = 7424
SLOPES = [2.0 ** (-8.0 * (i + 1) / 12) for i in range(12)]
DIL = [(128, 1), (512, 4), (2048, 16)]

WNAMES = ["ln_in_g", "ln_in_b", "w_in", "s5_a_re", "s5_a_im", "s5_log_dt", "s5_b_re", "s5_b_im", "s5_c_re",
          "s5_c_im", "s5_d", "s5_glu_w", "s5_glu_b", "conv_w", "conv_b", "swa_sink", "w_branch_a", "w_branch_b",
          "w_branch_c", "w_branch_d", "w_o", "ln1_g", "ln1_b", "router_group_w", "router_group_b",
          "router_expert_w", "router_expert_b", "expert_w_gate", "expert_w_up", "expert_w_down", "ln2_g", "ln2_b"]
WSHAPES = {
    "ln_in_g": [D], "ln_in_b": [D], "w_in": [2, D, IN_COLS], "s5_a_re": [2, 2, 24, 64], "s5_a_im": [2, 2, 24, 64],
    "s5_log_dt": [2, 2, 24], "s5_b_re": [2, 2, 24, 64, 16], "s5_b_im": [2, 2, 24, 64, 16],
    "s5_c_re": [2, 2, 24, 16, 64], "s5_c_im": [2, 2, 24, 16, 64], "s5_d": [2, 384], "s5_glu_w": [2, 384, 384],
    "s5_glu_b": [2, 384], "conv_w": [2, 3, 384], "conv_b": [2, 384], "swa_sink": [2, 6],
    "w_branch_a": [2, 384, D], "w_branch_b": [2, 384, D], "w_branch_c": [2, 128, D], "w_branch_d": [2, 384, D],
    "w_o": [2, D, D], "ln1_g": [2, D], "ln1_b": [2, D], "router_group_w": [2, D, 4], "router_group_b": [2, 4],
    "router_expert_w": [2, D, 16], "router_expert_b": [2, 16], "expert_w_gate": [2, 16, D, 256],
    "expert_w_up": [2, 16, D, 256], "expert_w_down": [2, 16, 256, D], "ln2_g": [2, D], "ln2_b": [2, D],
}


ENGS = ["sync", "scalar", "vector", "gpsimd", "tensor"]
SEM_ROLL = 30000


class Res:
    __slots__ = ("name", "w", "r")

    def __init__(self, name):
        self.name = name
        self.w = None
        self.r = []


class FW:
    def __init__(self, nc, es):
        self.nc = nc
        self.es = es
        self.q = {e: [] for e in ENGS}
        self.sems = {e: [es.enter_context(nc.semaphore("s_" + e + "0"))] for e in ENGS}
        self.cnt = {e: 0 for e in ENGS}
        self.seen = {e: {} for e in ENGS}
        self.dma_sems = [es.enter_context(nc.semaphore("d%d" % i)) for i in range(24)]
        self.dma_cnt = [0] * 24
        self.dma_i = 0
        self.n_ops = 0
        self.fence = []

    def barrier(self):
        f = []
        for e in ENGS:
            if self.cnt[e] > 0:
                f.append((self.sems[e][-1], self.cnt[e], e))
        for k in range(len(self.dma_sems)):
            if self.dma_cnt[k] > 0:
                f.append((self.dma_sems[k], self.dma_cnt[k], "dma"))
        self.fence = f

    def _ev_new(self, eng):
        if self.cnt[eng] >= SEM_ROLL:
            self.sems[eng].append(self.es.enter_context(self.nc.semaphore("s_%s%d" % (eng, len(self.sems[eng])))))
            self.cnt[eng] = 0
        self.cnt[eng] += 1
        return (self.sems[eng][-1], self.cnt[eng], eng)

    def _need(self, eng, ev, waits, pe_ok=False):
        if ev is None:
            return
        sem, val, src = ev
        if pe_ok and src == "tensor" and eng == "tensor":
            return
        key = id(sem)
        if self.seen[eng].get(key, 0) >= val:
            return
        if key not in waits or waits[key][1] < val:
            waits[key] = (sem, val)

    def op(self, eng, fn, reads=(), writes=(), pe_acc=False):
        waits = {}
        for ev in self.fence:
            self._need(eng, ev, waits)
        for r in reads:
            self._need(eng, r.w, waits)
        for w in writes:
            self._need(eng, w.w, waits, pe_ok=pe_acc)
            for ev in w.r:
                self._need(eng, ev, waits)
        for key, (sem, val) in waits.items():
            self.seen[eng][key] = val
        ev = self._ev_new(eng)
        self.q[eng].append((list(waits.values()), fn, (ev[0], 1)))
        for r in reads:
            r.r.append(ev)
        for w in writes:
            w.w = ev
            w.r = []
        self.n_ops += 1
        return ev

    def dma(self, eng, fn, reads=(), writes=()):
        if len(writes) == 0 and eng == "sync":
            eng = "scalar"
        waits = {}
        for ev in self.fence:
            self._need(eng, ev, waits)
        for r in reads:
            self._need(eng, r.w, waits)
        for w in writes:
            self._need(eng, w.w, waits)
            for ev in w.r:
                self._need(eng, ev, waits)
        k = self.dma_i % len(self.dma_sems)
        self.dma_i += 1
        sem = self.dma_sems[k]
        if self.dma_cnt[k] > 0:
            self._need(eng, (sem, self.dma_cnt[k], "dma"), waits)
        for key, (s, val) in waits.items():
            self.seen[eng][key] = val
        self.dma_cnt[k] += 16
        ev = (sem, self.dma_cnt[k], "dma")
        self.q[eng].append((list(waits.values()), fn, (sem, 16)))
        for r in reads:
            r.r.append(ev)
        for w in writes:
            w.w = ev
            w.r = []
        self.n_ops += 1
        return ev

    def finish(self, final_res):
        waits = {}
        for r in final_res:
            self._need("sync", r.w, waits)
        for k in range(len(self.dma_sems)):
            if self.dma_cnt[k] > 0:
                self._need("sync", (self.dma_sems[k], self.dma_cnt[k], "dma"), waits)
        tail = list(waits.values())
        q = self.q
        with self.nc.Block() as block:
            def replay(e, name):
                for ws, fn, inc in q[name]:
                    for sem, val in ws:
                        e.wait_ge(sem, val)
                    fn(e).then_inc(inc[0], inc[1])
                if name == "sync":
                    for sem, val in tail:
                        e.wait_ge(sem, val)

            @block.sync
            def _(e):
                replay(e, "sync")

            @block.scalar
            def _(e):
                replay(e, "scalar")

            @block.vector
            def _(e):
                replay(e, "vector")

            @block.gpsimd
            def _(e):
                replay(e, "gpsimd")

            @block.tensor
            def _(e):
                replay(e, "tensor")


def build(debug=False, stop_after=None, depth=DEPTH):
    nc = bass.Bass("TRN2", target_bir_lowering=False)
    x = nc.dram_tensor("x", [NTOK, D], F32, kind="ExternalInput").ap()
    flags_d = nc.dram_tensor("flags", [5], F32, kind="ExternalInput").ap()
    W = {n: nc.dram_tensor(n, WSHAPES[n], F32, kind="ExternalInput").ap() for n in WNAMES}
    y = nc.dram_tensor("y", [NTOK, D], F32, kind="ExternalOutput").ap()
    H0 = nc.dram_tensor("H0", [NTOK, D], F32).ap()
    H1 = nc.dram_tensor("H1", [NTOK, D], F32, kind=("ExternalOutput" if debug else "Internal")).ap()
    HT = nc.dram_tensor("HT", [8, 128, NTOK], BF16).ap()
    UAs = nc.dram_tensor("UAs", [3, 128, NTOK], F32).ap()
    CVs = nc.dram_tensor("CVs", [3, 128, NTOKP], BF16).ap()
    KTs = nc.dram_tensor("KTs", [4, 128, NTOKP], BF16).ap()
    VTs = nc.dram_tensor("VTs", [4, 128, NTOKP], BF16).ap()
    MTs = nc.dram_tensor("MTs", [8, 128, NTOK], BF16, kind=("ExternalOutput" if debug else "Internal")).ap()
    YA = nc.dram_tensor("YA", [2, 3, 128, NTOK], F32, kind=("ExternalOutput" if debug else "Internal")).ap()
    dbg = {}
    if debug:
        dbg["h0"] = nc.dram_tensor("dbg_h0", [NTOK, D], F32, kind="ExternalOutput").ap()
        dbg["ua"] = nc.dram_tensor("dbg_ua", [3, 128, NTOK], F32, kind="ExternalOutput").ap()
        dbg["mix"] = nc.dram_tensor("dbg_mix", [NTOK, D], F32, kind="ExternalOutput").ap()
        dbg["st"] = nc.dram_tensor("dbg_st", [NTOK, 8], F32, kind="ExternalOutput").ap()

    es = ExitStack()
    with es:
        fw = FW(nc, es)

        def sb(name, shape, dt=F32):
            return es.enter_context(nc.sbuf_tensor(name, shape, dt))

        def ps(name, shape, dt=F32):
            return es.enter_context(nc.psum_tensor(name, shape, dt))

        ident = sb("ident", [128, 128], BF16)
        r_ident = Res("ident")
        fw.op("gpsimd", lambda e: e.memset(ident[:], 0.0), writes=[r_ident])
        fw.op("gpsimd", lambda e: e.affine_select(out=ident[:], in_=ident[:], pattern=[[-1, 128]],
                                                  compare_op=ALU.not_equal, fill=1.0, base=0, channel_multiplier=1),
              reads=[r_ident], writes=[r_ident])
        flg = sb("flg", [128, 5])
        r_flg = Res("flg")
        fw.dma("sync", lambda e: e.dma_start(out=flg[:], in_=flags_d.partition_broadcast(128)), writes=[r_flg])
        gam = sb("gam", [128, D])
        bet = sb("bet", [128, D])
        r_gb = Res("gb")

        def load_gb(gname, bname, l):
            gsrc = W[gname] if l is None else W[gname][l]
            bsrc = W[bname] if l is None else W[bname][l]
            fw.dma("sync", lambda e: e.dma_start(out=gam[:], in_=gsrc.partition_broadcast(128)), writes=[r_gb])
            fw.dma("sync", lambda e: e.dma_start(out=bet[:], in_=bsrc.partition_broadcast(128)), writes=[r_gb])

        pT = [ps("pT%d" % i, [128, 8, 128], BF16) for i in range(2)]
        r_pT = [Res("pT%d" % i) for i in range(2)]
        pA = [ps("pA%d" % i, [128, 512]) for i in range(4)]
        r_pA = [Res("pA%d" % i) for i in range(4)]
        pX = [ps("pX%d" % i, [128, 512]) for i in range(2)]
        r_pX = [Res("pX%d" % i) for i in range(2)]
        cnt = {"w": 0, "pA": 0, "blk": 0, "uid": 0}

        def uname(n):
            cnt["uid"] += 1
            return "%s_%d" % (n, cnt["uid"])

        def padpos(t):
            return (t // SEG) * SEGP + PAD + (t % SEG)

        def phase_a(l):
            with ExitStack() as pes:
                def sb2(name, shape, dt=F32):
                    return pes.enter_context(nc.sbuf_tensor(uname("a_" + name), shape, dt))
                hT = sb2("hT", [128, 8, ST], BF16)
                r_hT = Res("hT")
                xb = [sb2("xb%d" % i, [128, D]) for i in range(2)]
                r_xb = [Res("xb%d" % i) for i in range(2)]
                tb = [sb2("tb%d" % i, [128, D]) for i in range(2)]
                r_tb = [Res("tb%d" % i) for i in range(2)]
                hb16 = [sb2("hb16_%d" % i, [128, D], BF16) for i in range(2)]
                r_hb16 = [Res("hb16_%d" % i) for i in range(2)]
                st4 = [sb2("st4_%d" % i, [128, 8]) for i in range(2)]
                r_st4 = [Res("st4_%d" % i) for i in range(2)]
                wst = [sb2("wst%d" % i, [128, 8, 128]) for i in range(2)]
                r_wst = [Res("wst%d" % i) for i in range(2)]
                wbf = [sb2("wbf%d" % i, [128, 8, 128], BF16) for i in range(2)]
                r_wbf = [Res("wbf%d" % i) for i in range(2)]
                zt0 = sb2("zt0", [128, ST], BF16)
                r_zt0 = Res("zt0")
                zf = [sb2("zf%d" % i, [128, 512]) for i in range(4)]
                r_zf = [Res("zf%d" % i) for i in range(4)]
                zb = [sb2("zb%d" % i, [128, 512], BF16) for i in range(4)]
                r_zb = [Res("zb%d" % i) for i in range(4)]

                def layer_norm_block(i):
                    ln_rows(fw, xb[i], r_xb[i], tb[i], r_tb[i], st4[i], r_st4[i], gam, bet, r_gb, xb[i], r_xb[i])

                def transpose_block(i, col0):
                    fw.op("scalar", lambda e: e.activation(out=hb16[i][:], in_=xb[i][:], func=AF.Copy),
                          reads=[r_xb[i]], writes=[r_hb16[i]])
                    for k in range(8):
                        fw.op("tensor", lambda e, k=k: e.transpose(pT[i][:, k, :], hb16[i][:, k * 128:(k + 1) * 128],
                                                                   ident[:]),
                              reads=[r_hb16[i], r_ident], writes=[r_pT[i]], pe_acc=True)
                    fw.op("vector", lambda e: e.tensor_copy(out=hT[:, :, col0:col0 + 128], in_=pT[i][:]),
                          reads=[r_pT[i]], writes=[r_hT])

                def load_w_chunk(src_ap):
                    j = cnt["w"] % 2
                    cnt["w"] += 1
                    fw.dma("sync", lambda e: e.dma_start(out=wst[j][:], in_=src_ap.rearrange("(k p) n -> p k n", p=128)),
                           writes=[r_wst[j]])
                    fw.op("gpsimd", lambda e: e.tensor_copy(out=wbf[j][:], in_=wst[j][:]),
                          reads=[r_wst[j]], writes=[r_wbf[j]])
                    return wbf[j], r_wbf[j]

                def proj_fm(wt, r_w, evac):
                    for ts in range(ST // 512):
                        j = cnt["pA"] % 4
                        cnt["pA"] += 1
                        for k in range(8):
                            fw.op("tensor", lambda e, k=k, j=j, ts=ts: e.matmul(
                                pA[j][:], lhsT=wt[:, k, :], rhs=hT[:, k, ts * 512:(ts + 1) * 512],
                                start=(k == 0), stop=(k == 7)),
                                reads=[r_w, r_hT], writes=[r_pA[j]], pe_acc=True)
                        evac(ts, j, pA[j], r_pA[j])

                if l == 0:
                    load_gb("ln_in_g", "ln_in_b", None)
                for st_i in range(NST):
                    t0 = st_i * ST
                    p0 = padpos(t0)
                    for b in range(ST // 128):
                        i = cnt["blk"] % 2
                        cnt["blk"] += 1
                        r0 = t0 + b * 128
                        if l == 0:
                            fw.dma("sync", lambda e, i=i, r0=r0: e.dma_start(out=xb[i][:], in_=x[r0:r0 + 128, :]),
                                   writes=[r_xb[i]])
                            layer_norm_block(i)
                            fw.dma("sync", lambda e, i=i, r0=r0: e.dma_start(out=H0[r0:r0 + 128, :], in_=xb[i][:]),
                                   reads=[r_xb[i]])
                            if debug:
                                fw.dma("sync", lambda e, i=i, r0=r0: e.dma_start(out=dbg["h0"][r0:r0 + 128, :],
                                                                                in_=xb[i][:]), reads=[r_xb[i]])
                        else:
                            fw.dma("sync", lambda e, i=i, r0=r0: e.dma_start(out=xb[i][:], in_=H0[r0:r0 + 128, :]),
                                   writes=[r_xb[i]])
                        transpose_block(i, b * 128)
                    fw.dma("sync", lambda e, t0=t0: e.dma_start(out=HT[:, :, t0:t0 + ST].rearrange("k p n -> p k n"),
                                                                in_=hT[:]), reads=[r_hT])

                    def store_plain(dst, c, t0=t0):
                        def ev(ts, j, pt, r_pt):
                            fw.op("scalar", lambda e: e.activation(out=zf[j][:], in_=pt[:], func=AF.Copy),
                                  reads=[r_pt], writes=[r_zf[j]])
                            fw.dma("sync", lambda e: e.dma_start(out=dst[c, :, t0 + ts * 512:t0 + (ts + 1) * 512],
                                                                 in_=zf[j][:]), reads=[r_zf[j]])
                            if debug and dst is UAs:
                                fw.dma("sync", lambda e: e.dma_start(
                                    out=dbg["ua"][c, :, t0 + ts * 512:t0 + (ts + 1) * 512], in_=zf[j][:]),
                                    reads=[r_zf[j]])
                        return ev

                    def store_pad(dst, c, p0=p0):
                        def ev(ts, j, pt, r_pt):
                            fw.op("scalar", lambda e: e.activation(out=zb[j][:], in_=pt[:], func=AF.Copy),
                                  reads=[r_pt], writes=[r_zb[j]])
                            fw.dma("sync", lambda e: e.dma_start(out=dst[c, :, p0 + ts * 512:p0 + (ts + 1) * 512],
                                                                 in_=zb[j][:]), reads=[r_zb[j]])
                        return ev

                    wi = W["w_in"][l]
                    for c in range(3):
                        wt, r_w = load_w_chunk(wi[:, c * 128:(c + 1) * 128])
                        proj_fm(wt, r_w, store_plain(UAs, c))
                    if stop_after == "ua":
                        continue
                    for c in range(3):
                        wt, r_w = load_w_chunk(wi[:, (15 + c) * 128:(16 + c) * 128])
                        proj_fm(wt, r_w, store_pad(KTs, c))
                    wt, r_w = load_w_chunk(wi[:, 24 * 128:25 * 128])
                    proj_fm(wt, r_w, store_pad(KTs, 3))
                    for c in range(3):
                        wt, r_w = load_w_chunk(wi[:, (18 + c) * 128:(19 + c) * 128])
                        proj_fm(wt, r_w, store_pad(VTs, c))
                    wt, r_w = load_w_chunk(wi[:, 25 * 128:26 * 128])
                    proj_fm(wt, r_w, store_pad(VTs, 3))
                    for c in range(3):
                        wt, r_w = load_w_chunk(wi[:, (3 + c) * 128:(4 + c) * 128])

                        def ev_vb(ts, j, pt, r_pt):
                            fw.op("scalar", lambda e: e.activation(out=zt0[:, ts * 512:(ts + 1) * 512], in_=pt[:],
                                                                   func=AF.Copy), reads=[r_pt], writes=[r_zt0])
                        proj_fm(wt, r_w, ev_vb)
                        wt, r_w = load_w_chunk(wi[:, (9 + c) * 128:(10 + c) * 128])

                        def ev_gc(ts, j, pt, r_pt, c=c, p0=p0):
                            fw.op("vector", lambda e: e.tensor_tensor(out=zb[j][:], in0=pt[:],
                                                                      in1=zt0[:, ts * 512:(ts + 1) * 512], op=ALU.mult),
                                  reads=[r_pt, r_zt0], writes=[r_zb[j]])
                            fw.dma("sync", lambda e: e.dma_start(out=CVs[c, :, p0 + ts * 512:p0 + (ts + 1) * 512],
                                                                 in_=zb[j][:]), reads=[r_zb[j]])
                        proj_fm(wt, r_w, ev_gc)
            fw.barrier()

        def phase_s5(l):
            with ExitStack() as pes:
                def sb2(name, shape, dt=F32):
                    return pes.enter_context(nc.sbuf_tensor(uname("s_" + name), shape, dt))
                r_p = Res("prm")
                names = ["are", "aim", "ldt", "dt", "rho", "th", "c", "s", "t1", "t2", "lr", "li", "nr", "den",
                         "numr", "numi", "kr", "ki", "nki"]
                P = {n: sb2(n, [128, 24]) for n in names}

                def tt(o, a, b, op):
                    fw.op("vector", lambda e: e.tensor_tensor(out=P[o][:], in0=P[a][:], in1=P[b][:], op=op),
                          reads=[r_p], writes=[r_p])

                def ts_(o, a, s1, op0, s2=None, op1=None):
                    if op1 is None:
                        fw.op("vector", lambda e: e.tensor_scalar(out=P[o][:], in0=P[a][:], scalar1=s1, scalar2=None,
                                                                  op0=op0), reads=[r_p], writes=[r_p])
                    else:
                        fw.op("vector", lambda e: e.tensor_scalar(out=P[o][:], in0=P[a][:], scalar1=s1, scalar2=s2,
                                                                  op0=op0, op1=op1), reads=[r_p], writes=[r_p])

                def act(o, a, func, scale=1.0):
                    fw.op("scalar", lambda e: e.activation(out=P[o][:], in_=P[a][:], func=func, scale=scale),
                          reads=[r_p], writes=[r_p])

                for d in range(2):
                    fw.dma("sync", lambda e, d=d: e.dma_start(
                        out=P["are"][:, d * 12:(d + 1) * 12],
                        in_=W["s5_a_re"][l, d].rearrange("(gp g2) p -> (g2 p) gp", g2=2),
                        allow_slow_non_contiguous=True), writes=[r_p])
                    fw.dma("sync", lambda e, d=d: e.dma_start(
                        out=P["aim"][:, d * 12:(d + 1) * 12],
                        in_=W["s5_a_im"][l, d].rearrange("(gp g2) p -> (g2 p) gp", g2=2),
                        allow_slow_non_contiguous=True), writes=[r_p])
                    for g2 in range(2):
                        fw.dma("sync", lambda e, d=d, g2=g2: e.dma_start(
                            out=P["ldt"][64 * g2:64 * g2 + 64, d * 12:(d + 1) * 12],
                            in_=W["s5_log_dt"][l, d].rearrange("(gp g2) -> g2 gp", g2=2)[g2].partition_broadcast(64),
                            allow_slow_non_contiguous=True), writes=[r_p])
                act("dt", "ldt", AF.Exp)
                tt("t1", "are", "dt", ALU.mult)
                act("rho", "t1", AF.Exp)
                tt("th", "aim", "dt", ALU.mult)
                act("t1", "th", AF.Sin, scale=1.0 / 128)
                tt("t2", "t1", "t1", ALU.mult)
                ts_("c", "t2", -2.0, ALU.mult, 1.0, ALU.add)
                act("s", "th", AF.Sin, scale=1.0 / 64)
                for _ in range(6):
                    tt("t1", "c", "c", ALU.mult)
                    tt("t2", "s", "s", ALU.mult)
                    fw.op("vector", lambda e: e.scalar_tensor_tensor(out=P["s"][:], in0=P["c"][:], scalar=2.0,
                                                                     in1=P["s"][:], op0=ALU.mult, op1=ALU.mult),
                          reads=[r_p], writes=[r_p])
                    tt("c", "t1", "t2", ALU.subtract)
                tt("lr", "rho", "c", ALU.mult)
                tt("li", "rho", "s", ALU.mult)
                ts_("nr", "lr", -1.0, ALU.add)
                tt("t1", "are", "are", ALU.mult)
                tt("t2", "aim", "aim", ALU.mult)
                tt("den", "t1", "t2", ALU.add)
                fw.op("vector", lambda e: e.reciprocal(out=P["den"][:], in_=P["den"][:]), reads=[r_p], writes=[r_p])
                tt("t1", "nr", "are", ALU.mult)
                tt("t2", "li", "aim", ALU.mult)
                tt("numr", "t1", "t2", ALU.add)
                tt("t1", "li", "are", ALU.mult)
                tt("t2", "nr", "aim", ALU.mult)
                tt("numi", "t1", "t2", ALU.subtract)
                tt("kr", "numr", "den", ALU.mult)
                tt("ki", "numi", "den", ALU.mult)
                ts_("nki", "ki", -1.0, ALU.mult)
                LRR = sb2("LRR", [128, 2, 24])
                LIS = sb2("LIS", [128, 2, 24])
                for hh in range(2):
                    fw.op("vector", lambda e, hh=hh: e.tensor_copy(out=LRR[:, hh, :], in_=P["lr"][:]),
                          reads=[r_p], writes=[r_p])
                fw.op("vector", lambda e: e.tensor_scalar(out=LIS[:, 0, :], in0=P["li"][:], scalar1=-1.0, scalar2=None,
                                                          op0=ALU.mult), reads=[r_p], writes=[r_p])
                fw.op("vector", lambda e: e.tensor_copy(out=LIS[:, 1, :], in_=P["li"][:]), reads=[r_p], writes=[r_p])

                for nm in ["l2r", "l2i"]:
                    P[nm] = sb2(nm, [128, 24])
                tt("t1", "lr", "lr", ALU.mult)
                tt("t2", "li", "li", ALU.mult)
                tt("l2r", "t1", "t2", ALU.subtract)
                fw.op("vector", lambda e: e.scalar_tensor_tensor(out=P["l2i"][:], in0=P["lr"][:], scalar=2.0,
                                                                 in1=P["li"][:], op0=ALU.mult, op1=ALU.mult),
                      reads=[r_p], writes=[r_p])
                L2RR = sb2("L2RR", [128, 2, 24])
                L2IS = sb2("L2IS", [128, 2, 24])
                for hh in range(2):
                    fw.op("vector", lambda e, hh=hh: e.tensor_copy(out=L2RR[:, hh, :], in_=P["l2r"][:]),
                          reads=[r_p], writes=[r_p])
                fw.op("vector", lambda e: e.tensor_scalar(out=L2IS[:, 0, :], in0=P["l2i"][:], scalar1=-1.0, scalar2=None,
                                                          op0=ALU.mult), reads=[r_p], writes=[r_p])
                fw.op("vector", lambda e: e.tensor_copy(out=L2IS[:, 1, :], in_=P["l2i"][:]), reads=[r_p], writes=[r_p])
                LRR64 = sb2("LRR64", [128, 2, 24, 64])
                LIS64 = sb2("LIS64", [128, 2, 24, 64])
                for (dst64, src3) in [(LRR64, LRR), (LIS64, LIS)]:
                    fw.op("vector", lambda e, dst64=dst64, src3=src3: e.tensor_copy(out=dst64[:, :, :, 0], in_=src3[:]),
                          reads=[r_p], writes=[r_p])
                    w_ = 1
                    while w_ < 64:
                        fw.op("vector", lambda e, dst64=dst64, w_=w_: e.tensor_copy(out=dst64[:, :, :, w_:2 * w_],
                                                                                   in_=dst64[:, :, :, 0:w_]),
                              reads=[r_p], writes=[r_p])
                        w_ *= 2
                T1 = sb2("T1", [128, 2, 24, 64])
                T2 = sb2("T2", [128, 2, 24, 64])
                CB = sb2("CB", [128, 2, 24, 64])
                r_t12 = Res("T12")
                r_cb = Res("CB")
                Bw = sb2("Bw", [128, 48, 128])
                Cw = sb2("Cw", [128, 48, 128])
                r_bw = Res("Bw")
                r_cw = Res("Cw")
                fw.op("gpsimd", lambda e: e.memset(Bw[:], 0.0), writes=[r_bw])
                fw.op("gpsimd", lambda e: e.memset(Cw[:], 0.0), writes=[r_cw])

                def widx(d, gp, ri):
                    return (d * 12 + gp) * 2 + ri
                for d in range(2):
                    for gp in range(12):
                        for g2 in range(2):
                            g = 2 * gp + g2
                            r0 = 16 * (g % 8)
                            for ri, (bn, cn) in enumerate([("s5_b_re", "s5_c_re"), ("s5_b_im", "s5_c_im")]):
                                fw.dma("sync", lambda e, d=d, gp=gp, g2=g2, g=g, r0=r0, ri=ri, bn=bn: e.dma_start(
                                    out=Bw[r0:r0 + 16, widx(d, gp, ri), 64 * g2:64 * g2 + 64],
                                    in_=W[bn][l, d, g].rearrange("p h -> h p"),
                                    allow_slow_non_contiguous=True), writes=[r_bw])
                                fw.dma("sync", lambda e, d=d, gp=gp, g2=g2, g=g, r0=r0, ri=ri, cn=cn: e.dma_start(
                                    out=Cw[64 * g2:64 * g2 + 64, widx(d, gp, ri), r0:r0 + 16],
                                    in_=W[cn][l, d, g].rearrange("h p -> p h"),
                                    allow_slow_non_contiguous=True), writes=[r_cw])
                Cw4 = Cw[:].rearrange("p (a r) n -> p a r n", r=2)
                fw.op("vector", lambda e: e.tensor_scalar(out=Cw4[:, :, 1, :], in0=Cw4[:, :, 1, :], scalar1=-1.0,
                                                          scalar2=None, op0=ALU.mult), reads=[r_cw], writes=[r_cw])

                XS = sb2("XS", [128, 2, 24, 129])
                BU = sb2("BU", [128, 2, 24, 128])
                PQ = sb2("PQ", [128, 2, 2, 24])
                r_xs = Res("XS")
                r_bu = Res("BU")
                r_pq = Res("PQ")
                r_pq1 = Res("PQ1")
                tmpb = [sb2("tmpb%d" % i, [128, 2, 128]) for i in range(2)]
                r_tmpb = [Res("tmpb%d" % i) for i in range(2)]
                ua = [[sb2("ua%d_%d" % (i, d), [128, 3, 128]) for d in range(2)] for i in range(2)]
                r_ua = [[Res("ua%d_%d" % (i, d)) for d in range(2)] for i in range(2)]
                yo = [sb2("yo%d" % i, [128, 128]) for i in range(2)]
                r_yo = [Res("yo%d" % i) for i in range(2)]
                fw.op("vector", lambda e: e.memset(XS[:], 0.0), writes=[r_xs])
                NT = NTOK // 128
                kcount = 0
                for i in range(NT):
                    tiles = [i, NT - 1 - i]
                    bi = i % 2
                    for d in range(2):
                        tk = tiles[d] * 128
                        fw.dma("sync", lambda e, d=d, tk=tk, bi=bi: e.dma_start(
                            out=ua[bi][d][:], in_=UAs[:, :, tk:tk + 128].rearrange("c p n -> p c n")),
                            writes=[r_ua[bi][d]])
                    for d in range(2):
                        for gp in range(12):
                            col = d * 12 + gp
                            c3 = gp // 4
                            pp = 2 * (col % 2)
                            for ri in range(2):
                                fw.op("tensor", lambda e, d=d, gp=gp, ri=ri, pp=pp, c3=c3, bi=bi: e.matmul(
                                    pA[pp + ri][:, 0:128], lhsT=Bw[:, widx(d, gp, ri), :], rhs=ua[bi][d][:, c3, :],
                                    start=True, stop=True),
                                    reads=[r_bw, r_ua[bi][d]], writes=[r_pA[pp + ri]])
                            tbk = tmpb[col % 2]
                            r_tbk = r_tmpb[col % 2]
                            if d == 0:
                                bre, bim = BU[:, 0, col, :], BU[:, 1, col, :]
                            else:
                                bre, bim = BU[:, 0, col, ::-1], BU[:, 1, col, ::-1]
                            fw.op("vector", lambda e, tbk=tbk, pp=pp, col=col: e.tensor_scalar(
                                out=tbk[:, 0, :], in0=pA[pp][:, 0:128], scalar1=P["kr"][:, col:col + 1], scalar2=None,
                                op0=ALU.mult), reads=[r_pA[pp], r_p], writes=[r_tbk])
                            fw.op("vector", lambda e, tbk=tbk, pp=pp, col=col, bre=bre: e.scalar_tensor_tensor(
                                out=bre, in0=pA[pp + 1][:, 0:128], scalar=P["nki"][:, col:col + 1], in1=tbk[:, 0, :],
                                op0=ALU.mult, op1=ALU.add), reads=[r_pA[pp + 1], r_p, r_tbk], writes=[r_bu])
                            fw.op("vector", lambda e, tbk=tbk, pp=pp, col=col: e.tensor_scalar(
                                out=tbk[:, 1, :], in0=pA[pp + 1][:, 0:128], scalar1=P["kr"][:, col:col + 1],
                                scalar2=None, op0=ALU.mult), reads=[r_pA[pp + 1], r_p], writes=[r_tbk])
                            fw.op("vector", lambda e, tbk=tbk, pp=pp, col=col, bim=bim: e.scalar_tensor_tensor(
                                out=bim, in0=pA[pp][:, 0:128], scalar=P["ki"][:, col:col + 1], in1=tbk[:, 1, :],
                                op0=ALU.mult, op1=ALU.add), reads=[r_pA[pp], r_p, r_tbk], writes=[r_bu])
                    BUe = BU[:, :, :, 0:128:2]
                    BUo = BU[:, :, :, 1:128:2]
                    BUes = BU[:, ::-1, :, 0:128:2]
                    fw.op("vector", lambda e, BUe=BUe: e.tensor_tensor(out=T1[:], in0=LRR64[:], in1=BUe, op=ALU.mult),
                          reads=[r_bu, r_p], writes=[r_t12])
                    fw.op("vector", lambda e, BUes=BUes: e.tensor_tensor(out=T2[:], in0=LIS64[:], in1=BUes, op=ALU.mult),
                          reads=[r_bu, r_p], writes=[r_t12])
                    fw.op("vector", lambda e: e.tensor_tensor(out=T1[:], in0=T1[:], in1=T2[:], op=ALU.add),
                          reads=[r_t12], writes=[r_t12])
                    fw.op("vector", lambda e, BUo=BUo: e.tensor_tensor(out=CB[:], in0=T1[:], in1=BUo, op=ALU.add),
                          reads=[r_t12, r_bu], writes=[r_cb])
                    for m in range(64):
                        j = 2 * m
                        fw.op("vector", lambda e, j=j: e.tensor_tensor(out=PQ[:, 0], in0=L2RR[:], in1=XS[:, :, :, j],
                                                                       op=ALU.mult),
                              reads=[r_xs, r_p], writes=[r_pq])
                        fw.op("vector", lambda e, j=j: e.tensor_tensor(out=PQ[:, 1], in0=L2IS[:], in1=XS[:, ::-1, :, j],
                                                                       op=ALU.mult),
                              reads=[r_xs, r_p], writes=[r_pq1])
                        fw.op("vector", lambda e: e.tensor_tensor(out=PQ[:, 0], in0=PQ[:, 0], in1=PQ[:, 1], op=ALU.add),
                              reads=[r_pq, r_pq1], writes=[r_pq])
                        fw.op("vector", lambda e, j=j, m=m: e.tensor_tensor(out=XS[:, :, :, j + 2], in0=PQ[:, 0],
                                                                            in1=CB[:, :, :, m], op=ALU.add),
                              reads=[r_pq, r_cb], writes=[r_xs])
                    XSe = XS[:, :, :, 0:128:2]
                    XSes = XS[:, ::-1, :, 0:128:2]
                    XSo = XS[:, :, :, 1:129:2]
                    fw.op("vector", lambda e, XSe=XSe: e.tensor_tensor(out=T1[:], in0=LRR64[:], in1=XSe, op=ALU.mult),
                          reads=[r_xs, r_p], writes=[r_t12])
                    fw.op("vector", lambda e, XSes=XSes: e.tensor_tensor(out=T2[:], in0=LIS64[:], in1=XSes, op=ALU.mult),
                          reads=[r_xs, r_p], writes=[r_t12])
                    fw.op("vector", lambda e: e.tensor_tensor(out=T1[:], in0=T1[:], in1=T2[:], op=ALU.add),
                          reads=[r_t12], writes=[r_t12])
                    fw.op("vector", lambda e, XSo=XSo, BUe=BUe: e.tensor_tensor(out=XSo, in0=T1[:], in1=BUe, op=ALU.add),
                          reads=[r_t12, r_bu], writes=[r_xs])
                    for d in range(2):
                        tk = tiles[d] * 128
                        for c3 in range(3):
                            pj = kcount % 2
                            kcount += 1
                            n = 0
                            for gq in range(4):
                                gp = c3 * 4 + gq
                                col = d * 12 + gp
                                for ri in range(2):
                                    fw.op("tensor", lambda e, d=d, gp=gp, ri=ri, col=col, pj=pj, n=n: e.matmul(
                                        pX[pj][:, 0:128], lhsT=Cw[:, widx(d, gp, ri), :], rhs=XS[:, ri, col, 1:129],
                                        start=(n == 0), stop=(n == 7)),
                                        reads=[r_cw, r_xs], writes=[r_pX[pj]], pe_acc=True)
                                    n += 1
                            ov = yo[pj][:, :] if d == 0 else yo[pj][:, ::-1]
                            fw.op("scalar", lambda e, pj=pj, ov=ov: e.activation(out=ov, in_=pX[pj][:, 0:128],
                                                                                 func=AF.Copy),
                                  reads=[r_pX[pj]], writes=[r_yo[pj]])
                            fw.dma("sync", lambda e, d=d, c3=c3, tk=tk, pj=pj: e.dma_start(
                                out=YA[d, c3, :, tk:tk + 128], in_=yo[pj][:]), reads=[r_yo[pj]])
                    fw.op("vector", lambda e: e.tensor_copy(out=XS[:, :, :, 0], in_=XS[:, :, :, 128]),
                          reads=[r_xs], writes=[r_xs])
                    if (i + 1) % 32 == 0 and i + 1 < NT:
                        sgn = (i + 1) // 32
                        fw.op("vector", lambda e, sgn=sgn: e.tensor_scalar(
                            out=XS[:, :, 0:12, 0], in0=XS[:, :, 0:12, 0], scalar1=flg[:, sgn:sgn + 1], scalar2=None,
                            op0=ALU.mult), reads=[r_xs, r_flg], writes=[r_xs])
                        fw.op("vector", lambda e, sgn=sgn: e.tensor_scalar(
                            out=XS[:, :, 12:24, 0], in0=XS[:, :, 12:24, 0], scalar1=flg[:, 4 - sgn:5 - sgn],
                            scalar2=None, op0=ALU.mult), reads=[r_xs, r_flg], writes=[r_xs])
            fw.barrier()

        def phase_h():
            with ExitStack() as pes:
                hb = [pes.enter_context(nc.sbuf_tensor(uname("h_hb%d" % i), [128, 4, PAD], BF16)) for i in range(2)]
                r_hb = [Res("hb%d" % i) for i in range(2)]
                k = 0
                for (T, nch) in [(KTs, 4), (VTs, 4), (CVs, 3)]:
                    for sg in range(NSEG):
                        jobs = []
                        src = ((sg - 1) * SEGP + SEG) if sg > 0 else (sg * SEGP + PAD)
                        jobs.append((src, sg * SEGP, sg))
                        src = ((sg + 1) * SEGP + PAD) if sg < NSEG - 1 else (sg * SEGP + SEG)
                        jobs.append((src, sg * SEGP + PAD + SEG, sg + 1))
                        for (src, dst, fc) in jobs:
                            b = k % 2
                            k += 1
                            fw.dma("sync", lambda e, T=T, nch=nch, src=src, b=b: e.dma_start(
                                out=hb[b][:, 0:nch, :], in_=T[0:nch, :, src:src + PAD].rearrange("c p n -> p c n")),
                                writes=[r_hb[b]])
                            fw.op("vector", lambda e, nch=nch, b=b, fc=fc: e.tensor_scalar(
                                out=hb[b][:, 0:nch, :], in0=hb[b][:, 0:nch, :], scalar1=flg[:, fc:fc + 1], scalar2=None,
                                op0=ALU.mult), reads=[r_hb[b], r_flg], writes=[r_hb[b]])
                            fw.dma("sync", lambda e, T=T, nch=nch, dst=dst, b=b: e.dma_start(
                                out=T[0:nch, :, dst:dst + PAD].rearrange("c p n -> p c n"), in_=hb[b][:, 0:nch, :]),
                                reads=[r_hb[b]])
            fw.barrier()

        maskD = sb("maskD", [128, 6, 256])
        maskS = sb("maskS", [128, 6, 384])
        r_mask = Res("mask")
        ones_col = sb("ones_col", [128, 1])
        fw.op("vector", lambda e: e.memset(ones_col[:], 1.0), writes=[r_mask])
        with ExitStack() as mes:
            ii = mes.enter_context(nc.sbuf_tensor("m_ii", [128, 128], mybir.dt.int32))
            fi = mes.enter_context(nc.sbuf_tensor("m_fi", [128, 128], F32))
            ta = mes.enter_context(nc.sbuf_tensor("m_ta", [128, 128], F32))
            tv = mes.enter_context(nc.sbuf_tensor("m_tv", [128, 128], F32))
            r_m = Res("m")
            fw.op("gpsimd", lambda e: e.iota(ii[:], pattern=[[-1, 128]], base=0, channel_multiplier=1), writes=[r_m])
            fw.op("vector", lambda e: e.tensor_copy(out=fi[:], in_=ii[:]), reads=[r_m], writes=[r_m])

            def mk_mask(dst, off, half, coef):
                fw.op("vector", lambda e: e.tensor_scalar(out=ta[:], in0=fi[:], scalar1=float(off), scalar2=None,
                                                          op0=ALU.add), reads=[r_m], writes=[r_m])
                fw.op("vector", lambda e: e.tensor_scalar(out=tv[:], in0=ta[:], scalar1=-1.0, scalar2=None,
                                                          op0=ALU.mult), reads=[r_m], writes=[r_m])
                fw.op("vector", lambda e: e.tensor_tensor(out=ta[:], in0=ta[:], in1=tv[:], op=ALU.max),
                      reads=[r_m], writes=[r_m])
                fw.op("vector", lambda e: e.tensor_scalar(out=tv[:], in0=ta[:], scalar1=-1.0, scalar2=float(half) + 0.5,
                                                          op0=ALU.mult, op1=ALU.add), reads=[r_m], writes=[r_m])
                fw.op("vector", lambda e: e.tensor_scalar(out=tv[:], in0=tv[:], scalar1=0.0, scalar2=0.5,
                                                          op0=ALU.max, op1=ALU.min), reads=[r_m], writes=[r_m])
                fw.op("scalar", lambda e: e.activation(out=ta[:], in_=ta[:], func=AF.Exp, scale=-float(coef)),
                      reads=[r_m], writes=[r_m])
                fw.op("vector", lambda e: e.scalar_tensor_tensor(out=dst, in0=ta[:], scalar=2.0, in1=tv[:],
                                                                 op0=ALU.mult, op1=ALU.mult),
                      reads=[r_m], writes=[r_m, r_mask])
            for gi, (win, dil) in enumerate(DIL):
                for h in range(2):
                    sl = SLOPES[6 + 2 * gi + h]
                    for kt in range(2):
                        mk_mask(maskD[:, 2 * gi + h, kt * 128:(kt + 1) * 128], -64 + 128 * kt, 64, sl * dil)
            for h in range(6):
                for kt in range(3):
                    mk_mask(maskS[:, h, kt * 128:(kt + 1) * 128], 128 * (kt - 1), 128, SLOPES[h])
        fw.barrier()

        def phase_b(l, last):
            with ExitStack() as L0:
                def sb0(name, shape, dt=F32):
                    return L0.enter_context(nc.sbuf_tensor(uname("b_" + name), shape, dt))
                hT = sb0("hT", [128, 8, ST], BF16)
                r_hT = Res("hT")
                vcol = sb0("vcol", [128, 2])
                r_vcol = Res("vcol")
                sexp = sb0("sexp", [128, 6])
                r_sexp = Res("sexp")
                fw.dma("sync", lambda e: e.dma_start(out=sexp[:], in_=W["swa_sink"][l].partition_broadcast(128)),
                       writes=[r_sexp])
                fw.op("scalar", lambda e: e.activation(out=sexp[:], in_=sexp[:], func=AF.Exp),
                      reads=[r_sexp], writes=[r_sexp])
                wi = W["w_in"][l]
                for st_i in range(NST):
                    t0 = st_i * ST
                    p0 = padpos(t0)
                    sg = st_i // 2
                    hf = st_i % 2
                    fw.dma("sync", lambda e, t0=t0: e.dma_start(
                        out=hT[:], in_=HT[:, :, t0:t0 + ST].rearrange("k p n -> p k n")), writes=[r_hT])
                    fw.op("vector", lambda e: e.memset(vcol[:], 1.0), writes=[r_vcol])
                    fw.op("vector", lambda e, sg=sg: e.tensor_copy(out=vcol[0:64, 0:1], in_=flg[0:64, sg:sg + 1]),
                          reads=[r_flg], writes=[r_vcol])
                    fw.op("vector", lambda e, sg=sg: e.tensor_copy(out=vcol[64:128, 1:2], in_=flg[64:128, sg + 1:sg + 2]),
                          reads=[r_flg], writes=[r_vcol])
                    with ExitStack() as L1:
                        def sb1(name, shape, dt=F32):
                            return L1.enter_context(nc.sbuf_tensor(uname("b1_" + name), shape, dt))
                        brT = sb1("brT", [128, 10, ST], BF16)
                        r_br = Res("brT")
                        wst = [sb1("wst%d" % i, [128, 8, 128]) for i in range(2)]
                        r_wst = [Res("wst%d" % i) for i in range(2)]
                        wbf = [sb1("wbf%d" % i, [128, 8, 128], BF16) for i in range(2)]
                        r_wbf = [Res("wbf%d" % i) for i in range(2)]

                        def load_w_chunk(parts):
                            j = cnt["w"] % 2
                            cnt["w"] += 1
                            for (src_ap, c0, n) in parts:
                                fw.dma("sync", lambda e, src_ap=src_ap, c0=c0, n=n, j=j: e.dma_start(
                                    out=wst[j][:, :, c0:c0 + n], in_=src_ap.rearrange("(k p) n -> p k n", p=128)),
                                    writes=[r_wst[j]])
                            fw.op("gpsimd", lambda e, j=j: e.tensor_copy(out=wbf[j][:], in_=wst[j][:]),
                                  reads=[r_wst[j]], writes=[r_wbf[j]])
                            return wbf[j], r_wbf[j]

                        def proj_fm(wt, r_w, evac):
                            for ts in range(ST // 512):
                                j = cnt["pA"] % 4
                                cnt["pA"] += 1
                                for k in range(8):
                                    fw.op("tensor", lambda e, k=k, j=j, ts=ts: e.matmul(
                                        pA[j][:], lhsT=wt[:, k, :], rhs=hT[:, k, ts * 512:(ts + 1) * 512],
                                        start=(k == 0), stop=(k == 7)),
                                        reads=[r_w, r_hT], writes=[r_pA[j]], pe_acc=True)
                                evac(ts, j, pA[j], r_pA[j])

                        with ExitStack() as S1:
                            def sbs(name, shape, dt=F32):
                                return S1.enter_context(nc.sbuf_tensor(uname("b2_" + name), shape, dt))
                            KT1 = sbs("KT1", [128, 2 * ST], BF16)
                            VT1 = sbs("VT1", [128, 2 * ST], BF16)
                            r_kv = Res("kv")
                            QT1 = sbs("QT1", [128, ST], BF16)
                            r_q = Res("q")
                            UACC = sbs("UACC", [128, 2, ST])
                            r_ua = Res("uacc")
                            RC = sbs("RC", [128, ST])
                            r_rc = Res("rc")
                            Et = [sbs("E%d" % i, [128, 384]) for i in range(2)]
                            r_E = [Res("E%d" % i) for i in range(2)]
                            Pt = [sbs("P%d" % i, [128, 384], BF16) for i in range(2)]
                            r_P = [Res("P%d" % i) for i in range(2)]
                            VE = [sbs("VE%d" % i, [128, 3, 2, 192], BF16) for i in range(2)]
                            r_VE = [Res("VE%d" % i) for i in range(2)]
                            sm = [sbs("sm%d" % i, [128, 128]) for i in range(2)]
                            r_sm = [Res("sm%d" % i) for i in range(2)]
                            for i in range(2):
                                fw.op("vector", lambda e, i=i: e.memset(VE[i][:], 1.0), writes=[r_VE[i]])
                            ac = {"u": 0}

                            def q_evac(ts, j, pt, r_pt):
                                fw.op("scalar", lambda e: e.activation(out=QT1[:, ts * 512:(ts + 1) * 512], in_=pt[:],
                                                                       func=AF.Copy), reads=[r_pt], writes=[r_q])

                            def load_kv(c, p0=p0):
                                fw.dma("sync", lambda e, c=c, p0=p0: e.dma_start(
                                    out=KT1[:], in_=KTs[c, :, p0 - PAD:p0 - PAD + 2 * ST]), writes=[r_kv])
                                fw.dma("sync", lambda e, c=c, p0=p0: e.dma_start(
                                    out=VT1[:], in_=VTs[c, :, p0 - PAD:p0 - PAD + 2 * ST]), writes=[r_kv])

                            def unit(nkt, kcols, qcols, heads, mask_of, valid_of, lhs_of, sink_dst):
                                u = ac["u"] % 2
                                ac["u"] += 1
                                for kt in range(nkt):
                                    fw.op("tensor", lambda e, kt=kt, u=u: e.transpose(
                                        pT[u][:, kt, :], VT1[:, kcols(kt)], ident[:]),
                                        reads=[r_kv, r_ident], writes=[r_pT[u]], pe_acc=True)
                                fw.op("vector", lambda e, u=u: e.tensor_copy(
                                    out=VE[u][:, 0:nkt, :, 64:128],
                                    in_=pT[u][:, 0:nkt, :].rearrange("p k (h d) -> p k h d", h=2)),
                                    reads=[r_pT[u]], writes=[r_VE[u]])
                                for (hrow, vslot, tag) in heads:
                                    j = cnt["pA"] % 4
                                    cnt["pA"] += 1
                                    j2 = cnt["pA"] % 4
                                    cnt["pA"] += 1
                                    ei = ac["u"] % 2
                                    for kt in range(nkt):
                                        fw.op("tensor", lambda e, kt=kt, j=j, hrow=hrow: e.matmul(
                                            pA[j][:, kt * 128:(kt + 1) * 128], lhsT=KT1[hrow:hrow + 64, kcols(kt)],
                                            rhs=QT1[hrow:hrow + 64, qcols], start=True, stop=True),
                                            reads=[r_kv, r_q], writes=[r_pA[j]], pe_acc=True)
                                    fw.op("scalar", lambda e, j=j, ei=ei: e.activation(
                                        out=Et[ei][:, 0:nkt * 128], in_=pA[j][:, 0:nkt * 128], func=AF.Exp, scale=0.125),
                                        reads=[r_pA[j]], writes=[r_E[ei]])
                                    for kt in range(nkt):
                                        vc = valid_of(kt)
                                        fw.op("vector", lambda e, kt=kt, ei=ei, vc=vc, tag=tag: e.scalar_tensor_tensor(
                                            out=Pt[ei][:, kt * 128:(kt + 1) * 128], in0=Et[ei][:, kt * 128:(kt + 1) * 128],
                                            scalar=vc, in1=mask_of(tag)[:, kt * 128:(kt + 1) * 128],
                                            op0=ALU.mult, op1=ALU.mult),
                                            reads=[r_E[ei], r_mask, r_vcol, r_flg], writes=[r_P[ei]])
                                    for kt in range(nkt):
                                        fw.op("tensor", lambda e, kt=kt, j2=j2, ei=ei, u=u, vslot=vslot, tag=tag: e.matmul(
                                            pA[j2][:, 0:128], lhsT=lhs_of(VE[u], kt, vslot, tag),
                                            rhs=Pt[ei][:, kt * 128:(kt + 1) * 128], start=(kt == 0), stop=(kt == nkt - 1)),
                                            reads=[r_VE[u], r_P[ei]], writes=[r_pA[j2]], pe_acc=True)
                                    sink_dst(tag, pA[j2], r_pA[j2])

                            for gi, (win, dil) in enumerate(DIL):
                                wt, r_w = load_w_chunk([(wi[:, (12 + gi) * 128:(13 + gi) * 128], 0, 128)])
                                proj_fm(wt, r_w, q_evac)
                                load_kv(gi)
                                nsub = ST // dil
                                for r in range(dil):
                                    for qb in range(nsub // 128):
                                        q0 = qb * 128
                                        c_lo = r + dil * q0

                                        def kcols(kt, c_lo=c_lo, dil=dil):
                                            b = PAD + c_lo + dil * (-64 + 128 * kt)
                                            return slice(b, b + 127 * dil + 1, dil)
                                        qcols = slice(c_lo, c_lo + 127 * dil + 1, dil)

                                        def valid_of(kt, qb=qb, nsub=nsub):
                                            if hf == 0 and qb == 0 and kt == 0:
                                                return vcol[:, 0:1]
                                            if hf == 1 and qb == nsub // 128 - 1 and kt == 1:
                                                return vcol[:, 1:2]
                                            return ones_col[:, 0:1]

                                        def mask_of(tag, gi=gi):
                                            return maskD[:, 2 * gi + tag, :]

                                        def lhs_of(ve, kt, vslot, tag):
                                            return ve[:, kt, tag, 64:192] if tag == 0 else ve[:, kt, tag, 0:128]

                                        def sink_dst(tag, pu, r_pu, gi=gi, qcols=qcols):
                                            dstv = UACC[:, tag, qcols]
                                            if gi == 0:
                                                fw.op("vector", lambda e: e.tensor_copy(out=dstv, in_=pu[:, 0:128]),
                                                      reads=[r_pu], writes=[r_ua])
                                            else:
                                                fw.op("vector", lambda e: e.tensor_tensor(out=dstv, in0=dstv,
                                                                                          in1=pu[:, 0:128], op=ALU.add),
                                                      reads=[r_pu, r_ua], writes=[r_ua])
                                        unit(2, kcols, qcols, [(0, 0, 0), (64, 1, 1)], mask_of, valid_of, lhs_of, sink_dst)
                            fw.op("vector", lambda e: e.reciprocal(out=RC[0:64, :], in_=UACC[64:128, 0, :]),
                                  reads=[r_ua], writes=[r_rc])
                            fw.op("vector", lambda e: e.reciprocal(out=RC[64:128, :], in_=UACC[0:64, 1, :]),
                                  reads=[r_ua], writes=[r_rc])
                            fw.op("vector", lambda e: e.tensor_tensor(out=brT[0:64, 6, :], in0=UACC[0:64, 0, :],
                                                                      in1=RC[0:64, :], op=ALU.mult),
                                  reads=[r_ua, r_rc], writes=[r_br])
                            fw.op("vector", lambda e: e.tensor_tensor(out=brT[64:128, 6, :], in0=UACC[64:128, 1, :],
                                                                      in1=RC[64:128, :], op=ALU.mult),
                                  reads=[r_ua, r_rc], writes=[r_br])
                            load_kv(3)
                            for jq in range(3):
                                wt, r_w = load_w_chunk([(wi[:, 2688 + 64 * jq:2688 + 64 * jq + 64], 0, 64),
                                                        (wi[:, 2688 + 64 * (jq + 3):2688 + 64 * (jq + 3) + 64], 64, 64)])
                                proj_fm(wt, r_w, q_evac)
                                for qb in range(ST // 128):
                                    q0 = qb * 128

                                    def kcols(kt, q0=q0):
                                        b = PAD + q0 - 128 + 128 * kt
                                        return slice(b, b + 128)
                                    qcols = slice(q0, q0 + 128)

                                    def valid_of(kt, qb=qb):
                                        if hf == 0 and qb == 0 and kt == 0:
                                            return flg[:, sg:sg + 1]
                                        if hf == 1 and qb == ST // 128 - 1 and kt == 2:
                                            return flg[:, sg + 1:sg + 2]
                                        return ones_col[:, 0:1]

                                    def mask_of(tag):
                                        return maskS[:, tag, :]

                                    def lhs_of(ve, kt, vslot, tag):
                                        return ve[:, kt, vslot, 64:192] if tag % 2 == 0 else ve[:, kt, vslot, 0:128]

                                    def sink_dst(tag, pu, r_pu, qcols=qcols):
                                        h = tag
                                        ch, half = 7 + h // 2, h % 2
                                        si = ac["u"] % 2
                                        if half == 0:
                                            urows, drows = slice(0, 64), slice(64, 128)
                                        else:
                                            urows, drows = slice(64, 128), slice(0, 64)
                                        fw.op("vector", lambda e: e.tensor_scalar(
                                            out=sm[si][drows, :], in0=pu[drows, 0:128], scalar1=sexp[drows, h:h + 1],
                                            scalar2=None, op0=ALU.add), reads=[r_pu, r_sexp], writes=[r_sm[si]])
                                        fw.op("vector", lambda e: e.reciprocal(out=sm[si][drows, :], in_=sm[si][drows, :]),
                                              reads=[r_sm[si]], writes=[r_sm[si]])
                                        fw.op("vector", lambda e: e.tensor_tensor(
                                            out=brT[urows, ch, qcols], in0=pu[urows, 0:128], in1=sm[si][drows, :],
                                            op=ALU.mult), reads=[r_pu, r_sm[si]], writes=[r_br])
                                    unit(3, kcols, qcols, [(0, 0, jq), (64, 1, jq + 3)], mask_of, valid_of, lhs_of, sink_dst)
                        fw.barrier()
                        if stop_after == "attn":
                            fw.dma("sync", lambda e, t0=t0: e.dma_start(
                                out=dbg["br"][:, :, t0:t0 + ST].rearrange("c p n -> p c n"), in_=brT[:]), reads=[r_br])
                            fw.barrier()
                            continue
                        with ExitStack() as S2:
                            def sbt(name, shape, dt=F32):
                                return S2.enter_context(nc.sbuf_tensor(uname("b3_" + name), shape, dt))
                            dvec = sbt("dvec", [128, 3])
                            glub = sbt("glub", [128, 3])
                            cwt = sbt("cwt", [128, 3, 3])
                            cbt = sbt("cbt", [128, 3])
                            r_sv = Res("sv")
                            fw.dma("sync", lambda e: e.dma_start(out=dvec[:], in_=W["s5_d"][l].rearrange("(c p) -> p c", p=128),
                                                                 allow_slow_non_contiguous=True), writes=[r_sv])
                            fw.dma("sync", lambda e: e.dma_start(out=glub[:], in_=W["s5_glu_b"][l].rearrange("(c p) -> p c", p=128),
                                                                 allow_slow_non_contiguous=True), writes=[r_sv])
                            fw.dma("sync", lambda e: e.dma_start(out=cwt[:], in_=W["conv_w"][l].rearrange("t (c p) -> p t c", p=128),
                                                                 allow_slow_non_contiguous=True), writes=[r_sv])
                            fw.dma("sync", lambda e: e.dma_start(out=cbt[:], in_=W["conv_b"][l].rearrange("(c p) -> p c", p=128),
                                                                 allow_slow_non_contiguous=True), writes=[r_sv])
                            gluw32 = sbt("gluw32", [128, 3, 384])
                            gluw = sbt("gluw", [128, 3, 384], BF16)
                            r_gluw = Res("gluw")
                            fw.dma("sync", lambda e: e.dma_start(out=gluw32[:], in_=W["s5_glu_w"][l].rearrange("(c p) n -> p c n", p=128)),
                                   writes=[r_gluw])
                            fw.op("gpsimd", lambda e: e.tensor_copy(out=gluw[:], in_=gluw32[:]), reads=[r_gluw], writes=[r_gluw])
                            yf = [sbt("yf%d" % i, [128, 512]) for i in range(2)]
                            yb_ = [sbt("yb%d" % i, [128, 512]) for i in range(2)]
                            uu = [sbt("uu%d" % i, [128, 512]) for i in range(2)]
                            r_y3 = [Res("y3_%d" % i) for i in range(2)]
                            zf32 = sbt("zf32", [128, 3, 512])
                            zb16 = sbt("zb16", [128, 3, 512], BF16)
                            r_z = Res("z")
                            gt = [sbt("gt%d" % i, [128, 512]) for i in range(2)]
                            r_gt = [Res("gt%d" % i) for i in range(2)]
                            kk = 0
                            for ts in range(ST // 512):
                                tk = t0 + ts * 512
                                for c in range(3):
                                    b = kk % 2
                                    kk += 1
                                    fw.dma("sync", lambda e, c=c, tk=tk, b=b: e.dma_start(out=yf[b][:], in_=YA[0, c, :, tk:tk + 512]),
                                           writes=[r_y3[b]])
                                    fw.dma("sync", lambda e, c=c, tk=tk, b=b: e.dma_start(out=yb_[b][:], in_=YA[1, c, :, tk:tk + 512]),
                                           writes=[r_y3[b]])
                                    fw.dma("sync", lambda e, c=c, tk=tk, b=b: e.dma_start(out=uu[b][:], in_=UAs[c, :, tk:tk + 512]),
                                           writes=[r_y3[b]])
                                    fw.op("vector", lambda e, b=b: e.tensor_tensor(out=yf[b][:], in0=yf[b][:], in1=yb_[b][:], op=ALU.add),
                                          reads=[r_y3[b]], writes=[r_y3[b]])
                                    fw.op("vector", lambda e, b=b, c=c: e.scalar_tensor_tensor(
                                        out=yf[b][:], in0=uu[b][:], scalar=dvec[:, c:c + 1], in1=yf[b][:], op0=ALU.mult, op1=ALU.add),
                                        reads=[r_y3[b], r_sv], writes=[r_y3[b]])
                                    fw.op("scalar", lambda e, b=b, c=c: e.activation(out=zf32[:, c, :], in_=yf[b][:], func=AF.Gelu),
                                          reads=[r_y3[b]], writes=[r_z])
                                    fw.op("vector", lambda e, c=c: e.tensor_copy(out=zb16[:, c, :], in_=zf32[:, c, :]),
                                          reads=[r_z], writes=[r_z])
                                for co in range(3):
                                    j = cnt["pA"] % 4
                                    cnt["pA"] += 1
                                    for ci in range(3):
                                        fw.op("tensor", lambda e, ci=ci, co=co, j=j: e.matmul(
                                            pA[j][:], lhsT=gluw[:, ci, co * 128:(co + 1) * 128], rhs=zb16[:, ci, :],
                                            start=(ci == 0), stop=(ci == 2)), reads=[r_gluw, r_z], writes=[r_pA[j]], pe_acc=True)
                                    g2 = co % 2
                                    fw.op("vector", lambda e, j=j, g2=g2, co=co: e.tensor_scalar(
                                        out=gt[g2][:], in0=pA[j][:], scalar1=glub[:, co:co + 1], scalar2=None, op0=ALU.add),
                                        reads=[r_pA[j], r_sv], writes=[r_gt[g2]])
                                    fw.op("scalar", lambda e, g2=g2: e.activation(out=gt[g2][:], in_=gt[g2][:], func=AF.Sigmoid),
                                          reads=[r_gt[g2]], writes=[r_gt[g2]])
                                    fw.op("vector", lambda e, g2=g2, co=co, ts=ts: e.tensor_tensor(
                                        out=brT[:, co, ts * 512:(ts + 1) * 512], in0=zf32[:, co, :], in1=gt[g2][:], op=ALU.mult),
                                        reads=[r_z, r_gt[g2]], writes=[r_br])
                            cvt = sbt("cvt", [128, ST + 2], BF16)
                            r_cvt = Res("cvt")
                            accf = [sbt("accf%d" % i, [128, 512]) for i in range(2)]
                            r_accf = [Res("accf%d" % i) for i in range(2)]
                            for c in range(3):
                                wt, r_w = load_w_chunk([(wi[:, (6 + c) * 128:(7 + c) * 128], 0, 128)])
                                fw.dma("sync", lambda e, c=c, p0=p0: e.dma_start(out=cvt[:], in_=CVs[c, :, p0 - 1:p0 + ST + 1]),
                                       writes=[r_cvt])

                                def ev_gb(ts, j, pt, r_pt, c=c):
                                    a = j % 2
                                    o = ts * 512
                                    fw.op("vector", lambda e: e.tensor_scalar(out=accf[a][:], in0=cvt[:, o:o + 512],
                                                                              scalar1=cwt[:, 0, c:c + 1], scalar2=None, op0=ALU.mult),
                                          reads=[r_cvt, r_sv], writes=[r_accf[a]])
                                    for tap in (1, 2):
                                        fw.op("vector", lambda e, tap=tap: e.scalar_tensor_tensor(
                                            out=accf[a][:], in0=cvt[:, o + tap:o + tap + 512], scalar=cwt[:, tap, c:c + 1],
                                            in1=accf[a][:], op0=ALU.mult, op1=ALU.add),
                                            reads=[r_cvt, r_sv, r_accf[a]], writes=[r_accf[a]])
                                    fw.op("vector", lambda e: e.scalar_tensor_tensor(
                                        out=brT[:, 3 + c, o:o + 512], in0=accf[a][:], scalar=cbt[:, c:c + 1], in1=pt[:],
                                        op0=ALU.add, op1=ALU.mult), reads=[r_accf[a], r_sv, r_pt], writes=[r_br])
                                proj_fm(wt, r_w, ev_gb)
                            if stop_after == "branches":
                                fw.dma("sync", lambda e, t0=t0: e.dma_start(
                                    out=dbg["br"][:, :, t0:t0 + ST].rearrange("c p n -> p c n"), in_=brT[:]), reads=[r_br])
                                fw.barrier()
                                continue
                            wbr32 = [sbt("wbr32_%d" % i, [128, 3, 128]) for i in range(2)]
                            wbr = [sbt("wbr%d" % i, [128, 3, 128], BF16) for i in range(2)]
                            r_wbr = [Res("wbr%d" % i) for i in range(2)]
                            mac = sbt("mac", [128, ST])
                            r_mac = Res("mac")
                            mbf = [sbt("mbf%d" % i, [128, ST], BF16) for i in range(2)]
                            r_mbf = [Res("mbf%d" % i) for i in range(2)]
                            sgt = [sbt("sgt%d" % i, [128, 512]) for i in range(2)]
                            r_sgt = [Res("sgt%d" % i) for i in range(2)]
                            BRS = [("w_branch_a", 3, 0), ("w_branch_b", 3, 3), ("w_branch_c", 1, 6), ("w_branch_d", 3, 7)]
                            q = 0
                            for jo in range(8):
                                for br, (wn, nch, ch0) in enumerate(BRS):
                                    wg, r_wg = load_w_chunk([(wi[:, 3328 + br * 1024 + jo * 128:3328 + br * 1024 + (jo + 1) * 128], 0, 128)])
                                    wb = q % 2
                                    q += 1
                                    fw.dma("sync", lambda e, wn=wn, nch=nch, jo=jo, wb=wb: e.dma_start(
                                        out=wbr32[wb][:, 0:nch, :],
                                        in_=W[wn][l][:, jo * 128:(jo + 1) * 128].rearrange("(c p) n -> p c n", p=128)),
                                        writes=[r_wbr[wb]])
                                    fw.op("gpsimd", lambda e, nch=nch, wb=wb: e.tensor_copy(out=wbr[wb][:, 0:nch, :],
                                                                                          in_=wbr32[wb][:, 0:nch, :]),
                                          reads=[r_wbr[wb]], writes=[r_wbr[wb]])
                                    for ts in range(ST // 512):
                                        j = cnt["pA"] % 4
                                        cnt["pA"] += 1
                                        xk = (q + ts) % 2
                                        o = ts * 512
                                        for k in range(8):
                                            fw.op("tensor", lambda e, k=k, j=j, o=o, wg=wg: e.matmul(
                                                pA[j][:], lhsT=wg[:, k, :], rhs=hT[:, k, o:o + 512], start=(k == 0), stop=(k == 7)),
                                                reads=[r_wg, r_hT], writes=[r_pA[j]], pe_acc=True)
                                        for c in range(nch):
                                            fw.op("tensor", lambda e, c=c, xk=xk, o=o, wb=wb, ch0=ch0, nch=nch: e.matmul(
                                                pX[xk][:], lhsT=wbr[wb][:, c, :], rhs=brT[:, ch0 + c, o:o + 512],
                                                start=(c == 0), stop=(c == nch - 1)),
                                                reads=[r_wbr[wb], r_br], writes=[r_pX[xk]], pe_acc=True)
                                        fw.op("scalar", lambda e, j=j, xk=xk: e.activation(out=sgt[xk][:], in_=pA[j][:], func=AF.Sigmoid),
                                              reads=[r_pA[j]], writes=[r_sgt[xk]])
                                        if br == 0:
                                            fw.op("vector", lambda e, xk=xk, o=o: e.tensor_tensor(
                                                out=mac[:, o:o + 512], in0=sgt[xk][:], in1=pX[xk][:], op=ALU.mult),
                                                reads=[r_sgt[xk], r_pX[xk]], writes=[r_mac])
                                        else:
                                            fw.op("vector", lambda e, xk=xk: e.tensor_tensor(
                                                out=sgt[xk][:], in0=sgt[xk][:], in1=pX[xk][:], op=ALU.mult),
                                                reads=[r_sgt[xk], r_pX[xk]], writes=[r_sgt[xk]])
                                            fw.op("vector", lambda e, xk=xk, o=o: e.tensor_tensor(
                                                out=mac[:, o:o + 512], in0=mac[:, o:o + 512], in1=sgt[xk][:], op=ALU.add),
                                                reads=[r_sgt[xk], r_mac], writes=[r_mac])
                                mi = jo % 2
                                fw.op("scalar", lambda e, mi=mi: e.activation(out=mbf[mi][:], in_=mac[:], func=AF.Copy),
                                      reads=[r_mac], writes=[r_mbf[mi]])
                                fw.dma("sync", lambda e, jo=jo, t0=t0, mi=mi: e.dma_start(out=MTs[jo, :, t0:t0 + ST], in_=mbf[mi][:]),
                                       reads=[r_mbf[mi]])
                    fw.barrier()
                    if stop_after in ("attn", "branches"):
                        continue
                    with ExitStack() as S3:
                        def sbu(name, shape, dt=F32):
                            return S3.enter_context(nc.sbuf_tensor(uname("b4_" + name), shape, dt))
                        fw.dma("sync", lambda e, t0=t0: e.dma_start(
                            out=hT[:], in_=MTs[:, :, t0:t0 + ST].rearrange("k p n -> p k n")), writes=[r_hT])
                        wo32 = [sbu("wo32_%d" % i, [128, 8, 256]) for i in range(2)]
                        r_wo32 = [Res("wo32_%d" % i) for i in range(2)]
                        wo = sbu("wo", [128, 8, D], BF16)
                        r_wo = Res("wo")
                        for pc in range(4):
                            a = pc % 2
                            fw.dma("sync", lambda e, pc=pc, a=a: e.dma_start(
                                out=wo32[a][:], in_=W["w_o"][l][:, pc * 256:(pc + 1) * 256].rearrange("(k p) n -> p k n", p=128)),
                                writes=[r_wo32[a]])
                            fw.op("gpsimd", lambda e, pc=pc, a=a: e.tensor_copy(out=wo[:, :, pc * 256:(pc + 1) * 256], in_=wo32[a][:]),
                                  reads=[r_wo32[a]], writes=[r_wo])
                        load_gb("ln1_g", "ln1_b", l)
                        xb = [sbu("xb%d" % i, [128, D]) for i in range(2)]
                        r_xb = [Res("xb%d" % i) for i in range(2)]
                        tb = [sbu("tb%d" % i, [128, D]) for i in range(2)]
                        r_tb = [Res("tb%d" % i) for i in range(2)]
                        hb16 = [sbu("hb16_%d" % i, [128, D], BF16) for i in range(2)]
                        r_hb16 = [Res("hb16_%d" % i) for i in range(2)]
                        st4 = [sbu("st4_%d" % i, [128, 8]) for i in range(2)]
                        r_st4 = [Res("st4_%d" % i) for i in range(2)]
                        mo = [sbu("mo%d" % i, [128, D]) for i in range(2)]
                        r_mo = [Res("mo%d" % i) for i in range(2)]
                        tkb = [sbu("tkb%d" % i, [128, 8, 128], BF16) for i in range(2)]
                        r_tkb = [Res("tkb%d" % i) for i in range(2)]
                        for blk in range(ST // 128):
                            i = blk % 2
                            r0 = t0 + blk * 128
                            fw.dma("sync", lambda e, i=i, r0=r0: e.dma_start(out=xb[i][:], in_=H0[r0:r0 + 128, :]), writes=[r_xb[i]])
                            for nh in range(2):
                                j = cnt["pA"] % 4
                                cnt["pA"] += 1
                                for k in range(8):
                                    fw.op("tensor", lambda e, k=k, j=j, nh=nh, blk=blk: e.matmul(
                                        pA[j][:], lhsT=hT[:, k, blk * 128:(blk + 1) * 128], rhs=wo[:, k, nh * 512:(nh + 1) * 512],
                                        start=(k == 0), stop=(k == 7)), reads=[r_hT, r_wo], writes=[r_pA[j]], pe_acc=True)
                                fw.op("scalar", lambda e, i=i, j=j, nh=nh: e.activation(
                                    out=mo[i][:, nh * 512:(nh + 1) * 512], in_=pA[j][:], func=AF.Copy),
                                    reads=[r_pA[j]], writes=[r_mo[i]])
                            if debug:
                                fw.dma("sync", lambda e, i=i, r0=r0: e.dma_start(out=dbg["mix"][r0:r0 + 128, :], in_=mo[i][:]),
                                       reads=[r_mo[i]])
                            fw.op("vector", lambda e, i=i: e.scalar_tensor_tensor(
                                out=xb[i][:], in0=xb[i][:], scalar=ALPHA, in1=mo[i][:], op0=ALU.mult, op1=ALU.add),
                                reads=[r_xb[i], r_mo[i]], writes=[r_xb[i]])
                            fw.dma("sync", lambda e, i=i, r0=r0: e.dma_start(out=H1[r0:r0 + 128, :], in_=xb[i][:]), reads=[r_xb[i]])
                        fw.barrier()
                        for blk in range(ST // 128):
                            i = blk % 2
                            r0 = t0 + blk * 128
                            fw.dma("sync", lambda e, i=i, r0=r0: e.dma_start(out=xb[i][:], in_=H1[r0:r0 + 128, :]), writes=[r_xb[i]])
                            ln_rows(fw, xb[i], r_xb[i], tb[i], r_tb[i], st4[i], r_st4[i], gam, bet, r_gb, xb[i], r_xb[i])
                            if debug:
                                fw.dma("sync", lambda e, i=i, r0=r0: e.dma_start(out=dbg["st"][r0:r0 + 128, :], in_=st4[i][:]),
                                       reads=[r_st4[i]])
                            fw.dma("sync", lambda e, i=i, r0=r0: e.dma_start(out=H1[r0:r0 + 128, :], in_=xb[i][:]), reads=[r_xb[i]])
                            fw.op("scalar", lambda e, i=i: e.activation(out=hb16[i][:], in_=xb[i][:], func=AF.Copy),
                                  reads=[r_xb[i]], writes=[r_hb16[i]])
                            for k in range(8):
                                fw.op("tensor", lambda e, k=k, i=i: e.transpose(pT[i][:, k, :], hb16[i][:, k * 128:(k + 1) * 128], ident[:]),
                                      reads=[r_hb16[i], r_ident], writes=[r_pT[i]], pe_acc=True)
                            fw.op("vector", lambda e, i=i: e.tensor_copy(out=tkb[i][:], in_=pT[i][:]), reads=[r_pT[i]], writes=[r_tkb[i]])
                            fw.dma("sync", lambda e, i=i, r0=r0: e.dma_start(
                                out=HT[:, :, r0:r0 + 128].rearrange("k p n -> p k n"), in_=tkb[i][:]), reads=[r_tkb[i]])
                    fw.barrier()
                    if stop_after == "ln1":
                        continue
                    with ExitStack() as S4:
                        def sbv(name, shape, dt=F32):
                            return S4.enter_context(nc.sbuf_tensor(uname("b5_" + name), shape, dt))
                        fw.dma("sync", lambda e, t0=t0: e.dma_start(
                            out=hT[:], in_=HT[:, :, t0:t0 + ST].rearrange("k p n -> p k n")), writes=[r_hT])
                        wr32 = sbv("wr32", [128, 8, 20])
                        wr = sbv("wr", [128, 8, 20], BF16)
                        r_wr = Res("wr")
                        fw.dma("sync", lambda e: e.dma_start(out=wr32[:, :, 0:4], in_=W["router_group_w"][l].rearrange("(k p) n -> p k n", p=128),
                                                             allow_slow_non_contiguous=True), writes=[r_wr])
                        fw.dma("sync", lambda e: e.dma_start(out=wr32[:, :, 4:20], in_=W["router_expert_w"][l].rearrange("(k p) n -> p k n", p=128),
                                                             allow_slow_non_contiguous=True), writes=[r_wr])
                        fw.op("vector", lambda e: e.tensor_copy(out=wr[:], in_=wr32[:]), reads=[r_wr], writes=[r_wr])
                        rb = sbv("rb", [128, 20])
                        r_rb = Res("rb")
                        fw.dma("sync", lambda e: e.dma_start(out=rb[:, 0:4], in_=W["router_group_b"][l].partition_broadcast(128)), writes=[r_rb])
                        fw.dma("sync", lambda e: e.dma_start(out=rb[:, 4:20], in_=W["router_expert_b"][l].partition_broadcast(128)), writes=[r_rb])
                        comb = sbv("comb", [128, ST // 128, 16])
                        r_comb = Res("comb")
                        rt = sbv("rt", [128, 64])
                        r_rt = Res("rt")
                        for blk in range(ST // 128):
                            xk = blk % 2
                            for k in range(8):
                                fw.op("tensor", lambda e, k=k, xk=xk, blk=blk: e.matmul(
                                    pX[xk][:, 0:20], lhsT=hT[:, k, blk * 128:(blk + 1) * 128], rhs=wr[:, k, :],
                                    start=(k == 0), stop=(k == 7)), reads=[r_hT, r_wr], writes=[r_pX[xk]], pe_acc=True)
                            lg = rt[:, 0:20]

                            def V(fn, extra_r=()):
                                fw.op("vector", fn, reads=[r_rt] + list(extra_r), writes=[r_rt])
                            fw.op("vector", lambda e, xk=xk: e.tensor_tensor(out=rt[:, 0:20], in0=pX[xk][:, 0:20], in1=rb[:], op=ALU.add),
                                  reads=[r_pX[xk], r_rb], writes=[r_rt])
                            V(lambda e: e.reduce_max(out=rt[:, 20:21], in_=rt[:, 0:4], axis=AX.X))
                            V(lambda e: e.tensor_scalar(out=rt[:, 24:28], in0=rt[:, 0:4], scalar1=rt[:, 20:21], scalar2=None, op0=ALU.subtract))
                            fw.op("scalar", lambda e: e.activation(out=rt[:, 28:32], in_=rt[:, 24:28], func=AF.Exp), reads=[r_rt], writes=[r_rt])
                            V(lambda e: e.reduce_sum(out=rt[:, 21:22], in_=rt[:, 28:32], axis=AX.X))
                            V(lambda e: e.reciprocal(out=rt[:, 21:22], in_=rt[:, 21:22]))
                            V(lambda e: e.tensor_scalar(out=rt[:, 24:28], in0=rt[:, 24:28], scalar1=-1e30, scalar2=1.0, op0=ALU.mult, op1=ALU.min))
                            V(lambda e: e.tensor_scalar(out=rt[:, 24:28], in0=rt[:, 24:28], scalar1=-1.0, scalar2=1.0, op0=ALU.mult, op1=ALU.add))
                            V(lambda e: e.tensor_scalar(out=rt[:, 32:36], in0=rt[:, 4:8], scalar1=rt[:, 24:25], scalar2=None, op0=ALU.mult))
                            for gq in range(1, 4):
                                V(lambda e, gq=gq: e.scalar_tensor_tensor(out=rt[:, 32:36], in0=rt[:, 4 + 4 * gq:8 + 4 * gq],
                                                                          scalar=rt[:, 24 + gq:25 + gq], in1=rt[:, 32:36],
                                                                          op0=ALU.mult, op1=ALU.add))
                            V(lambda e: e.reduce_max(out=rt[:, 22:23], in_=rt[:, 32:36], axis=AX.X))
                            V(lambda e: e.tensor_scalar(out=rt[:, 36:40], in0=rt[:, 32:36], scalar1=rt[:, 22:23], scalar2=None, op0=ALU.subtract))
                            V(lambda e: e.tensor_scalar(out=rt[:, 36:40], in0=rt[:, 36:40], scalar1=-1e30, scalar2=1.0, op0=ALU.mult, op1=ALU.min))
                            V(lambda e: e.tensor_scalar(out=rt[:, 36:40], in0=rt[:, 36:40], scalar1=-1.0, scalar2=1.0, op0=ALU.mult, op1=ALU.add))
                            V(lambda e: e.scalar_tensor_tensor(out=rt[:, 40:44], in0=rt[:, 36:40], scalar=-1e4, in1=rt[:, 32:36],
                                                               op0=ALU.mult, op1=ALU.add))
                            V(lambda e: e.reduce_max(out=rt[:, 23:24], in_=rt[:, 40:44], axis=AX.X))
                            V(lambda e: e.tensor_scalar(out=rt[:, 44:48], in0=rt[:, 40:44], scalar1=rt[:, 23:24], scalar2=None, op0=ALU.subtract))
                            V(lambda e: e.tensor_scalar(out=rt[:, 44:48], in0=rt[:, 44:48], scalar1=-1e30, scalar2=1.0, op0=ALU.mult, op1=ALU.min))
                            V(lambda e: e.tensor_scalar(out=rt[:, 44:48], in0=rt[:, 44:48], scalar1=-1.0, scalar2=1.0, op0=ALU.mult, op1=ALU.add))
                            V(lambda e: e.tensor_tensor(out=rt[:, 48:49], in0=rt[:, 23:24], in1=rt[:, 22:23], op=ALU.subtract))
                            fw.op("scalar", lambda e: e.activation(out=rt[:, 49:50], in_=rt[:, 48:49], func=AF.Exp), reads=[r_rt], writes=[r_rt])
                            V(lambda e: e.tensor_scalar(out=rt[:, 50:51], in0=rt[:, 49:50], scalar1=1.0, scalar2=None, op0=ALU.add))
                            V(lambda e: e.reciprocal(out=rt[:, 50:51], in_=rt[:, 50:51]))
                            V(lambda e: e.tensor_tensor(out=rt[:, 51:52], in0=rt[:, 49:50], in1=rt[:, 50:51], op=ALU.mult))
                            V(lambda e: e.tensor_tensor(out=rt[:, 50:51], in0=rt[:, 50:51], in1=rt[:, 21:22], op=ALU.mult))
                            V(lambda e: e.tensor_tensor(out=rt[:, 51:52], in0=rt[:, 51:52], in1=rt[:, 21:22], op=ALU.mult))
                            V(lambda e: e.tensor_scalar(out=rt[:, 52:56], in0=rt[:, 36:40], scalar1=rt[:, 50:51], scalar2=None, op0=ALU.mult))
                            V(lambda e: e.scalar_tensor_tensor(out=rt[:, 52:56], in0=rt[:, 44:48], scalar=rt[:, 51:52], in1=rt[:, 52:56],
                                                               op0=ALU.mult, op1=ALU.add))
                            for gq in range(4):
                                fw.op("vector", lambda e, gq=gq, blk=blk: e.tensor_scalar(
                                    out=comb[:, blk, 4 * gq:4 * gq + 4], in0=rt[:, 52:56], scalar1=rt[:, 24 + gq:25 + gq], scalar2=None,
                                    op0=ALU.mult), reads=[r_rt], writes=[r_comb])
                        wg32 = sbv("wg32", [128, 8, 256])
                        wu32 = sbv("wu32", [128, 8, 256])
                        wgb = sbv("wgb", [128, 8, 256], BF16)
                        wub = sbv("wub", [128, 8, 256], BF16)
                        wd32 = sbv("wd32", [128, 2, D])
                        wdb = sbv("wdb", [128, 2, D], BF16)
                        r_wg32, r_wu32, r_wd32 = Res("wg32"), Res("wu32"), Res("wd32")
                        r_wgb, r_wub, r_wdb = Res("wgb"), Res("wub"), Res("wdb")
                        HB = ST // 256
                        macc = sbv("macc", [128, HB, D])
                        r_macc = Res("macc")
                        actT = sbv("actT", [128, 2, ST // 2], BF16)
                        r_act = Res("actT")
                        sgl = [sbv("sgl%d" % i, [128, 512]) for i in range(2)]
                        r_sgl = [Res("sgl%d" % i) for i in range(2)]
                        xq = [sbv("xq%d" % i, [128, D]) for i in range(2)]
                        r_xq = [Res("xq%d" % i) for i in range(2)]
                        tq = [sbv("tq%d" % i, [128, D]) for i in range(2)]
                        r_tq = [Res("tq%d" % i) for i in range(2)]
                        sq4 = [sbv("sq4_%d" % i, [128, 8]) for i in range(2)]
                        r_sq4 = [Res("sq4_%d" % i) for i in range(2)]
                        load_gb("ln2_g", "ln2_b", l)
                        for hv in range(2):
                            c0 = hv * (ST // 2)
                            for ex in range(16):
                                fw.dma("sync", lambda e, ex=ex: e.dma_start(
                                    out=wg32[:], in_=W["expert_w_gate"][l, ex].rearrange("(k p) n -> p k n", p=128)), writes=[r_wg32])
                                fw.op("gpsimd", lambda e: e.tensor_copy(out=wgb[:], in_=wg32[:]), reads=[r_wg32], writes=[r_wgb])
                                fw.dma("sync", lambda e, ex=ex: e.dma_start(
                                    out=wu32[:], in_=W["expert_w_up"][l, ex].rearrange("(k p) n -> p k n", p=128)), writes=[r_wu32])
                                fw.op("gpsimd", lambda e: e.tensor_copy(out=wub[:], in_=wu32[:]), reads=[r_wu32], writes=[r_wub])
                                fw.dma("sync", lambda e, ex=ex: e.dma_start(
                                    out=wd32[:], in_=W["expert_w_down"][l, ex].rearrange("(c p) n -> p c n", p=128)), writes=[r_wd32])
                                fw.op("gpsimd", lambda e: e.tensor_copy(out=wdb[:], in_=wd32[:]), reads=[r_wd32], writes=[r_wdb])
                                for c in range(2):
                                    for ts in range(ST // 1024):
                                        o = c0 + ts * 512
                                        j = cnt["pA"] % 4
                                        cnt["pA"] += 1
                                        j2 = cnt["pA"] % 4
                                        cnt["pA"] += 1
                                        for k in range(8):
                                            fw.op("tensor", lambda e, k=k, j=j, c=c, o=o: e.matmul(
                                                pA[j][:], lhsT=wgb[:, k, c * 128:(c + 1) * 128], rhs=hT[:, k, o:o + 512],
                                                start=(k == 0), stop=(k == 7)), reads=[r_wgb, r_hT], writes=[r_pA[j]], pe_acc=True)
                                        for k in range(8):
                                            fw.op("tensor", lambda e, k=k, j2=j2, c=c, o=o: e.matmul(
                                                pA[j2][:], lhsT=wub[:, k, c * 128:(c + 1) * 128], rhs=hT[:, k, o:o + 512],
                                                start=(k == 0), stop=(k == 7)), reads=[r_wub, r_hT], writes=[r_pA[j2]], pe_acc=True)
                                        si = (c + ts) % 2
                                        fw.op("scalar", lambda e, j=j, si=si: e.activation(out=sgl[si][:], in_=pA[j][:], func=AF.Silu),
                                              reads=[r_pA[j]], writes=[r_sgl[si]])
                                        fw.op("vector", lambda e, j2=j2, si=si, c=c, ts=ts: e.tensor_tensor(
                                            out=actT[:, c, ts * 512:(ts + 1) * 512], in0=sgl[si][:], in1=pA[j2][:], op=ALU.mult),
                                            reads=[r_sgl[si], r_pA[j2]], writes=[r_act])
                                for bl in range(HB):
                                    blk = hv * HB + bl
                                    for nh in range(2):
                                        xk = (bl * 2 + nh) % 2
                                        for c in range(2):
                                            fw.op("tensor", lambda e, c=c, xk=xk, bl=bl, nh=nh: e.matmul(
                                                pX[xk][:], lhsT=actT[:, c, bl * 128:(bl + 1) * 128], rhs=wdb[:, c, nh * 512:(nh + 1) * 512],
                                                start=(c == 0), stop=(c == 1)), reads=[r_act, r_wdb], writes=[r_pX[xk]], pe_acc=True)
                                        if ex == 0:
                                            fw.op("vector", lambda e, xk=xk, bl=bl, nh=nh, blk=blk, ex=ex: e.tensor_scalar(
                                                out=macc[:, bl, nh * 512:(nh + 1) * 512], in0=pX[xk][:], scalar1=comb[:, blk, ex:ex + 1],
                                                scalar2=None, op0=ALU.mult), reads=[r_pX[xk], r_comb], writes=[r_macc])
                                        else:
                                            fw.op("vector", lambda e, xk=xk, bl=bl, nh=nh, blk=blk, ex=ex: e.scalar_tensor_tensor(
                                                out=macc[:, bl, nh * 512:(nh + 1) * 512], in0=pX[xk][:], scalar=comb[:, blk, ex:ex + 1],
                                                in1=macc[:, bl, nh * 512:(nh + 1) * 512], op0=ALU.mult, op1=ALU.add),
                                                reads=[r_pX[xk], r_comb, r_macc], writes=[r_macc])
                            dst = y if last else H0
                            for bl in range(HB):
                                i = bl % 2
                                r0 = t0 + (hv * HB + bl) * 128
                                fw.dma("sync", lambda e, i=i, r0=r0: e.dma_start(out=xq[i][:], in_=H1[r0:r0 + 128, :]), writes=[r_xq[i]])
                                fw.op("vector", lambda e, i=i, bl=bl: e.scalar_tensor_tensor(
                                    out=xq[i][:], in0=xq[i][:], scalar=ALPHA, in1=macc[:, bl, :], op0=ALU.mult, op1=ALU.add),
                                    reads=[r_xq[i], r_macc], writes=[r_xq[i]])
                                ln_rows(fw, xq[i], r_xq[i], tq[i], r_tq[i], sq4[i], r_sq4[i], gam, bet, r_gb, xq[i], r_xq[i])
                                fw.dma("sync", lambda e, i=i, r0=r0, dst=dst: e.dma_start(out=dst[r0:r0 + 128, :], in_=xq[i][:]),
                                       reads=[r_xq[i]])
                    fw.barrier()
            fw.barrier()

        if debug:
            dbg["br"] = nc.dram_tensor("dbg_br", [10, 128, NTOK], BF16, kind="ExternalOutput").ap()
        for l in range(depth):
            phase_a(l)
            if stop_after in ("a", "ua"):
                break
            phase_s5(l)
            if stop_after == "s5":
                break
            phase_h()
            phase_b(l, l == depth - 1)
            if stop_after in ("attn", "branches", "ln1"):
                break
    return nc, fw


def ln_rows(fw, xin, r_xin, tmp, r_tmp, s, r_s, gam, bet, r_gb, out_tile, r_out):
    fw.op("vector", lambda e: e.reduce_sum(out=s[:, 0:1], in_=xin[:], axis=AX.X), reads=[r_xin], writes=[r_s])
    fw.op("scalar", lambda e: e.activation(out=tmp[:], in_=xin[:], func=AF.Square), reads=[r_xin, r_s], writes=[r_tmp])
    fw.op("vector", lambda e: e.reduce_sum(out=s[:, 1:2], in_=tmp[:], axis=AX.X), reads=[r_tmp], writes=[r_s])
    fw.op("vector", lambda e: e.tensor_scalar(out=s[:, 2:3], in0=s[:, 0:1], scalar1=1.0 / D, scalar2=None,
                                              op0=ALU.mult), reads=[r_s], writes=[r_s])
    fw.op("vector", lambda e: e.tensor_tensor(out=s[:, 3:4], in0=s[:, 2:3], in1=s[:, 2:3], op=ALU.mult),
          reads=[r_s], writes=[r_s])
    fw.op("vector", lambda e: e.scalar_tensor_tensor(out=s[:, 4:5], in0=s[:, 1:2], scalar=1.0 / D, in1=s[:, 3:4],
                                                     op0=ALU.mult, op1=ALU.subtract), reads=[r_s], writes=[r_s])
    fw.op("vector", lambda e: e.tensor_scalar(out=s[:, 4:5], in0=s[:, 4:5], scalar1=LN_EPS, scalar2=None,
                                              op0=ALU.add), reads=[r_s], writes=[r_s])
    fw.op("scalar", lambda e: e.activation(out=s[:, 5:6], in_=s[:, 4:5], func=AF.Sqrt), reads=[r_s], writes=[r_s])
    fw.op("vector", lambda e: e.reciprocal(out=s[:, 6:7], in_=s[:, 5:6]), reads=[r_s], writes=[r_s])
    fw.op("vector", lambda e: e.scalar_tensor_tensor(out=s[:, 7:8], in0=s[:, 2:3], scalar=-1.0, in1=s[:, 6:7],
                                                     op0=ALU.mult, op1=ALU.mult), reads=[r_s], writes=[r_s])
    fw.op("vector", lambda e: e.tensor_scalar(out=tmp[:], in0=xin[:], scalar1=s[:, 2:3], scalar2=s[:, 6:7],
                                              op0=ALU.subtract, op1=ALU.mult), reads=[r_xin, r_s], writes=[r_tmp])
    fw.op("vector", lambda e: e.tensor_tensor(out=tmp[:], in0=tmp[:], in1=gam[:], op=ALU.mult),
          reads=[r_tmp, r_gb], writes=[r_tmp])
    fw.op("vector", lambda e: e.tensor_tensor(out=out_tile[:], in0=tmp[:], in1=bet[:], op=ALU.add),
          reads=[r_tmp, r_gb], writes=[r_out])


_CACHE = {}


def kernel(**inputs):
    xp = np.ascontiguousarray(inputs["x_prompt"], dtype=np.float32)
    xs = np.ascontiguousarray(inputs["x_sample"], dtype=np.float32)
    slots = {0: [("s", 0)], 1: [("s", 1)], 2: [("p", 0), ("p", 1)], 3: [("p", 2), ("p", 3)],
             4: [("p", 4)], 5: [("p", 5)], 6: [("p", 6)], 7: [("p", 7)]}
    in_maps = []
    for c in range(8):
        xc = np.zeros((NTOK, D), np.float32)
        fl = np.zeros(5, np.float32)
        if slots[c][0][0] == "s":
            xc[:] = xs[slots[c][0][1]]
            fl[1:4] = 1.0
        else:
            for j, (_, pi) in enumerate(slots[c]):
                xc[j * SEG:(j + 1) * SEG] = xp[pi]
        m = {"x": xc, "flags": fl}
        for n in WNAMES:
            m[n] = np.ascontiguousarray(inputs[n], dtype=np.float32)
        in_maps.append(m)
    if "nc" not in _CACHE:
        nc, fw = build()
        fw.finish(_CACHE.get("final", []))
        _CACHE["nc"] = nc
    res = run_bass_kernel_spmd(_CACHE["nc"], in_maps, core_ids=list(range(8)))
    yp = np.zeros_like(xp)
    ys = np.zeros_like(xs)
    for c in range(8):
        yc = np.asarray(res.results[c]["y"], dtype=np.float32)
        if slots[c][0][0] == "s":
            ys[slots[c][0][1]] = yc
        else:
            for j, (_, pi) in enumerate(slots[c]):
                yp[pi] = yc[j * SEG:(j + 1) * SEG]
    return (yp, ys)
```

```python
import math
import numpy as np
from contextlib import ExitStack
import concourse.bass as bass
import concourse.mybir as mybir
from concourse.bass_utils import run_bass_kernel_spmd

F32 = mybir.dt.float32
BF16 = mybir.dt.bfloat16
AF = mybir.ActivationFunctionType
ALU = mybir.AluOpType
AX = mybir.AxisListType

D = 1024
NSEG = 4
SEG = 4096
NTOK = NSEG * SEG
PAD = 1024
SEGP = SEG + 2 * PAD
NTOKP = NSEG * SEGP
ST = 2048
NST = NTOK // ST
DEPTH = 2
ALPHA = (2 * DEPTH) ** 0.25
LN_EPS = 1e-5
IN_COLS = 7424
SLOPES = [2.0 ** (-8.0 * (i + 1) / 12) for i in range(12)]
DIL = [(128, 1), (512, 4), (2048, 16)]

WNAMES = ["ln_in_g", "ln_in_b", "w_in", "s5_a_re", "s5_a_im", "s5_log_dt", "s5_b_re", "s5_b_im", "s5_c_re",
          "s5_c_im", "s5_d", "s5_glu_w", "s5_glu_b", "conv_w", "conv_b", "swa_sink", "w_branch_a", "w_branch_b",
          "w_branch_c", "w_branch_d", "w_o", "ln1_g", "ln1_b", "router_group_w", "router_group_b",
          "router_expert_w", "router_expert_b", "expert_w_gate", "expert_w_up", "expert_w_down", "ln2_g", "ln2_b"]
WSHAPES = {
    "ln_in_g": [D], "ln_in_b": [D], "w_in": [2, D, IN_COLS], "s5_a_re": [2, 2, 24, 64], "s5_a_im": [2, 2, 24, 64],
    "s5_log_dt": [2, 2, 24], "s5_b_re": [2, 2, 24, 64, 16], "s5_b_im": [2, 2, 24, 64, 16],
    "s5_c_re": [2, 2, 24, 16, 64], "s5_c_im": [2, 2, 24, 16, 64], "s5_d": [2, 384], "s5_glu_w": [2, 384, 384],
    "s5_glu_b": [2, 384], "conv_w": [2, 3, 384], "conv_b": [2, 384], "swa_sink": [2, 6],
    "w_branch_a": [2, 384, D], "w_branch_b": [2, 384, D], "w_branch_c": [2, 128, D], "w_branch_d": [2, 384, D],
    "w_o": [2, D, D], "ln1_g": [2, D], "ln1_b": [2, D], "router_group_w": [2, D, 4], "router_group_b": [2, 4],
    "router_expert_w": [2, D, 16], "router_expert_b": [2, 16], "expert_w_gate": [2, 16, D, 256],
    "expert_w_up": [2, 16, D, 256], "expert_w_down": [2, 16, 256, D], "ln2_g": [2, D], "ln2_b": [2, D],
}


ENGS = ["sync", "scalar", "vector", "gpsimd", "tensor"]
SEM_ROLL = 30000


class Res:
    __slots__ = ("name", "w", "r")

    def __init__(self, name):
        self.name = name
        self.w = None
        self.r = []


class FW:
    def __init__(self, nc, es):
        self.nc = nc
        self.es = es
        self.q = {e: [] for e in ENGS}
        self.sems = {e: [es.enter_context(nc.semaphore("s_" + e + "0"))] for e in ENGS}
        self.cnt = {e: 0 for e in ENGS}
        self.seen = {e: {} for e in ENGS}
        self.dma_sems = [es.enter_context(nc.semaphore("d%d" % i)) for i in range(24)]
        self.dma_cnt = [0] * 24
        self.dma_i = 0
        self.n_ops = 0
        self.fence = []

    def barrier(self):
        f = []
        for e in ENGS:
            if self.cnt[e] > 0:
                f.append((self.sems[e][-1], self.cnt[e], e))
        for k in range(len(self.dma_sems)):
            if self.dma_cnt[k] > 0:
                f.append((self.dma_sems[k], self.dma_cnt[k], "dma"))
        self.fence = f

    def _ev_new(self, eng):
        if self.cnt[eng] >= SEM_ROLL:
            self.sems[eng].append(self.es.enter_context(self.nc.semaphore("s_%s%d" % (eng, len(self.sems[eng])))))
            self.cnt[eng] = 0
        self.cnt[eng] += 1
        return (self.sems[eng][-1], self.cnt[eng], eng)

    def _need(self, eng, ev, waits, pe_ok=False):
        if ev is None:
            return
        sem, val, src = ev
        if pe_ok and src == "tensor" and eng == "tensor":
            return
        key = id(sem)
        if self.seen[eng].get(key, 0) >= val:
            return
        if key not in waits or waits[key][1] < val:
            waits[key] = (sem, val)

    def op(self, eng, fn, reads=(), writes=(), pe_acc=False):
        waits = {}
        for ev in self.fence:
            self._need(eng, ev, waits)
        for r in reads:
            self._need(eng, r.w, waits)
        for w in writes:
            self._need(eng, w.w, waits, pe_ok=pe_acc)
            for ev in w.r:
                self._need(eng, ev, waits)
        for key, (sem, val) in waits.items():
            self.seen[eng][key] = val
        ev = self._ev_new(eng)
        self.q[eng].append((list(waits.values()), fn, (ev[0], 1)))
        for r in reads:
            r.r.append(ev)
        for w in writes:
            w.w = ev
            w.r = []
        self.n_ops += 1
        return ev

    def dma(self, eng, fn, reads=(), writes=()):
        if len(writes) == 0 and eng == "sync":
            eng = "scalar"
        waits = {}
        for ev in self.fence:
            self._need(eng, ev, waits)
        for r in reads:
            self._need(eng, r.w, waits)
        for w in writes:
            self._need(eng, w.w, waits)
            for ev in w.r:
                self._need(eng, ev, waits)
        k = self.dma_i % len(self.dma_sems)
        self.dma_i += 1
        sem = self.dma_sems[k]
        if self.dma_cnt[k] > 0:
            self._need(eng, (sem, self.dma_cnt[k], "dma"), waits)
        for key, (s, val) in waits.items():
            self.seen[eng][key] = val
        self.dma_cnt[k] += 16
        ev = (sem, self.dma_cnt[k], "dma")
        self.q[eng].append((list(waits.values()), fn, (sem, 16)))
        for r in reads:
            r.r.append(ev)
        for w in writes:
            w.w = ev
            w.r = []
        self.n_ops += 1
        return ev

    def finish(self, final_res):
        waits = {}
        for r in final_res:
            self._need("sync", r.w, waits)
        for k in range(len(self.dma_sems)):
            if self.dma_cnt[k] > 0:
                self._need("sync", (self.dma_sems[k], self.dma_cnt[k], "dma"), waits)
        tail = list(waits.values())
        q = self.q
        with self.nc.Block() as block:
            def replay(e, name):
                for ws, fn, inc in q[name]:
                    for sem, val in ws:
                        e.wait_ge(sem, val)
                    fn(e).then_inc(inc[0], inc[1])
                if name == "sync":
                    for sem, val in tail:
                        e.wait_ge(sem, val)

            @block.sync
            def _(e):
                replay(e, "sync")

            @block.scalar
            def _(e):
                replay(e, "scalar")

            @block.vector
            def _(e):
                replay(e, "vector")

            @block.gpsimd
            def _(e):
                replay(e, "gpsimd")

            @block.tensor
            def _(e):
                replay(e, "tensor")


def build(debug=False, stop_after=None, depth=DEPTH):
    nc = bass.Bass("TRN2", target_bir_lowering=False)
    x = nc.dram_tensor("x", [NTOK, D], F32, kind="ExternalInput").ap()
    flags_d = nc.dram_tensor("flags", [5], F32, kind="ExternalInput").ap()
    W = {n: nc.dram_tensor(n, WSHAPES[n], F32, kind="ExternalInput").ap() for n in WNAMES}
    y = nc.dram_tensor("y", [NTOK, D], F32, kind="ExternalOutput").ap()
    H0 = nc.dram_tensor("H0", [NTOK, D], F32).ap()
    H1 = nc.dram_tensor("H1", [NTOK, D], F32, kind=("ExternalOutput" if debug else "Internal")).ap()
    HT = nc.dram_tensor("HT", [8, 128, NTOK], BF16).ap()
    UAs = nc.dram_tensor("UAs", [3, 128, NTOK], F32).ap()
    CVs = nc.dram_tensor("CVs", [3, 128, NTOKP], BF16).ap()
    KTs = nc.dram_tensor("KTs", [4, 128, NTOKP], BF16).ap()
    VTs = nc.dram_tensor("VTs", [4, 128, NTOKP], BF16).ap()
    MTs = nc.dram_tensor("MTs", [8, 128, NTOK], BF16, kind=("ExternalOutput" if debug else "Internal")).ap()
    YA = nc.dram_tensor("YA", [2, 3, 128, NTOK], F32, kind=("ExternalOutput" if debug else "Internal")).ap()
    dbg = {}
    if debug:
        dbg["h0"] = nc.dram_tensor("dbg_h0", [NTOK, D], F32, kind="ExternalOutput").ap()
        dbg["ua"] = nc.dram_tensor("dbg_ua", [3, 128, NTOK], F32, kind="ExternalOutput").ap()
        dbg["mix"] = nc.dram_tensor("dbg_mix", [NTOK, D], F32, kind="ExternalOutput").ap()
        dbg["st"] = nc.dram_tensor("dbg_st", [NTOK, 8], F32, kind="ExternalOutput").ap()

    es = ExitStack()
    with es:
        fw = FW(nc, es)

        def sb(name, shape, dt=F32):
            return es.enter_context(nc.sbuf_tensor(name, shape, dt))

        def ps(name, shape, dt=F32):
            return es.enter_context(nc.psum_tensor(name, shape, dt))

        ident = sb("ident", [128, 128], BF16)
        r_ident = Res("ident")
        fw.op("gpsimd", lambda e: e.memset(ident[:], 0.0), writes=[r_ident])
        fw.op("gpsimd", lambda e: e.affine_select(out=ident[:], in_=ident[:], pattern=[[-1, 128]],
                                                  compare_op=ALU.not_equal, fill=1.0, base=0, channel_multiplier=1),
              reads=[r_ident], writes=[r_ident])
        flg = sb("flg", [128, 5])
        r_flg = Res("flg")
        fw.dma("sync", lambda e: e.dma_start(out=flg[:], in_=flags_d.partition_broadcast(128)), writes=[r_flg])
        gam = sb("gam", [128, D])
        bet = sb("bet", [128, D])
        r_gb = Res("gb")

        def load_gb(gname, bname, l):
            gsrc = W[gname] if l is None else W[gname][l]
            bsrc = W[bname] if l is None else W[bname][l]
            fw.dma("sync", lambda e: e.dma_start(out=gam[:], in_=gsrc.partition_broadcast(128)), writes=[r_gb])
            fw.dma("sync", lambda e: e.dma_start(out=bet[:], in_=bsrc.partition_broadcast(128)), writes=[r_gb])

        pT = [ps("pT%d" % i, [128, 8, 128], BF16) for i in range(2)]
        r_pT = [Res("pT%d" % i) for i in range(2)]
        pA = [ps("pA%d" % i, [128, 512]) for i in range(4)]
        r_pA = [Res("pA%d" % i) for i in range(4)]
        pX = [ps("pX%d" % i, [128, 512]) for i in range(2)]
        r_pX = [Res("pX%d" % i) for i in range(2)]
        cnt = {"w": 0, "pA": 0, "blk": 0, "uid": 0}

        def uname(n):
            cnt["uid"] += 1
            return "%s_%d" % (n, cnt["uid"])

        def padpos(t):
            return (t // SEG) * SEGP + PAD + (t % SEG)

        def phase_a(l):
            with ExitStack() as pes:
                def sb2(name, shape, dt=F32):
                    return pes.enter_context(nc.sbuf_tensor(uname("a_" + name), shape, dt))
                hT = sb2("hT", [128, 8, ST], BF16)
                r_hT = Res("hT")
                xb = [sb2("xb%d" % i, [128, D]) for i in range(2)]
                r_xb = [Res("xb%d" % i) for i in range(2)]
                tb = [sb2("tb%d" % i, [128, D]) for i in range(2)]
                r_tb = [Res("tb%d" % i) for i in range(2)]
                hb16 = [sb2("hb16_%d" % i, [128, D], BF16) for i in range(2)]
                r_hb16 = [Res("hb16_%d" % i) for i in range(2)]
                st4 = [sb2("st4_%d" % i, [128, 8]) for i in range(2)]
                r_st4 = [Res("st4_%d" % i) for i in range(2)]
                wst = [sb2("wst%d" % i, [128, 8, 128]) for i in range(2)]
                r_wst = [Res("wst%d" % i) for i in range(2)]
                wbf = [sb2("wbf%d" % i, [128, 8, 128], BF16) for i in range(2)]
                r_wbf = [Res("wbf%d" % i) for i in range(2)]
                zt0 = sb2("zt0", [128, ST], BF16)
                r_zt0 = Res("zt0")
                zf = [sb2("zf%d" % i, [128, 512]) for i in range(4)]
                r_zf = [Res("zf%d" % i) for i in range(4)]
                zb = [sb2("zb%d" % i, [128, 512], BF16) for i in range(4)]
                r_zb = [Res("zb%d" % i) for i in range(4)]

                def layer_norm_block(i):
                    ln_rows(fw, xb[i], r_xb[i], tb[i], r_tb[i], st4[i], r_st4[i], gam, bet, r_gb, xb[i], r_xb[i])

                def transpose_block(i, col0):
                    fw.op("scalar", lambda e: e.activation(out=hb16[i][:], in_=xb[i][:], func=AF.Copy),
                          reads=[r_xb[i]], writes=[r_hb16[i]])
                    for k in range(8):
                        fw.op("tensor", lambda e, k=k: e.transpose(pT[i][:, k, :], hb16[i][:, k * 128:(k + 1) * 128],
                                                                   ident[:]),
                              reads=[r_hb16[i], r_ident], writes=[r_pT[i]], pe_acc=True)
                    fw.op("vector", lambda e: e.tensor_copy(out=hT[:, :, col0:col0 + 128], in_=pT[i][:]),
                          reads=[r_pT[i]], writes=[r_hT])

                def load_w_chunk(src_ap):
                    j = cnt["w"] % 2
                    cnt["w"] += 1
                    fw.dma("sync", lambda e: e.dma_start(out=wst[j][:], in_=src_ap.rearrange("(k p) n -> p k n", p=128)),
                           writes=[r_wst[j]])
                    fw.op("gpsimd", lambda e: e.tensor_copy(out=wbf[j][:], in_=wst[j][:]),
                          reads=[r_wst[j]], writes=[r_wbf[j]])
                    return wbf[j], r_wbf[j]

                def proj_fm(wt, r_w, evac):
                    for ts in range(ST // 512):
                        j = cnt["pA"] % 4
                        cnt["pA"] += 1
                        for k in range(8):
                            fw.op("tensor", lambda e, k=k, j=j, ts=ts: e.matmul(
                                pA[j][:], lhsT=wt[:, k, :], rhs=hT[:, k, ts * 512:(ts + 1) * 512],
                                start=(k == 0), stop=(k == 7)),
                                reads=[r_w, r_hT], writes=[r_pA[j]], pe_acc=True)
                        evac(ts, j, pA[j], r_pA[j])

                if l == 0:
                    load_gb("ln_in_g", "ln_in_b", None)
                for st_i in range(NST):
                    t0 = st_i * ST
                    p0 = padpos(t0)
                    for b in range(ST // 128):
                        i = cnt["blk"] % 2
                        cnt["blk"] += 1
                        r0 = t0 + b * 128
                        if l == 0:
                            fw.dma("sync", lambda e, i=i, r0=r0: e.dma_start(out=xb[i][:], in_=x[r0:r0 + 128, :]),
                                   writes=[r_xb[i]])
                            layer_norm_block(i)
                            fw.dma("sync", lambda e, i=i, r0=r0: e.dma_start(out=H0[r0:r0 + 128, :], in_=xb[i][:]),
                                   reads=[r_xb[i]])
                            if debug:
                                fw.dma("sync", lambda e, i=i, r0=r0: e.dma_start(out=dbg["h0"][r0:r0 + 128, :],
                                                                                in_=xb[i][:]), reads=[r_xb[i]])
                        else:
                            fw.dma("sync", lambda e, i=i, r0=r0: e.dma_start(out=xb[i][:], in_=H0[r0:r0 + 128, :]),
                                   writes=[r_xb[i]])
                        transpose_block(i, b * 128)
                    fw.dma("sync", lambda e, t0=t0: e.dma_start(out=HT[:, :, t0:t0 + ST].rearrange("k p n -> p k n"),
                                                                in_=hT[:]), reads=[r_hT])

                    def store_plain(dst, c, t0=t0):
                        def ev(ts, j, pt, r_pt):
                            fw.op("scalar", lambda e: e.activation(out=zf[j][:], in_=pt[:], func=AF.Copy),
                                  reads=[r_pt], writes=[r_zf[j]])
                            fw.dma("sync", lambda e: e.dma_start(out=dst[c, :, t0 + ts * 512:t0 + (ts + 1) * 512],
                                                                 in_=zf[j][:]), reads=[r_zf[j]])
                            if debug and dst is UAs:
                                fw.dma("sync", lambda e: e.dma_start(
                                    out=dbg["ua"][c, :, t0 + ts * 512:t0 + (ts + 1) * 512], in_=zf[j][:]),
                                    reads=[r_zf[j]])
                        return ev

                    def store_pad(dst, c, p0=p0):
                        def ev(ts, j, pt, r_pt):
                            fw.op("scalar", lambda e: e.activation(out=zb[j][:], in_=pt[:], func=AF.Copy),
                                  reads=[r_pt], writes=[r_zb[j]])
                            fw.dma("sync", lambda e: e.dma_start(out=dst[c, :, p0 + ts * 512:p0 + (ts + 1) * 512],
                                                                 in_=zb[j][:]), reads=[r_zb[j]])
                        return ev

                    wi = W["w_in"][l]
                    for c in range(3):
                        wt, r_w = load_w_chunk(wi[:, c * 128:(c + 1) * 128])
                        proj_fm(wt, r_w, store_plain(UAs, c))
                    if stop_after == "ua":
                        continue
                    for c in range(3):
                        wt, r_w = load_w_chunk(wi[:, (15 + c) * 128:(16 + c) * 128])
                        proj_fm(wt, r_w, store_pad(KTs, c))
                    wt, r_w = load_w_chunk(wi[:, 24 * 128:25 * 128])
                    proj_fm(wt, r_w, store_pad(KTs, 3))
                    for c in range(3):
                        wt, r_w = load_w_chunk(wi[:, (18 + c) * 128:(19 + c) * 128])
                        proj_fm(wt, r_w, store_pad(VTs, c))
                    wt, r_w = load_w_chunk(wi[:, 25 * 128:26 * 128])
                    proj_fm(wt, r_w, store_pad(VTs, 3))
                    for c in range(3):
                        wt, r_w = load_w_chunk(wi[:, (3 + c) * 128:(4 + c) * 128])

                        def ev_vb(ts, j, pt, r_pt):
                            fw.op("scalar", lambda e: e.activation(out=zt0[:, ts * 512:(ts + 1) * 512], in_=pt[:],
                                                                   func=AF.Copy), reads=[r_pt], writes=[r_zt0])
                        proj_fm(wt, r_w, ev_vb)
                        wt, r_w = load_w_chunk(wi[:, (9 + c) * 128:(10 + c) * 128])

                        def ev_gc(ts, j, pt, r_pt, c=c, p0=p0):
                            fw.op("vector", lambda e: e.tensor_tensor(out=zb[j][:], in0=pt[:],
                                                                      in1=zt0[:, ts * 512:(ts + 1) * 512], op=ALU.mult),
                                  reads=[r_pt, r_zt0], writes=[r_zb[j]])
                            fw.dma("sync", lambda e: e.dma_start(out=CVs[c, :, p0 + ts * 512:p0 + (ts + 1) * 512],
                                                                 in_=zb[j][:]), reads=[r_zb[j]])
                        proj_fm(wt, r_w, ev_gc)
            fw.barrier()

        def phase_s5(l):
            with ExitStack() as pes:
                def sb2(name, shape, dt=F32):
                    return pes.enter_context(nc.sbuf_tensor(uname("s_" + name), shape, dt))
                r_p = Res("prm")
                names = ["are", "aim", "ldt", "dt", "rho", "th", "c", "s", "t1", "t2", "lr", "li", "nr", "den",
                         "numr", "numi", "kr", "ki", "nki"]
                P = {n: sb2(n, [128, 24]) for n in names}

                def tt(o, a, b, op):
                    fw.op("vector", lambda e: e.tensor_tensor(out=P[o][:], in0=P[a][:], in1=P[b][:], op=op),
                          reads=[r_p], writes=[r_p])

                def ts_(o, a, s1, op0, s2=None, op1=None):
                    if op1 is None:
                        fw.op("vector", lambda e: e.tensor_scalar(out=P[o][:], in0=P[a][:], scalar1=s1, scalar2=None,
                                                                  op0=op0), reads=[r_p], writes=[r_p])
                    else:
                        fw.op("vector", lambda e: e.tensor_scalar(out=P[o][:], in0=P[a][:], scalar1=s1, scalar2=s2,
                                                                  op0=op0, op1=op1), reads=[r_p], writes=[r_p])

                def act(o, a, func, scale=1.0):
                    fw.op("scalar", lambda e: e.activation(out=P[o][:], in_=P[a][:], func=func, scale=scale),
                          reads=[r_p], writes=[r_p])

                for d in range(2):
                    fw.dma("sync", lambda e, d=d: e.dma_start(
                        out=P["are"][:, d * 12:(d + 1) * 12],
                        in_=W["s5_a_re"][l, d].rearrange("(gp g2) p -> (g2 p) gp", g2=2),
                        allow_slow_non_contiguous=True), writes=[r_p])
                    fw.dma("sync", lambda e, d=d: e.dma_start(
                        out=P["aim"][:, d * 12:(d + 1) * 12],
                        in_=W["s5_a_im"][l, d].rearrange("(gp g2) p -> (g2 p) gp", g2=2),
                        allow_slow_non_contiguous=True), writes=[r_p])
                    for g2 in range(2):
                        fw.dma("sync", lambda e, d=d, g2=g2: e.dma_start(
                            out=P["ldt"][64 * g2:64 * g2 + 64, d * 12:(d + 1) * 12],
                            in_=W["s5_log_dt"][l, d].rearrange("(gp g2) -> g2 gp", g2=2)[g2].partition_broadcast(64),
                            allow_slow_non_contiguous=True), writes=[r_p])
                act("dt", "ldt", AF.Exp)
                tt("t1", "are", "dt", ALU.mult)
                act("rho", "t1", AF.Exp)
                tt("th", "aim", "dt", ALU.mult)
                act("t1", "th", AF.Sin, scale=1.0 / 128)
                tt("t2", "t1", "t1", ALU.mult)
                ts_("c", "t2", -2.0, ALU.mult, 1.0, ALU.add)
                act("s", "th", AF.Sin, scale=1.0 / 64)
                for _ in range(6):
                    tt("t1", "c", "c", ALU.mult)
                    tt("t2", "s", "s", ALU.mult)
                    fw.op("vector", lambda e: e.scalar_tensor_tensor(out=P["s"][:], in0=P["c"][:], scalar=2.0,
                                                                     in1=P["s"][:], op0=ALU.mult, op1=ALU.mult),
                          reads=[r_p], writes=[r_p])
                    tt("c", "t1", "t2", ALU.subtract)
                tt("lr", "rho", "c", ALU.mult)
                tt("li", "rho", "s", ALU.mult)
                ts_("nr", "lr", -1.0, ALU.add)
                tt("t1", "are", "are", ALU.mult)
                tt("t2", "aim", "aim", ALU.mult)
                tt("den", "t1", "t2", ALU.add)
                fw.op("vector", lambda e: e.reciprocal(out=P["den"][:], in_=P["den"][:]), reads=[r_p], writes=[r_p])
                tt("t1", "nr", "are", ALU.mult)
                tt("t2", "li", "aim", ALU.mult)
                tt("numr", "t1", "t2", ALU.add)
                tt("t1", "li", "are", ALU.mult)
                tt("t2", "nr", "aim", ALU.mult)
                tt("numi", "t1", "t2", ALU.subtract)
                tt("kr", "numr", "den", ALU.mult)
                tt("ki", "numi", "den", ALU.mult)
                ts_("nki", "ki", -1.0, ALU.mult)
                LRR = sb2("LRR", [128, 2, 24])
                LIS = sb2("LIS", [128, 2, 24])
                for hh in range(2):
                    fw.op("vector", lambda e, hh=hh: e.tensor_copy(out=LRR[:, hh, :], in_=P["lr"][:]),
                          reads=[r_p], writes=[r_p])
                fw.op("vector", lambda e: e.tensor_scalar(out=LIS[:, 0, :], in0=P["li"][:], scalar1=-1.0, scalar2=None,
                                                          op0=ALU.mult), reads=[r_p], writes=[r_p])
                fw.op("vector", lambda e: e.tensor_copy(out=LIS[:, 1, :], in_=P["li"][:]), reads=[r_p], writes=[r_p])

                for nm in ["l2r", "l2i"]:
                    P[nm] = sb2(nm, [128, 24])
                tt("t1", "lr", "lr", ALU.mult)
                tt("t2", "li", "li", ALU.mult)
                tt("l2r", "t1", "t2", ALU.subtract)
                fw.op("vector", lambda e: e.scalar_tensor_tensor(out=P["l2i"][:], in0=P["lr"][:], scalar=2.0,
                                                                 in1=P["li"][:], op0=ALU.mult, op1=ALU.mult),
                      reads=[r_p], writes=[r_p])
                L2RR = sb2("L2RR", [128, 2, 24])
                L2IS = sb2("L2IS", [128, 2, 24])
                for hh in range(2):
                    fw.op("vector", lambda e, hh=hh: e.tensor_copy(out=L2RR[:, hh, :], in_=P["l2r"][:]),
                          reads=[r_p], writes=[r_p])
                fw.op("vector", lambda e: e.tensor_scalar(out=L2IS[:, 0, :], in0=P["l2i"][:], scalar1=-1.0, scalar2=None,
                                                          op0=ALU.mult), reads=[r_p], writes=[r_p])
                fw.op("vector", lambda e: e.tensor_copy(out=L2IS[:, 1, :], in_=P["l2i"][:]), reads=[r_p], writes=[r_p])
                LRR64 = sb2("LRR64", [128, 2, 24, 64])
                LIS64 = sb2("LIS64", [128, 2, 24, 64])
                for (dst64, src3) in [(LRR64, LRR), (LIS64, LIS)]:
                    fw.op("vector", lambda e, dst64=dst64, src3=src3: e.tensor_copy(out=dst64[:, :, :, 0], in_=src3[:]),
                          reads=[r_p], writes=[r_p])
                    w_ = 1
                    while w_ < 64:
                        fw.op("vector", lambda e, dst64=dst64, w_=w_: e.tensor_copy(out=dst64[:, :, :, w_:2 * w_],
                                                                                   in_=dst64[:, :, :, 0:w_]),
                              reads=[r_p], writes=[r_p])
                        w_ *= 2
                for nm in ["l4r", "l4i"]:
                    P[nm] = sb2(nm, [128, 24])
                tt("t1", "l2r", "l2r", ALU.mult)
                tt("t2", "l2i", "l2i", ALU.mult)
                tt("l4r", "t1", "t2", ALU.subtract)
                fw.op("vector", lambda e: e.scalar_tensor_tensor(out=P["l4i"][:], in0=P["l2r"][:], scalar=2.0,
                                                                 in1=P["l2i"][:], op0=ALU.mult, op1=ALU.mult),
                      reads=[r_p], writes=[r_p])
                L4RR = sb2("L4RR", [128, 2, 24])
                L4IS = sb2("L4IS", [128, 2, 24])
                for hh in range(2):
                    fw.op("vector", lambda e, hh=hh: e.tensor_copy(out=L4RR[:, hh, :], in_=P["l4r"][:]),
                          reads=[r_p], writes=[r_p])
                fw.op("vector", lambda e: e.tensor_scalar(out=L4IS[:, 0, :], in0=P["l4i"][:], scalar1=-1.0, scalar2=None,
                                                          op0=ALU.mult), reads=[r_p], writes=[r_p])
                fw.op("vector", lambda e: e.tensor_copy(out=L4IS[:, 1, :], in_=P["l4i"][:]), reads=[r_p], writes=[r_p])
                L2R32 = sb2("L2R32", [128, 2, 24, 32])
                L2I32 = sb2("L2I32", [128, 2, 24, 32])
                for (dst32, src3) in [(L2R32, L2RR), (L2I32, L2IS)]:
                    fw.op("vector", lambda e, dst32=dst32, src3=src3: e.tensor_copy(out=dst32[:, :, :, 0], in_=src3[:]),
                          reads=[r_p], writes=[r_p])
                    w_ = 1
                    while w_ < 32:
                        fw.op("vector", lambda e, dst32=dst32, w_=w_: e.tensor_copy(out=dst32[:, :, :, w_:2 * w_],
                                                                                   in_=dst32[:, :, :, 0:w_]),
                              reads=[r_p], writes=[r_p])
                        w_ *= 2
                T1 = sb2("T1", [128, 2, 24, 64])
                T2 = sb2("T2", [128, 2, 24, 64])
                CB2 = T2[:, :, :, 32:64]
                CB = sb2("CB", [128, 2, 24, 64])
                r_t12 = Res("T12")
                r_cb = Res("CB")
                Bw = sb2("Bw", [128, 48, 128])
                Cw = sb2("Cw", [128, 48, 128])
                r_bw = Res("Bw")
                r_cw = Res("Cw")
                fw.op("gpsimd", lambda e: e.memset(Bw[:], 0.0), writes=[r_bw])
                fw.op("gpsimd", lambda e: e.memset(Cw[:], 0.0), writes=[r_cw])

                def widx(d, gp, ri):
                    return (d * 12 + gp) * 2 + ri
                for d in range(2):
                    for gp in range(12):
                        for g2 in range(2):
                            g = 2 * gp + g2
                            r0 = 16 * (g % 8)
                            for ri, (bn, cn) in enumerate([("s5_b_re", "s5_c_re"), ("s5_b_im", "s5_c_im")]):
                                fw.dma("sync", lambda e, d=d, gp=gp, g2=g2, g=g, r0=r0, ri=ri, bn=bn: e.dma_start(
                                    out=Bw[r0:r0 + 16, widx(d, gp, ri), 64 * g2:64 * g2 + 64],
                                    in_=W[bn][l, d, g].rearrange("p h -> h p"),
                                    allow_slow_non_contiguous=True), writes=[r_bw])
                                fw.dma("sync", lambda e, d=d, gp=gp, g2=g2, g=g, r0=r0, ri=ri, cn=cn: e.dma_start(
                                    out=Cw[64 * g2:64 * g2 + 64, widx(d, gp, ri), r0:r0 + 16],
                                    in_=W[cn][l, d, g].rearrange("h p -> p h"),
                                    allow_slow_non_contiguous=True), writes=[r_cw])
                Cw4 = Cw[:].rearrange("p (a r) n -> p a r n", r=2)
                fw.op("vector", lambda e: e.tensor_scalar(out=Cw4[:, :, 1, :], in0=Cw4[:, :, 1, :], scalar1=-1.0,
                                                          scalar2=None, op0=ALU.mult), reads=[r_cw], writes=[r_cw])

                XS = sb2("XS", [128, 2, 24, 129])
                BU = sb2("BU", [128, 2, 24, 128])
                PQ = sb2("PQ", [128, 2, 2, 24])
                r_xs = Res("XS")
                r_bu = Res("BU")
                r_pq = Res("PQ")
                r_pq1 = Res("PQ1")
                tmpb = [sb2("tmpb%d" % i, [128, 2, 128]) for i in range(2)]
                r_tmpb = [Res("tmpb%d" % i) for i in range(2)]
                ua = [[sb2("ua%d_%d" % (i, d), [128, 3, 128]) for d in range(2)] for i in range(2)]
                r_ua = [[Res("ua%d_%d" % (i, d)) for d in range(2)] for i in range(2)]
                yo = [sb2("yo%d" % i, [128, 128]) for i in range(2)]
                r_yo = [Res("yo%d" % i) for i in range(2)]
                fw.op("vector", lambda e: e.memset(XS[:], 0.0), writes=[r_xs])
                NT = NTOK // 128
                kcount = 0
                for i in range(NT):
                    tiles = [i, NT - 1 - i]
                    bi = i % 2
                    for d in range(2):
                        tk = tiles[d] * 128
                        fw.dma("sync", lambda e, d=d, tk=tk, bi=bi: e.dma_start(
                            out=ua[bi][d][:], in_=UAs[:, :, tk:tk + 128].rearrange("c p n -> p c n")),
                            writes=[r_ua[bi][d]])
                    for d in range(2):
                        for gp in range(12):
                            col = d * 12 + gp
                            c3 = gp // 4
                            pp = 2 * (col % 2)
                            for ri in range(2):
                                fw.op("tensor", lambda e, d=d, gp=gp, ri=ri, pp=pp, c3=c3, bi=bi: e.matmul(
                                    pA[pp + ri][:, 0:128], lhsT=Bw[:, widx(d, gp, ri), :], rhs=ua[bi][d][:, c3, :],
                                    start=True, stop=True),
                                    reads=[r_bw, r_ua[bi][d]], writes=[r_pA[pp + ri]])
                            tbk = tmpb[col % 2]
                            r_tbk = r_tmpb[col % 2]
                            if d == 0:
                                bre, bim = BU[:, 0, col, :], BU[:, 1, col, :]
                            else:
                                bre, bim = BU[:, 0, col, ::-1], BU[:, 1, col, ::-1]
                            fw.op("vector", lambda e, tbk=tbk, pp=pp, col=col: e.tensor_scalar(
                                out=tbk[:, 0, :], in0=pA[pp][:, 0:128], scalar1=P["kr"][:, col:col + 1], scalar2=None,
                                op0=ALU.mult), reads=[r_pA[pp], r_p], writes=[r_tbk])
                            fw.op("vector", lambda e, tbk=tbk, pp=pp, col=col, bre=bre: e.scalar_tensor_tensor(
                                out=bre, in0=pA[pp + 1][:, 0:128], scalar=P["nki"][:, col:col + 1], in1=tbk[:, 0, :],
                                op0=ALU.mult, op1=ALU.add), reads=[r_pA[pp + 1], r_p, r_tbk], writes=[r_bu])
                            fw.op("vector", lambda e, tbk=tbk, pp=pp, col=col: e.tensor_scalar(
                                out=tbk[:, 1, :], in0=pA[pp + 1][:, 0:128], scalar1=P["kr"][:, col:col + 1],
                                scalar2=None, op0=ALU.mult), reads=[r_pA[pp + 1], r_p], writes=[r_tbk])
                            fw.op("vector", lambda e, tbk=tbk, pp=pp, col=col, bim=bim: e.scalar_tensor_tensor(
                                out=bim, in0=pA[pp][:, 0:128], scalar=P["ki"][:, col:col + 1], in1=tbk[:, 1, :],
                                op0=ALU.mult, op1=ALU.add), reads=[r_pA[pp], r_p, r_tbk], writes=[r_bu])
                    BUe = BU[:, :, :, 0:128:2]
                    BUo = BU[:, :, :, 1:128:2]
                    BUes = BU[:, ::-1, :, 0:128:2]
                    fw.op("vector", lambda e, BUe=BUe: e.tensor_tensor(out=T1[:], in0=LRR64[:], in1=BUe, op=ALU.mult),
                          reads=[r_bu, r_p], writes=[r_t12])
                    fw.op("vector", lambda e, BUes=BUes: e.tensor_tensor(out=T2[:], in0=LIS64[:], in1=BUes, op=ALU.mult),
                          reads=[r_bu, r_p], writes=[r_t12])
                    fw.op("vector", lambda e: e.tensor_tensor(out=T1[:], in0=T1[:], in1=T2[:], op=ALU.add),
                          reads=[r_t12], writes=[r_t12])
                    fw.op("vector", lambda e, BUo=BUo: e.tensor_tensor(out=CB[:], in0=T1[:], in1=BUo, op=ALU.add),
                          reads=[r_t12, r_bu], writes=[r_cb])
                    C1e = CB[:, :, :, 0:64:2]
                    C1o = CB[:, :, :, 1:64:2]
                    C1es = CB[:, ::-1, :, 0:64:2]
                    fw.op("vector", lambda e, C1e=C1e: e.tensor_tensor(out=T1[:, :, :, 0:32], in0=L2R32[:], in1=C1e, op=ALU.mult),
                          reads=[r_cb, r_p], writes=[r_t12])
                    fw.op("vector", lambda e, C1es=C1es: e.tensor_tensor(out=T2[:, :, :, 0:32], in0=L2I32[:], in1=C1es, op=ALU.mult),
                          reads=[r_cb, r_p], writes=[r_t12])
                    fw.op("vector", lambda e: e.tensor_tensor(out=T1[:, :, :, 0:32], in0=T1[:, :, :, 0:32], in1=T2[:, :, :, 0:32],
                                                              op=ALU.add), reads=[r_t12], writes=[r_t12])
                    fw.op("vector", lambda e, C1o=C1o: e.tensor_tensor(out=CB2, in0=T1[:, :, :, 0:32], in1=C1o, op=ALU.add),
                          reads=[r_t12, r_cb], writes=[r_t12])
                    for n_ in range(32):
                        j = 4 * n_
                        fw.op("vector", lambda e, j=j: e.tensor_tensor(out=PQ[:, 0], in0=L4RR[:], in1=XS[:, :, :, j],
                                                                       op=ALU.mult),
                              reads=[r_xs, r_p], writes=[r_pq])
                        fw.op("vector", lambda e, j=j: e.tensor_tensor(out=PQ[:, 1], in0=L4IS[:], in1=XS[:, ::-1, :, j],
                                                                       op=ALU.mult),
                              reads=[r_xs, r_p], writes=[r_pq1])
                        fw.op("vector", lambda e: e.tensor_tensor(out=PQ[:, 0], in0=PQ[:, 0], in1=PQ[:, 1], op=ALU.add),
                              reads=[r_pq, r_pq1], writes=[r_pq])
                        fw.op("vector", lambda e, j=j, n_=n_: e.tensor_tensor(out=XS[:, :, :, j + 4], in0=PQ[:, 0],
                                                                              in1=T2[:, :, :, 32 + n_], op=ALU.add),
                              reads=[r_pq, r_t12], writes=[r_xs])
                    X4 = XS[:, :, :, 0:128:4]
                    X4s = XS[:, ::-1, :, 0:128:4]
                    X42 = XS[:, :, :, 2:129:4]
                    fw.op("vector", lambda e, X4=X4: e.tensor_tensor(out=T1[:, :, :, 0:32], in0=L2R32[:], in1=X4, op=ALU.mult),
                          reads=[r_xs, r_p], writes=[r_t12])
                    fw.op("vector", lambda e, X4s=X4s: e.tensor_tensor(out=T2[:, :, :, 0:32], in0=L2I32[:], in1=X4s, op=ALU.mult),
                          reads=[r_xs, r_p], writes=[r_t12])
                    fw.op("vector", lambda e: e.tensor_tensor(out=T1[:, :, :, 0:32], in0=T1[:, :, :, 0:32], in1=T2[:, :, :, 0:32],
                                                              op=ALU.add), reads=[r_t12], writes=[r_t12])
                    fw.op("vector", lambda e, X42=X42, C1e=C1e: e.tensor_tensor(out=X42, in0=T1[:, :, :, 0:32], in1=C1e, op=ALU.add),
                          reads=[r_t12, r_cb], writes=[r_xs])
                    XSe = XS[:, :, :, 0:128:2]
                    XSes = XS[:, ::-1, :, 0:128:2]
                    XSo = XS[:, :, :, 1:129:2]
                    fw.op("vector", lambda e, XSe=XSe: e.tensor_tensor(out=T1[:], in0=LRR64[:], in1=XSe, op=ALU.mult),
                          reads=[r_xs, r_p], writes=[r_t12])
                    fw.op("vector", lambda e, XSes=XSes: e.tensor_tensor(out=T2[:], in0=LIS64[:], in1=XSes, op=ALU.mult),
                          reads=[r_xs, r_p], writes=[r_t12])
                    fw.op("vector", lambda e: e.tensor_tensor(out=T1[:], in0=T1[:], in1=T2[:], op=ALU.add),
                          reads=[r_t12], writes=[r_t12])
                    fw.op("vector", lambda e, XSo=XSo, BUe=BUe: e.tensor_tensor(out=XSo, in0=T1[:], in1=BUe, op=ALU.add),
                          reads=[r_t12, r_bu], writes=[r_xs])
                    for d in range(2):
                        tk = tiles[d] * 128
                        for c3 in range(3):
                            pj = kcount % 2
                            kcount += 1
                            n = 0
                            for gq in range(4):
                                gp = c3 * 4 + gq
                                col = d * 12 + gp
                                for ri in range(2):
                                    fw.op("tensor", lambda e, d=d, gp=gp, ri=ri, col=col, pj=pj, n=n: e.matmul(
                                        pX[pj][:, 0:128], lhsT=Cw[:, widx(d, gp, ri), :], rhs=XS[:, ri, col, 1:129],
                                        start=(n == 0), stop=(n == 7)),
                                        reads=[r_cw, r_xs], writes=[r_pX[pj]], pe_acc=True)
                                    n += 1
                            ov = yo[pj][:, :] if d == 0 else yo[pj][:, ::-1]
                            fw.op("scalar", lambda e, pj=pj, ov=ov: e.activation(out=ov, in_=pX[pj][:, 0:128],
                                                                                 func=AF.Copy),
                                  reads=[r_pX[pj]], writes=[r_yo[pj]])
                            fw.dma("sync", lambda e, d=d, c3=c3, tk=tk, pj=pj: e.dma_start(
                                out=YA[d, c3, :, tk:tk + 128], in_=yo[pj][:]), reads=[r_yo[pj]])
                    fw.op("vector", lambda e: e.tensor_copy(out=XS[:, :, :, 0], in_=XS[:, :, :, 128]),
                          reads=[r_xs], writes=[r_xs])
                    if (i + 1) % 32 == 0 and i + 1 < NT:
                        sgn = (i + 1) // 32
                        fw.op("vector", lambda e, sgn=sgn: e.tensor_scalar(
                            out=XS[:, :, 0:12, 0], in0=XS[:, :, 0:12, 0], scalar1=flg[:, sgn:sgn + 1], scalar2=None,
                            op0=ALU.mult), reads=[r_xs, r_flg], writes=[r_xs])
                        fw.op("vector", lambda e, sgn=sgn: e.tensor_scalar(
                            out=XS[:, :, 12:24, 0], in0=XS[:, :, 12:24, 0], scalar1=flg[:, 4 - sgn:5 - sgn],
                            scalar2=None, op0=ALU.mult), reads=[r_xs, r_flg], writes=[r_xs])
            fw.barrier()

        def phase_h():
            with ExitStack() as pes:
                hb = [pes.enter_context(nc.sbuf_tensor(uname("h_hb%d" % i), [128, 4, PAD], BF16)) for i in range(2)]
                r_hb = [Res("hb%d" % i) for i in range(2)]
                k = 0
                for (T, nch) in [(KTs, 4), (VTs, 4), (CVs, 3)]:
                    for sg in range(NSEG):
                        jobs = []
                        src = ((sg - 1) * SEGP + SEG) if sg > 0 else (sg * SEGP + PAD)
                        jobs.append((src, sg * SEGP, sg))
                        src = ((sg + 1) * SEGP + PAD) if sg < NSEG - 1 else (sg * SEGP + SEG)
                        jobs.append((src, sg * SEGP + PAD + SEG, sg + 1))
                        for (src, dst, fc) in jobs:
                            b = k % 2
                            k += 1
                            fw.dma("sync", lambda e, T=T, nch=nch, src=src, b=b: e.dma_start(
                                out=hb[b][:, 0:nch, :], in_=T[0:nch, :, src:src + PAD].rearrange("c p n -> p c n")),
                                writes=[r_hb[b]])
                            fw.op("vector", lambda e, nch=nch, b=b, fc=fc: e.tensor_scalar(
                                out=hb[b][:, 0:nch, :], in0=hb[b][:, 0:nch, :], scalar1=flg[:, fc:fc + 1], scalar2=None,
                                op0=ALU.mult), reads=[r_hb[b], r_flg], writes=[r_hb[b]])
                            fw.dma("sync", lambda e, T=T, nch=nch, dst=dst, b=b: e.dma_start(
                                out=T[0:nch, :, dst:dst + PAD].rearrange("c p n -> p c n"), in_=hb[b][:, 0:nch, :]),
                                reads=[r_hb[b]])
            fw.barrier()

        maskD = sb("maskD", [128, 6, 256])
        maskS = sb("maskS", [128, 6, 384])
        r_mask = Res("mask")
        ones_col = sb("ones_col", [128, 1])
        fw.op("vector", lambda e: e.memset(ones_col[:], 1.0), writes=[r_mask])
        with ExitStack() as mes:
            ii = mes.enter_context(nc.sbuf_tensor("m_ii", [128, 128], mybir.dt.int32))
            fi = mes.enter_context(nc.sbuf_tensor("m_fi", [128, 128], F32))
            ta = mes.enter_context(nc.sbuf_tensor("m_ta", [128, 128], F32))
            tv = mes.enter_context(nc.sbuf_tensor("m_tv", [128, 128], F32))
            r_m = Res("m")
            fw.op("gpsimd", lambda e: e.iota(ii[:], pattern=[[-1, 128]], base=0, channel_multiplier=1), writes=[r_m])
            fw.op("vector", lambda e: e.tensor_copy(out=fi[:], in_=ii[:]), reads=[r_m], writes=[r_m])

            def mk_mask(dst, off, half, coef):
                fw.op("vector", lambda e: e.tensor_scalar(out=ta[:], in0=fi[:], scalar1=float(off), scalar2=None,
                                                          op0=ALU.add), reads=[r_m], writes=[r_m])
                fw.op("vector", lambda e: e.tensor_scalar(out=tv[:], in0=ta[:], scalar1=-1.0, scalar2=None,
                                                          op0=ALU.mult), reads=[r_m], writes=[r_m])
                fw.op("vector", lambda e: e.tensor_tensor(out=ta[:], in0=ta[:], in1=tv[:], op=ALU.max),
                      reads=[r_m], writes=[r_m])
                fw.op("vector", lambda e: e.tensor_scalar(out=tv[:], in0=ta[:], scalar1=-1.0, scalar2=float(half) + 0.5,
                                                          op0=ALU.mult, op1=ALU.add), reads=[r_m], writes=[r_m])
                fw.op("vector", lambda e: e.tensor_scalar(out=tv[:], in0=tv[:], scalar1=0.0, scalar2=0.5,
                                                          op0=ALU.max, op1=ALU.min), reads=[r_m], writes=[r_m])
                fw.op("scalar", lambda e: e.activation(out=ta[:], in_=ta[:], func=AF.Exp, scale=-float(coef)),
                      reads=[r_m], writes=[r_m])
                fw.op("vector", lambda e: e.scalar_tensor_tensor(out=dst, in0=ta[:], scalar=2.0, in1=tv[:],
                                                                 op0=ALU.mult, op1=ALU.mult),
                      reads=[r_m], writes=[r_m, r_mask])
            for gi, (win, dil) in enumerate(DIL):
                for h in range(2):
                    sl = SLOPES[6 + 2 * gi + h]
                    for kt in range(2):
                        mk_mask(maskD[:, 2 * gi + h, kt * 128:(kt + 1) * 128], -64 + 128 * kt, 64, sl * dil)
            for h in range(6):
                for kt in range(3):
                    mk_mask(maskS[:, h, kt * 128:(kt + 1) * 128], 128 * (kt - 1), 128, SLOPES[h])
        fw.barrier()

        def phase_b(l, last):
            with ExitStack() as L0:
                def sb0(name, shape, dt=F32):
                    return L0.enter_context(nc.sbuf_tensor(uname("b_" + name), shape, dt))
                hT = sb0("hT", [128, 8, ST], BF16)
                r_hT = Res("hT")
                vcol = sb0("vcol", [128, 2])
                r_vcol = Res("vcol")
                sexp = sb0("sexp", [128, 6])
                r_sexp = Res("sexp")
                fw.dma("sync", lambda e: e.dma_start(out=sexp[:], in_=W["swa_sink"][l].partition_broadcast(128)),
                       writes=[r_sexp])
                fw.op("scalar", lambda e: e.activation(out=sexp[:], in_=sexp[:], func=AF.Exp),
                      reads=[r_sexp], writes=[r_sexp])
                wi = W["w_in"][l]
                for st_i in range(NST):
                    t0 = st_i * ST
                    p0 = padpos(t0)
                    sg = st_i // 2
                    hf = st_i % 2
                    fw.dma("sync", lambda e, t0=t0: e.dma_start(
                        out=hT[:], in_=HT[:, :, t0:t0 + ST].rearrange("k p n -> p k n")), writes=[r_hT])
                    fw.op("vector", lambda e: e.memset(vcol[:], 1.0), writes=[r_vcol])
                    fw.op("vector", lambda e, sg=sg: e.tensor_copy(out=vcol[0:64, 0:1], in_=flg[0:64, sg:sg + 1]),
                          reads=[r_flg], writes=[r_vcol])
                    fw.op("vector", lambda e, sg=sg: e.tensor_copy(out=vcol[64:128, 1:2], in_=flg[64:128, sg + 1:sg + 2]),
                          reads=[r_flg], writes=[r_vcol])
                    with ExitStack() as L1:
                        def sb1(name, shape, dt=F32):
                            return L1.enter_context(nc.sbuf_tensor(uname("b1_" + name), shape, dt))
                        brT = sb1("brT", [128, 10, ST], BF16)
                        r_br = Res("brT")
                        wst = [sb1("wst%d" % i, [128, 8, 128]) for i in range(2)]
                        r_wst = [Res("wst%d" % i) for i in range(2)]
                        wbf = [sb1("wbf%d" % i, [128, 8, 128], BF16) for i in range(2)]
                        r_wbf = [Res("wbf%d" % i) for i in range(2)]

                        def load_w_chunk(parts):
                            j = cnt["w"] % 2
                            cnt["w"] += 1
                            for (src_ap, c0, n) in parts:
                                fw.dma("sync", lambda e, src_ap=src_ap, c0=c0, n=n, j=j: e.dma_start(
                                    out=wst[j][:, :, c0:c0 + n], in_=src_ap.rearrange("(k p) n -> p k n", p=128)),
                                    writes=[r_wst[j]])
                            fw.op("gpsimd", lambda e, j=j: e.tensor_copy(out=wbf[j][:], in_=wst[j][:]),
                                  reads=[r_wst[j]], writes=[r_wbf[j]])
                            return wbf[j], r_wbf[j]

                        def proj_fm(wt, r_w, evac):
                            for ts in range(ST // 512):
                                j = cnt["pA"] % 4
                                cnt["pA"] += 1
                                for k in range(8):
                                    fw.op("tensor", lambda e, k=k, j=j, ts=ts: e.matmul(
                                        pA[j][:], lhsT=wt[:, k, :], rhs=hT[:, k, ts * 512:(ts + 1) * 512],
                                        start=(k == 0), stop=(k == 7)),
                                        reads=[r_w, r_hT], writes=[r_pA[j]], pe_acc=True)
                                evac(ts, j, pA[j], r_pA[j])

                        with ExitStack() as S1:
                            def sbs(name, shape, dt=F32):
                                return S1.enter_context(nc.sbuf_tensor(uname("b2_" + name), shape, dt))
                            KT1 = sbs("KT1", [128, 2 * ST], BF16)
                            VT1 = sbs("VT1", [128, 2 * ST], BF16)
                            r_kv = Res("kv")
                            QT1 = sbs("QT1", [128, ST], BF16)
                            r_q = Res("q")
                            UACC = sbs("UACC", [128, 2, ST])
                            r_ua = Res("uacc")
                            RC = sbs("RC", [128, ST])
                            r_rc = Res("rc")
                            Et = [sbs("E%d" % i, [128, 384]) for i in range(2)]
                            r_E = [Res("E%d" % i) for i in range(2)]
                            Pt = [sbs("P%d" % i, [128, 384], BF16) for i in range(2)]
                            r_P = [Res("P%d" % i) for i in range(2)]
                            VE = [sbs("VE%d" % i, [128, 3, 2, 192], BF16) for i in range(2)]
                            r_VE = [Res("VE%d" % i) for i in range(2)]
                            sm = [sbs("sm%d" % i, [128, 128]) for i in range(2)]
                            r_sm = [Res("sm%d" % i) for i in range(2)]
                            for i in range(2):
                                fw.op("vector", lambda e, i=i: e.memset(VE[i][:], 1.0), writes=[r_VE[i]])
                            ac = {"u": 0}

                            def q_evac(ts, j, pt, r_pt):
                                fw.op("scalar", lambda e: e.activation(out=QT1[:, ts * 512:(ts + 1) * 512], in_=pt[:],
                                                                       func=AF.Copy), reads=[r_pt], writes=[r_q])

                            def load_kv(c, p0=p0):
                                fw.dma("sync", lambda e, c=c, p0=p0: e.dma_start(
                                    out=KT1[:], in_=KTs[c, :, p0 - PAD:p0 - PAD + 2 * ST]), writes=[r_kv])
                                fw.dma("sync", lambda e, c=c, p0=p0: e.dma_start(
                                    out=VT1[:], in_=VTs[c, :, p0 - PAD:p0 - PAD + 2 * ST]), writes=[r_kv])

                            def unit(nkt, kcols, qcols, heads, mask_of, valid_of, lhs_of, sink_dst):
                                u = ac["u"] % 2
                                ac["u"] += 1
                                for kt in range(nkt):
                                    fw.op("tensor", lambda e, kt=kt, u=u: e.transpose(
                                        pT[u][:, kt, :], VT1[:, kcols(kt)], ident[:]),
                                        reads=[r_kv, r_ident], writes=[r_pT[u]], pe_acc=True)
                                fw.op("vector", lambda e, u=u: e.tensor_copy(
                                    out=VE[u][:, 0:nkt, :, 64:128],
                                    in_=pT[u][:, 0:nkt, :].rearrange("p k (h d) -> p k h d", h=2)),
                                    reads=[r_pT[u]], writes=[r_VE[u]])
                                for (hrow, vslot, tag) in heads:
                                    j = cnt["pA"] % 4
                                    cnt["pA"] += 1
                                    j2 = cnt["pA"] % 4
                                    cnt["pA"] += 1
                                    ei = ac["u"] % 2
                                    for kt in range(nkt):
                                        fw.op("tensor", lambda e, kt=kt, j=j, hrow=hrow: e.matmul(
                                            pA[j][:, kt * 128:(kt + 1) * 128], lhsT=KT1[hrow:hrow + 64, kcols(kt)],
                                            rhs=QT1[hrow:hrow + 64, qcols], start=True, stop=True),
                                            reads=[r_kv, r_q], writes=[r_pA[j]], pe_acc=True)
                                    fw.op("scalar", lambda e, j=j, ei=ei: e.activation(
                                        out=Et[ei][:, 0:nkt * 128], in_=pA[j][:, 0:nkt * 128], func=AF.Exp, scale=0.125),
                                        reads=[r_pA[j]], writes=[r_E[ei]])
                                    for kt in range(nkt):
                                        vc = valid_of(kt)
                                        fw.op("vector", lambda e, kt=kt, ei=ei, vc=vc, tag=tag: e.scalar_tensor_tensor(
                                            out=Pt[ei][:, kt * 128:(kt + 1) * 128], in0=Et[ei][:, kt * 128:(kt + 1) * 128],
                                            scalar=vc, in1=mask_of(tag)[:, kt * 128:(kt + 1) * 128],
                                            op0=ALU.mult, op1=ALU.mult),
                                            reads=[r_E[ei], r_mask, r_vcol, r_flg], writes=[r_P[ei]])
                                    for kt in range(nkt):
                                        fw.op("tensor", lambda e, kt=kt, j2=j2, ei=ei, u=u, vslot=vslot, tag=tag: e.matmul(
                                            pA[j2][:, 0:128], lhsT=lhs_of(VE[u], kt, vslot, tag),
                                            rhs=Pt[ei][:, kt * 128:(kt + 1) * 128], start=(kt == 0), stop=(kt == nkt - 1)),
                                            reads=[r_VE[u], r_P[ei]], writes=[r_pA[j2]], pe_acc=True)
                                    sink_dst(tag, pA[j2], r_pA[j2])

                            for gi, (win, dil) in enumerate(DIL):
                                wt, r_w = load_w_chunk([(wi[:, (12 + gi) * 128:(13 + gi) * 128], 0, 128)])
                                proj_fm(wt, r_w, q_evac)
                                load_kv(gi)
                                nsub = ST // dil
                                for r in range(dil):
                                    for qb in range(nsub // 128):
                                        q0 = qb * 128
                                        c_lo = r + dil * q0

                                        def kcols(kt, c_lo=c_lo, dil=dil):
                                            b = PAD + c_lo + dil * (-64 + 128 * kt)
                                            return slice(b, b + 127 * dil + 1, dil)
                                        qcols = slice(c_lo, c_lo + 127 * dil + 1, dil)

                                        def valid_of(kt, qb=qb, nsub=nsub):
                                            if hf == 0 and qb == 0 and kt == 0:
                                                return vcol[:, 0:1]
                                            if hf == 1 and qb == nsub // 128 - 1 and kt == 1:
                                                return vcol[:, 1:2]
                                            return ones_col[:, 0:1]

                                        def mask_of(tag, gi=gi):
                                            return maskD[:, 2 * gi + tag, :]

                                        def lhs_of(ve, kt, vslot, tag):
                                            return ve[:, kt, tag, 64:192] if tag == 0 else ve[:, kt, tag, 0:128]

                                        def sink_dst(tag, pu, r_pu, gi=gi, qcols=qcols):
                                            dstv = UACC[:, tag, qcols]
                                            if gi == 0:
                                                fw.op("vector", lambda e: e.tensor_copy(out=dstv, in_=pu[:, 0:128]),
                                                      reads=[r_pu], writes=[r_ua])
                                            else:
                                                fw.op("vector", lambda e: e.tensor_tensor(out=dstv, in0=dstv,
                                                                                          in1=pu[:, 0:128], op=ALU.add),
                                                      reads=[r_pu, r_ua], writes=[r_ua])
                                        unit(2, kcols, qcols, [(0, 0, 0), (64, 1, 1)], mask_of, valid_of, lhs_of, sink_dst)
                            fw.op("vector", lambda e: e.reciprocal(out=RC[0:64, :], in_=UACC[64:128, 0, :]),
                                  reads=[r_ua], writes=[r_rc])
                            fw.op("vector", lambda e: e.reciprocal(out=RC[64:128, :], in_=UACC[0:64, 1, :]),
                                  reads=[r_ua], writes=[r_rc])
                            fw.op("vector", lambda e: e.tensor_tensor(out=brT[0:64, 6, :], in0=UACC[0:64, 0, :],
                                                                      in1=RC[0:64, :], op=ALU.mult),
                                  reads=[r_ua, r_rc], writes=[r_br])
                            fw.op("vector", lambda e: e.tensor_tensor(out=brT[64:128, 6, :], in0=UACC[64:128, 1, :],
                                                                      in1=RC[64:128, :], op=ALU.mult),
                                  reads=[r_ua, r_rc], writes=[r_br])
                            load_kv(3)
                            for jq in range(3):
                                wt, r_w = load_w_chunk([(wi[:, 2688 + 64 * jq:2688 + 64 * jq + 64], 0, 64),
                                                        (wi[:, 2688 + 64 * (jq + 3):2688 + 64 * (jq + 3) + 64], 64, 64)])
                                proj_fm(wt, r_w, q_evac)
                                for qb in range(ST // 128):
                                    q0 = qb * 128

                                    def kcols(kt, q0=q0):
                                        b = PAD + q0 - 128 + 128 * kt
                                        return slice(b, b + 128)
                                    qcols = slice(q0, q0 + 128)

                                    def valid_of(kt, qb=qb):
                                        if hf == 0 and qb == 0 and kt == 0:
                                            return flg[:, sg:sg + 1]
                                        if hf == 1 and qb == ST // 128 - 1 and kt == 2:
                                            return flg[:, sg + 1:sg + 2]
                                        return ones_col[:, 0:1]

                                    def mask_of(tag):
                                        return maskS[:, tag, :]

                                    def lhs_of(ve, kt, vslot, tag):
                                        return ve[:, kt, vslot, 64:192] if tag % 2 == 0 else ve[:, kt, vslot, 0:128]

                                    def sink_dst(tag, pu, r_pu, qcols=qcols):
                                        h = tag
                                        ch, half = 7 + h // 2, h % 2
                                        si = ac["u"] % 2
                                        if half == 0:
                                            urows, drows = slice(0, 64), slice(64, 128)
                                        else:
                                            urows, drows = slice(64, 128), slice(0, 64)
                                        fw.op("vector", lambda e: e.tensor_scalar(
                                            out=sm[si][drows, :], in0=pu[drows, 0:128], scalar1=sexp[drows, h:h + 1],
                                            scalar2=None, op0=ALU.add), reads=[r_pu, r_sexp], writes=[r_sm[si]])
                                        fw.op("vector", lambda e: e.reciprocal(out=sm[si][drows, :], in_=sm[si][drows, :]),
                                              reads=[r_sm[si]], writes=[r_sm[si]])
                                        fw.op("vector", lambda e: e.tensor_tensor(
                                            out=brT[urows, ch, qcols], in0=pu[urows, 0:128], in1=sm[si][drows, :],
                                            op=ALU.mult), reads=[r_pu, r_sm[si]], writes=[r_br])
                                    unit(3, kcols, qcols, [(0, 0, jq), (64, 1, jq + 3)], mask_of, valid_of, lhs_of, sink_dst)
                        fw.barrier()
                        if stop_after == "attn":
                            fw.dma("sync", lambda e, t0=t0: e.dma_start(
                                out=dbg["br"][:, :, t0:t0 + ST].rearrange("c p n -> p c n"), in_=brT[:]), reads=[r_br])
                            fw.barrier()
                            continue
                        with ExitStack() as S2:
                            def sbt(name, shape, dt=F32):
                                return S2.enter_context(nc.sbuf_tensor(uname("b3_" + name), shape, dt))
                            dvec = sbt("dvec", [128, 3])
                            glub = sbt("glub", [128, 3])
                            cwt = sbt("cwt", [128, 3, 3])
                            cbt = sbt("cbt", [128, 3])
                            r_sv = Res("sv")
                            fw.dma("sync", lambda e: e.dma_start(out=dvec[:], in_=W["s5_d"][l].rearrange("(c p) -> p c", p=128),
                                                                 allow_slow_non_contiguous=True), writes=[r_sv])
                            fw.dma("sync", lambda e: e.dma_start(out=glub[:], in_=W["s5_glu_b"][l].rearrange("(c p) -> p c", p=128),
                                                                 allow_slow_non_contiguous=True), writes=[r_sv])
                            fw.dma("sync", lambda e: e.dma_start(out=cwt[:], in_=W["conv_w"][l].rearrange("t (c p) -> p t c", p=128),
                                                                 allow_slow_non_contiguous=True), writes=[r_sv])
                            fw.dma("sync", lambda e: e.dma_start(out=cbt[:], in_=W["conv_b"][l].rearrange("(c p) -> p c", p=128),
                                                                 allow_slow_non_contiguous=True), writes=[r_sv])
                            gluw32 = sbt("gluw32", [128, 3, 384])
                            gluw = sbt("gluw", [128, 3, 384], BF16)
                            r_gluw = Res("gluw")
                            fw.dma("sync", lambda e: e.dma_start(out=gluw32[:], in_=W["s5_glu_w"][l].rearrange("(c p) n -> p c n", p=128)),
                                   writes=[r_gluw])
                            fw.op("gpsimd", lambda e: e.tensor_copy(out=gluw[:], in_=gluw32[:]), reads=[r_gluw], writes=[r_gluw])
                            yf = [sbt("yf%d" % i, [128, 512]) for i in range(2)]
                            yb_ = [sbt("yb%d" % i, [128, 512]) for i in range(2)]
                            uu = [sbt("uu%d" % i, [128, 512]) for i in range(2)]
                            r_y3 = [Res("y3_%d" % i) for i in range(2)]
                            zf32 = sbt("zf32", [128, 3, 512])
                            zb16 = sbt("zb16", [128, 3, 512], BF16)
                            r_z = Res("z")
                            gt = [sbt("gt%d" % i, [128, 512]) for i in range(2)]
                            r_gt = [Res("gt%d" % i) for i in range(2)]
                            kk = 0
                            for ts in range(ST // 512):
                                tk = t0 + ts * 512
                                for c in range(3):
                                    b = kk % 2
                                    kk += 1
                                    fw.dma("sync", lambda e, c=c, tk=tk, b=b: e.dma_start(out=yf[b][:], in_=YA[0, c, :, tk:tk + 512]),
                                           writes=[r_y3[b]])
                                    fw.dma("sync", lambda e, c=c, tk=tk, b=b: e.dma_start(out=yb_[b][:], in_=YA[1, c, :, tk:tk + 512]),
                                           writes=[r_y3[b]])
                                    fw.dma("sync", lambda e, c=c, tk=tk, b=b: e.dma_start(out=uu[b][:], in_=UAs[c, :, tk:tk + 512]),
                                           writes=[r_y3[b]])
                                    fw.op("vector", lambda e, b=b: e.tensor_tensor(out=yf[b][:], in0=yf[b][:], in1=yb_[b][:], op=ALU.add),
                                          reads=[r_y3[b]], writes=[r_y3[b]])
                                    fw.op("vector", lambda e, b=b, c=c: e.scalar_tensor_tensor(
                                        out=yf[b][:], in0=uu[b][:], scalar=dvec[:, c:c + 1], in1=yf[b][:], op0=ALU.mult, op1=ALU.add),
                                        reads=[r_y3[b], r_sv], writes=[r_y3[b]])
                                    fw.op("scalar", lambda e, b=b, c=c: e.activation(out=zf32[:, c, :], in_=yf[b][:], func=AF.Gelu),
                                          reads=[r_y3[b]], writes=[r_z])
                                    fw.op("vector", lambda e, c=c: e.tensor_copy(out=zb16[:, c, :], in_=zf32[:, c, :]),
                                          reads=[r_z], writes=[r_z])
                                for co in range(3):
                                    j = cnt["pA"] % 4
                                    cnt["pA"] += 1
                                    for ci in range(3):
                                        fw.op("tensor", lambda e, ci=ci, co=co, j=j: e.matmul(
                                            pA[j][:], lhsT=gluw[:, ci, co * 128:(co + 1) * 128], rhs=zb16[:, ci, :],
                                            start=(ci == 0), stop=(ci == 2)), reads=[r_gluw, r_z], writes=[r_pA[j]], pe_acc=True)
                                    g2 = co % 2
                                    fw.op("vector", lambda e, j=j, g2=g2, co=co: e.tensor_scalar(
                                        out=gt[g2][:], in0=pA[j][:], scalar1=glub[:, co:co + 1], scalar2=None, op0=ALU.add),
                                        reads=[r_pA[j], r_sv], writes=[r_gt[g2]])
                                    fw.op("scalar", lambda e, g2=g2: e.activation(out=gt[g2][:], in_=gt[g2][:], func=AF.Sigmoid),
                                          reads=[r_gt[g2]], writes=[r_gt[g2]])
                                    fw.op("vector", lambda e, g2=g2, co=co, ts=ts: e.tensor_tensor(
                                        out=brT[:, co, ts * 512:(ts + 1) * 512], in0=zf32[:, co, :], in1=gt[g2][:], op=ALU.mult),
                                        reads=[r_z, r_gt[g2]], writes=[r_br])
                            cvt = sbt("cvt", [128, ST + 2], BF16)
                            r_cvt = Res("cvt")
                            accf = [sbt("accf%d" % i, [128, 512]) for i in range(2)]
                            r_accf = [Res("accf%d" % i) for i in range(2)]
                            for c in range(3):
                                wt, r_w = load_w_chunk([(wi[:, (6 + c) * 128:(7 + c) * 128], 0, 128)])
                                fw.dma("sync", lambda e, c=c, p0=p0: e.dma_start(out=cvt[:], in_=CVs[c, :, p0 - 1:p0 + ST + 1]),
                                       writes=[r_cvt])

                                def ev_gb(ts, j, pt, r_pt, c=c):
                                    a = j % 2
                                    o = ts * 512
                                    fw.op("vector", lambda e: e.tensor_scalar(out=accf[a][:], in0=cvt[:, o:o + 512],
                                                                              scalar1=cwt[:, 0, c:c + 1], scalar2=None, op0=ALU.mult),
                                          reads=[r_cvt, r_sv], writes=[r_accf[a]])
                                    for tap in (1, 2):
                                        fw.op("vector", lambda e, tap=tap: e.scalar_tensor_tensor(
                                            out=accf[a][:], in0=cvt[:, o + tap:o + tap + 512], scalar=cwt[:, tap, c:c + 1],
                                            in1=accf[a][:], op0=ALU.mult, op1=ALU.add),
                                            reads=[r_cvt, r_sv, r_accf[a]], writes=[r_accf[a]])
                                    fw.op("vector", lambda e: e.scalar_tensor_tensor(
                                        out=brT[:, 3 + c, o:o + 512], in0=accf[a][:], scalar=cbt[:, c:c + 1], in1=pt[:],
                                        op0=ALU.add, op1=ALU.mult), reads=[r_accf[a], r_sv, r_pt], writes=[r_br])
                                proj_fm(wt, r_w, ev_gb)
                            if stop_after == "branches":
                                fw.dma("sync", lambda e, t0=t0: e.dma_start(
                                    out=dbg["br"][:, :, t0:t0 + ST].rearrange("c p n -> p c n"), in_=brT[:]), reads=[r_br])
                                fw.barrier()
                                continue
                            wbr32 = [sbt("wbr32_%d" % i, [128, 3, 128]) for i in range(2)]
                            wbr = [sbt("wbr%d" % i, [128, 3, 128], BF16) for i in range(2)]
                            r_wbr = [Res("wbr%d" % i) for i in range(2)]
                            mac = sbt("mac", [128, ST])
                            r_mac = Res("mac")
                            mbf = [sbt("mbf%d" % i, [128, ST], BF16) for i in range(2)]
                            r_mbf = [Res("mbf%d" % i) for i in range(2)]
                            sgt = [sbt("sgt%d" % i, [128, 512]) for i in range(2)]
                            r_sgt = [Res("sgt%d" % i) for i in range(2)]
                            BRS = [("w_branch_a", 3, 0), ("w_branch_b", 3, 3), ("w_branch_c", 1, 6), ("w_branch_d", 3, 7)]
                            q = 0
                            for jo in range(8):
                                for br, (wn, nch, ch0) in enumerate(BRS):
                                    wg, r_wg = load_w_chunk([(wi[:, 3328 + br * 1024 + jo * 128:3328 + br * 1024 + (jo + 1) * 128], 0, 128)])
                                    wb = q % 2
                                    q += 1
                                    fw.dma("sync", lambda e, wn=wn, nch=nch, jo=jo, wb=wb: e.dma_start(
                                        out=wbr32[wb][:, 0:nch, :],
                                        in_=W[wn][l][:, jo * 128:(jo + 1) * 128].rearrange("(c p) n -> p c n", p=128)),
                                        writes=[r_wbr[wb]])
                                    fw.op("gpsimd", lambda e, nch=nch, wb=wb: e.tensor_copy(out=wbr[wb][:, 0:nch, :],
                                                                                          in_=wbr32[wb][:, 0:nch, :]),
                                          reads=[r_wbr[wb]], writes=[r_wbr[wb]])
                                    for ts in range(ST // 512):
                                        j = cnt["pA"] % 4
                                        cnt["pA"] += 1
                                        xk = (q + ts) % 2
                                        o = ts * 512
                                        for k in range(8):
                                            fw.op("tensor", lambda e, k=k, j=j, o=o, wg=wg: e.matmul(
                                                pA[j][:], lhsT=wg[:, k, :], rhs=hT[:, k, o:o + 512], start=(k == 0), stop=(k == 7)),
                                                reads=[r_wg, r_hT], writes=[r_pA[j]], pe_acc=True)
                                        for c in range(nch):
                                            fw.op("tensor", lambda e, c=c, xk=xk, o=o, wb=wb, ch0=ch0, nch=nch: e.matmul(
                                                pX[xk][:], lhsT=wbr[wb][:, c, :], rhs=brT[:, ch0 + c, o:o + 512],
                                                start=(c == 0), stop=(c == nch - 1)),
                                                reads=[r_wbr[wb], r_br], writes=[r_pX[xk]], pe_acc=True)
                                        fw.op("scalar", lambda e, j=j, xk=xk: e.activation(out=sgt[xk][:], in_=pA[j][:], func=AF.Sigmoid),
                                              reads=[r_pA[j]], writes=[r_sgt[xk]])
                                        if br == 0:
                                            fw.op("vector", lambda e, xk=xk, o=o: e.tensor_tensor(
                                                out=mac[:, o:o + 512], in0=sgt[xk][:], in1=pX[xk][:], op=ALU.mult),
                                                reads=[r_sgt[xk], r_pX[xk]], writes=[r_mac])
                                        else:
                                            fw.op("vector", lambda e, xk=xk: e.tensor_tensor(
                                                out=sgt[xk][:], in0=sgt[xk][:], in1=pX[xk][:], op=ALU.mult),
                                                reads=[r_sgt[xk], r_pX[xk]], writes=[r_sgt[xk]])
                                            fw.op("vector", lambda e, xk=xk, o=o: e.tensor_tensor(
                                                out=mac[:, o:o + 512], in0=mac[:, o:o + 512], in1=sgt[xk][:], op=ALU.add),
                                                reads=[r_sgt[xk], r_mac], writes=[r_mac])
                                mi = jo % 2
                                fw.op("scalar", lambda e, mi=mi: e.activation(out=mbf[mi][:], in_=mac[:], func=AF.Copy),
                                      reads=[r_mac], writes=[r_mbf[mi]])
                                fw.dma("sync", lambda e, jo=jo, t0=t0, mi=mi: e.dma_start(out=MTs[jo, :, t0:t0 + ST], in_=mbf[mi][:]),
                                       reads=[r_mbf[mi]])
                    fw.barrier()
                    if stop_after in ("attn", "branches"):
                        continue
                    with ExitStack() as S3:
                        def sbu(name, shape, dt=F32):
                            return S3.enter_context(nc.sbuf_tensor(uname("b4_" + name), shape, dt))
                        fw.dma("sync", lambda e, t0=t0: e.dma_start(
                            out=hT[:], in_=MTs[:, :, t0:t0 + ST].rearrange("k p n -> p k n")), writes=[r_hT])
                        wo32 = [sbu("wo32_%d" % i, [128, 8, 256]) for i in range(2)]
                        r_wo32 = [Res("wo32_%d" % i) for i in range(2)]
                        wo = sbu("wo", [128, 8, D], BF16)
                        r_wo = Res("wo")
                        for pc in range(4):
                            a = pc % 2
                            fw.dma("sync", lambda e, pc=pc, a=a: e.dma_start(
                                out=wo32[a][:], in_=W["w_o"][l][:, pc * 256:(pc + 1) * 256].rearrange("(k p) n -> p k n", p=128)),
                                writes=[r_wo32[a]])
                            fw.op("gpsimd", lambda e, pc=pc, a=a: e.tensor_copy(out=wo[:, :, pc * 256:(pc + 1) * 256], in_=wo32[a][:]),
                                  reads=[r_wo32[a]], writes=[r_wo])
                        load_gb("ln1_g", "ln1_b", l)
                        xb = [sbu("xb%d" % i, [128, D]) for i in range(2)]
                        r_xb = [Res("xb%d" % i) for i in range(2)]
                        tb = [sbu("tb%d" % i, [128, D]) for i in range(2)]
                        r_tb = [Res("tb%d" % i) for i in range(2)]
                        hb16 = [sbu("hb16_%d" % i, [128, D], BF16) for i in range(2)]
                        r_hb16 = [Res("hb16_%d" % i) for i in range(2)]
                        st4 = [sbu("st4_%d" % i, [128, 8]) for i in range(2)]
                        r_st4 = [Res("st4_%d" % i) for i in range(2)]
                        mo = [sbu("mo%d" % i, [128, D]) for i in range(2)]
                        r_mo = [Res("mo%d" % i) for i in range(2)]
                        tkb = [sbu("tkb%d" % i, [128, 8, 128], BF16) for i in range(2)]
                        r_tkb = [Res("tkb%d" % i) for i in range(2)]
                        for blk in range(ST // 128):
                            i = blk % 2
                            r0 = t0 + blk * 128
                            fw.dma("sync", lambda e, i=i, r0=r0: e.dma_start(out=xb[i][:], in_=H0[r0:r0 + 128, :]), writes=[r_xb[i]])
                            for nh in range(2):
                                j = cnt["pA"] % 4
                                cnt["pA"] += 1
                                for k in range(8):
                                    fw.op("tensor", lambda e, k=k, j=j, nh=nh, blk=blk: e.matmul(
                                        pA[j][:], lhsT=hT[:, k, blk * 128:(blk + 1) * 128], rhs=wo[:, k, nh * 512:(nh + 1) * 512],
                                        start=(k == 0), stop=(k == 7)), reads=[r_hT, r_wo], writes=[r_pA[j]], pe_acc=True)
                                fw.op("scalar", lambda e, i=i, j=j, nh=nh: e.activation(
                                    out=mo[i][:, nh * 512:(nh + 1) * 512], in_=pA[j][:], func=AF.Copy),
                                    reads=[r_pA[j]], writes=[r_mo[i]])
                            if debug:
                                fw.dma("sync", lambda e, i=i, r0=r0: e.dma_start(out=dbg["mix"][r0:r0 + 128, :], in_=mo[i][:]),
                                       reads=[r_mo[i]])
                            fw.op("vector", lambda e, i=i: e.scalar_tensor_tensor(
                                out=xb[i][:], in0=xb[i][:], scalar=ALPHA, in1=mo[i][:], op0=ALU.mult, op1=ALU.add),
                                reads=[r_xb[i], r_mo[i]], writes=[r_xb[i]])
                            fw.dma("sync", lambda e, i=i, r0=r0: e.dma_start(out=H1[r0:r0 + 128, :], in_=xb[i][:]), reads=[r_xb[i]])
                        fw.barrier()
                        for blk in range(ST // 128):
                            i = blk % 2
                            r0 = t0 + blk * 128
                            fw.dma("sync", lambda e, i=i, r0=r0: e.dma_start(out=xb[i][:], in_=H1[r0:r0 + 128, :]), writes=[r_xb[i]])
                            ln_rows(fw, xb[i], r_xb[i], tb[i], r_tb[i], st4[i], r_st4[i], gam, bet, r_gb, xb[i], r_xb[i])
                            if debug:
                                fw.dma("sync", lambda e, i=i, r0=r0: e.dma_start(out=dbg["st"][r0:r0 + 128, :], in_=st4[i][:]),
                                       reads=[r_st4[i]])
                            fw.dma("sync", lambda e, i=i, r0=r0: e.dma_start(out=H1[r0:r0 + 128, :], in_=xb[i][:]), reads=[r_xb[i]])
                            fw.op("scalar", lambda e, i=i: e.activation(out=hb16[i][:], in_=xb[i][:], func=AF.Copy),
                                  reads=[r_xb[i]], writes=[r_hb16[i]])
                            for k in range(8):
                                fw.op("tensor", lambda e, k=k, i=i: e.transpose(pT[i][:, k, :], hb16[i][:, k * 128:(k + 1) * 128], ident[:]),
                                      reads=[r_hb16[i], r_ident], writes=[r_pT[i]], pe_acc=True)
                            fw.op("vector", lambda e, i=i: e.tensor_copy(out=tkb[i][:], in_=pT[i][:]), reads=[r_pT[i]], writes=[r_tkb[i]])
                            fw.dma("sync", lambda e, i=i, r0=r0: e.dma_start(
                                out=HT[:, :, r0:r0 + 128].rearrange("k p n -> p k n"), in_=tkb[i][:]), reads=[r_tkb[i]])
                    fw.barrier()
                    if stop_after == "ln1":
                        continue
                    with ExitStack() as S4:
                        def sbv(name, shape, dt=F32):
                            return S4.enter_context(nc.sbuf_tensor(uname("b5_" + name), shape, dt))
                        fw.dma("sync", lambda e, t0=t0: e.dma_start(
                            out=hT[:], in_=HT[:, :, t0:t0 + ST].rearrange("k p n -> p k n")), writes=[r_hT])
                        wr32 = sbv("wr32", [128, 8, 20])
                        wr = sbv("wr", [128, 8, 20], BF16)
                        r_wr = Res("wr")
                        fw.dma("sync", lambda e: e.dma_start(out=wr32[:, :, 0:4], in_=W["router_group_w"][l].rearrange("(k p) n -> p k n", p=128),
                                                             allow_slow_non_contiguous=True), writes=[r_wr])
                        fw.dma("sync", lambda e: e.dma_start(out=wr32[:, :, 4:20], in_=W["router_expert_w"][l].rearrange("(k p) n -> p k n", p=128),
                                                             allow_slow_non_contiguous=True), writes=[r_wr])
                        fw.op("vector", lambda e: e.tensor_copy(out=wr[:], in_=wr32[:]), reads=[r_wr], writes=[r_wr])
                        rb = sbv("rb", [128, 20])
                        r_rb = Res("rb")
                        fw.dma("sync", lambda e: e.dma_start(out=rb[:, 0:4], in_=W["router_group_b"][l].partition_broadcast(128)), writes=[r_rb])
                        fw.dma("sync", lambda e: e.dma_start(out=rb[:, 4:20], in_=W["router_expert_b"][l].partition_broadcast(128)), writes=[r_rb])
                        comb = sbv("comb", [128, ST // 128, 16])
                        r_comb = Res("comb")
                        rt = sbv("rt", [128, 64])
                        r_rt = Res("rt")
                        for blk in range(ST // 128):
                            xk = blk % 2
                            for k in range(8):
                                fw.op("tensor", lambda e, k=k, xk=xk, blk=blk: e.matmul(
                                    pX[xk][:, 0:20], lhsT=hT[:, k, blk * 128:(blk + 1) * 128], rhs=wr[:, k, :],
                                    start=(k == 0), stop=(k == 7)), reads=[r_hT, r_wr], writes=[r_pX[xk]], pe_acc=True)
                            lg = rt[:, 0:20]

                            def V(fn, extra_r=()):
                                fw.op("vector", fn, reads=[r_rt] + list(extra_r), writes=[r_rt])
                            fw.op("vector", lambda e, xk=xk: e.tensor_tensor(out=rt[:, 0:20], in0=pX[xk][:, 0:20], in1=rb[:], op=ALU.add),
                                  reads=[r_pX[xk], r_rb], writes=[r_rt])
                            V(lambda e: e.reduce_max(out=rt[:, 20:21], in_=rt[:, 0:4], axis=AX.X))
                            V(lambda e: e.tensor_scalar(out=rt[:, 24:28], in0=rt[:, 0:4], scalar1=rt[:, 20:21], scalar2=None, op0=ALU.subtract))
                            fw.op("scalar", lambda e: e.activation(out=rt[:, 28:32], in_=rt[:, 24:28], func=AF.Exp), reads=[r_rt], writes=[r_rt])
                            V(lambda e: e.reduce_sum(out=rt[:, 21:22], in_=rt[:, 28:32], axis=AX.X))
                            V(lambda e: e.reciprocal(out=rt[:, 21:22], in_=rt[:, 21:22]))
                            V(lambda e: e.tensor_scalar(out=rt[:, 24:28], in0=rt[:, 24:28], scalar1=-1e30, scalar2=1.0, op0=ALU.mult, op1=ALU.min))
                            V(lambda e: e.tensor_scalar(out=rt[:, 24:28], in0=rt[:, 24:28], scalar1=-1.0, scalar2=1.0, op0=ALU.mult, op1=ALU.add))
                            V(lambda e: e.tensor_scalar(out=rt[:, 32:36], in0=rt[:, 4:8], scalar1=rt[:, 24:25], scalar2=None, op0=ALU.mult))
                            for gq in range(1, 4):
                                V(lambda e, gq=gq: e.scalar_tensor_tensor(out=rt[:, 32:36], in0=rt[:, 4 + 4 * gq:8 + 4 * gq],
                                                                          scalar=rt[:, 24 + gq:25 + gq], in1=rt[:, 32:36],
                                                                          op0=ALU.mult, op1=ALU.add))
                            V(lambda e: e.reduce_max(out=rt[:, 22:23], in_=rt[:, 32:36], axis=AX.X))
                            V(lambda e: e.tensor_scalar(out=rt[:, 36:40], in0=rt[:, 32:36], scalar1=rt[:, 22:23], scalar2=None, op0=ALU.subtract))
                            V(lambda e: e.tensor_scalar(out=rt[:, 36:40], in0=rt[:, 36:40], scalar1=-1e30, scalar2=1.0, op0=ALU.mult, op1=ALU.min))
                            V(lambda e: e.tensor_scalar(out=rt[:, 36:40], in0=rt[:, 36:40], scalar1=-1.0, scalar2=1.0, op0=ALU.mult, op1=ALU.add))
                            V(lambda e: e.scalar_tensor_tensor(out=rt[:, 40:44], in0=rt[:, 36:40], scalar=-1e4, in1=rt[:, 32:36],
                                                               op0=ALU.mult, op1=ALU.add))
                            V(lambda e: e.reduce_max(out=rt[:, 23:24], in_=rt[:, 40:44], axis=AX.X))
                            V(lambda e: e.tensor_scalar(out=rt[:, 44:48], in0=rt[:, 40:44], scalar1=rt[:, 23:24], scalar2=None, op0=ALU.subtract))
                            V(lambda e: e.tensor_scalar(out=rt[:, 44:48], in0=rt[:, 44:48], scalar1=-1e30, scalar2=1.0, op0=ALU.mult, op1=ALU.min))
                            V(lambda e: e.tensor_scalar(out=rt[:, 44:48], in0=rt[:, 44:48], scalar1=-1.0, scalar2=1.0, op0=ALU.mult, op1=ALU.add))
                            V(lambda e: e.tensor_tensor(out=rt[:, 48:49], in0=rt[:, 23:24], in1=rt[:, 22:23], op=ALU.subtract))
                            fw.op("scalar", lambda e: e.activation(out=rt[:, 49:50], in_=rt[:, 48:49], func=AF.Exp), reads=[r_rt], writes=[r_rt])
                            V(lambda e: e.tensor_scalar(out=rt[:, 50:51], in0=rt[:, 49:50], scalar1=1.0, scalar2=None, op0=ALU.add))
                            V(lambda e: e.reciprocal(out=rt[:, 50:51], in_=rt[:, 50:51]))
                            V(lambda e: e.tensor_tensor(out=rt[:, 51:52], in0=rt[:, 49:50], in1=rt[:, 50:51], op=ALU.mult))
                            V(lambda e: e.tensor_tensor(out=rt[:, 50:51], in0=rt[:, 50:51], in1=rt[:, 21:22], op=ALU.mult))
                            V(lambda e: e.tensor_tensor(out=rt[:, 51:52], in0=rt[:, 51:52], in1=rt[:, 21:22], op=ALU.mult))
                            V(lambda e: e.tensor_scalar(out=rt[:, 52:56], in0=rt[:, 36:40], scalar1=rt[:, 50:51], scalar2=None, op0=ALU.mult))
                            V(lambda e: e.scalar_tensor_tensor(out=rt[:, 52:56], in0=rt[:, 44:48], scalar=rt[:, 51:52], in1=rt[:, 52:56],
                                                               op0=ALU.mult, op1=ALU.add))
                            for gq in range(4):
                                fw.op("vector", lambda e, gq=gq, blk=blk: e.tensor_scalar(
                                    out=comb[:, blk, 4 * gq:4 * gq + 4], in0=rt[:, 52:56], scalar1=rt[:, 24 + gq:25 + gq], scalar2=None,
                                    op0=ALU.mult), reads=[r_rt], writes=[r_comb])
                        wg32 = sbv("wg32", [128, 8, 256])
                        wu32 = sbv("wu32", [128, 8, 256])
                        wgb = sbv("wgb", [128, 8, 256], BF16)
                        wub = sbv("wub", [128, 8, 256], BF16)
                        wd32 = sbv("wd32", [128, 2, D])
                        wdb = sbv("wdb", [128, 2, D], BF16)
                        r_wg32, r_wu32, r_wd32 = Res("wg32"), Res("wu32"), Res("wd32")
                        r_wgb, r_wub, r_wdb = Res("wgb"), Res("wub"), Res("wdb")
                        HB = ST // 256
                        macc = sbv("macc", [128, HB, D])
                        r_macc = Res("macc")
                        actT = sbv("actT", [128, 2, ST // 2], BF16)
                        r_act = Res("actT")
                        sgl = [sbv("sgl%d" % i, [128, 512]) for i in range(2)]
                        r_sgl = [Res("sgl%d" % i) for i in range(2)]
                        xq = [sbv("xq%d" % i, [128, D]) for i in range(2)]
                        r_xq = [Res("xq%d" % i) for i in range(2)]
                        tq = [sbv("tq%d" % i, [128, D]) for i in range(2)]
                        r_tq = [Res("tq%d" % i) for i in range(2)]
                        sq4 = [sbv("sq4_%d" % i, [128, 8]) for i in range(2)]
                        r_sq4 = [Res("sq4_%d" % i) for i in range(2)]
                        load_gb("ln2_g", "ln2_b", l)
                        for hv in range(2):
                            c0 = hv * (ST // 2)
                            for ex in range(16):
                                fw.dma("sync", lambda e, ex=ex: e.dma_start(
                                    out=wg32[:], in_=W["expert_w_gate"][l, ex].rearrange("(k p) n -> p k n", p=128)), writes=[r_wg32])
                                fw.op("gpsimd", lambda e: e.tensor_copy(out=wgb[:], in_=wg32[:]), reads=[r_wg32], writes=[r_wgb])
                                fw.dma("sync", lambda e, ex=ex: e.dma_start(
                                    out=wu32[:], in_=W["expert_w_up"][l, ex].rearrange("(k p) n -> p k n", p=128)), writes=[r_wu32])
                                fw.op("gpsimd", lambda e: e.tensor_copy(out=wub[:], in_=wu32[:]), reads=[r_wu32], writes=[r_wub])
                                fw.dma("sync", lambda e, ex=ex: e.dma_start(
                                    out=wd32[:], in_=W["expert_w_down"][l, ex].rearrange("(c p) n -> p c n", p=128)), writes=[r_wd32])
                                fw.op("gpsimd", lambda e: e.tensor_copy(out=wdb[:], in_=wd32[:]), reads=[r_wd32], writes=[r_wdb])
                                for c in range(2):
                                    for ts in range(ST // 1024):
                                        o = c0 + ts * 512
                                        j = cnt["pA"] % 4
                                        cnt["pA"] += 1
                                        j2 = cnt["pA"] % 4
                                        cnt["pA"] += 1
                                        for k in range(8):
                                            fw.op("tensor", lambda e, k=k, j=j, c=c, o=o: e.matmul(
                                                pA[j][:], lhsT=wgb[:, k, c * 128:(c + 1) * 128], rhs=hT[:, k, o:o + 512],
                                                start=(k == 0), stop=(k == 7)), reads=[r_wgb, r_hT], writes=[r_pA[j]], pe_acc=True)
                                        for k in range(8):
                                            fw.op("tensor", lambda e, k=k, j2=j2, c=c, o=o: e.matmul(
                                                pA[j2][:], lhsT=wub[:, k, c * 128:(c + 1) * 128], rhs=hT[:, k, o:o + 512],
                                                start=(k == 0), stop=(k == 7)), reads=[r_wub, r_hT], writes=[r_pA[j2]], pe_acc=True)
                                        si = (c + ts) % 2
                                        fw.op("scalar", lambda e, j=j, si=si: e.activation(out=sgl[si][:], in_=pA[j][:], func=AF.Silu),
                                              reads=[r_pA[j]], writes=[r_sgl[si]])
                                        fw.op("vector", lambda e, j2=j2, si=si, c=c, ts=ts: e.tensor_tensor(
                                            out=actT[:, c, ts * 512:(ts + 1) * 512], in0=sgl[si][:], in1=pA[j2][:], op=ALU.mult),
                                            reads=[r_sgl[si], r_pA[j2]], writes=[r_act])
                                for bl in range(HB):
                                    blk = hv * HB + bl
                                    for nh in range(2):
                                        xk = (bl * 2 + nh) % 2
                                        for c in range(2):
                                            fw.op("tensor", lambda e, c=c, xk=xk, bl=bl, nh=nh: e.matmul(
                                                pX[xk][:], lhsT=actT[:, c, bl * 128:(bl + 1) * 128], rhs=wdb[:, c, nh * 512:(nh + 1) * 512],
                                                start=(c == 0), stop=(c == 1)), reads=[r_act, r_wdb], writes=[r_pX[xk]], pe_acc=True)
                                        if ex == 0:
                                            fw.op("vector", lambda e, xk=xk, bl=bl, nh=nh, blk=blk, ex=ex: e.tensor_scalar(
                                                out=macc[:, bl, nh * 512:(nh + 1) * 512], in0=pX[xk][:], scalar1=comb[:, blk, ex:ex + 1],
                                                scalar2=None, op0=ALU.mult), reads=[r_pX[xk], r_comb], writes=[r_macc])
                                        else:
                                            fw.op("vector", lambda e, xk=xk, bl=bl, nh=nh, blk=blk, ex=ex: e.scalar_tensor_tensor(
                                                out=macc[:, bl, nh * 512:(nh + 1) * 512], in0=pX[xk][:], scalar=comb[:, blk, ex:ex + 1],
                                                in1=macc[:, bl, nh * 512:(nh + 1) * 512], op0=ALU.mult, op1=ALU.add),
                                                reads=[r_pX[xk], r_comb, r_macc], writes=[r_macc])
                            dst = y if last else H0
                            for bl in range(HB):
                                i = bl % 2
                                r0 = t0 + (hv * HB + bl) * 128
                                fw.dma("sync", lambda e, i=i, r0=r0: e.dma_start(out=xq[i][:], in_=H1[r0:r0 + 128, :]), writes=[r_xq[i]])
                                fw.op("vector", lambda e, i=i, bl=bl: e.scalar_tensor_tensor(
                                    out=xq[i][:], in0=xq[i][:], scalar=ALPHA, in1=macc[:, bl, :], op0=ALU.mult, op1=ALU.add),
                                    reads=[r_xq[i], r_macc], writes=[r_xq[i]])
                                ln_rows(fw, xq[i], r_xq[i], tq[i], r_tq[i], sq4[i], r_sq4[i], gam, bet, r_gb, xq[i], r_xq[i])
                                fw.dma("sync", lambda e, i=i, r0=r0, dst=dst: e.dma_start(out=dst[r0:r0 + 128, :], in_=xq[i][:]),
                                       reads=[r_xq[i]])
                    fw.barrier()
            fw.barrier()

        if debug:
            dbg["br"] = nc.dram_tensor("dbg_br", [10, 128, NTOK], BF16, kind="ExternalOutput").ap()
        for l in range(depth):
            phase_a(l)
            if stop_after in ("a", "ua"):
                break
            phase_s5(l)
            if stop_after == "s5":
                break
            phase_h()
            phase_b(l, l == depth - 1)
            if stop_after in ("attn", "branches", "ln1"):
                break
    return nc, fw


def ln_rows(fw, xin, r_xin, tmp, r_tmp, s, r_s, gam, bet, r_gb, out_tile, r_out):
    fw.op("vector", lambda e: e.reduce_sum(out=s[:, 0:1], in_=xin[:], axis=AX.X), reads=[r_xin], writes=[r_s])
    fw.op("scalar", lambda e: e.activation(out=tmp[:], in_=xin[:], func=AF.Square), reads=[r_xin, r_s], writes=[r_tmp])
    fw.op("vector", lambda e: e.reduce_sum(out=s[:, 1:2], in_=tmp[:], axis=AX.X), reads=[r_tmp], writes=[r_s])
    fw.op("vector", lambda e: e.tensor_scalar(out=s[:, 2:3], in0=s[:, 0:1], scalar1=1.0 / D, scalar2=None,
                                              op0=ALU.mult), reads=[r_s], writes=[r_s])
    fw.op("vector", lambda e: e.tensor_tensor(out=s[:, 3:4], in0=s[:, 2:3], in1=s[:, 2:3], op=ALU.mult),
          reads=[r_s], writes=[r_s])
    fw.op("vector", lambda e: e.scalar_tensor_tensor(out=s[:, 4:5], in0=s[:, 1:2], scalar=1.0 / D, in1=s[:, 3:4],
                                                     op0=ALU.mult, op1=ALU.subtract), reads=[r_s], writes=[r_s])
    fw.op("vector", lambda e: e.tensor_scalar(out=s[:, 4:5], in0=s[:, 4:5], scalar1=LN_EPS, scalar2=None,
                                              op0=ALU.add), reads=[r_s], writes=[r_s])
    fw.op("scalar", lambda e: e.activation(out=s[:, 5:6], in_=s[:, 4:5], func=AF.Sqrt), reads=[r_s], writes=[r_s])
    fw.op("vector", lambda e: e.reciprocal(out=s[:, 6:7], in_=s[:, 5:6]), reads=[r_s], writes=[r_s])
    fw.op("vector", lambda e: e.scalar_tensor_tensor(out=s[:, 7:8], in0=s[:, 2:3], scalar=-1.0, in1=s[:, 6:7],
                                                     op0=ALU.mult, op1=ALU.mult), reads=[r_s], writes=[r_s])
    fw.op("vector", lambda e: e.tensor_scalar(out=tmp[:], in0=xin[:], scalar1=s[:, 2:3], scalar2=s[:, 6:7],
                                              op0=ALU.subtract, op1=ALU.mult), reads=[r_xin, r_s], writes=[r_tmp])
    fw.op("vector", lambda e: e.tensor_tensor(out=tmp[:], in0=tmp[:], in1=gam[:], op=ALU.mult),
          reads=[r_tmp, r_gb], writes=[r_tmp])
    fw.op("vector", lambda e: e.tensor_tensor(out=out_tile[:], in0=tmp[:], in1=bet[:], op=ALU.add),
          reads=[r_tmp, r_gb], writes=[r_out])


_CACHE = {}


def kernel(**inputs):
    xp = np.ascontiguousarray(inputs["x_prompt"], dtype=np.float32)
    xs = np.ascontiguousarray(inputs["x_sample"], dtype=np.float32)
    slots = {0: [("s", 0)], 1: [("s", 1)], 2: [("p", 0), ("p", 1)], 3: [("p", 2), ("p", 3)],
             4: [("p", 4)], 5: [("p", 5)], 6: [("p", 6)], 7: [("p", 7)]}
    in_maps = []
    for c in range(8):
        xc = np.zeros((NTOK, D), np.float32)
        fl = np.zeros(5, np.float32)
        if slots[c][0][0] == "s":
            xc[:] = xs[slots[c][0][1]]
            fl[1:4] = 1.0
        else:
            for j, (_, pi) in enumerate(slots[c]):
                xc[j * SEG:(j + 1) * SEG] = xp[pi]
        m = {"x": xc, "flags": fl}
        for n in WNAMES:
            m[n] = np.ascontiguousarray(inputs[n], dtype=np.float32)
        in_maps.append(m)
    if "nc" not in _CACHE:
        nc, fw = build()
        fw.finish(_CACHE.get("final", []))
        _CACHE["nc"] = nc
    res = run_bass_kernel_spmd(_CACHE["nc"], in_maps, core_ids=list(range(8)))
    yp = np.zeros_like(xp)
    ys = np.zeros_like(xs)
    for c in range(8):
        yc = np.asarray(res.results[c]["y"], dtype=np.float32)
        if slots[c][0][0] == "s":
            ys[slots[c][0][1]] = yc
        else:
            for j, (_, pi) in enumerate(slots[c]):
                yp[pi] = yc[j * SEG:(j + 1) * SEG]
    return (yp, ys)
```

```python
import math
import numpy as np
from contextlib import ExitStack
import concourse.bass as bass
import concourse.mybir as mybir
from concourse.bass_utils import run_bass_kernel_spmd

F32 = mybir.dt.float32
BF16 = mybir.dt.bfloat16
AF = mybir.ActivationFunctionType
ALU = mybir.AluOpType
AX = mybir.AxisListType

D = 1024
NSEG = 4
SEG = 4096
NTOK = NSEG * SEG
PAD = 1024
SEGP = SEG + 2 * PAD
NTOKP = NSEG * SEGP
ST = 2048
NST = NTOK // ST
DEPTH = 2
ALPHA = (2 * DEPTH) ** 0.25
LN_EPS = 1e-5
IN_COLS = 7424
SLOPES = [2.0 ** (-8.0 * (i + 1) / 12) for i in range(12)]
DIL = [(128, 1), (512, 4), (2048, 16)]

WNAMES = ["ln_in_g", "ln_in_b", "w_in", "s5_a_re", "s5_a_im", "s5_log_dt", "s5_b_re", "s5_b_im", "s5_c_re",
          "s5_c_im", "s5_d", "s5_glu_w", "s5_glu_b", "conv_w", "conv_b", "swa_sink", "w_branch_a", "w_branch_b",
          "w_branch_c", "w_branch_d", "w_o", "ln1_g", "ln1_b", "router_group_w", "router_group_b",
          "router_expert_w", "router_expert_b", "expert_w_gate", "expert_w_up", "expert_w_down", "ln2_g", "ln2_b"]
WSHAPES = {
    "ln_in_g": [D], "ln_in_b": [D], "w_in": [2, D, IN_COLS], "s5_a_re": [2, 2, 24, 64], "s5_a_im": [2, 2, 24, 64],
    "s5_log_dt": [2, 2, 24], "s5_b_re": [2, 2, 24, 64, 16], "s5_b_im": [2, 2, 24, 64, 16],
    "s5_c_re": [2, 2, 24, 16, 64], "s5_c_im": [2, 2, 24, 16, 64], "s5_d": [2, 384], "s5_glu_w": [2, 384, 384],
    "s5_glu_b": [2, 384], "conv_w": [2, 3, 384], "conv_b": [2, 384], "swa_sink": [2, 6],
    "w_branch_a": [2, 384, D], "w_branch_b": [2, 384, D], "w_branch_c": [2, 128, D], "w_branch_d": [2, 384, D],
    "w_o": [2, D, D], "ln1_g": [2, D], "ln1_b": [2, D], "router_group_w": [2, D, 4], "router_group_b": [2, 4],
    "router_expert_w": [2, D, 16], "router_expert_b": [2, 16], "expert_w_gate": [2, 16, D, 256],
    "expert_w_up": [2, 16, D, 256], "expert_w_down": [2, 16, 256, D], "ln2_g": [2, D], "ln2_b": [2, D],
}


ENGS = ["sync", "scalar", "vector", "gpsimd", "tensor"]
SEM_ROLL = 30000


class Res:
    __slots__ = ("name", "w", "r")

    def __init__(self, name):
        self.name = name
        self.w = None
        self.r = []


class FW:
    def __init__(self, nc, es):
        self.nc = nc
        self.es = es
        self.q = {e: [] for e in ENGS}
        self.sems = {e: [es.enter_context(nc.semaphore("s_" + e + "0"))] for e in ENGS}
        self.cnt = {e: 0 for e in ENGS}
        self.seen = {e: {} for e in ENGS}
        self.dma_sems = [es.enter_context(nc.semaphore("d%d" % i)) for i in range(24)]
        self.dma_cnt = [0] * 24
        self.dma_i = 0
        self.n_ops = 0
        self.fence = []

    def barrier(self):
        f = []
        for e in ENGS:
            if self.cnt[e] > 0:
                f.append((self.sems[e][-1], self.cnt[e], e))
        for k in range(len(self.dma_sems)):
            if self.dma_cnt[k] > 0:
                f.append((self.dma_sems[k], self.dma_cnt[k], "dma"))
        self.fence = f

    def _ev_new(self, eng):
        if self.cnt[eng] >= SEM_ROLL:
            self.sems[eng].append(self.es.enter_context(self.nc.semaphore("s_%s%d" % (eng, len(self.sems[eng])))))
            self.cnt[eng] = 0
        self.cnt[eng] += 1
        return (self.sems[eng][-1], self.cnt[eng], eng)

    def _need(self, eng, ev, waits, pe_ok=False):
        if ev is None:
            return
        sem, val, src = ev
        if pe_ok and src == "tensor" and eng == "tensor":
            return
        key = id(sem)
        if self.seen[eng].get(key, 0) >= val:
            return
        if key not in waits or waits[key][1] < val:
            waits[key] = (sem, val)

    def op(self, eng, fn, reads=(), writes=(), pe_acc=False):
        waits = {}
        for ev in self.fence:
            self._need(eng, ev, waits)
        for r in reads:
            self._need(eng, r.w, waits)
        for w in writes:
            self._need(eng, w.w, waits, pe_ok=pe_acc)
            for ev in w.r:
                self._need(eng, ev, waits)
        for key, (sem, val) in waits.items():
            self.seen[eng][key] = val
        ev = self._ev_new(eng)
        self.q[eng].append((list(waits.values()), fn, (ev[0], 1)))
        for r in reads:
            r.r.append(ev)
        for w in writes:
            w.w = ev
            w.r = []
        self.n_ops += 1
        return ev

    def dma(self, eng, fn, reads=(), writes=()):
        if len(writes) == 0 and eng == "sync":
            eng = "scalar"
        waits = {}
        for ev in self.fence:
            self._need(eng, ev, waits)
        for r in reads:
            self._need(eng, r.w, waits)
        for w in writes:
            self._need(eng, w.w, waits)
            for ev in w.r:
                self._need(eng, ev, waits)
        k = self.dma_i % len(self.dma_sems)
        self.dma_i += 1
        sem = self.dma_sems[k]
        if self.dma_cnt[k] > 0:
            self._need(eng, (sem, self.dma_cnt[k], "dma"), waits)
        for key, (s, val) in waits.items():
            self.seen[eng][key] = val
        self.dma_cnt[k] += 16
        ev = (sem, self.dma_cnt[k], "dma")
        self.q[eng].append((list(waits.values()), fn, (sem, 16)))
        for r in reads:
            r.r.append(ev)
        for w in writes:
            w.w = ev
            w.r = []
        self.n_ops += 1
        return ev

    def finish(self, final_res):
        waits = {}
        for r in final_res:
            self._need("sync", r.w, waits)
        for k in range(len(self.dma_sems)):
            if self.dma_cnt[k] > 0:
                self._need("sync", (self.dma_sems[k], self.dma_cnt[k], "dma"), waits)
        tail = list(waits.values())
        q = self.q
        with self.nc.Block() as block:
            def replay(e, name):
                for ws, fn, inc in q[name]:
                    for sem, val in ws:
                        e.wait_ge(sem, val)
                    fn(e).then_inc(inc[0], inc[1])
                if name == "sync":
                    for sem, val in tail:
                        e.wait_ge(sem, val)

            @block.sync
            def _(e):
                replay(e, "sync")

            @block.scalar
            def _(e):
                replay(e, "scalar")

            @block.vector
            def _(e):
                replay(e, "vector")

            @block.gpsimd
            def _(e):
                replay(e, "gpsimd")

            @block.tensor
            def _(e):
                replay(e, "tensor")


def build(debug=False, stop_after=None, depth=DEPTH):
    nc = bass.Bass("TRN2", target_bir_lowering=False)
    x = nc.dram_tensor("x", [NTOK, D], F32, kind="ExternalInput").ap()
    flags_d = nc.dram_tensor("flags", [5], F32, kind="ExternalInput").ap()
    W = {n: nc.dram_tensor(n, WSHAPES[n], F32, kind="ExternalInput").ap() for n in WNAMES}
    y = nc.dram_tensor("y", [NTOK, D], F32, kind="ExternalOutput").ap()
    H0 = nc.dram_tensor("H0", [NTOK, D], F32).ap()
    H1 = nc.dram_tensor("H1", [NTOK, D], F32, kind=("ExternalOutput" if debug else "Internal")).ap()
    HT = nc.dram_tensor("HT", [8, 128, NTOK], BF16).ap()
    UAs = nc.dram_tensor("UAs", [3, 128, NTOK], F32).ap()
    CVs = nc.dram_tensor("CVs", [3, 128, NTOKP], BF16).ap()
    KTs = nc.dram_tensor("KTs", [4, 128, NTOKP], BF16).ap()
    VTs = nc.dram_tensor("VTs", [4, 128, NTOKP], BF16).ap()
    MTs = nc.dram_tensor("MTs", [8, 128, NTOK], BF16, kind=("ExternalOutput" if debug else "Internal")).ap()
    YA = nc.dram_tensor("YA", [2, 3, 128, NTOK], F32, kind=("ExternalOutput" if debug else "Internal")).ap()
    dbg = {}
    if debug:
        dbg["h0"] = nc.dram_tensor("dbg_h0", [NTOK, D], F32, kind="ExternalOutput").ap()
        dbg["ua"] = nc.dram_tensor("dbg_ua", [3, 128, NTOK], F32, kind="ExternalOutput").ap()
        dbg["mix"] = nc.dram_tensor("dbg_mix", [NTOK, D], F32, kind="ExternalOutput").ap()
        dbg["st"] = nc.dram_tensor("dbg_st", [NTOK, 8], F32, kind="ExternalOutput").ap()

    es = ExitStack()
    with es:
        fw = FW(nc, es)

        def sb(name, shape, dt=F32):
            return es.enter_context(nc.sbuf_tensor(name, shape, dt))

        def ps(name, shape, dt=F32):
            return es.enter_context(nc.psum_tensor(name, shape, dt))

        ident = sb("ident", [128, 128], BF16)
        r_ident = Res("ident")
        fw.op("gpsimd", lambda e: e.memset(ident[:], 0.0), writes=[r_ident])
        fw.op("gpsimd", lambda e: e.affine_select(out=ident[:], in_=ident[:], pattern=[[-1, 128]],
                                                  compare_op=ALU.not_equal, fill=1.0, base=0, channel_multiplier=1),
              reads=[r_ident], writes=[r_ident])
        flg = sb("flg", [128, 5])
        r_flg = Res("flg")
        fw.dma("sync", lambda e: e.dma_start(out=flg[:], in_=flags_d.partition_broadcast(128)), writes=[r_flg])
        gam = sb("gam", [128, D])
        bet = sb("bet", [128, D])
        r_gb = Res("gb")

        def load_gb(gname, bname, l):
            gsrc = W[gname] if l is None else W[gname][l]
            bsrc = W[bname] if l is None else W[bname][l]
            fw.dma("sync", lambda e: e.dma_start(out=gam[:], in_=gsrc.partition_broadcast(128)), writes=[r_gb])
            fw.dma("sync", lambda e: e.dma_start(out=bet[:], in_=bsrc.partition_broadcast(128)), writes=[r_gb])

        pT = [ps("pT%d" % i, [128, 8, 128], BF16) for i in range(2)]
        r_pT = [Res("pT%d" % i) for i in range(2)]
        pA = [ps("pA%d" % i, [128, 512]) for i in range(4)]
        r_pA = [Res("pA%d" % i) for i in range(4)]
        pX = [ps("pX%d" % i, [128, 512]) for i in range(2)]
        r_pX = [Res("pX%d" % i) for i in range(2)]
        cnt = {"w": 0, "pA": 0, "blk": 0, "uid": 0}

        def uname(n):
            cnt["uid"] += 1
            return "%s_%d" % (n, cnt["uid"])

        def padpos(t):
            return (t // SEG) * SEGP + PAD + (t % SEG)

        def phase_a(l):
            with ExitStack() as pes:
                def sb2(name, shape, dt=F32):
                    return pes.enter_context(nc.sbuf_tensor(uname("a_" + name), shape, dt))
                hT = sb2("hT", [128, 8, ST], BF16)
                r_hT = Res("hT")
                xb = [sb2("xb%d" % i, [128, D]) for i in range(2)]
                r_xb = [Res("xb%d" % i) for i in range(2)]
                tb = [sb2("tb%d" % i, [128, D]) for i in range(2)]
                r_tb = [Res("tb%d" % i) for i in range(2)]
                hb16 = [sb2("hb16_%d" % i, [128, D], BF16) for i in range(2)]
                r_hb16 = [Res("hb16_%d" % i) for i in range(2)]
                st4 = [sb2("st4_%d" % i, [128, 8]) for i in range(2)]
                r_st4 = [Res("st4_%d" % i) for i in range(2)]
                wst = [sb2("wst%d" % i, [128, 8, 128]) for i in range(2)]
                r_wst = [Res("wst%d" % i) for i in range(2)]
                wbf = [sb2("wbf%d" % i, [128, 8, 128], BF16) for i in range(2)]
                r_wbf = [Res("wbf%d" % i) for i in range(2)]
                zt0 = sb2("zt0", [128, ST], BF16)
                r_zt0 = Res("zt0")
                zf = [sb2("zf%d" % i, [128, 512]) for i in range(4)]
                r_zf = [Res("zf%d" % i) for i in range(4)]
                zb = [sb2("zb%d" % i, [128, 512], BF16) for i in range(4)]
                r_zb = [Res("zb%d" % i) for i in range(4)]

                def layer_norm_block(i):
                    ln_rows(fw, xb[i], r_xb[i], tb[i], r_tb[i], st4[i], r_st4[i], gam, bet, r_gb, xb[i], r_xb[i])

                def transpose_block(i, col0):
                    fw.op("scalar", lambda e: e.activation(out=hb16[i][:], in_=xb[i][:], func=AF.Copy),
                          reads=[r_xb[i]], writes=[r_hb16[i]])
                    for k in range(8):
                        fw.op("tensor", lambda e, k=k: e.transpose(pT[i][:, k, :], hb16[i][:, k * 128:(k + 1) * 128],
                                                                   ident[:]),
                              reads=[r_hb16[i], r_ident], writes=[r_pT[i]], pe_acc=True)
                    fw.op("vector", lambda e: e.tensor_copy(out=hT[:, :, col0:col0 + 128], in_=pT[i][:]),
                          reads=[r_pT[i]], writes=[r_hT])

                def load_w_chunk(src_ap):
                    j = cnt["w"] % 2
                    cnt["w"] += 1
                    fw.dma("sync", lambda e: e.dma_start(out=wst[j][:], in_=src_ap.rearrange("(k p) n -> p k n", p=128)),
                           writes=[r_wst[j]])
                    fw.op("gpsimd", lambda e: e.tensor_copy(out=wbf[j][:], in_=wst[j][:]),
                          reads=[r_wst[j]], writes=[r_wbf[j]])
                    return wbf[j], r_wbf[j]

                def proj_fm(wt, r_w, evac):
                    for ts in range(ST // 512):
                        j = cnt["pA"] % 4
                        cnt["pA"] += 1
                        for k in range(8):
                            fw.op("tensor", lambda e, k=k, j=j, ts=ts: e.matmul(
                                pA[j][:], lhsT=wt[:, k, :], rhs=hT[:, k, ts * 512:(ts + 1) * 512],
                                start=(k == 0), stop=(k == 7)),
                                reads=[r_w, r_hT], writes=[r_pA[j]], pe_acc=True)
                        evac(ts, j, pA[j], r_pA[j])

                if l == 0:
                    load_gb("ln_in_g", "ln_in_b", None)
                for st_i in range(NST):
                    t0 = st_i * ST
                    p0 = padpos(t0)
                    for b in range(ST // 128):
                        i = cnt["blk"] % 2
                        cnt["blk"] += 1
                        r0 = t0 + b * 128
                        if l == 0:
                            fw.dma("sync", lambda e, i=i, r0=r0: e.dma_start(out=xb[i][:], in_=x[r0:r0 + 128, :]),
                                   writes=[r_xb[i]])
                            layer_norm_block(i)
                            fw.dma("sync", lambda e, i=i, r0=r0: e.dma_start(out=H0[r0:r0 + 128, :], in_=xb[i][:]),
                                   reads=[r_xb[i]])
                            if debug:
                                fw.dma("sync", lambda e, i=i, r0=r0: e.dma_start(out=dbg["h0"][r0:r0 + 128, :],
                                                                                in_=xb[i][:]), reads=[r_xb[i]])
                        else:
                            fw.dma("sync", lambda e, i=i, r0=r0: e.dma_start(out=xb[i][:], in_=H0[r0:r0 + 128, :]),
                                   writes=[r_xb[i]])
                        transpose_block(i, b * 128)
                    fw.dma("sync", lambda e, t0=t0: e.dma_start(out=HT[:, :, t0:t0 + ST].rearrange("k p n -> p k n"),
                                                                in_=hT[:]), reads=[r_hT])

                    def store_plain(dst, c, t0=t0):
                        def ev(ts, j, pt, r_pt):
                            fw.op("scalar", lambda e: e.activation(out=zf[j][:], in_=pt[:], func=AF.Copy),
                                  reads=[r_pt], writes=[r_zf[j]])
                            fw.dma("sync", lambda e: e.dma_start(out=dst[c, :, t0 + ts * 512:t0 + (ts + 1) * 512],
                                                                 in_=zf[j][:]), reads=[r_zf[j]])
                            if debug and dst is UAs:
                                fw.dma("sync", lambda e: e.dma_start(
                                    out=dbg["ua"][c, :, t0 + ts * 512:t0 + (ts + 1) * 512], in_=zf[j][:]),
                                    reads=[r_zf[j]])
                        return ev

                    def store_pad(dst, c, p0=p0):
                        def ev(ts, j, pt, r_pt):
                            fw.op("scalar", lambda e: e.activation(out=zb[j][:], in_=pt[:], func=AF.Copy),
                                  reads=[r_pt], writes=[r_zb[j]])
                            fw.dma("sync", lambda e: e.dma_start(out=dst[c, :, p0 + ts * 512:p0 + (ts + 1) * 512],
                                                                 in_=zb[j][:]), reads=[r_zb[j]])
                        return ev

                    wi = W["w_in"][l]
                    for c in range(3):
                        wt, r_w = load_w_chunk(wi[:, c * 128:(c + 1) * 128])
                        proj_fm(wt, r_w, store_plain(UAs, c))
                    if stop_after == "ua":
                        continue
                    for c in range(3):
                        wt, r_w = load_w_chunk(wi[:, (15 + c) * 128:(16 + c) * 128])
                        proj_fm(wt, r_w, store_pad(KTs, c))
                    wt, r_w = load_w_chunk(wi[:, 24 * 128:25 * 128])
                    proj_fm(wt, r_w, store_pad(KTs, 3))
                    for c in range(3):
                        wt, r_w = load_w_chunk(wi[:, (18 + c) * 128:(19 + c) * 128])
                        proj_fm(wt, r_w, store_pad(VTs, c))
                    wt, r_w = load_w_chunk(wi[:, 25 * 128:26 * 128])
                    proj_fm(wt, r_w, store_pad(VTs, 3))
                    for c in range(3):
                        wt, r_w = load_w_chunk(wi[:, (3 + c) * 128:(4 + c) * 128])

                        def ev_vb(ts, j, pt, r_pt):
                            fw.op("scalar", lambda e: e.activation(out=zt0[:, ts * 512:(ts + 1) * 512], in_=pt[:],
                                                                   func=AF.Copy), reads=[r_pt], writes=[r_zt0])
                        proj_fm(wt, r_w, ev_vb)
                        wt, r_w = load_w_chunk(wi[:, (9 + c) * 128:(10 + c) * 128])

                        def ev_gc(ts, j, pt, r_pt, c=c, p0=p0):
                            fw.op("vector", lambda e: e.tensor_tensor(out=zb[j][:], in0=pt[:],
                                                                      in1=zt0[:, ts * 512:(ts + 1) * 512], op=ALU.mult),
                                  reads=[r_pt, r_zt0], writes=[r_zb[j]])
                            fw.dma("sync", lambda e: e.dma_start(out=CVs[c, :, p0 + ts * 512:p0 + (ts + 1) * 512],
                                                                 in_=zb[j][:]), reads=[r_zb[j]])
                        proj_fm(wt, r_w, ev_gc)
            fw.barrier()

        def phase_s5(l):
            with ExitStack() as pes:
                def sb2(name, shape, dt=F32):
                    return pes.enter_context(nc.sbuf_tensor(uname("s_" + name), shape, dt))
                r_p = Res("prm")
                names = ["are", "aim", "ldt", "dt", "rho", "th", "c", "s", "t1", "t2", "lr", "li", "nr", "den",
                         "numr", "numi", "kr", "ki", "nki"]
                P = {n: sb2(n, [128, 24]) for n in names}

                def tt(o, a, b, op):
                    fw.op("vector", lambda e: e.tensor_tensor(out=P[o][:], in0=P[a][:], in1=P[b][:], op=op),
                          reads=[r_p], writes=[r_p])

                def ts_(o, a, s1, op0, s2=None, op1=None):
                    if op1 is None:
                        fw.op("vector", lambda e: e.tensor_scalar(out=P[o][:], in0=P[a][:], scalar1=s1, scalar2=None,
                                                                  op0=op0), reads=[r_p], writes=[r_p])
                    else:
                        fw.op("vector", lambda e: e.tensor_scalar(out=P[o][:], in0=P[a][:], scalar1=s1, scalar2=s2,
                                                                  op0=op0, op1=op1), reads=[r_p], writes=[r_p])

                def act(o, a, func, scale=1.0):
                    fw.op("scalar", lambda e: e.activation(out=P[o][:], in_=P[a][:], func=func, scale=scale),
                          reads=[r_p], writes=[r_p])

                for d in range(2):
                    fw.dma("sync", lambda e, d=d: e.dma_start(
                        out=P["are"][:, d * 12:(d + 1) * 12],
                        in_=W["s5_a_re"][l, d].rearrange("(gp g2) p -> (g2 p) gp", g2=2),
                        allow_slow_non_contiguous=True), writes=[r_p])
                    fw.dma("sync", lambda e, d=d: e.dma_start(
                        out=P["aim"][:, d * 12:(d + 1) * 12],
                        in_=W["s5_a_im"][l, d].rearrange("(gp g2) p -> (g2 p) gp", g2=2),
                        allow_slow_non_contiguous=True), writes=[r_p])
                    for g2 in range(2):
                        fw.dma("sync", lambda e, d=d, g2=g2: e.dma_start(
                            out=P["ldt"][64 * g2:64 * g2 + 64, d * 12:(d + 1) * 12],
                            in_=W["s5_log_dt"][l, d].rearrange("(gp g2) -> g2 gp", g2=2)[g2].partition_broadcast(64),
                            allow_slow_non_contiguous=True), writes=[r_p])
                act("dt", "ldt", AF.Exp)
                tt("t1", "are", "dt", ALU.mult)
                act("rho", "t1", AF.Exp)
                tt("th", "aim", "dt", ALU.mult)
                act("t1", "th", AF.Sin, scale=1.0 / 128)
                tt("t2", "t1", "t1", ALU.mult)
                ts_("c", "t2", -2.0, ALU.mult, 1.0, ALU.add)
                act("s", "th", AF.Sin, scale=1.0 / 64)
                for _ in range(6):
                    tt("t1", "c", "c", ALU.mult)
                    tt("t2", "s", "s", ALU.mult)
                    fw.op("vector", lambda e: e.scalar_tensor_tensor(out=P["s"][:], in0=P["c"][:], scalar=2.0,
                                                                     in1=P["s"][:], op0=ALU.mult, op1=ALU.mult),
                          reads=[r_p], writes=[r_p])
                    tt("c", "t1", "t2", ALU.subtract)
                tt("lr", "rho", "c", ALU.mult)
                tt("li", "rho", "s", ALU.mult)
                ts_("nr", "lr", -1.0, ALU.add)
                tt("t1", "are", "are", ALU.mult)
                tt("t2", "aim", "aim", ALU.mult)
                tt("den", "t1", "t2", ALU.add)
                fw.op("vector", lambda e: e.reciprocal(out=P["den"][:], in_=P["den"][:]), reads=[r_p], writes=[r_p])
                tt("t1", "nr", "are", ALU.mult)
                tt("t2", "li", "aim", ALU.mult)
                tt("numr", "t1", "t2", ALU.add)
                tt("t1", "li", "are", ALU.mult)
                tt("t2", "nr", "aim", ALU.mult)
                tt("numi", "t1", "t2", ALU.subtract)
                tt("kr", "numr", "den", ALU.mult)
                tt("ki", "numi", "den", ALU.mult)
                ts_("nki", "ki", -1.0, ALU.mult)
                LRR = sb2("LRR", [128, 2, 24])
                LIS = sb2("LIS", [128, 2, 24])
                for hh in range(2):
                    fw.op("vector", lambda e, hh=hh: e.tensor_copy(out=LRR[:, hh, :], in_=P["lr"][:]),
                          reads=[r_p], writes=[r_p])
                fw.op("vector", lambda e: e.tensor_scalar(out=LIS[:, 0, :], in0=P["li"][:], scalar1=-1.0, scalar2=None,
                                                          op0=ALU.mult), reads=[r_p], writes=[r_p])
                fw.op("vector", lambda e: e.tensor_copy(out=LIS[:, 1, :], in_=P["li"][:]), reads=[r_p], writes=[r_p])

                for nm in ["l2r", "l2i"]:
                    P[nm] = sb2(nm, [128, 24])
                tt("t1", "lr", "lr", ALU.mult)
                tt("t2", "li", "li", ALU.mult)
                tt("l2r", "t1", "t2", ALU.subtract)
                fw.op("vector", lambda e: e.scalar_tensor_tensor(out=P["l2i"][:], in0=P["lr"][:], scalar=2.0,
                                                                 in1=P["li"][:], op0=ALU.mult, op1=ALU.mult),
                      reads=[r_p], writes=[r_p])
                L2RR = sb2("L2RR", [128, 2, 24])
                L2IS = sb2("L2IS", [128, 2, 24])
                for hh in range(2):
                    fw.op("vector", lambda e, hh=hh: e.tensor_copy(out=L2RR[:, hh, :], in_=P["l2r"][:]),
                          reads=[r_p], writes=[r_p])
                fw.op("vector", lambda e: e.tensor_scalar(out=L2IS[:, 0, :], in0=P["l2i"][:], scalar1=-1.0, scalar2=None,
                                                          op0=ALU.mult), reads=[r_p], writes=[r_p])
                fw.op("vector", lambda e: e.tensor_copy(out=L2IS[:, 1, :], in_=P["l2i"][:]), reads=[r_p], writes=[r_p])
                LRR64 = sb2("LRR64", [128, 2, 24, 64])
                LIS64 = sb2("LIS64", [128, 2, 24, 64])
                for (dst64, src3) in [(LRR64, LRR), (LIS64, LIS)]:
                    fw.op("vector", lambda e, dst64=dst64, src3=src3: e.tensor_copy(out=dst64[:, :, :, 0], in_=src3[:]),
                          reads=[r_p], writes=[r_p])
                    w_ = 1
                    while w_ < 64:
                        fw.op("vector", lambda e, dst64=dst64, w_=w_: e.tensor_copy(out=dst64[:, :, :, w_:2 * w_],
                                                                                   in_=dst64[:, :, :, 0:w_]),
                              reads=[r_p], writes=[r_p])
                        w_ *= 2
                for nm in ["l4r", "l4i"]:
                    P[nm] = sb2(nm, [128, 24])
                tt("t1", "l2r", "l2r", ALU.mult)
                tt("t2", "l2i", "l2i", ALU.mult)
                tt("l4r", "t1", "t2", ALU.subtract)
                fw.op("vector", lambda e: e.scalar_tensor_tensor(out=P["l4i"][:], in0=P["l2r"][:], scalar=2.0,
                                                                 in1=P["l2i"][:], op0=ALU.mult, op1=ALU.mult),
                      reads=[r_p], writes=[r_p])
                L4RR = sb2("L4RR", [128, 2, 24])
                L4IS = sb2("L4IS", [128, 2, 24])
                for hh in range(2):
                    fw.op("vector", lambda e, hh=hh: e.tensor_copy(out=L4RR[:, hh, :], in_=P["l4r"][:]),
                          reads=[r_p], writes=[r_p])
                fw.op("vector", lambda e: e.tensor_scalar(out=L4IS[:, 0, :], in0=P["l4i"][:], scalar1=-1.0, scalar2=None,
                                                          op0=ALU.mult), reads=[r_p], writes=[r_p])
                fw.op("vector", lambda e: e.tensor_copy(out=L4IS[:, 1, :], in_=P["l4i"][:]), reads=[r_p], writes=[r_p])
                L2R32 = sb2("L2R32", [128, 2, 24, 32])
                L2I32 = sb2("L2I32", [128, 2, 24, 32])
                for (dst32, src3) in [(L2R32, L2RR), (L2I32, L2IS)]:
                    fw.op("vector", lambda e, dst32=dst32, src3=src3: e.tensor_copy(out=dst32[:, :, :, 0], in_=src3[:]),
                          reads=[r_p], writes=[r_p])
                    w_ = 1
                    while w_ < 32:
                        fw.op("vector", lambda e, dst32=dst32, w_=w_: e.tensor_copy(out=dst32[:, :, :, w_:2 * w_],
                                                                                   in_=dst32[:, :, :, 0:w_]),
                              reads=[r_p], writes=[r_p])
                        w_ *= 2
                T1 = sb2("T1", [128, 2, 24, 64])
                T2 = sb2("T2", [128, 2, 24, 64])
                CB2 = T2[:, :, :, 32:64]
                CB = sb2("CB", [128, 2, 24, 64])
                r_t12 = Res("T12")
                r_cb = Res("CB")
                Bw = sb2("Bw", [128, 48, 128])
                Cw = sb2("Cw", [128, 48, 128])
                r_bw = Res("Bw")
                r_cw = Res("Cw")
                fw.op("gpsimd", lambda e: e.memset(Bw[:], 0.0), writes=[r_bw])
                fw.op("gpsimd", lambda e: e.memset(Cw[:], 0.0), writes=[r_cw])

                def widx(d, gp, ri):
                    return (d * 12 + gp) * 2 + ri
                for d in range(2):
                    for gp in range(12):
                        for g2 in range(2):
                            g = 2 * gp + g2
                            r0 = 16 * (g % 8)
                            for ri, (bn, cn) in enumerate([("s5_b_re", "s5_c_re"), ("s5_b_im", "s5_c_im")]):
                                fw.dma("sync", lambda e, d=d, gp=gp, g2=g2, g=g, r0=r0, ri=ri, bn=bn: e.dma_start(
                                    out=Bw[r0:r0 + 16, widx(d, gp, ri), 64 * g2:64 * g2 + 64],
                                    in_=W[bn][l, d, g].rearrange("p h -> h p"),
                                    allow_slow_non_contiguous=True), writes=[r_bw])
                                fw.dma("sync", lambda e, d=d, gp=gp, g2=g2, g=g, r0=r0, ri=ri, cn=cn: e.dma_start(
                                    out=Cw[64 * g2:64 * g2 + 64, widx(d, gp, ri), r0:r0 + 16],
                                    in_=W[cn][l, d, g].rearrange("h p -> p h"),
                                    allow_slow_non_contiguous=True), writes=[r_cw])
                Cw4 = Cw[:].rearrange("p (a r) n -> p a r n", r=2)
                fw.op("vector", lambda e: e.tensor_scalar(out=Cw4[:, :, 1, :], in0=Cw4[:, :, 1, :], scalar1=-1.0,
                                                          scalar2=None, op0=ALU.mult), reads=[r_cw], writes=[r_cw])

                XS = sb2("XS", [128, 2, 24, 129])
                BU = sb2("BU", [128, 2, 24, 128])
                PQ = sb2("PQ", [128, 2, 2, 24])
                r_xs = Res("XS")
                r_bu = Res("BU")
                r_pq = Res("PQ")
                r_pq1 = Res("PQ1")
                tmpb = [sb2("tmpb%d" % i, [128, 2, 128]) for i in range(2)]
                r_tmpb = [Res("tmpb%d" % i) for i in range(2)]
                ua = [[sb2("ua%d_%d" % (i, d), [128, 3, 128]) for d in range(2)] for i in range(2)]
                r_ua = [[Res("ua%d_%d" % (i, d)) for d in range(2)] for i in range(2)]
                yo = [sb2("yo%d" % i, [128, 128]) for i in range(2)]
                r_yo = [Res("yo%d" % i) for i in range(2)]
                fw.op("vector", lambda e: e.memset(XS[:], 0.0), writes=[r_xs])
                NT = NTOK // 128
                kcount = 0
                for i in range(NT):
                    tiles = [i, NT - 1 - i]
                    bi = i % 2
                    for d in range(2):
                        tk = tiles[d] * 128
                        fw.dma("sync", lambda e, d=d, tk=tk, bi=bi: e.dma_start(
                            out=ua[bi][d][:], in_=UAs[:, :, tk:tk + 128].rearrange("c p n -> p c n")),
                            writes=[r_ua[bi][d]])
                    for d in range(2):
                        for gp in range(12):
                            col = d * 12 + gp
                            c3 = gp // 4
                            pp = 2 * (col % 2)
                            for ri in range(2):
                                fw.op("tensor", lambda e, d=d, gp=gp, ri=ri, pp=pp, c3=c3, bi=bi: e.matmul(
                                    pA[pp + ri][:, 0:128], lhsT=Bw[:, widx(d, gp, ri), :], rhs=ua[bi][d][:, c3, :],
                                    start=True, stop=True),
                                    reads=[r_bw, r_ua[bi][d]], writes=[r_pA[pp + ri]])
                            tbk = tmpb[col % 2]
                            r_tbk = r_tmpb[col % 2]
                            if d == 0:
                                bre, bim = BU[:, 0, col, :], BU[:, 1, col, :]
                            else:
                                bre, bim = BU[:, 0, col, ::-1], BU[:, 1, col, ::-1]
                            fw.op("scalar", lambda e, tbk=tbk, pp=pp, col=col: e.activation(
                                out=tbk[:, 0, :], in_=pA[pp][:, 0:128], func=AF.Identity, scale=P["kr"][:, col:col + 1]),
                                reads=[r_pA[pp], r_p], writes=[r_tbk])
                            fw.op("vector", lambda e, tbk=tbk, pp=pp, col=col, bre=bre: e.scalar_tensor_tensor(
                                out=bre, in0=pA[pp + 1][:, 0:128], scalar=P["nki"][:, col:col + 1], in1=tbk[:, 0, :],
                                op0=ALU.mult, op1=ALU.add), reads=[r_pA[pp + 1], r_p, r_tbk], writes=[r_bu])
                            fw.op("scalar", lambda e, tbk=tbk, pp=pp, col=col: e.activation(
                                out=tbk[:, 1, :], in_=pA[pp + 1][:, 0:128], func=AF.Identity, scale=P["kr"][:, col:col + 1]),
                                reads=[r_pA[pp + 1], r_p], writes=[r_tbk])
                            fw.op("vector", lambda e, tbk=tbk, pp=pp, col=col, bim=bim: e.scalar_tensor_tensor(
                                out=bim, in0=pA[pp][:, 0:128], scalar=P["ki"][:, col:col + 1], in1=tbk[:, 1, :],
                                op0=ALU.mult, op1=ALU.add), reads=[r_pA[pp], r_p, r_tbk], writes=[r_bu])
                    BUe = BU[:, :, :, 0:128:2]
                    BUo = BU[:, :, :, 1:128:2]
                    BUes = BU[:, ::-1, :, 0:128:2]
                    fw.op("vector", lambda e, BUe=BUe: e.tensor_tensor(out=T1[:], in0=LRR64[:], in1=BUe, op=ALU.mult),
                          reads=[r_bu, r_p], writes=[r_t12])
                    fw.op("vector", lambda e, BUes=BUes: e.tensor_tensor(out=T2[:], in0=LIS64[:], in1=BUes, op=ALU.mult),
                          reads=[r_bu, r_p], writes=[r_t12])
                    fw.op("vector", lambda e: e.tensor_tensor(out=T1[:], in0=T1[:], in1=T2[:], op=ALU.add),
                          reads=[r_t12], writes=[r_t12])
                    fw.op("vector", lambda e, BUo=BUo: e.tensor_tensor(out=CB[:], in0=T1[:], in1=BUo, op=ALU.add),
                          reads=[r_t12, r_bu], writes=[r_cb])
                    C1e = CB[:, :, :, 0:64:2]
                    C1o = CB[:, :, :, 1:64:2]
                    C1es = CB[:, ::-1, :, 0:64:2]
                    fw.op("vector", lambda e, C1e=C1e: e.tensor_tensor(out=T1[:, :, :, 0:32], in0=L2R32[:], in1=C1e, op=ALU.mult),
                          reads=[r_cb, r_p], writes=[r_t12])
                    fw.op("vector", lambda e, C1es=C1es: e.tensor_tensor(out=T2[:, :, :, 0:32], in0=L2I32[:], in1=C1es, op=ALU.mult),
                          reads=[r_cb, r_p], writes=[r_t12])
                    fw.op("vector", lambda e: e.tensor_tensor(out=T1[:, :, :, 0:32], in0=T1[:, :, :, 0:32], in1=T2[:, :, :, 0:32],
                                                              op=ALU.add), reads=[r_t12], writes=[r_t12])
                    fw.op("vector", lambda e, C1o=C1o: e.tensor_tensor(out=CB2, in0=T1[:, :, :, 0:32], in1=C1o, op=ALU.add),
                          reads=[r_t12, r_cb], writes=[r_t12])
                    for n_ in range(32):
                        j = 4 * n_
                        fw.op("vector", lambda e, j=j: e.tensor_tensor(out=PQ[:, 0], in0=L4RR[:], in1=XS[:, :, :, j],
                                                                       op=ALU.mult),
                              reads=[r_xs, r_p], writes=[r_pq])
                        fw.op("vector", lambda e, j=j: e.tensor_tensor(out=PQ[:, 1], in0=L4IS[:], in1=XS[:, ::-1, :, j],
                                                                       op=ALU.mult),
                              reads=[r_xs, r_p], writes=[r_pq1])
                        fw.op("vector", lambda e: e.tensor_tensor(out=PQ[:, 0], in0=PQ[:, 0], in1=PQ[:, 1], op=ALU.add),
                              reads=[r_pq, r_pq1], writes=[r_pq])
                        fw.op("vector", lambda e, j=j, n_=n_: e.tensor_tensor(out=XS[:, :, :, j + 4], in0=PQ[:, 0],
                                                                              in1=T2[:, :, :, 32 + n_], op=ALU.add),
                              reads=[r_pq, r_t12], writes=[r_xs])
                    X4 = XS[:, :, :, 0:128:4]
                    X4s = XS[:, ::-1, :, 0:128:4]
                    X42 = XS[:, :, :, 2:129:4]
                    fw.op("vector", lambda e, X4=X4: e.tensor_tensor(out=T1[:, :, :, 0:32], in0=L2R32[:], in1=X4, op=ALU.mult),
                          reads=[r_xs, r_p], writes=[r_t12])
                    fw.op("vector", lambda e, X4s=X4s: e.tensor_tensor(out=T2[:, :, :, 0:32], in0=L2I32[:], in1=X4s, op=ALU.mult),
                          reads=[r_xs, r_p], writes=[r_t12])
                    fw.op("vector", lambda e: e.tensor_tensor(out=T1[:, :, :, 0:32], in0=T1[:, :, :, 0:32], in1=T2[:, :, :, 0:32],
                                                              op=ALU.add), reads=[r_t12], writes=[r_t12])
                    fw.op("vector", lambda e, X42=X42, C1e=C1e: e.tensor_tensor(out=X42, in0=T1[:, :, :, 0:32], in1=C1e, op=ALU.add),
                          reads=[r_t12, r_cb], writes=[r_xs])
                    XSe = XS[:, :, :, 0:128:2]
                    XSes = XS[:, ::-1, :, 0:128:2]
                    XSo = XS[:, :, :, 1:129:2]
                    fw.op("vector", lambda e, XSe=XSe: e.tensor_tensor(out=T1[:], in0=LRR64[:], in1=XSe, op=ALU.mult),
                          reads=[r_xs, r_p], writes=[r_t12])
                    fw.op("vector", lambda e, XSes=XSes: e.tensor_tensor(out=T2[:], in0=LIS64[:], in1=XSes, op=ALU.mult),
                          reads=[r_xs, r_p], writes=[r_t12])
                    fw.op("vector", lambda e: e.tensor_tensor(out=T1[:], in0=T1[:], in1=T2[:], op=ALU.add),
                          reads=[r_t12], writes=[r_t12])
                    fw.op("vector", lambda e, XSo=XSo, BUe=BUe: e.tensor_tensor(out=XSo, in0=T1[:], in1=BUe, op=ALU.add),
                          reads=[r_t12, r_bu], writes=[r_xs])
                    for d in range(2):
                        tk = tiles[d] * 128
                        for c3 in range(3):
                            pj = kcount % 2
                            kcount += 1
                            n = 0
                            for gq in range(4):
                                gp = c3 * 4 + gq
                                col = d * 12 + gp
                                for ri in range(2):
                                    fw.op("tensor", lambda e, d=d, gp=gp, ri=ri, col=col, pj=pj, n=n: e.matmul(
                                        pX[pj][:, 0:128], lhsT=Cw[:, widx(d, gp, ri), :], rhs=XS[:, ri, col, 1:129],
                                        start=(n == 0), stop=(n == 7)),
                                        reads=[r_cw, r_xs], writes=[r_pX[pj]], pe_acc=True)
                                    n += 1
                            ov = yo[pj][:, :] if d == 0 else yo[pj][:, ::-1]
                            fw.op("scalar", lambda e, pj=pj, ov=ov: e.activation(out=ov, in_=pX[pj][:, 0:128],
                                                                                 func=AF.Copy),
                                  reads=[r_pX[pj]], writes=[r_yo[pj]])
                            fw.dma("sync", lambda e, d=d, c3=c3, tk=tk, pj=pj: e.dma_start(
                                out=YA[d, c3, :, tk:tk + 128], in_=yo[pj][:]), reads=[r_yo[pj]])
                    fw.op("vector", lambda e: e.tensor_copy(out=XS[:, :, :, 0], in_=XS[:, :, :, 128]),
                          reads=[r_xs], writes=[r_xs])
                    if (i + 1) % 32 == 0 and i + 1 < NT:
                        sgn = (i + 1) // 32
                        fw.op("vector", lambda e, sgn=sgn: e.tensor_scalar(
                            out=XS[:, :, 0:12, 0], in0=XS[:, :, 0:12, 0], scalar1=flg[:, sgn:sgn + 1], scalar2=None,
                            op0=ALU.mult), reads=[r_xs, r_flg], writes=[r_xs])
                        fw.op("vector", lambda e, sgn=sgn: e.tensor_scalar(
                            out=XS[:, :, 12:24, 0], in0=XS[:, :, 12:24, 0], scalar1=flg[:, 4 - sgn:5 - sgn],
                            scalar2=None, op0=ALU.mult), reads=[r_xs, r_flg], writes=[r_xs])
            fw.barrier()

        def phase_h():
            with ExitStack() as pes:
                hb = [pes.enter_context(nc.sbuf_tensor(uname("h_hb%d" % i), [128, 4, PAD], BF16)) for i in range(2)]
                r_hb = [Res("hb%d" % i) for i in range(2)]
                k = 0
                for (T, nch) in [(KTs, 4), (VTs, 4), (CVs, 3)]:
                    for sg in range(NSEG):
                        jobs = []
                        src = ((sg - 1) * SEGP + SEG) if sg > 0 else (sg * SEGP + PAD)
                        jobs.append((src, sg * SEGP, sg))
                        src = ((sg + 1) * SEGP + PAD) if sg < NSEG - 1 else (sg * SEGP + SEG)
                        jobs.append((src, sg * SEGP + PAD + SEG, sg + 1))
                        for (src, dst, fc) in jobs:
                            b = k % 2
                            k += 1
                            fw.dma("sync", lambda e, T=T, nch=nch, src=src, b=b: e.dma_start(
                                out=hb[b][:, 0:nch, :], in_=T[0:nch, :, src:src + PAD].rearrange("c p n -> p c n")),
                                writes=[r_hb[b]])
                            fw.op("vector", lambda e, nch=nch, b=b, fc=fc: e.tensor_scalar(
                                out=hb[b][:, 0:nch, :], in0=hb[b][:, 0:nch, :], scalar1=flg[:, fc:fc + 1], scalar2=None,
                                op0=ALU.mult), reads=[r_hb[b], r_flg], writes=[r_hb[b]])
                            fw.dma("sync", lambda e, T=T, nch=nch, dst=dst, b=b: e.dma_start(
                                out=T[0:nch, :, dst:dst + PAD].rearrange("c p n -> p c n"), in_=hb[b][:, 0:nch, :]),
                                reads=[r_hb[b]])
            fw.barrier()

        maskD = sb("maskD", [128, 6, 256])
        maskS = sb("maskS", [128, 6, 384])
        r_mask = Res("mask")
        ones_col = sb("ones_col", [128, 1])
        fw.op("vector", lambda e: e.memset(ones_col[:], 1.0), writes=[r_mask])
        with ExitStack() as mes:
            ii = mes.enter_context(nc.sbuf_tensor("m_ii", [128, 128], mybir.dt.int32))
            fi = mes.enter_context(nc.sbuf_tensor("m_fi", [128, 128], F32))
            ta = mes.enter_context(nc.sbuf_tensor("m_ta", [128, 128], F32))
            tv = mes.enter_context(nc.sbuf_tensor("m_tv", [128, 128], F32))
            r_m = Res("m")
            fw.op("gpsimd", lambda e: e.iota(ii[:], pattern=[[-1, 128]], base=0, channel_multiplier=1), writes=[r_m])
            fw.op("vector", lambda e: e.tensor_copy(out=fi[:], in_=ii[:]), reads=[r_m], writes=[r_m])

            def mk_mask(dst, off, half, coef):
                fw.op("vector", lambda e: e.tensor_scalar(out=ta[:], in0=fi[:], scalar1=float(off), scalar2=None,
                                                          op0=ALU.add), reads=[r_m], writes=[r_m])
                fw.op("vector", lambda e: e.tensor_scalar(out=tv[:], in0=ta[:], scalar1=-1.0, scalar2=None,
                                                          op0=ALU.mult), reads=[r_m], writes=[r_m])
                fw.op("vector", lambda e: e.tensor_tensor(out=ta[:], in0=ta[:], in1=tv[:], op=ALU.max),
                      reads=[r_m], writes=[r_m])
                fw.op("vector", lambda e: e.tensor_scalar(out=tv[:], in0=ta[:], scalar1=-1.0, scalar2=float(half) + 0.5,
                                                          op0=ALU.mult, op1=ALU.add), reads=[r_m], writes=[r_m])
                fw.op("vector", lambda e: e.tensor_scalar(out=tv[:], in0=tv[:], scalar1=0.0, scalar2=0.5,
                                                          op0=ALU.max, op1=ALU.min), reads=[r_m], writes=[r_m])
                fw.op("scalar", lambda e: e.activation(out=ta[:], in_=ta[:], func=AF.Exp, scale=-float(coef)),
                      reads=[r_m], writes=[r_m])
                fw.op("vector", lambda e: e.scalar_tensor_tensor(out=dst, in0=ta[:], scalar=2.0, in1=tv[:],
                                                                 op0=ALU.mult, op1=ALU.mult),
                      reads=[r_m], writes=[r_m, r_mask])
            for gi, (win, dil) in enumerate(DIL):
                for h in range(2):
                    sl = SLOPES[6 + 2 * gi + h]
                    for kt in range(2):
                        mk_mask(maskD[:, 2 * gi + h, kt * 128:(kt + 1) * 128], -64 + 128 * kt, 64, sl * dil)
            for h in range(6):
                for kt in range(3):
                    mk_mask(maskS[:, h, kt * 128:(kt + 1) * 128], 128 * (kt - 1), 128, SLOPES[h])
        fw.barrier()

        def phase_b(l, last):
            with ExitStack() as L0:
                def sb0(name, shape, dt=F32):
                    return L0.enter_context(nc.sbuf_tensor(uname("b_" + name), shape, dt))
                hT = sb0("hT", [128, 8, ST], BF16)
                r_hT = Res("hT")
                vcol = sb0("vcol", [128, 2])
                r_vcol = Res("vcol")
                sexp = sb0("sexp", [128, 6])
                r_sexp = Res("sexp")
                fw.dma("sync", lambda e: e.dma_start(out=sexp[:], in_=W["swa_sink"][l].partition_broadcast(128)),
                       writes=[r_sexp])
                fw.op("scalar", lambda e: e.activation(out=sexp[:], in_=sexp[:], func=AF.Exp),
                      reads=[r_sexp], writes=[r_sexp])
                wi = W["w_in"][l]
                for st_i in range(NST):
                    t0 = st_i * ST
                    p0 = padpos(t0)
                    sg = st_i // 2
                    hf = st_i % 2
                    fw.dma("sync", lambda e, t0=t0: e.dma_start(
                        out=hT[:], in_=HT[:, :, t0:t0 + ST].rearrange("k p n -> p k n")), writes=[r_hT])
                    fw.op("vector", lambda e: e.memset(vcol[:], 1.0), writes=[r_vcol])
                    fw.op("vector", lambda e, sg=sg: e.tensor_copy(out=vcol[0:64, 0:1], in_=flg[0:64, sg:sg + 1]),
                          reads=[r_flg], writes=[r_vcol])
                    fw.op("vector", lambda e, sg=sg: e.tensor_copy(out=vcol[64:128, 1:2], in_=flg[64:128, sg + 1:sg + 2]),
                          reads=[r_flg], writes=[r_vcol])
                    with ExitStack() as L1:
                        def sb1(name, shape, dt=F32):
                            return L1.enter_context(nc.sbuf_tensor(uname("b1_" + name), shape, dt))
                        brT = sb1("brT", [128, 10, ST], BF16)
                        r_br = Res("brT")
                        wst = [sb1("wst%d" % i, [128, 8, 128]) for i in range(2)]
                        r_wst = [Res("wst%d" % i) for i in range(2)]
                        wbf = [sb1("wbf%d" % i, [128, 8, 128], BF16) for i in range(2)]
                        r_wbf = [Res("wbf%d" % i) for i in range(2)]

                        def load_w_chunk(parts):
                            j = cnt["w"] % 2
                            cnt["w"] += 1
                            for (src_ap, c0, n) in parts:
                                fw.dma("sync", lambda e, src_ap=src_ap, c0=c0, n=n, j=j: e.dma_start(
                                    out=wst[j][:, :, c0:c0 + n], in_=src_ap.rearrange("(k p) n -> p k n", p=128)),
                                    writes=[r_wst[j]])
                            fw.op("gpsimd", lambda e, j=j: e.tensor_copy(out=wbf[j][:], in_=wst[j][:]),
                                  reads=[r_wst[j]], writes=[r_wbf[j]])
                            return wbf[j], r_wbf[j]

                        def proj_fm(wt, r_w, evac):
                            for ts in range(ST // 512):
                                j = cnt["pA"] % 4
                                cnt["pA"] += 1
                                for k in range(8):
                                    fw.op("tensor", lambda e, k=k, j=j, ts=ts: e.matmul(
                                        pA[j][:], lhsT=wt[:, k, :], rhs=hT[:, k, ts * 512:(ts + 1) * 512],
                                        start=(k == 0), stop=(k == 7)),
                                        reads=[r_w, r_hT], writes=[r_pA[j]], pe_acc=True)
                                evac(ts, j, pA[j], r_pA[j])

                        with ExitStack() as S1:
                            def sbs(name, shape, dt=F32):
                                return S1.enter_context(nc.sbuf_tensor(uname("b2_" + name), shape, dt))
                            KT1 = sbs("KT1", [128, 2 * ST], BF16)
                            VT1 = sbs("VT1", [128, 2 * ST], BF16)
                            r_kv = Res("kv")
                            QT1 = sbs("QT1", [128, ST], BF16)
                            r_q = Res("q")
                            UACC = sbs("UACC", [128, 2, ST])
                            r_ua = Res("uacc")
                            RC = sbs("RC", [128, ST])
                            r_rc = Res("rc")
                            Et = [sbs("E%d" % i, [128, 384]) for i in range(2)]
                            r_E = [Res("E%d" % i) for i in range(2)]
                            Pt = [sbs("P%d" % i, [128, 384], BF16) for i in range(2)]
                            r_P = [Res("P%d" % i) for i in range(2)]
                            VE = [sbs("VE%d" % i, [128, 3, 2, 192], BF16) for i in range(2)]
                            r_VE = [Res("VE%d" % i) for i in range(2)]
                            sm = [sbs("sm%d" % i, [128, 128]) for i in range(2)]
                            r_sm = [Res("sm%d" % i) for i in range(2)]
                            for i in range(2):
                                fw.op("vector", lambda e, i=i: e.memset(VE[i][:], 1.0), writes=[r_VE[i]])
                            ac = {"u": 0}

                            def q_evac(ts, j, pt, r_pt):
                                fw.op("scalar", lambda e: e.activation(out=QT1[:, ts * 512:(ts + 1) * 512], in_=pt[:],
                                                                       func=AF.Copy), reads=[r_pt], writes=[r_q])

                            def load_kv(c, p0=p0):
                                fw.dma("sync", lambda e, c=c, p0=p0: e.dma_start(
                                    out=KT1[:], in_=KTs[c, :, p0 - PAD:p0 - PAD + 2 * ST]), writes=[r_kv])
                                fw.dma("sync", lambda e, c=c, p0=p0: e.dma_start(
                                    out=VT1[:], in_=VTs[c, :, p0 - PAD:p0 - PAD + 2 * ST]), writes=[r_kv])

                            def unit(nkt, kcols, qcols, heads, mask_of, valid_of, lhs_of, sink_dst):
                                u = ac["u"] % 2
                                ac["u"] += 1
                                for kt in range(nkt):
                                    fw.op("tensor", lambda e, kt=kt, u=u: e.transpose(
                                        pT[u][:, kt, :], VT1[:, kcols(kt)], ident[:]),
                                        reads=[r_kv, r_ident], writes=[r_pT[u]], pe_acc=True)
                                fw.op("vector", lambda e, u=u: e.tensor_copy(
                                    out=VE[u][:, 0:nkt, :, 64:128],
                                    in_=pT[u][:, 0:nkt, :].rearrange("p k (h d) -> p k h d", h=2)),
                                    reads=[r_pT[u]], writes=[r_VE[u]])
                                for (hrow, vslot, tag) in heads:
                                    j = cnt["pA"] % 4
                                    cnt["pA"] += 1
                                    j2 = cnt["pA"] % 4
                                    cnt["pA"] += 1
                                    ei = ac["u"] % 2
                                    for kt in range(nkt):
                                        fw.op("tensor", lambda e, kt=kt, j=j, hrow=hrow: e.matmul(
                                            pA[j][:, kt * 128:(kt + 1) * 128], lhsT=KT1[hrow:hrow + 64, kcols(kt)],
                                            rhs=QT1[hrow:hrow + 64, qcols], start=True, stop=True),
                                            reads=[r_kv, r_q], writes=[r_pA[j]], pe_acc=True)
                                    fw.op("scalar", lambda e, j=j, ei=ei: e.activation(
                                        out=Et[ei][:, 0:nkt * 128], in_=pA[j][:, 0:nkt * 128], func=AF.Exp, scale=0.125),
                                        reads=[r_pA[j]], writes=[r_E[ei]])
                                    for kt in range(nkt):
                                        vc = valid_of(kt)
                                        fw.op("vector", lambda e, kt=kt, ei=ei, vc=vc, tag=tag: e.scalar_tensor_tensor(
                                            out=Pt[ei][:, kt * 128:(kt + 1) * 128], in0=Et[ei][:, kt * 128:(kt + 1) * 128],
                                            scalar=vc, in1=mask_of(tag)[:, kt * 128:(kt + 1) * 128],
                                            op0=ALU.mult, op1=ALU.mult),
                                            reads=[r_E[ei], r_mask, r_vcol, r_flg], writes=[r_P[ei]])
                                    for kt in range(nkt):
                                        fw.op("tensor", lambda e, kt=kt, j2=j2, ei=ei, u=u, vslot=vslot, tag=tag: e.matmul(
                                            pA[j2][:, 0:128], lhsT=lhs_of(VE[u], kt, vslot, tag),
                                            rhs=Pt[ei][:, kt * 128:(kt + 1) * 128], start=(kt == 0), stop=(kt == nkt - 1)),
                                            reads=[r_VE[u], r_P[ei]], writes=[r_pA[j2]], pe_acc=True)
                                    sink_dst(tag, pA[j2], r_pA[j2])

                            for gi, (win, dil) in enumerate(DIL):
                                wt, r_w = load_w_chunk([(wi[:, (12 + gi) * 128:(13 + gi) * 128], 0, 128)])
                                proj_fm(wt, r_w, q_evac)
                                load_kv(gi)
                                nsub = ST // dil
                                for r in range(dil):
                                    for qb in range(nsub // 128):
                                        q0 = qb * 128
                                        c_lo = r + dil * q0

                                        def kcols(kt, c_lo=c_lo, dil=dil):
                                            b = PAD + c_lo + dil * (-64 + 128 * kt)
                                            return slice(b, b + 127 * dil + 1, dil)
                                        qcols = slice(c_lo, c_lo + 127 * dil + 1, dil)

                                        def valid_of(kt, qb=qb, nsub=nsub):
                                            if hf == 0 and qb == 0 and kt == 0:
                                                return vcol[:, 0:1]
                                            if hf == 1 and qb == nsub // 128 - 1 and kt == 1:
                                                return vcol[:, 1:2]
                                            return ones_col[:, 0:1]

                                        def mask_of(tag, gi=gi):
                                            return maskD[:, 2 * gi + tag, :]

                                        def lhs_of(ve, kt, vslot, tag):
                                            return ve[:, kt, tag, 64:192] if tag == 0 else ve[:, kt, tag, 0:128]

                                        def sink_dst(tag, pu, r_pu, gi=gi, qcols=qcols):
                                            dstv = UACC[:, tag, qcols]
                                            if gi == 0:
                                                fw.op("vector", lambda e: e.tensor_copy(out=dstv, in_=pu[:, 0:128]),
                                                      reads=[r_pu], writes=[r_ua])
                                            else:
                                                fw.op("vector", lambda e: e.tensor_tensor(out=dstv, in0=dstv,
                                                                                          in1=pu[:, 0:128], op=ALU.add),
                                                      reads=[r_pu, r_ua], writes=[r_ua])
                                        unit(2, kcols, qcols, [(0, 0, 0), (64, 1, 1)], mask_of, valid_of, lhs_of, sink_dst)
                            fw.op("vector", lambda e: e.reciprocal(out=RC[0:64, :], in_=UACC[64:128, 0, :]),
                                  reads=[r_ua], writes=[r_rc])
                            fw.op("vector", lambda e: e.reciprocal(out=RC[64:128, :], in_=UACC[0:64, 1, :]),
                                  reads=[r_ua], writes=[r_rc])
                            fw.op("vector", lambda e: e.tensor_tensor(out=brT[0:64, 6, :], in0=UACC[0:64, 0, :],
                                                                      in1=RC[0:64, :], op=ALU.mult),
                                  reads=[r_ua, r_rc], writes=[r_br])
                            fw.op("vector", lambda e: e.tensor_tensor(out=brT[64:128, 6, :], in0=UACC[64:128, 1, :],
                                                                      in1=RC[64:128, :], op=ALU.mult),
                                  reads=[r_ua, r_rc], writes=[r_br])
                            load_kv(3)
                            for jq in range(3):
                                wt, r_w = load_w_chunk([(wi[:, 2688 + 64 * jq:2688 + 64 * jq + 64], 0, 64),
                                                        (wi[:, 2688 + 64 * (jq + 3):2688 + 64 * (jq + 3) + 64], 64, 64)])
                                proj_fm(wt, r_w, q_evac)
                                for qb in range(ST // 128):
                                    q0 = qb * 128

                                    def kcols(kt, q0=q0):
                                        b = PAD + q0 - 128 + 128 * kt
                                        return slice(b, b + 128)
                                    qcols = slice(q0, q0 + 128)

                                    def valid_of(kt, qb=qb):
                                        if hf == 0 and qb == 0 and kt == 0:
                                            return flg[:, sg:sg + 1]
                                        if hf == 1 and qb == ST // 128 - 1 and kt == 2:
                                            return flg[:, sg + 1:sg + 2]
                                        return ones_col[:, 0:1]

                                    def mask_of(tag):
                                        return maskS[:, tag, :]

                                    def lhs_of(ve, kt, vslot, tag):
                                        return ve[:, kt, vslot, 64:192] if tag % 2 == 0 else ve[:, kt, vslot, 0:128]

                                    def sink_dst(tag, pu, r_pu, qcols=qcols):
                                        h = tag
                                        ch, half = 7 + h // 2, h % 2
                                        si = ac["u"] % 2
                                        if half == 0:
                                            urows, drows = slice(0, 64), slice(64, 128)
                                        else:
                                            urows, drows = slice(64, 128), slice(0, 64)
                                        fw.op("vector", lambda e: e.tensor_scalar(
                                            out=sm[si][drows, :], in0=pu[drows, 0:128], scalar1=sexp[drows, h:h + 1],
                                            scalar2=None, op0=ALU.add), reads=[r_pu, r_sexp], writes=[r_sm[si]])
                                        fw.op("vector", lambda e: e.reciprocal(out=sm[si][drows, :], in_=sm[si][drows, :]),
                                              reads=[r_sm[si]], writes=[r_sm[si]])
                                        fw.op("vector", lambda e: e.tensor_tensor(
                                            out=brT[urows, ch, qcols], in0=pu[urows, 0:128], in1=sm[si][drows, :],
                                            op=ALU.mult), reads=[r_pu, r_sm[si]], writes=[r_br])
                                    unit(3, kcols, qcols, [(0, 0, jq), (64, 1, jq + 3)], mask_of, valid_of, lhs_of, sink_dst)
                        fw.barrier()
                        if stop_after == "attn":
                            fw.dma("sync", lambda e, t0=t0: e.dma_start(
                                out=dbg["br"][:, :, t0:t0 + ST].rearrange("c p n -> p c n"), in_=brT[:]), reads=[r_br])
                            fw.barrier()
                            continue
                        with ExitStack() as S2:
                            def sbt(name, shape, dt=F32):
                                return S2.enter_context(nc.sbuf_tensor(uname("b3_" + name), shape, dt))
                            dvec = sbt("dvec", [128, 3])
                            glub = sbt("glub", [128, 3])
                            cwt = sbt("cwt", [128, 3, 3])
                            cbt = sbt("cbt", [128, 3])
                            r_sv = Res("sv")
                            fw.dma("sync", lambda e: e.dma_start(out=dvec[:], in_=W["s5_d"][l].rearrange("(c p) -> p c", p=128),
                                                                 allow_slow_non_contiguous=True), writes=[r_sv])
                            fw.dma("sync", lambda e: e.dma_start(out=glub[:], in_=W["s5_glu_b"][l].rearrange("(c p) -> p c", p=128),
                                                                 allow_slow_non_contiguous=True), writes=[r_sv])
                            fw.dma("sync", lambda e: e.dma_start(out=cwt[:], in_=W["conv_w"][l].rearrange("t (c p) -> p t c", p=128),
                                                                 allow_slow_non_contiguous=True), writes=[r_sv])
                            fw.dma("sync", lambda e: e.dma_start(out=cbt[:], in_=W["conv_b"][l].rearrange("(c p) -> p c", p=128),
                                                                 allow_slow_non_contiguous=True), writes=[r_sv])
                            gluw32 = sbt("gluw32", [128, 3, 384])
                            gluw = sbt("gluw", [128, 3, 384], BF16)
                            r_gluw = Res("gluw")
                            fw.dma("sync", lambda e: e.dma_start(out=gluw32[:], in_=W["s5_glu_w"][l].rearrange("(c p) n -> p c n", p=128)),
                                   writes=[r_gluw])
                            fw.op("gpsimd", lambda e: e.tensor_copy(out=gluw[:], in_=gluw32[:]), reads=[r_gluw], writes=[r_gluw])
                            yf = [sbt("yf%d" % i, [128, 512]) for i in range(2)]
                            yb_ = [sbt("yb%d" % i, [128, 512]) for i in range(2)]
                            uu = [sbt("uu%d" % i, [128, 512]) for i in range(2)]
                            r_y3 = [Res("y3_%d" % i) for i in range(2)]
                            zf32 = sbt("zf32", [128, 3, 512])
                            zb16 = sbt("zb16", [128, 3, 512], BF16)
                            r_z = Res("z")
                            gt = [sbt("gt%d" % i, [128, 512]) for i in range(2)]
                            r_gt = [Res("gt%d" % i) for i in range(2)]
                            kk = 0
                            for ts in range(ST // 512):
                                tk = t0 + ts * 512
                                for c in range(3):
                                    b = kk % 2
                                    kk += 1
                                    fw.dma("sync", lambda e, c=c, tk=tk, b=b: e.dma_start(out=yf[b][:], in_=YA[0, c, :, tk:tk + 512]),
                                           writes=[r_y3[b]])
                                    fw.dma("sync", lambda e, c=c, tk=tk, b=b: e.dma_start(out=yb_[b][:], in_=YA[1, c, :, tk:tk + 512]),
                                           writes=[r_y3[b]])
                                    fw.dma("sync", lambda e, c=c, tk=tk, b=b: e.dma_start(out=uu[b][:], in_=UAs[c, :, tk:tk + 512]),
                                           writes=[r_y3[b]])
                                    fw.op("vector", lambda e, b=b: e.tensor_tensor(out=yf[b][:], in0=yf[b][:], in1=yb_[b][:], op=ALU.add),
                                          reads=[r_y3[b]], writes=[r_y3[b]])
                                    fw.op("vector", lambda e, b=b, c=c: e.scalar_tensor_tensor(
                                        out=yf[b][:], in0=uu[b][:], scalar=dvec[:, c:c + 1], in1=yf[b][:], op0=ALU.mult, op1=ALU.add),
                                        reads=[r_y3[b], r_sv], writes=[r_y3[b]])
                                    fw.op("scalar", lambda e, b=b, c=c: e.activation(out=zf32[:, c, :], in_=yf[b][:], func=AF.Gelu),
                                          reads=[r_y3[b]], writes=[r_z])
                                    fw.op("vector", lambda e, c=c: e.tensor_copy(out=zb16[:, c, :], in_=zf32[:, c, :]),
                                          reads=[r_z], writes=[r_z])
                                for co in range(3):
                                    j = cnt["pA"] % 4
                                    cnt["pA"] += 1
                                    for ci in range(3):
                                        fw.op("tensor", lambda e, ci=ci, co=co, j=j: e.matmul(
                                            pA[j][:], lhsT=gluw[:, ci, co * 128:(co + 1) * 128], rhs=zb16[:, ci, :],
                                            start=(ci == 0), stop=(ci == 2)), reads=[r_gluw, r_z], writes=[r_pA[j]], pe_acc=True)
                                    g2 = co % 2
                                    fw.op("vector", lambda e, j=j, g2=g2, co=co: e.tensor_scalar(
                                        out=gt[g2][:], in0=pA[j][:], scalar1=glub[:, co:co + 1], scalar2=None, op0=ALU.add),
                                        reads=[r_pA[j], r_sv], writes=[r_gt[g2]])
                                    fw.op("scalar", lambda e, g2=g2: e.activation(out=gt[g2][:], in_=gt[g2][:], func=AF.Sigmoid),
                                          reads=[r_gt[g2]], writes=[r_gt[g2]])
                                    fw.op("vector", lambda e, g2=g2, co=co, ts=ts: e.tensor_tensor(
                                        out=brT[:, co, ts * 512:(ts + 1) * 512], in0=zf32[:, co, :], in1=gt[g2][:], op=ALU.mult),
                                        reads=[r_z, r_gt[g2]], writes=[r_br])
                            cvt = sbt("cvt", [128, ST + 2], BF16)
                            r_cvt = Res("cvt")
                            accf = [sbt("accf%d" % i, [128, 512]) for i in range(2)]
                            r_accf = [Res("accf%d" % i) for i in range(2)]
                            for c in range(3):
                                wt, r_w = load_w_chunk([(wi[:, (6 + c) * 128:(7 + c) * 128], 0, 128)])
                                fw.dma("sync", lambda e, c=c, p0=p0: e.dma_start(out=cvt[:], in_=CVs[c, :, p0 - 1:p0 + ST + 1]),
                                       writes=[r_cvt])

                                def ev_gb(ts, j, pt, r_pt, c=c):
                                    a = j % 2
                                    o = ts * 512
                                    fw.op("vector", lambda e: e.tensor_scalar(out=accf[a][:], in0=cvt[:, o:o + 512],
                                                                              scalar1=cwt[:, 0, c:c + 1], scalar2=None, op0=ALU.mult),
                                          reads=[r_cvt, r_sv], writes=[r_accf[a]])
                                    for tap in (1, 2):
                                        fw.op("vector", lambda e, tap=tap: e.scalar_tensor_tensor(
                                            out=accf[a][:], in0=cvt[:, o + tap:o + tap + 512], scalar=cwt[:, tap, c:c + 1],
                                            in1=accf[a][:], op0=ALU.mult, op1=ALU.add),
                                            reads=[r_cvt, r_sv, r_accf[a]], writes=[r_accf[a]])
                                    fw.op("vector", lambda e: e.scalar_tensor_tensor(
                                        out=brT[:, 3 + c, o:o + 512], in0=accf[a][:], scalar=cbt[:, c:c + 1], in1=pt[:],
                                        op0=ALU.add, op1=ALU.mult), reads=[r_accf[a], r_sv, r_pt], writes=[r_br])
                                proj_fm(wt, r_w, ev_gb)
                            if stop_after == "branches":
                                fw.dma("sync", lambda e, t0=t0: e.dma_start(
                                    out=dbg["br"][:, :, t0:t0 + ST].rearrange("c p n -> p c n"), in_=brT[:]), reads=[r_br])
                                fw.barrier()
                                continue
                            wbr32 = [sbt("wbr32_%d" % i, [128, 3, 128]) for i in range(2)]
                            wbr = [sbt("wbr%d" % i, [128, 3, 128], BF16) for i in range(2)]
                            r_wbr = [Res("wbr%d" % i) for i in range(2)]
                            mac = sbt("mac", [128, ST])
                            r_mac = Res("mac")
                            mbf = [sbt("mbf%d" % i, [128, ST], BF16) for i in range(2)]
                            r_mbf = [Res("mbf%d" % i) for i in range(2)]
                            sgt = [sbt("sgt%d" % i, [128, 512]) for i in range(2)]
                            r_sgt = [Res("sgt%d" % i) for i in range(2)]
                            BRS = [("w_branch_a", 3, 0), ("w_branch_b", 3, 3), ("w_branch_c", 1, 6), ("w_branch_d", 3, 7)]
                            q = 0
                            for jo in range(8):
                                for br, (wn, nch, ch0) in enumerate(BRS):
                                    wg, r_wg = load_w_chunk([(wi[:, 3328 + br * 1024 + jo * 128:3328 + br * 1024 + (jo + 1) * 128], 0, 128)])
                                    wb = q % 2
                                    q += 1
                                    fw.dma("sync", lambda e, wn=wn, nch=nch, jo=jo, wb=wb: e.dma_start(
                                        out=wbr32[wb][:, 0:nch, :],
                                        in_=W[wn][l][:, jo * 128:(jo + 1) * 128].rearrange("(c p) n -> p c n", p=128)),
                                        writes=[r_wbr[wb]])
                                    fw.op("gpsimd", lambda e, nch=nch, wb=wb: e.tensor_copy(out=wbr[wb][:, 0:nch, :],
                                                                                          in_=wbr32[wb][:, 0:nch, :]),
                                          reads=[r_wbr[wb]], writes=[r_wbr[wb]])
                                    for ts in range(ST // 512):
                                        j = cnt["pA"] % 4
                                        cnt["pA"] += 1
                                        xk = (q + ts) % 2
                                        o = ts * 512
                                        for k in range(8):
                                            fw.op("tensor", lambda e, k=k, j=j, o=o, wg=wg: e.matmul(
                                                pA[j][:], lhsT=wg[:, k, :], rhs=hT[:, k, o:o + 512], start=(k == 0), stop=(k == 7)),
                                                reads=[r_wg, r_hT], writes=[r_pA[j]], pe_acc=True)
                                        for c in range(nch):
                                            fw.op("tensor", lambda e, c=c, xk=xk, o=o, wb=wb, ch0=ch0, nch=nch: e.matmul(
                                                pX[xk][:], lhsT=wbr[wb][:, c, :], rhs=brT[:, ch0 + c, o:o + 512],
                                                start=(c == 0), stop=(c == nch - 1)),
                                                reads=[r_wbr[wb], r_br], writes=[r_pX[xk]], pe_acc=True)
                                        fw.op("scalar", lambda e, j=j, xk=xk: e.activation(out=sgt[xk][:], in_=pA[j][:], func=AF.Sigmoid),
                                              reads=[r_pA[j]], writes=[r_sgt[xk]])
                                        if br == 0:
                                            fw.op("vector", lambda e, xk=xk, o=o: e.tensor_tensor(
                                                out=mac[:, o:o + 512], in0=sgt[xk][:], in1=pX[xk][:], op=ALU.mult),
                                                reads=[r_sgt[xk], r_pX[xk]], writes=[r_mac])
                                        else:
                                            fw.op("vector", lambda e, xk=xk: e.tensor_tensor(
                                                out=sgt[xk][:], in0=sgt[xk][:], in1=pX[xk][:], op=ALU.mult),
                                                reads=[r_sgt[xk], r_pX[xk]], writes=[r_sgt[xk]])
                                            fw.op("vector", lambda e, xk=xk, o=o: e.tensor_tensor(
                                                out=mac[:, o:o + 512], in0=mac[:, o:o + 512], in1=sgt[xk][:], op=ALU.add),
                                                reads=[r_sgt[xk], r_mac], writes=[r_mac])
                                mi = jo % 2
                                fw.op("scalar", lambda e, mi=mi: e.activation(out=mbf[mi][:], in_=mac[:], func=AF.Copy),
                                      reads=[r_mac], writes=[r_mbf[mi]])
                                fw.dma("sync", lambda e, jo=jo, t0=t0, mi=mi: e.dma_start(out=MTs[jo, :, t0:t0 + ST], in_=mbf[mi][:]),
                                       reads=[r_mbf[mi]])
                    fw.barrier()
                    if stop_after in ("attn", "branches"):
                        continue
                    with ExitStack() as S3:
                        def sbu(name, shape, dt=F32):
                            return S3.enter_context(nc.sbuf_tensor(uname("b4_" + name), shape, dt))
                        fw.dma("sync", lambda e, t0=t0: e.dma_start(
                            out=hT[:], in_=MTs[:, :, t0:t0 + ST].rearrange("k p n -> p k n")), writes=[r_hT])
                        wo32 = [sbu("wo32_%d" % i, [128, 8, 256]) for i in range(2)]
                        r_wo32 = [Res("wo32_%d" % i) for i in range(2)]
                        wo = sbu("wo", [128, 8, D], BF16)
                        r_wo = Res("wo")
                        for pc in range(4):
                            a = pc % 2
                            fw.dma("sync", lambda e, pc=pc, a=a: e.dma_start(
                                out=wo32[a][:], in_=W["w_o"][l][:, pc * 256:(pc + 1) * 256].rearrange("(k p) n -> p k n", p=128)),
                                writes=[r_wo32[a]])
                            fw.op("gpsimd", lambda e, pc=pc, a=a: e.tensor_copy(out=wo[:, :, pc * 256:(pc + 1) * 256], in_=wo32[a][:]),
                                  reads=[r_wo32[a]], writes=[r_wo])
                        load_gb("ln1_g", "ln1_b", l)
                        xb = [sbu("xb%d" % i, [128, D]) for i in range(2)]
                        r_xb = [Res("xb%d" % i) for i in range(2)]
                        tb = [sbu("tb%d" % i, [128, D]) for i in range(2)]
                        r_tb = [Res("tb%d" % i) for i in range(2)]
                        hb16 = [sbu("hb16_%d" % i, [128, D], BF16) for i in range(2)]
                        r_hb16 = [Res("hb16_%d" % i) for i in range(2)]
                        st4 = [sbu("st4_%d" % i, [128, 8]) for i in range(2)]
                        r_st4 = [Res("st4_%d" % i) for i in range(2)]
                        mo = [sbu("mo%d" % i, [128, D]) for i in range(2)]
                        r_mo = [Res("mo%d" % i) for i in range(2)]
                        tkb = [sbu("tkb%d" % i, [128, 8, 128], BF16) for i in range(2)]
                        r_tkb = [Res("tkb%d" % i) for i in range(2)]
                        for blk in range(ST // 128):
                            i = blk % 2
                            r0 = t0 + blk * 128
                            fw.dma("sync", lambda e, i=i, r0=r0: e.dma_start(out=xb[i][:], in_=H0[r0:r0 + 128, :]), writes=[r_xb[i]])
                            for nh in range(2):
                                j = cnt["pA"] % 4
                                cnt["pA"] += 1
                                for k in range(8):
                                    fw.op("tensor", lambda e, k=k, j=j, nh=nh, blk=blk: e.matmul(
                                        pA[j][:], lhsT=hT[:, k, blk * 128:(blk + 1) * 128], rhs=wo[:, k, nh * 512:(nh + 1) * 512],
                                        start=(k == 0), stop=(k == 7)), reads=[r_hT, r_wo], writes=[r_pA[j]], pe_acc=True)
                                fw.op("scalar", lambda e, i=i, j=j, nh=nh: e.activation(
                                    out=mo[i][:, nh * 512:(nh + 1) * 512], in_=pA[j][:], func=AF.Copy),
                                    reads=[r_pA[j]], writes=[r_mo[i]])
                            if debug:
                                fw.dma("sync", lambda e, i=i, r0=r0: e.dma_start(out=dbg["mix"][r0:r0 + 128, :], in_=mo[i][:]),
                                       reads=[r_mo[i]])
                            fw.op("vector", lambda e, i=i: e.scalar_tensor_tensor(
                                out=xb[i][:], in0=xb[i][:], scalar=ALPHA, in1=mo[i][:], op0=ALU.mult, op1=ALU.add),
                                reads=[r_xb[i], r_mo[i]], writes=[r_xb[i]])
                            ln_rows(fw, xb[i], r_xb[i], tb[i], r_tb[i], st4[i], r_st4[i], gam, bet, r_gb, xb[i], r_xb[i])
                            if debug:
                                fw.dma("sync", lambda e, i=i, r0=r0: e.dma_start(out=dbg["st"][r0:r0 + 128, :], in_=st4[i][:]),
                                       reads=[r_st4[i]])
                            fw.dma("sync", lambda e, i=i, r0=r0: e.dma_start(out=H1[r0:r0 + 128, :], in_=xb[i][:]), reads=[r_xb[i]])
                            fw.op("scalar", lambda e, i=i: e.activation(out=hb16[i][:], in_=xb[i][:], func=AF.Copy),
                                  reads=[r_xb[i]], writes=[r_hb16[i]])
                            for k in range(8):
                                fw.op("tensor", lambda e, k=k, i=i: e.transpose(pT[i][:, k, :], hb16[i][:, k * 128:(k + 1) * 128], ident[:]),
                                      reads=[r_hb16[i], r_ident], writes=[r_pT[i]], pe_acc=True)
                            fw.op("vector", lambda e, i=i: e.tensor_copy(out=tkb[i][:], in_=pT[i][:]), reads=[r_pT[i]], writes=[r_tkb[i]])
                            fw.dma("sync", lambda e, i=i, r0=r0: e.dma_start(
                                out=HT[:, :, r0:r0 + 128].rearrange("k p n -> p k n"), in_=tkb[i][:]), reads=[r_tkb[i]])
                    fw.barrier()
                    if stop_after == "ln1":
                        continue
                    with ExitStack() as S4:
                        def sbv(name, shape, dt=F32):
                            return S4.enter_context(nc.sbuf_tensor(uname("b5_" + name), shape, dt))
                        fw.dma("sync", lambda e, t0=t0: e.dma_start(
                            out=hT[:], in_=HT[:, :, t0:t0 + ST].rearrange("k p n -> p k n")), writes=[r_hT])
                        wr32 = sbv("wr32", [128, 8, 20])
                        wr = sbv("wr", [128, 8, 20], BF16)
                        r_wr = Res("wr")
                        fw.dma("sync", lambda e: e.dma_start(out=wr32[:, :, 0:4], in_=W["router_group_w"][l].rearrange("(k p) n -> p k n", p=128),
                                                             allow_slow_non_contiguous=True), writes=[r_wr])
                        fw.dma("sync", lambda e: e.dma_start(out=wr32[:, :, 4:20], in_=W["router_expert_w"][l].rearrange("(k p) n -> p k n", p=128),
                                                             allow_slow_non_contiguous=True), writes=[r_wr])
                        fw.op("vector", lambda e: e.tensor_copy(out=wr[:], in_=wr32[:]), reads=[r_wr], writes=[r_wr])
                        rb = sbv("rb", [128, 20])
                        r_rb = Res("rb")
                        fw.dma("sync", lambda e: e.dma_start(out=rb[:, 0:4], in_=W["router_group_b"][l].partition_broadcast(128)), writes=[r_rb])
                        fw.dma("sync", lambda e: e.dma_start(out=rb[:, 4:20], in_=W["router_expert_b"][l].partition_broadcast(128)), writes=[r_rb])
                        comb = sbv("comb", [128, ST // 128, 16])
                        r_comb = Res("comb")
                        rt = sbv("rt", [128, 64])
                        r_rt = Res("rt")
                        for blk in range(ST // 128):
                            xk = blk % 2
                            for k in range(8):
                                fw.op("tensor", lambda e, k=k, xk=xk, blk=blk: e.matmul(
                                    pX[xk][:, 0:20], lhsT=hT[:, k, blk * 128:(blk + 1) * 128], rhs=wr[:, k, :],
                                    start=(k == 0), stop=(k == 7)), reads=[r_hT, r_wr], writes=[r_pX[xk]], pe_acc=True)
                            lg = rt[:, 0:20]

                            def V(fn, extra_r=()):
                                fw.op("vector", fn, reads=[r_rt] + list(extra_r), writes=[r_rt])
                            fw.op("vector", lambda e, xk=xk: e.tensor_tensor(out=rt[:, 0:20], in0=pX[xk][:, 0:20], in1=rb[:], op=ALU.add),
                                  reads=[r_pX[xk], r_rb], writes=[r_rt])
                            V(lambda e: e.reduce_max(out=rt[:, 20:21], in_=rt[:, 0:4], axis=AX.X))
                            V(lambda e: e.tensor_scalar(out=rt[:, 24:28], in0=rt[:, 0:4], scalar1=rt[:, 20:21], scalar2=None, op0=ALU.subtract))
                            fw.op("scalar", lambda e: e.activation(out=rt[:, 28:32], in_=rt[:, 24:28], func=AF.Exp), reads=[r_rt], writes=[r_rt])
                            V(lambda e: e.reduce_sum(out=rt[:, 21:22], in_=rt[:, 28:32], axis=AX.X))
                            V(lambda e: e.reciprocal(out=rt[:, 21:22], in_=rt[:, 21:22]))
                            V(lambda e: e.tensor_scalar(out=rt[:, 24:28], in0=rt[:, 24:28], scalar1=-1e30, scalar2=1.0, op0=ALU.mult, op1=ALU.min))
                            V(lambda e: e.tensor_scalar(out=rt[:, 24:28], in0=rt[:, 24:28], scalar1=-1.0, scalar2=1.0, op0=ALU.mult, op1=ALU.add))
                            V(lambda e: e.tensor_scalar(out=rt[:, 32:36], in0=rt[:, 4:8], scalar1=rt[:, 24:25], scalar2=None, op0=ALU.mult))
                            for gq in range(1, 4):
                                V(lambda e, gq=gq: e.scalar_tensor_tensor(out=rt[:, 32:36], in0=rt[:, 4 + 4 * gq:8 + 4 * gq],
                                                                          scalar=rt[:, 24 + gq:25 + gq], in1=rt[:, 32:36],
                                                                          op0=ALU.mult, op1=ALU.add))
                            V(lambda e: e.reduce_max(out=rt[:, 22:23], in_=rt[:, 32:36], axis=AX.X))
                            V(lambda e: e.tensor_scalar(out=rt[:, 36:40], in0=rt[:, 32:36], scalar1=rt[:, 22:23], scalar2=None, op0=ALU.subtract))
                            V(lambda e: e.tensor_scalar(out=rt[:, 36:40], in0=rt[:, 36:40], scalar1=-1e30, scalar2=1.0, op0=ALU.mult, op1=ALU.min))
                            V(lambda e: e.tensor_scalar(out=rt[:, 36:40], in0=rt[:, 36:40], scalar1=-1.0, scalar2=1.0, op0=ALU.mult, op1=ALU.add))
                            V(lambda e: e.scalar_tensor_tensor(out=rt[:, 40:44], in0=rt[:, 36:40], scalar=-1e4, in1=rt[:, 32:36],
                                                               op0=ALU.mult, op1=ALU.add))
                            V(lambda e: e.reduce_max(out=rt[:, 23:24], in_=rt[:, 40:44], axis=AX.X))
                            V(lambda e: e.tensor_scalar(out=rt[:, 44:48], in0=rt[:, 40:44], scalar1=rt[:, 23:24], scalar2=None, op0=ALU.subtract))
                            V(lambda e: e.tensor_scalar(out=rt[:, 44:48], in0=rt[:, 44:48], scalar1=-1e30, scalar2=1.0, op0=ALU.mult, op1=ALU.min))
                            V(lambda e: e.tensor_scalar(out=rt[:, 44:48], in0=rt[:, 44:48], scalar1=-1.0, scalar2=1.0, op0=ALU.mult, op1=ALU.add))
                            V(lambda e: e.tensor_tensor(out=rt[:, 48:49], in0=rt[:, 23:24], in1=rt[:, 22:23], op=ALU.subtract))
                            fw.op("scalar", lambda e: e.activation(out=rt[:, 49:50], in_=rt[:, 48:49], func=AF.Exp), reads=[r_rt], writes=[r_rt])
                            V(lambda e: e.tensor_scalar(out=rt[:, 50:51], in0=rt[:, 49:50], scalar1=1.0, scalar2=None, op0=ALU.add))
                            V(lambda e: e.reciprocal(out=rt[:, 50:51], in_=rt[:, 50:51]))
                            V(lambda e: e.tensor_tensor(out=rt[:, 51:52], in0=rt[:, 49:50], in1=rt[:, 50:51], op=ALU.mult))
                            V(lambda e: e.tensor_tensor(out=rt[:, 50:51], in0=rt[:, 50:51], in1=rt[:, 21:22], op=ALU.mult))
                            V(lambda e: e.tensor_tensor(out=rt[:, 51:52], in0=rt[:, 51:52], in1=rt[:, 21:22], op=ALU.mult))
                            V(lambda e: e.tensor_scalar(out=rt[:, 52:56], in0=rt[:, 36:40], scalar1=rt[:, 50:51], scalar2=None, op0=ALU.mult))
                            V(lambda e: e.scalar_tensor_tensor(out=rt[:, 52:56], in0=rt[:, 44:48], scalar=rt[:, 51:52], in1=rt[:, 52:56],
                                                               op0=ALU.mult, op1=ALU.add))
                            for gq in range(4):
                                fw.op("vector", lambda e, gq=gq, blk=blk: e.tensor_scalar(
                                    out=comb[:, blk, 4 * gq:4 * gq + 4], in0=rt[:, 52:56], scalar1=rt[:, 24 + gq:25 + gq], scalar2=None,
                                    op0=ALU.mult), reads=[r_rt], writes=[r_comb])
                        wg32 = sbv("wg32", [128, 8, 256])
                        wu32 = sbv("wu32", [128, 8, 256])
                        wgb = sbv("wgb", [128, 8, 256], BF16)
                        wub = sbv("wub", [128, 8, 256], BF16)
                        wd32 = sbv("wd32", [128, 2, D])
                        wdb = sbv("wdb", [128, 2, D], BF16)
                        r_wg32, r_wu32, r_wd32 = Res("wg32"), Res("wu32"), Res("wd32")
                        r_wgb, r_wub, r_wdb = Res("wgb"), Res("wub"), Res("wdb")
                        HB = ST // 256
                        macc = sbv("macc", [128, HB, D])
                        r_macc = Res("macc")
                        actT = sbv("actT", [128, 2, ST // 2], BF16)
                        r_act = Res("actT")
                        sgl = [sbv("sgl%d" % i, [128, 512]) for i in range(2)]
                        r_sgl = [Res("sgl%d" % i) for i in range(2)]
                        xq = [sbv("xq%d" % i, [128, D]) for i in range(2)]
                        r_xq = [Res("xq%d" % i) for i in range(2)]
                        tq = [sbv("tq%d" % i, [128, D]) for i in range(2)]
                        r_tq = [Res("tq%d" % i) for i in range(2)]
                        sq4 = [sbv("sq4_%d" % i, [128, 8]) for i in range(2)]
                        r_sq4 = [Res("sq4_%d" % i) for i in range(2)]
                        load_gb("ln2_g", "ln2_b", l)
                        for hv in range(2):
                            c0 = hv * (ST // 2)
                            for ex in range(16):
                                fw.dma("sync", lambda e, ex=ex: e.dma_start(
                                    out=wg32[:], in_=W["expert_w_gate"][l, ex].rearrange("(k p) n -> p k n", p=128)), writes=[r_wg32])
                                fw.op("gpsimd", lambda e: e.tensor_copy(out=wgb[:], in_=wg32[:]), reads=[r_wg32], writes=[r_wgb])
                                fw.dma("sync", lambda e, ex=ex: e.dma_start(
                                    out=wu32[:], in_=W["expert_w_up"][l, ex].rearrange("(k p) n -> p k n", p=128)), writes=[r_wu32])
                                fw.op("gpsimd", lambda e: e.tensor_copy(out=wub[:], in_=wu32[:]), reads=[r_wu32], writes=[r_wub])
                                fw.dma("sync", lambda e, ex=ex: e.dma_start(
                                    out=wd32[:], in_=W["expert_w_down"][l, ex].rearrange("(c p) n -> p c n", p=128)), writes=[r_wd32])
                                fw.op("gpsimd", lambda e: e.tensor_copy(out=wdb[:], in_=wd32[:]), reads=[r_wd32], writes=[r_wdb])
                                for c in range(2):
                                    for ts in range(ST // 1024):
                                        o = c0 + ts * 512
                                        j = cnt["pA"] % 4
                                        cnt["pA"] += 1
                                        j2 = cnt["pA"] % 4
                                        cnt["pA"] += 1
                                        for k in range(8):
                                            fw.op("tensor", lambda e, k=k, j=j, c=c, o=o: e.matmul(
                                                pA[j][:], lhsT=wgb[:, k, c * 128:(c + 1) * 128], rhs=hT[:, k, o:o + 512],
                                                start=(k == 0), stop=(k == 7)), reads=[r_wgb, r_hT], writes=[r_pA[j]], pe_acc=True)
                                        for k in range(8):
                                            fw.op("tensor", lambda e, k=k, j2=j2, c=c, o=o: e.matmul(
                                                pA[j2][:], lhsT=wub[:, k, c * 128:(c + 1) * 128], rhs=hT[:, k, o:o + 512],
                                                start=(k == 0), stop=(k == 7)), reads=[r_wub, r_hT], writes=[r_pA[j2]], pe_acc=True)
                                        si = (c + ts) % 2
                                        fw.op("scalar", lambda e, j=j, si=si: e.activation(out=sgl[si][:], in_=pA[j][:], func=AF.Silu),
                                              reads=[r_pA[j]], writes=[r_sgl[si]])
                                        fw.op("vector", lambda e, j2=j2, si=si, c=c, ts=ts: e.tensor_tensor(
                                            out=actT[:, c, ts * 512:(ts + 1) * 512], in0=sgl[si][:], in1=pA[j2][:], op=ALU.mult),
                                            reads=[r_sgl[si], r_pA[j2]], writes=[r_act])
                                for bl in range(HB):
                                    blk = hv * HB + bl
                                    for nh in range(2):
                                        xk = (bl * 2 + nh) % 2
                                        for c in range(2):
                                            fw.op("tensor", lambda e, c=c, xk=xk, bl=bl, nh=nh: e.matmul(
                                                pX[xk][:], lhsT=actT[:, c, bl * 128:(bl + 1) * 128], rhs=wdb[:, c, nh * 512:(nh + 1) * 512],
                                                start=(c == 0), stop=(c == 1)), reads=[r_act, r_wdb], writes=[r_pX[xk]], pe_acc=True)
                                        if ex == 0:
                                            fw.op("vector", lambda e, xk=xk, bl=bl, nh=nh, blk=blk, ex=ex: e.tensor_scalar(
                                                out=macc[:, bl, nh * 512:(nh + 1) * 512], in0=pX[xk][:], scalar1=comb[:, blk, ex:ex + 1],
                                                scalar2=None, op0=ALU.mult), reads=[r_pX[xk], r_comb], writes=[r_macc])
                                        else:
                                            fw.op("vector", lambda e, xk=xk, bl=bl, nh=nh, blk=blk, ex=ex: e.scalar_tensor_tensor(
                                                out=macc[:, bl, nh * 512:(nh + 1) * 512], in0=pX[xk][:], scalar=comb[:, blk, ex:ex + 1],
                                                in1=macc[:, bl, nh * 512:(nh + 1) * 512], op0=ALU.mult, op1=ALU.add),
                                                reads=[r_pX[xk], r_comb, r_macc], writes=[r_macc])
                            dst = y if last else H0
                            for bl in range(HB):
                                i = bl % 2
                                r0 = t0 + (hv * HB + bl) * 128
                                fw.dma("sync", lambda e, i=i, r0=r0: e.dma_start(out=xq[i][:], in_=H1[r0:r0 + 128, :]), writes=[r_xq[i]])
                                fw.op("vector", lambda e, i=i, bl=bl: e.scalar_tensor_tensor(
                                    out=xq[i][:], in0=xq[i][:], scalar=ALPHA, in1=macc[:, bl, :], op0=ALU.mult, op1=ALU.add),
                                    reads=[r_xq[i], r_macc], writes=[r_xq[i]])
                                ln_rows(fw, xq[i], r_xq[i], tq[i], r_tq[i], sq4[i], r_sq4[i], gam, bet, r_gb, xq[i], r_xq[i])
                                fw.dma("sync", lambda e, i=i, r0=r0, dst=dst: e.dma_start(out=dst[r0:r0 + 128, :], in_=xq[i][:]),
                                       reads=[r_xq[i]])
                    fw.barrier()
            fw.barrier()

        if debug:
            dbg["br"] = nc.dram_tensor("dbg_br", [10, 128, NTOK], BF16, kind="ExternalOutput").ap()
        for l in range(depth):
            phase_a(l)
            if stop_after in ("a", "ua"):
                break
            phase_s5(l)
            if stop_after == "s5":
                break
            phase_h()
            phase_b(l, l == depth - 1)
            if stop_after in ("attn", "branches", "ln1"):
                break
    return nc, fw


def ln_rows(fw, xin, r_xin, tmp, r_tmp, s, r_s, gam, bet, r_gb, out_tile, r_out):
    fw.op("vector", lambda e: e.reduce_sum(out=s[:, 0:1], in_=xin[:], axis=AX.X), reads=[r_xin], writes=[r_s])
    fw.op("scalar", lambda e: e.activation(out=tmp[:], in_=xin[:], func=AF.Square), reads=[r_xin, r_s], writes=[r_tmp])
    fw.op("vector", lambda e: e.reduce_sum(out=s[:, 1:2], in_=tmp[:], axis=AX.X), reads=[r_tmp], writes=[r_s])
    fw.op("vector", lambda e: e.tensor_scalar(out=s[:, 2:3], in0=s[:, 0:1], scalar1=1.0 / D, scalar2=None,
                                              op0=ALU.mult), reads=[r_s], writes=[r_s])
    fw.op("vector", lambda e: e.tensor_tensor(out=s[:, 3:4], in0=s[:, 2:3], in1=s[:, 2:3], op=ALU.mult),
          reads=[r_s], writes=[r_s])
    fw.op("vector", lambda e: e.scalar_tensor_tensor(out=s[:, 4:5], in0=s[:, 1:2], scalar=1.0 / D, in1=s[:, 3:4],
                                                     op0=ALU.mult, op1=ALU.subtract), reads=[r_s], writes=[r_s])
    fw.op("vector", lambda e: e.tensor_scalar(out=s[:, 4:5], in0=s[:, 4:5], scalar1=LN_EPS, scalar2=None,
                                              op0=ALU.add), reads=[r_s], writes=[r_s])
    fw.op("scalar", lambda e: e.activation(out=s[:, 5:6], in_=s[:, 4:5], func=AF.Sqrt), reads=[r_s], writes=[r_s])
    fw.op("vector", lambda e: e.reciprocal(out=s[:, 6:7], in_=s[:, 5:6]), reads=[r_s], writes=[r_s])
    fw.op("vector", lambda e: e.scalar_tensor_tensor(out=s[:, 7:8], in0=s[:, 2:3], scalar=-1.0, in1=s[:, 6:7],
                                                     op0=ALU.mult, op1=ALU.mult), reads=[r_s], writes=[r_s])
    fw.op("vector", lambda e: e.tensor_scalar(out=tmp[:], in0=xin[:], scalar1=s[:, 2:3], scalar2=s[:, 6:7],
                                              op0=ALU.subtract, op1=ALU.mult), reads=[r_xin, r_s], writes=[r_tmp])
    fw.op("vector", lambda e: e.tensor_tensor(out=tmp[:], in0=tmp[:], in1=gam[:], op=ALU.mult),
          reads=[r_tmp, r_gb], writes=[r_tmp])
    fw.op("vector", lambda e: e.tensor_tensor(out=out_tile[:], in0=tmp[:], in1=bet[:], op=ALU.add),
          reads=[r_tmp, r_gb], writes=[r_out])


_CACHE = {}


def kernel(**inputs):
    xp = np.ascontiguousarray(inputs["x_prompt"], dtype=np.float32)
    xs = np.ascontiguousarray(inputs["x_sample"], dtype=np.float32)
    slots = {0: [("s", 0)], 1: [("s", 1)], 2: [("p", 0), ("p", 1)], 3: [("p", 2), ("p", 3)],
             4: [("p", 4)], 5: [("p", 5)], 6: [("p", 6)], 7: [("p", 7)]}
    in_maps = []
    for c in range(8):
        xc = np.zeros((NTOK, D), np.float32)
        fl = np.zeros(5, np.float32)
        if slots[c][0][0] == "s":
            xc[:] = xs[slots[c][0][1]]
            fl[1:4] = 1.0
        else:
            for j, (_, pi) in enumerate(slots[c]):
                xc[j * SEG:(j + 1) * SEG] = xp[pi]
        m = {"x": xc, "flags": fl}
        for n in WNAMES:
            m[n] = np.ascontiguousarray(inputs[n], dtype=np.float32)
        in_maps.append(m)
    if "nc" not in _CACHE:
        nc, fw = build()
        fw.finish(_CACHE.get("final", []))
        _CACHE["nc"] = nc
    res = run_bass_kernel_spmd(_CACHE["nc"], in_maps, core_ids=list(range(8)))
    yp = np.zeros_like(xp)
    ys = np.zeros_like(xs)
    for c in range(8):
        yc = np.asarray(res.results[c]["y"], dtype=np.float32)
        if slots[c][0][0] == "s":
            ys[slots[c][0][1]] = yc
        else:
            for j, (_, pi) in enumerate(slots[c]):
                yp[pi] = yc[j * SEG:(j + 1) * SEG]
    return (yp, ys)
```

```python
import math
import numpy as np
from contextlib import ExitStack
import concourse.bass as bass
import concourse.mybir as mybir
from concourse.bass_utils import run_bass_kernel_spmd

F32 = mybir.dt.float32
BF16 = mybir.dt.bfloat16
AF = mybir.ActivationFunctionType
ALU = mybir.AluOpType
AX = mybir.AxisListType

D = 1024
NSEG = 4
SEG = 4096
NTOK = NSEG * SEG
PAD = 1024
SEGP = SEG + 2 * PAD
NTOKP = NSEG * SEGP
ST = 2048
NST = NTOK // ST
DEPTH = 2
ALPHA = (2 * DEPTH) ** 0.25
LN_EPS = 1e-5
IN_COLS = 7424
SLOPES = [2.0 ** (-8.0 * (i + 1) / 12) for i in range(12)]
DIL = [(128, 1), (512, 4), (2048, 16)]

WNAMES = ["ln_in_g", "ln_in_b", "w_in", "s5_a_re", "s5_a_im", "s5_log_dt", "s5_b_re", "s5_b_im", "s5_c_re",
          "s5_c_im", "s5_d", "s5_glu_w", "s5_glu_b", "conv_w", "conv_b", "swa_sink", "w_branch_a", "w_branch_b",
          "w_branch_c", "w_branch_d", "w_o", "ln1_g", "ln1_b", "router_group_w", "router_group_b",
          "router_expert_w", "router_expert_b", "expert_w_gate", "expert_w_up", "expert_w_down", "ln2_g", "ln2_b"]
WSHAPES = {
    "ln_in_g": [D], "ln_in_b": [D], "w_in": [2, D, IN_COLS], "s5_a_re": [2, 2, 24, 64], "s5_a_im": [2, 2, 24, 64],
    "s5_log_dt": [2, 2, 24], "s5_b_re": [2, 2, 24, 64, 16], "s5_b_im": [2, 2, 24, 64, 16],
    "s5_c_re": [2, 2, 24, 16, 64], "s5_c_im": [2, 2, 24, 16, 64], "s5_d": [2, 384], "s5_glu_w": [2, 384, 384],
    "s5_glu_b": [2, 384], "conv_w": [2, 3, 384], "conv_b": [2, 384], "swa_sink": [2, 6],
    "w_branch_a": [2, 384, D], "w_branch_b": [2, 384, D], "w_branch_c": [2, 128, D], "w_branch_d": [2, 384, D],
    "w_o": [2, D, D], "ln1_g": [2, D], "ln1_b": [2, D], "router_group_w": [2, D, 4], "router_group_b": [2, 4],
    "router_expert_w": [2, D, 16], "router_expert_b": [2, 16], "expert_w_gate": [2, 16, D, 256],
    "expert_w_up": [2, 16, D, 256], "expert_w_down": [2, 16, 256, D], "ln2_g": [2, D], "ln2_b": [2, D],
}


ENGS = ["sync", "scalar", "vector", "gpsimd", "tensor"]
SEM_ROLL = 30000


class Res:
    __slots__ = ("name", "w", "r")

    def __init__(self, name):
        self.name = name
        self.w = None
        self.r = []


class FW:
    def __init__(self, nc, es):
        self.nc = nc
        self.es = es
        self.q = {e: [] for e in ENGS}
        self.sems = {e: [es.enter_context(nc.semaphore("s_" + e + "0"))] for e in ENGS}
        self.cnt = {e: 0 for e in ENGS}
        self.seen = {e: {} for e in ENGS}
        self.dma_sems = [es.enter_context(nc.semaphore("d%d" % i)) for i in range(24)]
        self.dma_cnt = [0] * 24
        self.dma_i = 0
        self.n_ops = 0
        self.fence = []

    def barrier(self):
        f = []
        for e in ENGS:
            if self.cnt[e] > 0:
                f.append((self.sems[e][-1], self.cnt[e], e))
        for k in range(len(self.dma_sems)):
            if self.dma_cnt[k] > 0:
                f.append((self.dma_sems[k], self.dma_cnt[k], "dma"))
        self.fence = f

    def _ev_new(self, eng):
        if self.cnt[eng] >= SEM_ROLL:
            self.sems[eng].append(self.es.enter_context(self.nc.semaphore("s_%s%d" % (eng, len(self.sems[eng])))))
            self.cnt[eng] = 0
        self.cnt[eng] += 1
        return (self.sems[eng][-1], self.cnt[eng], eng)

    def _need(self, eng, ev, waits, pe_ok=False):
        if ev is None:
            return
        sem, val, src = ev
        if pe_ok and src == "tensor" and eng == "tensor":
            return
        key = id(sem)
        if self.seen[eng].get(key, 0) >= val:
            return
        if key not in waits or waits[key][1] < val:
            waits[key] = (sem, val)

    def op(self, eng, fn, reads=(), writes=(), pe_acc=False):
        waits = {}
        for ev in self.fence:
            self._need(eng, ev, waits)
        for r in reads:
            self._need(eng, r.w, waits)
        for w in writes:
            self._need(eng, w.w, waits, pe_ok=pe_acc)
            for ev in w.r:
                self._need(eng, ev, waits)
        for key, (sem, val) in waits.items():
            self.seen[eng][key] = val
        ev = self._ev_new(eng)
        self.q[eng].append((list(waits.values()), fn, (ev[0], 1)))
        for r in reads:
            r.r.append(ev)
        for w in writes:
            w.w = ev
            w.r = []
        self.n_ops += 1
        return ev

    def dma(self, eng, fn, reads=(), writes=()):
        if len(writes) == 0 and eng == "sync":
            eng = "scalar"
        waits = {}
        for ev in self.fence:
            self._need(eng, ev, waits)
        for r in reads:
            self._need(eng, r.w, waits)
        for w in writes:
            self._need(eng, w.w, waits)
            for ev in w.r:
                self._need(eng, ev, waits)
        k = self.dma_i % len(self.dma_sems)
        self.dma_i += 1
        sem = self.dma_sems[k]
        if self.dma_cnt[k] > 0:
            self._need(eng, (sem, self.dma_cnt[k], "dma"), waits)
        for key, (s, val) in waits.items():
            self.seen[eng][key] = val
        self.dma_cnt[k] += 16
        ev = (sem, self.dma_cnt[k], "dma")
        self.q[eng].append((list(waits.values()), fn, (sem, 16)))
        for r in reads:
            r.r.append(ev)
        for w in writes:
            w.w = ev
            w.r = []
        self.n_ops += 1
        return ev

    def finish(self, final_res):
        waits = {}
        for r in final_res:
            self._need("sync", r.w, waits)
        for k in range(len(self.dma_sems)):
            if self.dma_cnt[k] > 0:
                self._need("sync", (self.dma_sems[k], self.dma_cnt[k], "dma"), waits)
        tail = list(waits.values())
        q = self.q
        with self.nc.Block() as block:
            def replay(e, name):
                for ws, fn, inc in q[name]:
                    for sem, val in ws:
                        e.wait_ge(sem, val)
                    fn(e).then_inc(inc[0], inc[1])
                if name == "sync":
                    for sem, val in tail:
                        e.wait_ge(sem, val)

            @block.sync
            def _(e):
                replay(e, "sync")

            @block.scalar
            def _(e):
                replay(e, "scalar")

            @block.vector
            def _(e):
                replay(e, "vector")

            @block.gpsimd
            def _(e):
                replay(e, "gpsimd")

            @block.tensor
            def _(e):
                replay(e, "tensor")


def build(debug=False, stop_after=None, depth=DEPTH):
    nc = bass.Bass("TRN2", target_bir_lowering=False)
    x = nc.dram_tensor("x", [NTOK, D], F32, kind="ExternalInput").ap()
    flags_d = nc.dram_tensor("flags", [5], F32, kind="ExternalInput").ap()
    W = {n: nc.dram_tensor(n, WSHAPES[n], F32, kind="ExternalInput").ap() for n in WNAMES}
    y = nc.dram_tensor("y", [NTOK, D], F32, kind="ExternalOutput").ap()
    H0 = nc.dram_tensor("H0", [NTOK, D], F32).ap()
    H1 = nc.dram_tensor("H1", [NTOK, D], F32, kind=("ExternalOutput" if debug else "Internal")).ap()
    HT = nc.dram_tensor("HT", [8, 128, NTOK], BF16).ap()
    UAs = nc.dram_tensor("UAs", [3, 128, NTOK], F32).ap()
    CVs = nc.dram_tensor("CVs", [3, 128, NTOKP], BF16).ap()
    KTs = nc.dram_tensor("KTs", [4, 128, NTOKP], BF16).ap()
    VTs = nc.dram_tensor("VTs", [4, 128, NTOKP], BF16).ap()
    MTs = nc.dram_tensor("MTs", [8, 128, NTOK], BF16, kind=("ExternalOutput" if debug else "Internal")).ap()
    YA = nc.dram_tensor("YA", [2, 3, 128, NTOK], F32, kind=("ExternalOutput" if debug else "Internal")).ap()
    dbg = {}
    if debug:
        dbg["h0"] = nc.dram_tensor("dbg_h0", [NTOK, D], F32, kind="ExternalOutput").ap()
        dbg["ua"] = nc.dram_tensor("dbg_ua", [3, 128, NTOK], F32, kind="ExternalOutput").ap()
        dbg["mix"] = nc.dram_tensor("dbg_mix", [NTOK, D], F32, kind="ExternalOutput").ap()
        dbg["st"] = nc.dram_tensor("dbg_st", [NTOK, 8], F32, kind="ExternalOutput").ap()

    es = ExitStack()
    with es:
        fw = FW(nc, es)

        def sb(name, shape, dt=F32):
            return es.enter_context(nc.sbuf_tensor(name, shape, dt))

        def ps(name, shape, dt=F32):
            return es.enter_context(nc.psum_tensor(name, shape, dt))

        ident = sb("ident", [128, 128], BF16)
        r_ident = Res("ident")
        fw.op("gpsimd", lambda e: e.memset(ident[:], 0.0), writes=[r_ident])
        fw.op("gpsimd", lambda e: e.affine_select(out=ident[:], in_=ident[:], pattern=[[-1, 128]],
                                                  compare_op=ALU.not_equal, fill=1.0, base=0, channel_multiplier=1),
              reads=[r_ident], writes=[r_ident])
        flg = sb("flg", [128, 5])
        r_flg = Res("flg")
        fw.dma("sync", lambda e: e.dma_start(out=flg[:], in_=flags_d.partition_broadcast(128)), writes=[r_flg])
        gam = sb("gam", [128, D])
        bet = sb("bet", [128, D])
        r_gb = Res("gb")

        def load_gb(gname, bname, l):
            gsrc = W[gname] if l is None else W[gname][l]
            bsrc = W[bname] if l is None else W[bname][l]
            fw.dma("sync", lambda e: e.dma_start(out=gam[:], in_=gsrc.partition_broadcast(128)), writes=[r_gb])
            fw.dma("sync", lambda e: e.dma_start(out=bet[:], in_=bsrc.partition_broadcast(128)), writes=[r_gb])

        pT = [ps("pT%d" % i, [128, 8, 128], BF16) for i in range(2)]
        r_pT = [Res("pT%d" % i) for i in range(2)]
        pA = [ps("pA%d" % i, [128, 512]) for i in range(4)]
        r_pA = [Res("pA%d" % i) for i in range(4)]
        pX = [ps("pX%d" % i, [128, 512]) for i in range(2)]
        r_pX = [Res("pX%d" % i) for i in range(2)]
        cnt = {"w": 0, "pA": 0, "blk": 0, "uid": 0}

        def uname(n):
            cnt["uid"] += 1
            return "%s_%d" % (n, cnt["uid"])

        def padpos(t):
            return (t // SEG) * SEGP + PAD + (t % SEG)

        def phase_a(l):
            with ExitStack() as pes:
                def sb2(name, shape, dt=F32):
                    return pes.enter_context(nc.sbuf_tensor(uname("a_" + name), shape, dt))
                hT = sb2("hT", [128, 8, ST], BF16)
                r_hT = Res("hT")
                xb = [sb2("xb%d" % i, [128, D]) for i in range(2)]
                r_xb = [Res("xb%d" % i) for i in range(2)]
                tb = [sb2("tb%d" % i, [128, D]) for i in range(2)]
                r_tb = [Res("tb%d" % i) for i in range(2)]
                hb16 = [sb2("hb16_%d" % i, [128, D], BF16) for i in range(2)]
                r_hb16 = [Res("hb16_%d" % i) for i in range(2)]
                st4 = [sb2("st4_%d" % i, [128, 8]) for i in range(2)]
                r_st4 = [Res("st4_%d" % i) for i in range(2)]
                wst = [sb2("wst%d" % i, [128, 8, 128]) for i in range(2)]
                r_wst = [Res("wst%d" % i) for i in range(2)]
                wbf = [sb2("wbf%d" % i, [128, 8, 128], BF16) for i in range(2)]
                r_wbf = [Res("wbf%d" % i) for i in range(2)]
                zt0 = sb2("zt0", [128, ST], BF16)
                r_zt0 = Res("zt0")
                zf = [sb2("zf%d" % i, [128, 512]) for i in range(4)]
                r_zf = [Res("zf%d" % i) for i in range(4)]
                zb = [sb2("zb%d" % i, [128, 512], BF16) for i in range(4)]
                r_zb = [Res("zb%d" % i) for i in range(4)]

                def layer_norm_block(i):
                    ln_rows(fw, xb[i], r_xb[i], tb[i], r_tb[i], st4[i], r_st4[i], gam, bet, r_gb, xb[i], r_xb[i])

                def transpose_block(i, col0):
                    fw.op("scalar", lambda e: e.activation(out=hb16[i][:], in_=xb[i][:], func=AF.Copy),
                          reads=[r_xb[i]], writes=[r_hb16[i]])
                    for k in range(8):
                        fw.op("tensor", lambda e, k=k: e.transpose(pT[i][:, k, :], hb16[i][:, k * 128:(k + 1) * 128],
                                                                   ident[:]),
                              reads=[r_hb16[i], r_ident], writes=[r_pT[i]], pe_acc=True)
                    fw.op("vector", lambda e: e.tensor_copy(out=hT[:, :, col0:col0 + 128], in_=pT[i][:]),
                          reads=[r_pT[i]], writes=[r_hT])

                def load_w_chunk(src_ap):
                    j = cnt["w"] % 2
                    cnt["w"] += 1
                    fw.dma("sync", lambda e: e.dma_start(out=wst[j][:], in_=src_ap.rearrange("(k p) n -> p k n", p=128)),
                           writes=[r_wst[j]])
                    fw.op("gpsimd", lambda e: e.tensor_copy(out=wbf[j][:], in_=wst[j][:]),
                          reads=[r_wst[j]], writes=[r_wbf[j]])
                    return wbf[j], r_wbf[j]

                def proj_fm(wt, r_w, evac):
                    for ts in range(ST // 512):
                        j = cnt["pA"] % 4
                        cnt["pA"] += 1
                        for k in range(8):
                            fw.op("tensor", lambda e, k=k, j=j, ts=ts: e.matmul(
                                pA[j][:], lhsT=wt[:, k, :], rhs=hT[:, k, ts * 512:(ts + 1) * 512],
                                start=(k == 0), stop=(k == 7)),
                                reads=[r_w, r_hT], writes=[r_pA[j]], pe_acc=True)
                        evac(ts, j, pA[j], r_pA[j])

                if l == 0:
                    load_gb("ln_in_g", "ln_in_b", None)
                for st_i in range(NST):
                    t0 = st_i * ST
                    p0 = padpos(t0)
                    for b in range(ST // 128):
                        i = cnt["blk"] % 2
                        cnt["blk"] += 1
                        r0 = t0 + b * 128
                        if l == 0:
                            fw.dma("sync", lambda e, i=i, r0=r0: e.dma_start(out=xb[i][:], in_=x[r0:r0 + 128, :]),
                                   writes=[r_xb[i]])
                            layer_norm_block(i)
                            fw.dma("sync", lambda e, i=i, r0=r0: e.dma_start(out=H0[r0:r0 + 128, :], in_=xb[i][:]),
                                   reads=[r_xb[i]])
                            if debug:
                                fw.dma("sync", lambda e, i=i, r0=r0: e.dma_start(out=dbg["h0"][r0:r0 + 128, :],
                                                                                in_=xb[i][:]), reads=[r_xb[i]])
                        else:
                            fw.dma("sync", lambda e, i=i, r0=r0: e.dma_start(out=xb[i][:], in_=H0[r0:r0 + 128, :]),
                                   writes=[r_xb[i]])
                        transpose_block(i, b * 128)
                    fw.dma("sync", lambda e, t0=t0: e.dma_start(out=HT[:, :, t0:t0 + ST].rearrange("k p n -> p k n"),
                                                                in_=hT[:]), reads=[r_hT])

                    def store_plain(dst, c, t0=t0):
                        def ev(ts, j, pt, r_pt):
                            fw.op("scalar", lambda e: e.activation(out=zf[j][:], in_=pt[:], func=AF.Copy),
                                  reads=[r_pt], writes=[r_zf[j]])
                            fw.dma("sync", lambda e: e.dma_start(out=dst[c, :, t0 + ts * 512:t0 + (ts + 1) * 512],
                                                                 in_=zf[j][:]), reads=[r_zf[j]])
                            if debug and dst is UAs:
                                fw.dma("sync", lambda e: e.dma_start(
                                    out=dbg["ua"][c, :, t0 + ts * 512:t0 + (ts + 1) * 512], in_=zf[j][:]),
                                    reads=[r_zf[j]])
                        return ev

                    def store_pad(dst, c, p0=p0):
                        def ev(ts, j, pt, r_pt):
                            fw.op("scalar", lambda e: e.activation(out=zb[j][:], in_=pt[:], func=AF.Copy),
                                  reads=[r_pt], writes=[r_zb[j]])
                            fw.dma("sync", lambda e: e.dma_start(out=dst[c, :, p0 + ts * 512:p0 + (ts + 1) * 512],
                                                                 in_=zb[j][:]), reads=[r_zb[j]])
                        return ev

                    wi = W["w_in"][l]
                    for c in range(3):
                        wt, r_w = load_w_chunk(wi[:, c * 128:(c + 1) * 128])
                        proj_fm(wt, r_w, store_plain(UAs, c))
                    if stop_after == "ua":
                        continue
                    for c in range(3):
                        wt, r_w = load_w_chunk(wi[:, (15 + c) * 128:(16 + c) * 128])
                        proj_fm(wt, r_w, store_pad(KTs, c))
                    wt, r_w = load_w_chunk(wi[:, 24 * 128:25 * 128])
                    proj_fm(wt, r_w, store_pad(KTs, 3))
                    for c in range(3):
                        wt, r_w = load_w_chunk(wi[:, (18 + c) * 128:(19 + c) * 128])
                        proj_fm(wt, r_w, store_pad(VTs, c))
                    wt, r_w = load_w_chunk(wi[:, 25 * 128:26 * 128])
                    proj_fm(wt, r_w, store_pad(VTs, 3))
                    for c in range(3):
                        wt, r_w = load_w_chunk(wi[:, (3 + c) * 128:(4 + c) * 128])

                        def ev_vb(ts, j, pt, r_pt):
                            fw.op("scalar", lambda e: e.activation(out=zt0[:, ts * 512:(ts + 1) * 512], in_=pt[:],
                                                                   func=AF.Copy), reads=[r_pt], writes=[r_zt0])
                        proj_fm(wt, r_w, ev_vb)
                        wt, r_w = load_w_chunk(wi[:, (9 + c) * 128:(10 + c) * 128])

                        def ev_gc(ts, j, pt, r_pt, c=c, p0=p0):
                            fw.op("vector", lambda e: e.tensor_tensor(out=zb[j][:], in0=pt[:],
                                                                      in1=zt0[:, ts * 512:(ts + 1) * 512], op=ALU.mult),
                                  reads=[r_pt, r_zt0], writes=[r_zb[j]])
                            fw.dma("sync", lambda e: e.dma_start(out=CVs[c, :, p0 + ts * 512:p0 + (ts + 1) * 512],
                                                                 in_=zb[j][:]), reads=[r_zb[j]])
                        proj_fm(wt, r_w, ev_gc)
            fw.barrier()

        def phase_s5(l):
            with ExitStack() as pes:
                def sb2(name, shape, dt=F32):
                    return pes.enter_context(nc.sbuf_tensor(uname("s_" + name), shape, dt))
                r_p = Res("prm")
                names = ["are", "aim", "ldt", "dt", "rho", "th", "c", "s", "t1", "t2", "lr", "li", "nr", "den",
                         "numr", "numi", "kr", "ki", "nki"]
                P = {n: sb2(n, [128, 24]) for n in names}

                def tt(o, a, b, op):
                    fw.op("vector", lambda e: e.tensor_tensor(out=P[o][:], in0=P[a][:], in1=P[b][:], op=op),
                          reads=[r_p], writes=[r_p])

                def ts_(o, a, s1, op0, s2=None, op1=None):
                    if op1 is None:
                        fw.op("vector", lambda e: e.tensor_scalar(out=P[o][:], in0=P[a][:], scalar1=s1, scalar2=None,
                                                                  op0=op0), reads=[r_p], writes=[r_p])
                    else:
                        fw.op("vector", lambda e: e.tensor_scalar(out=P[o][:], in0=P[a][:], scalar1=s1, scalar2=s2,
                                                                  op0=op0, op1=op1), reads=[r_p], writes=[r_p])

                def act(o, a, func, scale=1.0):
                    fw.op("scalar", lambda e: e.activation(out=P[o][:], in_=P[a][:], func=func, scale=scale),
                          reads=[r_p], writes=[r_p])

                for d in range(2):
                    fw.dma("sync", lambda e, d=d: e.dma_start(
                        out=P["are"][:, d * 12:(d + 1) * 12],
                        in_=W["s5_a_re"][l, d].rearrange("(gp g2) p -> (g2 p) gp", g2=2),
                        allow_slow_non_contiguous=True), writes=[r_p])
                    fw.dma("sync", lambda e, d=d: e.dma_start(
                        out=P["aim"][:, d * 12:(d + 1) * 12],
                        in_=W["s5_a_im"][l, d].rearrange("(gp g2) p -> (g2 p) gp", g2=2),
                        allow_slow_non_contiguous=True), writes=[r_p])
                    for g2 in range(2):
                        fw.dma("sync", lambda e, d=d, g2=g2: e.dma_start(
                            out=P["ldt"][64 * g2:64 * g2 + 64, d * 12:(d + 1) * 12],
                            in_=W["s5_log_dt"][l, d].rearrange("(gp g2) -> g2 gp", g2=2)[g2].partition_broadcast(64),
                            allow_slow_non_contiguous=True), writes=[r_p])
                act("dt", "ldt", AF.Exp)
                tt("t1", "are", "dt", ALU.mult)
                act("rho", "t1", AF.Exp)
                tt("th", "aim", "dt", ALU.mult)
                act("t1", "th", AF.Sin, scale=1.0 / 128)
                tt("t2", "t1", "t1", ALU.mult)
                ts_("c", "t2", -2.0, ALU.mult, 1.0, ALU.add)
                act("s", "th", AF.Sin, scale=1.0 / 64)
                for _ in range(6):
                    tt("t1", "c", "c", ALU.mult)
                    tt("t2", "s", "s", ALU.mult)
                    fw.op("vector", lambda e: e.scalar_tensor_tensor(out=P["s"][:], in0=P["c"][:], scalar=2.0,
                                                                     in1=P["s"][:], op0=ALU.mult, op1=ALU.mult),
                          reads=[r_p], writes=[r_p])
                    tt("c", "t1", "t2", ALU.subtract)
                tt("lr", "rho", "c", ALU.mult)
                tt("li", "rho", "s", ALU.mult)
                ts_("nr", "lr", -1.0, ALU.add)
                tt("t1", "are", "are", ALU.mult)
                tt("t2", "aim", "aim", ALU.mult)
                tt("den", "t1", "t2", ALU.add)
                fw.op("vector", lambda e: e.reciprocal(out=P["den"][:], in_=P["den"][:]), reads=[r_p], writes=[r_p])
                tt("t1", "nr", "are", ALU.mult)
                tt("t2", "li", "aim", ALU.mult)
                tt("numr", "t1", "t2", ALU.add)
                tt("t1", "li", "are", ALU.mult)
                tt("t2", "nr", "aim", ALU.mult)
                tt("numi", "t1", "t2", ALU.subtract)
                tt("kr", "numr", "den", ALU.mult)
                tt("ki", "numi", "den", ALU.mult)
                ts_("nki", "ki", -1.0, ALU.mult)
                LRR = sb2("LRR", [128, 2, 24])
                LIS = sb2("LIS", [128, 2, 24])
                for hh in range(2):
                    fw.op("vector", lambda e, hh=hh: e.tensor_copy(out=LRR[:, hh, :], in_=P["lr"][:]),
                          reads=[r_p], writes=[r_p])
                fw.op("vector", lambda e: e.tensor_scalar(out=LIS[:, 0, :], in0=P["li"][:], scalar1=-1.0, scalar2=None,
                                                          op0=ALU.mult), reads=[r_p], writes=[r_p])
                fw.op("vector", lambda e: e.tensor_copy(out=LIS[:, 1, :], in_=P["li"][:]), reads=[r_p], writes=[r_p])

                for nm in ["l2r", "l2i"]:
                    P[nm] = sb2(nm, [128, 24])
                tt("t1", "lr", "lr", ALU.mult)
                tt("t2", "li", "li", ALU.mult)
                tt("l2r", "t1", "t2", ALU.subtract)
                fw.op("vector", lambda e: e.scalar_tensor_tensor(out=P["l2i"][:], in0=P["lr"][:], scalar=2.0,
                                                                 in1=P["li"][:], op0=ALU.mult, op1=ALU.mult),
                      reads=[r_p], writes=[r_p])
                L2RR = sb2("L2RR", [128, 2, 24])
                L2IS = sb2("L2IS", [128, 2, 24])
                for hh in range(2):
                    fw.op("vector", lambda e, hh=hh: e.tensor_copy(out=L2RR[:, hh, :], in_=P["l2r"][:]),
                          reads=[r_p], writes=[r_p])
                fw.op("vector", lambda e: e.tensor_scalar(out=L2IS[:, 0, :], in0=P["l2i"][:], scalar1=-1.0, scalar2=None,
                                                          op0=ALU.mult), reads=[r_p], writes=[r_p])
                fw.op("vector", lambda e: e.tensor_copy(out=L2IS[:, 1, :], in_=P["l2i"][:]), reads=[r_p], writes=[r_p])
                LRR64 = sb2("LRR64", [128, 2, 24, 64])
                LIS64 = sb2("LIS64", [128, 2, 24, 64])
                for (dst64, src3) in [(LRR64, LRR), (LIS64, LIS)]:
                    fw.op("vector", lambda e, dst64=dst64, src3=src3: e.tensor_copy(out=dst64[:, :, :, 0], in_=src3[:]),
                          reads=[r_p], writes=[r_p])
                    w_ = 1
                    while w_ < 64:
                        fw.op("vector", lambda e, dst64=dst64, w_=w_: e.tensor_copy(out=dst64[:, :, :, w_:2 * w_],
                                                                                   in_=dst64[:, :, :, 0:w_]),
                              reads=[r_p], writes=[r_p])
                        w_ *= 2
                for nm in ["l4r", "l4i"]:
                    P[nm] = sb2(nm, [128, 24])
                tt("t1", "l2r", "l2r", ALU.mult)
                tt("t2", "l2i", "l2i", ALU.mult)
                tt("l4r", "t1", "t2", ALU.subtract)
                fw.op("vector", lambda e: e.scalar_tensor_tensor(out=P["l4i"][:], in0=P["l2r"][:], scalar=2.0,
                                                                 in1=P["l2i"][:], op0=ALU.mult, op1=ALU.mult),
                      reads=[r_p], writes=[r_p])
                L4RR = sb2("L4RR", [128, 2, 24])
                L4IS = sb2("L4IS", [128, 2, 24])
                for hh in range(2):
                    fw.op("vector", lambda e, hh=hh: e.tensor_copy(out=L4RR[:, hh, :], in_=P["l4r"][:]),
                          reads=[r_p], writes=[r_p])
                fw.op("vector", lambda e: e.tensor_scalar(out=L4IS[:, 0, :], in0=P["l4i"][:], scalar1=-1.0, scalar2=None,
                                                          op0=ALU.mult), reads=[r_p], writes=[r_p])
                fw.op("vector", lambda e: e.tensor_copy(out=L4IS[:, 1, :], in_=P["l4i"][:]), reads=[r_p], writes=[r_p])
                L2R32 = sb2("L2R32", [128, 2, 24, 32])
                L2I32 = sb2("L2I32", [128, 2, 24, 32])
                for (dst32, src3) in [(L2R32, L2RR), (L2I32, L2IS)]:
                    fw.op("vector", lambda e, dst32=dst32, src3=src3: e.tensor_copy(out=dst32[:, :, :, 0], in_=src3[:]),
                          reads=[r_p], writes=[r_p])
                    w_ = 1
                    while w_ < 32:
                        fw.op("vector", lambda e, dst32=dst32, w_=w_: e.tensor_copy(out=dst32[:, :, :, w_:2 * w_],
                                                                                   in_=dst32[:, :, :, 0:w_]),
                              reads=[r_p], writes=[r_p])
                        w_ *= 2
                T1 = sb2("T1", [128, 2, 24, 64])
                T2 = sb2("T2", [128, 2, 24, 64])
                CB2 = T2[:, :, :, 32:64]
                CB = sb2("CB", [128, 2, 24, 64])
                r_t12 = Res("T12")
                r_cb = Res("CB")
                Bw = sb2("Bw", [128, 48, 128])
                Cw = sb2("Cw", [128, 48, 128])
                r_bw = Res("Bw")
                r_cw = Res("Cw")
                fw.op("gpsimd", lambda e: e.memset(Bw[:], 0.0), writes=[r_bw])
                fw.op("gpsimd", lambda e: e.memset(Cw[:], 0.0), writes=[r_cw])

                def widx(d, gp, ri):
                    return (d * 12 + gp) * 2 + ri
                for d in range(2):
                    for gp in range(12):
                        for g2 in range(2):
                            g = 2 * gp + g2
                            r0 = 16 * (g % 8)
                            for ri, (bn, cn) in enumerate([("s5_b_re", "s5_c_re"), ("s5_b_im", "s5_c_im")]):
                                fw.dma("sync", lambda e, d=d, gp=gp, g2=g2, g=g, r0=r0, ri=ri, bn=bn: e.dma_start(
                                    out=Bw[r0:r0 + 16, widx(d, gp, ri), 64 * g2:64 * g2 + 64],
                                    in_=W[bn][l, d, g].rearrange("p h -> h p"),
                                    allow_slow_non_contiguous=True), writes=[r_bw])
                                fw.dma("sync", lambda e, d=d, gp=gp, g2=g2, g=g, r0=r0, ri=ri, cn=cn: e.dma_start(
                                    out=Cw[64 * g2:64 * g2 + 64, widx(d, gp, ri), r0:r0 + 16],
                                    in_=W[cn][l, d, g].rearrange("h p -> p h"),
                                    allow_slow_non_contiguous=True), writes=[r_cw])
                Cw4 = Cw[:].rearrange("p (a r) n -> p a r n", r=2)
                fw.op("vector", lambda e: e.tensor_scalar(out=Cw4[:, :, 1, :], in0=Cw4[:, :, 1, :], scalar1=-1.0,
                                                          scalar2=None, op0=ALU.mult), reads=[r_cw], writes=[r_cw])

                XS = sb2("XS", [128, 2, 24, 129])
                BU = sb2("BU", [128, 2, 24, 128])
                PQ = sb2("PQ", [128, 2, 2, 24])
                r_xs = Res("XS")
                r_bu = Res("BU")
                r_pq = Res("PQ")
                r_pq1 = Res("PQ1")
                tmpb = [sb2("tmpb%d" % i, [128, 2, 128]) for i in range(2)]
                r_tmpb = [Res("tmpb%d" % i) for i in range(2)]
                ua = [[sb2("ua%d_%d" % (i, d), [128, 3, 128]) for d in range(2)] for i in range(2)]
                r_ua = [[Res("ua%d_%d" % (i, d)) for d in range(2)] for i in range(2)]
                yo = [sb2("yo%d" % i, [128, 128]) for i in range(2)]
                r_yo = [Res("yo%d" % i) for i in range(2)]
                fw.op("vector", lambda e: e.memset(XS[:], 0.0), writes=[r_xs])
                NT = NTOK // 128
                kcount = 0
                for i in range(NT):
                    tiles = [i, NT - 1 - i]
                    bi = i % 2
                    for d in range(2):
                        tk = tiles[d] * 128
                        fw.dma("sync", lambda e, d=d, tk=tk, bi=bi: e.dma_start(
                            out=ua[bi][d][:], in_=UAs[:, :, tk:tk + 128].rearrange("c p n -> p c n")),
                            writes=[r_ua[bi][d]])
                    for d in range(2):
                        for gp in range(12):
                            col = d * 12 + gp
                            c3 = gp // 4
                            pp = 2 * (col % 2)
                            for ri in range(2):
                                fw.op("tensor", lambda e, d=d, gp=gp, ri=ri, pp=pp, c3=c3, bi=bi: e.matmul(
                                    pA[pp + ri][:, 0:128], lhsT=Bw[:, widx(d, gp, ri), :], rhs=ua[bi][d][:, c3, :],
                                    start=True, stop=True),
                                    reads=[r_bw, r_ua[bi][d]], writes=[r_pA[pp + ri]])
                            tbk = tmpb[col % 2]
                            r_tbk = r_tmpb[col % 2]
                            if d == 0:
                                bre, bim = BU[:, 0, col, :], BU[:, 1, col, :]
                            else:
                                bre, bim = BU[:, 0, col, ::-1], BU[:, 1, col, ::-1]
                            fw.op("scalar", lambda e, tbk=tbk, pp=pp, col=col: e.activation(
                                out=tbk[:, 0, :], in_=pA[pp][:, 0:128], func=AF.Identity, scale=P["kr"][:, col:col + 1]),
                                reads=[r_pA[pp], r_p], writes=[r_tbk])
                            fw.op("vector", lambda e, tbk=tbk, pp=pp, col=col, bre=bre: e.scalar_tensor_tensor(
                                out=bre, in0=pA[pp + 1][:, 0:128], scalar=P["nki"][:, col:col + 1], in1=tbk[:, 0, :],
                                op0=ALU.mult, op1=ALU.add), reads=[r_pA[pp + 1], r_p, r_tbk], writes=[r_bu])
                            fw.op("scalar", lambda e, tbk=tbk, pp=pp, col=col: e.activation(
                                out=tbk[:, 1, :], in_=pA[pp + 1][:, 0:128], func=AF.Identity, scale=P["kr"][:, col:col + 1]),
                                reads=[r_pA[pp + 1], r_p], writes=[r_tbk])
                            fw.op("vector", lambda e, tbk=tbk, pp=pp, col=col, bim=bim: e.scalar_tensor_tensor(
                                out=bim, in0=pA[pp][:, 0:128], scalar=P["ki"][:, col:col + 1], in1=tbk[:, 1, :],
                                op0=ALU.mult, op1=ALU.add), reads=[r_pA[pp], r_p, r_tbk], writes=[r_bu])
                    BUe = BU[:, :, :, 0:128:2]
                    BUo = BU[:, :, :, 1:128:2]
                    BUes = BU[:, ::-1, :, 0:128:2]
                    fw.op("vector", lambda e, BUe=BUe: e.tensor_tensor(out=T1[:], in0=LRR64[:], in1=BUe, op=ALU.mult),
                          reads=[r_bu, r_p], writes=[r_t12])
                    fw.op("vector", lambda e, BUes=BUes: e.tensor_tensor(out=T2[:], in0=LIS64[:], in1=BUes, op=ALU.mult),
                          reads=[r_bu, r_p], writes=[r_t12])
                    fw.op("vector", lambda e: e.tensor_tensor(out=T1[:], in0=T1[:], in1=T2[:], op=ALU.add),
                          reads=[r_t12], writes=[r_t12])
                    fw.op("vector", lambda e, BUo=BUo: e.tensor_tensor(out=CB[:], in0=T1[:], in1=BUo, op=ALU.add),
                          reads=[r_t12, r_bu], writes=[r_cb])
                    C1e = CB[:, :, :, 0:64:2]
                    C1o = CB[:, :, :, 1:64:2]
                    C1es = CB[:, ::-1, :, 0:64:2]
                    fw.op("vector", lambda e, C1e=C1e: e.tensor_tensor(out=T1[:, :, :, 0:32], in0=L2R32[:], in1=C1e, op=ALU.mult),
                          reads=[r_cb, r_p], writes=[r_t12])
                    fw.op("vector", lambda e, C1es=C1es: e.tensor_tensor(out=T2[:, :, :, 0:32], in0=L2I32[:], in1=C1es, op=ALU.mult),
                          reads=[r_cb, r_p], writes=[r_t12])
                    fw.op("vector", lambda e: e.tensor_tensor(out=T1[:, :, :, 0:32], in0=T1[:, :, :, 0:32], in1=T2[:, :, :, 0:32],
                                                              op=ALU.add), reads=[r_t12], writes=[r_t12])
                    fw.op("vector", lambda e, C1o=C1o: e.tensor_tensor(out=CB2, in0=T1[:, :, :, 0:32], in1=C1o, op=ALU.add),
                          reads=[r_t12, r_cb], writes=[r_t12])
                    for n_ in range(32):
                        j = 4 * n_
                        fw.op("vector", lambda e, j=j: e.tensor_tensor(out=PQ[:, 0], in0=L4RR[:], in1=XS[:, :, :, j],
                                                                       op=ALU.mult),
                              reads=[r_xs, r_p], writes=[r_pq])
                        fw.op("vector", lambda e, j=j: e.tensor_tensor(out=PQ[:, 1], in0=L4IS[:], in1=XS[:, ::-1, :, j],
                                                                       op=ALU.mult),
                              reads=[r_xs, r_p], writes=[r_pq1])
                        fw.op("vector", lambda e: e.tensor_tensor(out=PQ[:, 0], in0=PQ[:, 0], in1=PQ[:, 1], op=ALU.add),
                              reads=[r_pq, r_pq1], writes=[r_pq])
                        fw.op("vector", lambda e, j=j, n_=n_: e.tensor_tensor(out=XS[:, :, :, j + 4], in0=PQ[:, 0],
                                                                              in1=T2[:, :, :, 32 + n_], op=ALU.add),
                              reads=[r_pq, r_t12], writes=[r_xs])
                    X4 = XS[:, :, :, 0:128:4]
                    X4s = XS[:, ::-1, :, 0:128:4]
                    X42 = XS[:, :, :, 2:129:4]
                    fw.op("vector", lambda e, X4=X4: e.tensor_tensor(out=T1[:, :, :, 0:32], in0=L2R32[:], in1=X4, op=ALU.mult),
                          reads=[r_xs, r_p], writes=[r_t12])
                    fw.op("vector", lambda e, X4s=X4s: e.tensor_tensor(out=T2[:, :, :, 0:32], in0=L2I32[:], in1=X4s, op=ALU.mult),
                          reads=[r_xs, r_p], writes=[r_t12])
                    fw.op("vector", lambda e: e.tensor_tensor(out=T1[:, :, :, 0:32], in0=T1[:, :, :, 0:32], in1=T2[:, :, :, 0:32],
                                                              op=ALU.add), reads=[r_t12], writes=[r_t12])
                    fw.op("vector", lambda e, X42=X42, C1e=C1e: e.tensor_tensor(out=X42, in0=T1[:, :, :, 0:32], in1=C1e, op=ALU.add),
                          reads=[r_t12, r_cb], writes=[r_xs])
                    XSe = XS[:, :, :, 0:128:2]
                    XSes = XS[:, ::-1, :, 0:128:2]
                    XSo = XS[:, :, :, 1:129:2]
                    fw.op("vector", lambda e, XSe=XSe: e.tensor_tensor(out=T1[:], in0=LRR64[:], in1=XSe, op=ALU.mult),
                          reads=[r_xs, r_p], writes=[r_t12])
                    fw.op("vector", lambda e, XSes=XSes: e.tensor_tensor(out=T2[:], in0=LIS64[:], in1=XSes, op=ALU.mult),
                          reads=[r_xs, r_p], writes=[r_t12])
                    fw.op("vector", lambda e: e.tensor_tensor(out=T1[:], in0=T1[:], in1=T2[:], op=ALU.add),
                          reads=[r_t12], writes=[r_t12])
                    fw.op("vector", lambda e, XSo=XSo, BUe=BUe: e.tensor_tensor(out=XSo, in0=T1[:], in1=BUe, op=ALU.add),
                          reads=[r_t12, r_bu], writes=[r_xs])
                    for d in range(2):
                        tk = tiles[d] * 128
                        for c3 in range(3):
                            pj = kcount % 2
                            kcount += 1
                            n = 0
                            for gq in range(4):
                                gp = c3 * 4 + gq
                                col = d * 12 + gp
                                for ri in range(2):
                                    fw.op("tensor", lambda e, d=d, gp=gp, ri=ri, col=col, pj=pj, n=n: e.matmul(
                                        pX[pj][:, 0:128], lhsT=Cw[:, widx(d, gp, ri), :], rhs=XS[:, ri, col, 1:129],
                                        start=(n == 0), stop=(n == 7)),
                                        reads=[r_cw, r_xs], writes=[r_pX[pj]], pe_acc=True)
                                    n += 1
                            ov = yo[pj][:, :] if d == 0 else yo[pj][:, ::-1]
                            fw.op("scalar", lambda e, pj=pj, ov=ov: e.activation(out=ov, in_=pX[pj][:, 0:128],
                                                                                 func=AF.Copy),
                                  reads=[r_pX[pj]], writes=[r_yo[pj]])
                            fw.dma("sync", lambda e, d=d, c3=c3, tk=tk, pj=pj: e.dma_start(
                                out=YA[d, c3, :, tk:tk + 128], in_=yo[pj][:]), reads=[r_yo[pj]])
                    fw.op("vector", lambda e: e.tensor_copy(out=XS[:, :, :, 0], in_=XS[:, :, :, 128]),
                          reads=[r_xs], writes=[r_xs])
                    if (i + 1) % 32 == 0 and i + 1 < NT:
                        sgn = (i + 1) // 32
                        fw.op("vector", lambda e, sgn=sgn: e.tensor_scalar(
                            out=XS[:, :, 0:12, 0], in0=XS[:, :, 0:12, 0], scalar1=flg[:, sgn:sgn + 1], scalar2=None,
                            op0=ALU.mult), reads=[r_xs, r_flg], writes=[r_xs])
                        fw.op("vector", lambda e, sgn=sgn: e.tensor_scalar(
                            out=XS[:, :, 12:24, 0], in0=XS[:, :, 12:24, 0], scalar1=flg[:, 4 - sgn:5 - sgn],
                            scalar2=None, op0=ALU.mult), reads=[r_xs, r_flg], writes=[r_xs])
            fw.barrier()

        def phase_h():
            with ExitStack() as pes:
                hb = [pes.enter_context(nc.sbuf_tensor(uname("h_hb%d" % i), [128, 4, PAD], BF16)) for i in range(2)]
                r_hb = [Res("hb%d" % i) for i in range(2)]
                k = 0
                for (T, nch) in [(KTs, 4), (VTs, 4), (CVs, 3)]:
                    for sg in range(NSEG):
                        jobs = []
                        src = ((sg - 1) * SEGP + SEG) if sg > 0 else (sg * SEGP + PAD)
                        jobs.append((src, sg * SEGP, sg))
                        src = ((sg + 1) * SEGP + PAD) if sg < NSEG - 1 else (sg * SEGP + SEG)
                        jobs.append((src, sg * SEGP + PAD + SEG, sg + 1))
                        for (src, dst, fc) in jobs:
                            b = k % 2
                            k += 1
                            fw.dma("sync", lambda e, T=T, nch=nch, src=src, b=b: e.dma_start(
                                out=hb[b][:, 0:nch, :], in_=T[0:nch, :, src:src + PAD].rearrange("c p n -> p c n")),
                                writes=[r_hb[b]])
                            fw.op("vector", lambda e, nch=nch, b=b, fc=fc: e.tensor_scalar(
                                out=hb[b][:, 0:nch, :], in0=hb[b][:, 0:nch, :], scalar1=flg[:, fc:fc + 1], scalar2=None,
                                op0=ALU.mult), reads=[r_hb[b], r_flg], writes=[r_hb[b]])
                            fw.dma("sync", lambda e, T=T, nch=nch, dst=dst, b=b: e.dma_start(
                                out=T[0:nch, :, dst:dst + PAD].rearrange("c p n -> p c n"), in_=hb[b][:, 0:nch, :]),
                                reads=[r_hb[b]])
            fw.barrier()

        maskD = sb("maskD", [128, 6, 256])
        maskS = sb("maskS", [128, 6, 384])
        r_mask = Res("mask")
        ones_col = sb("ones_col", [128, 1])
        fw.op("vector", lambda e: e.memset(ones_col[:], 1.0), writes=[r_mask])
        with ExitStack() as mes:
            ii = mes.enter_context(nc.sbuf_tensor("m_ii", [128, 128], mybir.dt.int32))
            fi = mes.enter_context(nc.sbuf_tensor("m_fi", [128, 128], F32))
            ta = mes.enter_context(nc.sbuf_tensor("m_ta", [128, 128], F32))
            tv = mes.enter_context(nc.sbuf_tensor("m_tv", [128, 128], F32))
            r_m = Res("m")
            fw.op("gpsimd", lambda e: e.iota(ii[:], pattern=[[-1, 128]], base=0, channel_multiplier=1), writes=[r_m])
            fw.op("vector", lambda e: e.tensor_copy(out=fi[:], in_=ii[:]), reads=[r_m], writes=[r_m])

            def mk_mask(dst, off, half, coef):
                fw.op("vector", lambda e: e.tensor_scalar(out=ta[:], in0=fi[:], scalar1=float(off), scalar2=None,
                                                          op0=ALU.add), reads=[r_m], writes=[r_m])
                fw.op("vector", lambda e: e.tensor_scalar(out=tv[:], in0=ta[:], scalar1=-1.0, scalar2=None,
                                                          op0=ALU.mult), reads=[r_m], writes=[r_m])
                fw.op("vector", lambda e: e.tensor_tensor(out=ta[:], in0=ta[:], in1=tv[:], op=ALU.max),
                      reads=[r_m], writes=[r_m])
                fw.op("vector", lambda e: e.tensor_scalar(out=tv[:], in0=ta[:], scalar1=-1.0, scalar2=float(half) + 0.5,
                                                          op0=ALU.mult, op1=ALU.add), reads=[r_m], writes=[r_m])
                fw.op("vector", lambda e: e.tensor_scalar(out=tv[:], in0=tv[:], scalar1=0.0, scalar2=0.5,
                                                          op0=ALU.max, op1=ALU.min), reads=[r_m], writes=[r_m])
                fw.op("scalar", lambda e: e.activation(out=ta[:], in_=ta[:], func=AF.Exp, scale=-float(coef)),
                      reads=[r_m], writes=[r_m])
                fw.op("vector", lambda e: e.scalar_tensor_tensor(out=dst, in0=ta[:], scalar=2.0, in1=tv[:],
                                                                 op0=ALU.mult, op1=ALU.mult),
                      reads=[r_m], writes=[r_m, r_mask])
            for gi, (win, dil) in enumerate(DIL):
                for h in range(2):
                    sl = SLOPES[6 + 2 * gi + h]
                    for kt in range(2):
                        mk_mask(maskD[:, 2 * gi + h, kt * 128:(kt + 1) * 128], -64 + 128 * kt, 64, sl * dil)
            for h in range(6):
                for kt in range(3):
                    mk_mask(maskS[:, h, kt * 128:(kt + 1) * 128], 128 * (kt - 1), 128, SLOPES[h])
        fw.barrier()

        def phase_b(l, last):
            with ExitStack() as L0:
                def sb0(name, shape, dt=F32):
                    return L0.enter_context(nc.sbuf_tensor(uname("b_" + name), shape, dt))
                hT = sb0("hT", [128, 8, ST], BF16)
                r_hT = Res("hT")
                vcol = sb0("vcol", [128, 2])
                r_vcol = Res("vcol")
                sexp = sb0("sexp", [128, 6])
                r_sexp = Res("sexp")
                fw.dma("sync", lambda e: e.dma_start(out=sexp[:], in_=W["swa_sink"][l].partition_broadcast(128)),
                       writes=[r_sexp])
                fw.op("scalar", lambda e: e.activation(out=sexp[:], in_=sexp[:], func=AF.Exp),
                      reads=[r_sexp], writes=[r_sexp])
                wi = W["w_in"][l]
                for st_i in range(NST):
                    t0 = st_i * ST
                    p0 = padpos(t0)
                    sg = st_i // 2
                    hf = st_i % 2
                    fw.dma("sync", lambda e, t0=t0: e.dma_start(
                        out=hT[:], in_=HT[:, :, t0:t0 + ST].rearrange("k p n -> p k n")), writes=[r_hT])
                    fw.op("vector", lambda e: e.memset(vcol[:], 1.0), writes=[r_vcol])
                    fw.op("vector", lambda e, sg=sg: e.tensor_copy(out=vcol[0:64, 0:1], in_=flg[0:64, sg:sg + 1]),
                          reads=[r_flg], writes=[r_vcol])
                    fw.op("vector", lambda e, sg=sg: e.tensor_copy(out=vcol[64:128, 1:2], in_=flg[64:128, sg + 1:sg + 2]),
                          reads=[r_flg], writes=[r_vcol])
                    with ExitStack() as L1:
                        def sb1(name, shape, dt=F32):
                            return L1.enter_context(nc.sbuf_tensor(uname("b1_" + name), shape, dt))
                        brT = sb1("brT", [128, 10, ST], BF16)
                        r_br = Res("brT")
                        wst = [sb1("wst%d" % i, [128, 8, 128]) for i in range(2)]
                        r_wst = [Res("wst%d" % i) for i in range(2)]
                        wbf = [sb1("wbf%d" % i, [128, 8, 128], BF16) for i in range(2)]
                        r_wbf = [Res("wbf%d" % i) for i in range(2)]

                        def load_w_chunk(parts):
                            j = cnt["w"] % 2
                            cnt["w"] += 1
                            for (src_ap, c0, n) in parts:
                                fw.dma("sync", lambda e, src_ap=src_ap, c0=c0, n=n, j=j: e.dma_start(
                                    out=wst[j][:, :, c0:c0 + n], in_=src_ap.rearrange("(k p) n -> p k n", p=128)),
                                    writes=[r_wst[j]])
                            fw.op("gpsimd", lambda e, j=j: e.tensor_copy(out=wbf[j][:], in_=wst[j][:]),
                                  reads=[r_wst[j]], writes=[r_wbf[j]])
                            return wbf[j], r_wbf[j]

                        def proj_fm(wt, r_w, evac):
                            for ts in range(ST // 512):
                                j = cnt["pA"] % 4
                                cnt["pA"] += 1
                                for k in range(8):
                                    fw.op("tensor", lambda e, k=k, j=j, ts=ts: e.matmul(
                                        pA[j][:], lhsT=wt[:, k, :], rhs=hT[:, k, ts * 512:(ts + 1) * 512],
                                        start=(k == 0), stop=(k == 7)),
                                        reads=[r_w, r_hT], writes=[r_pA[j]], pe_acc=True)
                                evac(ts, j, pA[j], r_pA[j])

                        with ExitStack() as S1:
                            def sbs(name, shape, dt=F32):
                                return S1.enter_context(nc.sbuf_tensor(uname("b2_" + name), shape, dt))
                            KT1 = sbs("KT1", [128, 2 * ST], BF16)
                            VT1 = sbs("VT1", [128, 2 * ST], BF16)
                            r_kv = Res("kv")
                            QT1 = sbs("QT1", [128, ST], BF16)
                            r_q = Res("q")
                            UACC = sbs("UACC", [128, 2, ST])
                            r_ua = Res("uacc")
                            RC = sbs("RC", [128, ST])
                            r_rc = Res("rc")
                            Et = [sbs("E%d" % i, [128, 384]) for i in range(2)]
                            r_E = [Res("E%d" % i) for i in range(2)]
                            Pt = [sbs("P%d" % i, [128, 384], BF16) for i in range(2)]
                            r_P = [Res("P%d" % i) for i in range(2)]
                            VE = [sbs("VE%d" % i, [128, 3, 2, 192], BF16) for i in range(2)]
                            r_VE = [Res("VE%d" % i) for i in range(2)]
                            sm = [sbs("sm%d" % i, [128, 128]) for i in range(2)]
                            r_sm = [Res("sm%d" % i) for i in range(2)]
                            for i in range(2):
                                fw.op("vector", lambda e, i=i: e.memset(VE[i][:], 1.0), writes=[r_VE[i]])
                            ac = {"u": 0}

                            def q_evac(ts, j, pt, r_pt):
                                fw.op("scalar", lambda e: e.activation(out=QT1[:, ts * 512:(ts + 1) * 512], in_=pt[:],
                                                                       func=AF.Copy), reads=[r_pt], writes=[r_q])

                            def load_kv(c, p0=p0):
                                fw.dma("sync", lambda e, c=c, p0=p0: e.dma_start(
                                    out=KT1[:], in_=KTs[c, :, p0 - PAD:p0 - PAD + 2 * ST]), writes=[r_kv])
                                fw.dma("sync", lambda e, c=c, p0=p0: e.dma_start(
                                    out=VT1[:], in_=VTs[c, :, p0 - PAD:p0 - PAD + 2 * ST]), writes=[r_kv])

                            def unit(nkt, kcols, qcols, heads, mask_of, valid_of, lhs_of, sink_dst):
                                u = ac["u"] % 2
                                ac["u"] += 1
                                for kt in range(nkt):
                                    fw.op("tensor", lambda e, kt=kt, u=u: e.transpose(
                                        pT[u][:, kt, :], VT1[:, kcols(kt)], ident[:]),
                                        reads=[r_kv, r_ident], writes=[r_pT[u]], pe_acc=True)
                                fw.op("vector", lambda e, u=u: e.tensor_copy(
                                    out=VE[u][:, 0:nkt, :, 64:128],
                                    in_=pT[u][:, 0:nkt, :].rearrange("p k (h d) -> p k h d", h=2)),
                                    reads=[r_pT[u]], writes=[r_VE[u]])
                                for (hrow, vslot, tag) in heads:
                                    j = cnt["pA"] % 4
                                    cnt["pA"] += 1
                                    j2 = cnt["pA"] % 4
                                    cnt["pA"] += 1
                                    ei = ac["u"] % 2
                                    for kt in range(nkt):
                                        fw.op("tensor", lambda e, kt=kt, j=j, hrow=hrow: e.matmul(
                                            pA[j][:, kt * 128:(kt + 1) * 128], lhsT=KT1[hrow:hrow + 64, kcols(kt)],
                                            rhs=QT1[hrow:hrow + 64, qcols], start=True, stop=True),
                                            reads=[r_kv, r_q], writes=[r_pA[j]], pe_acc=True)
                                    fw.op("scalar", lambda e, j=j, ei=ei: e.activation(
                                        out=Et[ei][:, 0:nkt * 128], in_=pA[j][:, 0:nkt * 128], func=AF.Exp, scale=0.125),
                                        reads=[r_pA[j]], writes=[r_E[ei]])
                                    for kt in range(nkt):
                                        vc = valid_of(kt)
                                        fw.op("vector", lambda e, kt=kt, ei=ei, vc=vc, tag=tag: e.scalar_tensor_tensor(
                                            out=Pt[ei][:, kt * 128:(kt + 1) * 128], in0=Et[ei][:, kt * 128:(kt + 1) * 128],
                                            scalar=vc, in1=mask_of(tag)[:, kt * 128:(kt + 1) * 128],
                                            op0=ALU.mult, op1=ALU.mult),
                                            reads=[r_E[ei], r_mask, r_vcol, r_flg], writes=[r_P[ei]])
                                    for kt in range(nkt):
                                        fw.op("tensor", lambda e, kt=kt, j2=j2, ei=ei, u=u, vslot=vslot, tag=tag: e.matmul(
                                            pA[j2][:, 0:128], lhsT=lhs_of(VE[u], kt, vslot, tag),
                                            rhs=Pt[ei][:, kt * 128:(kt + 1) * 128], start=(kt == 0), stop=(kt == nkt - 1)),
                                            reads=[r_VE[u], r_P[ei]], writes=[r_pA[j2]], pe_acc=True)
                                    sink_dst(tag, pA[j2], r_pA[j2])

                            for gi, (win, dil) in enumerate(DIL):
                                wt, r_w = load_w_chunk([(wi[:, (12 + gi) * 128:(13 + gi) * 128], 0, 128)])
                                proj_fm(wt, r_w, q_evac)
                                load_kv(gi)
                                nsub = ST // dil
                                for r in range(dil):
                                    for qb in range(nsub // 128):
                                        q0 = qb * 128
                                        c_lo = r + dil * q0

                                        def kcols(kt, c_lo=c_lo, dil=dil):
                                            b = PAD + c_lo + dil * (-64 + 128 * kt)
                                            return slice(b, b + 127 * dil + 1, dil)
                                        qcols = slice(c_lo, c_lo + 127 * dil + 1, dil)

                                        def valid_of(kt, qb=qb, nsub=nsub):
                                            if hf == 0 and qb == 0 and kt == 0:
                                                return vcol[:, 0:1]
                                            if hf == 1 and qb == nsub // 128 - 1 and kt == 1:
                                                return vcol[:, 1:2]
                                            return ones_col[:, 0:1]

                                        def mask_of(tag, gi=gi):
                                            return maskD[:, 2 * gi + tag, :]

                                        def lhs_of(ve, kt, vslot, tag):
                                            return ve[:, kt, tag, 64:192] if tag == 0 else ve[:, kt, tag, 0:128]

                                        def sink_dst(tag, pu, r_pu, gi=gi, qcols=qcols):
                                            dstv = UACC[:, tag, qcols]
                                            if gi == 0:
                                                fw.op("vector", lambda e: e.tensor_copy(out=dstv, in_=pu[:, 0:128]),
                                                      reads=[r_pu], writes=[r_ua])
                                            else:
                                                fw.op("vector", lambda e: e.tensor_tensor(out=dstv, in0=dstv,
                                                                                          in1=pu[:, 0:128], op=ALU.add),
                                                      reads=[r_pu, r_ua], writes=[r_ua])
                                        unit(2, kcols, qcols, [(0, 0, 0), (64, 1, 1)], mask_of, valid_of, lhs_of, sink_dst)
                            fw.op("vector", lambda e: e.reciprocal(out=RC[0:64, :], in_=UACC[64:128, 0, :]),
                                  reads=[r_ua], writes=[r_rc])
                            fw.op("vector", lambda e: e.reciprocal(out=RC[64:128, :], in_=UACC[0:64, 1, :]),
                                  reads=[r_ua], writes=[r_rc])
                            fw.op("vector", lambda e: e.tensor_tensor(out=brT[0:64, 6, :], in0=UACC[0:64, 0, :],
                                                                      in1=RC[0:64, :], op=ALU.mult),
                                  reads=[r_ua, r_rc], writes=[r_br])
                            fw.op("vector", lambda e: e.tensor_tensor(out=brT[64:128, 6, :], in0=UACC[64:128, 1, :],
                                                                      in1=RC[64:128, :], op=ALU.mult),
                                  reads=[r_ua, r_rc], writes=[r_br])
                            load_kv(3)
                            for jq in range(3):
                                wt, r_w = load_w_chunk([(wi[:, 2688 + 64 * jq:2688 + 64 * jq + 64], 0, 64),
                                                        (wi[:, 2688 + 64 * (jq + 3):2688 + 64 * (jq + 3) + 64], 64, 64)])
                                proj_fm(wt, r_w, q_evac)
                                for qb in range(ST // 128):
                                    q0 = qb * 128

                                    def kcols(kt, q0=q0):
                                        b = PAD + q0 - 128 + 128 * kt
                                        return slice(b, b + 128)
                                    qcols = slice(q0, q0 + 128)

                                    def valid_of(kt, qb=qb):
                                        if hf == 0 and qb == 0 and kt == 0:
                                            return flg[:, sg:sg + 1]
                                        if hf == 1 and qb == ST // 128 - 1 and kt == 2:
                                            return flg[:, sg + 1:sg + 2]
                                        return ones_col[:, 0:1]

                                    def mask_of(tag):
                                        return maskS[:, tag, :]

                                    def lhs_of(ve, kt, vslot, tag):
                                        return ve[:, kt, vslot, 64:192] if tag % 2 == 0 else ve[:, kt, vslot, 0:128]

                                    def sink_dst(tag, pu, r_pu, qcols=qcols):
                                        h = tag
                                        ch, half = 7 + h // 2, h % 2
                                        si = ac["u"] % 2
                                        if half == 0:
                                            urows, drows = slice(0, 64), slice(64, 128)
                                        else:
                                            urows, drows = slice(64, 128), slice(0, 64)
                                        fw.op("vector", lambda e: e.tensor_scalar(
                                            out=sm[si][drows, :], in0=pu[drows, 0:128], scalar1=sexp[drows, h:h + 1],
                                            scalar2=None, op0=ALU.add), reads=[r_pu, r_sexp], writes=[r_sm[si]])
                                        fw.op("vector", lambda e: e.reciprocal(out=sm[si][drows, :], in_=sm[si][drows, :]),
                                              reads=[r_sm[si]], writes=[r_sm[si]])
                                        fw.op("vector", lambda e: e.tensor_tensor(
                                            out=brT[urows, ch, qcols], in0=pu[urows, 0:128], in1=sm[si][drows, :],
                                            op=ALU.mult), reads=[r_pu, r_sm[si]], writes=[r_br])
                                    unit(3, kcols, qcols, [(0, 0, jq), (64, 1, jq + 3)], mask_of, valid_of, lhs_of, sink_dst)
                        fw.barrier()
                        if stop_after == "attn":
                            fw.dma("sync", lambda e, t0=t0: e.dma_start(
                                out=dbg["br"][:, :, t0:t0 + ST].rearrange("c p n -> p c n"), in_=brT[:]), reads=[r_br])
                            fw.barrier()
                            continue
                        with ExitStack() as S2:
                            def sbt(name, shape, dt=F32):
                                return S2.enter_context(nc.sbuf_tensor(uname("b3_" + name), shape, dt))
                            dvec = sbt("dvec", [128, 3])
                            glub = sbt("glub", [128, 3])
                            cwt = sbt("cwt", [128, 3, 3])
                            cbt = sbt("cbt", [128, 3])
                            r_sv = Res("sv")
                            fw.dma("sync", lambda e: e.dma_start(out=dvec[:], in_=W["s5_d"][l].rearrange("(c p) -> p c", p=128),
                                                                 allow_slow_non_contiguous=True), writes=[r_sv])
                            fw.dma("sync", lambda e: e.dma_start(out=glub[:], in_=W["s5_glu_b"][l].rearrange("(c p) -> p c", p=128),
                                                                 allow_slow_non_contiguous=True), writes=[r_sv])
                            fw.dma("sync", lambda e: e.dma_start(out=cwt[:], in_=W["conv_w"][l].rearrange("t (c p) -> p t c", p=128),
                                                                 allow_slow_non_contiguous=True), writes=[r_sv])
                            fw.dma("sync", lambda e: e.dma_start(out=cbt[:], in_=W["conv_b"][l].rearrange("(c p) -> p c", p=128),
                                                                 allow_slow_non_contiguous=True), writes=[r_sv])
                            gluw32 = sbt("gluw32", [128, 3, 384])
                            gluw = sbt("gluw", [128, 3, 384], BF16)
                            r_gluw = Res("gluw")
                            fw.dma("sync", lambda e: e.dma_start(out=gluw32[:], in_=W["s5_glu_w"][l].rearrange("(c p) n -> p c n", p=128)),
                                   writes=[r_gluw])
                            fw.op("gpsimd", lambda e: e.tensor_copy(out=gluw[:], in_=gluw32[:]), reads=[r_gluw], writes=[r_gluw])
                            yf = [sbt("yf%d" % i, [128, 512]) for i in range(2)]
                            yb_ = [sbt("yb%d" % i, [128, 512]) for i in range(2)]
                            uu = [sbt("uu%d" % i, [128, 512]) for i in range(2)]
                            r_y3 = [Res("y3_%d" % i) for i in range(2)]
                            zf32 = sbt("zf32", [128, 3, 512])
                            zb16 = sbt("zb16", [128, 3, 512], BF16)
                            r_z = Res("z")
                            gt = [sbt("gt%d" % i, [128, 512]) for i in range(2)]
                            r_gt = [Res("gt%d" % i) for i in range(2)]
                            kk = 0
                            for ts in range(ST // 512):
                                tk = t0 + ts * 512
                                for c in range(3):
                                    b = kk % 2
                                    kk += 1
                                    fw.dma("sync", lambda e, c=c, tk=tk, b=b: e.dma_start(out=yf[b][:], in_=YA[0, c, :, tk:tk + 512]),
                                           writes=[r_y3[b]])
                                    fw.dma("sync", lambda e, c=c, tk=tk, b=b: e.dma_start(out=yb_[b][:], in_=YA[1, c, :, tk:tk + 512]),
                                           writes=[r_y3[b]])
                                    fw.dma("sync", lambda e, c=c, tk=tk, b=b: e.dma_start(out=uu[b][:], in_=UAs[c, :, tk:tk + 512]),
                                           writes=[r_y3[b]])
                                    fw.op("vector", lambda e, b=b: e.tensor_tensor(out=yf[b][:], in0=yf[b][:], in1=yb_[b][:], op=ALU.add),
                                          reads=[r_y3[b]], writes=[r_y3[b]])
                                    fw.op("vector", lambda e, b=b, c=c: e.scalar_tensor_tensor(
                                        out=yf[b][:], in0=uu[b][:], scalar=dvec[:, c:c + 1], in1=yf[b][:], op0=ALU.mult, op1=ALU.add),
                                        reads=[r_y3[b], r_sv], writes=[r_y3[b]])
                                    fw.op("scalar", lambda e, b=b, c=c: e.activation(out=zf32[:, c, :], in_=yf[b][:], func=AF.Gelu),
                                          reads=[r_y3[b]], writes=[r_z])
                                    fw.op("vector", lambda e, c=c: e.tensor_copy(out=zb16[:, c, :], in_=zf32[:, c, :]),
                                          reads=[r_z], writes=[r_z])
                                for co in range(3):
                                    j = cnt["pA"] % 4
                                    cnt["pA"] += 1
                                    for ci in range(3):
                                        fw.op("tensor", lambda e, ci=ci, co=co, j=j: e.matmul(
                                            pA[j][:], lhsT=gluw[:, ci, co * 128:(co + 1) * 128], rhs=zb16[:, ci, :],
                                            start=(ci == 0), stop=(ci == 2)), reads=[r_gluw, r_z], writes=[r_pA[j]], pe_acc=True)
                                    g2 = co % 2
                                    fw.op("vector", lambda e, j=j, g2=g2, co=co: e.tensor_scalar(
                                        out=gt[g2][:], in0=pA[j][:], scalar1=glub[:, co:co + 1], scalar2=None, op0=ALU.add),
                                        reads=[r_pA[j], r_sv], writes=[r_gt[g2]])
                                    fw.op("scalar", lambda e, g2=g2: e.activation(out=gt[g2][:], in_=gt[g2][:], func=AF.Sigmoid),
                                          reads=[r_gt[g2]], writes=[r_gt[g2]])
                                    fw.op("vector", lambda e, g2=g2, co=co, ts=ts: e.tensor_tensor(
                                        out=brT[:, co, ts * 512:(ts + 1) * 512], in0=zf32[:, co, :], in1=gt[g2][:], op=ALU.mult),
                                        reads=[r_z, r_gt[g2]], writes=[r_br])
                            cvt = sbt("cvt", [128, ST + 2], BF16)
                            r_cvt = Res("cvt")
                            accf = [sbt("accf%d" % i, [128, 512]) for i in range(2)]
                            r_accf = [Res("accf%d" % i) for i in range(2)]
                            for c in range(3):
                                wt, r_w = load_w_chunk([(wi[:, (6 + c) * 128:(7 + c) * 128], 0, 128)])
                                fw.dma("sync", lambda e, c=c, p0=p0: e.dma_start(out=cvt[:], in_=CVs[c, :, p0 - 1:p0 + ST + 1]),
                                       writes=[r_cvt])

                                def ev_gb(ts, j, pt, r_pt, c=c):
                                    a = j % 2
                                    o = ts * 512
                                    fw.op("vector", lambda e: e.tensor_scalar(out=accf[a][:], in0=cvt[:, o:o + 512],
                                                                              scalar1=cwt[:, 0, c:c + 1], scalar2=None, op0=ALU.mult),
                                          reads=[r_cvt, r_sv], writes=[r_accf[a]])
                                    for tap in (1, 2):
                                        fw.op("vector", lambda e, tap=tap: e.scalar_tensor_tensor(
                                            out=accf[a][:], in0=cvt[:, o + tap:o + tap + 512], scalar=cwt[:, tap, c:c + 1],
                                            in1=accf[a][:], op0=ALU.mult, op1=ALU.add),
                                            reads=[r_cvt, r_sv, r_accf[a]], writes=[r_accf[a]])
                                    fw.op("vector", lambda e: e.scalar_tensor_tensor(
                                        out=brT[:, 3 + c, o:o + 512], in0=accf[a][:], scalar=cbt[:, c:c + 1], in1=pt[:],
                                        op0=ALU.add, op1=ALU.mult), reads=[r_accf[a], r_sv, r_pt], writes=[r_br])
                                proj_fm(wt, r_w, ev_gb)
                            if stop_after == "branches":
                                fw.dma("sync", lambda e, t0=t0: e.dma_start(
                                    out=dbg["br"][:, :, t0:t0 + ST].rearrange("c p n -> p c n"), in_=brT[:]), reads=[r_br])
                                fw.barrier()
                                continue
                            wbr32 = [sbt("wbr32_%d" % i, [128, 3, 128]) for i in range(2)]
                            wbr = [sbt("wbr%d" % i, [128, 3, 128], BF16) for i in range(2)]
                            r_wbr = [Res("wbr%d" % i) for i in range(2)]
                            mac = sbt("mac", [128, ST])
                            r_mac = Res("mac")
                            mbf = [sbt("mbf%d" % i, [128, ST], BF16) for i in range(2)]
                            r_mbf = [Res("mbf%d" % i) for i in range(2)]
                            sgt = [sbt("sgt%d" % i, [128, 512]) for i in range(2)]
                            r_sgt = [Res("sgt%d" % i) for i in range(2)]
                            BRS = [("w_branch_a", 3, 0), ("w_branch_b", 3, 3), ("w_branch_c", 1, 6), ("w_branch_d", 3, 7)]
                            q = 0
                            for jo in range(8):
                                for br, (wn, nch, ch0) in enumerate(BRS):
                                    wg, r_wg = load_w_chunk([(wi[:, 3328 + br * 1024 + jo * 128:3328 + br * 1024 + (jo + 1) * 128], 0, 128)])
                                    wb = q % 2
                                    q += 1
                                    fw.dma("sync", lambda e, wn=wn, nch=nch, jo=jo, wb=wb: e.dma_start(
                                        out=wbr32[wb][:, 0:nch, :],
                                        in_=W[wn][l][:, jo * 128:(jo + 1) * 128].rearrange("(c p) n -> p c n", p=128)),
                                        writes=[r_wbr[wb]])
                                    fw.op("gpsimd", lambda e, nch=nch, wb=wb: e.tensor_copy(out=wbr[wb][:, 0:nch, :],
                                                                                          in_=wbr32[wb][:, 0:nch, :]),
                                          reads=[r_wbr[wb]], writes=[r_wbr[wb]])
                                    for ts in range(ST // 512):
                                        j = cnt["pA"] % 4
                                        cnt["pA"] += 1
                                        xk = (q + ts) % 2
                                        o = ts * 512
                                        for k in range(8):
                                            fw.op("tensor", lambda e, k=k, j=j, o=o, wg=wg: e.matmul(
                                                pA[j][:], lhsT=wg[:, k, :], rhs=hT[:, k, o:o + 512], start=(k == 0), stop=(k == 7)),
                                                reads=[r_wg, r_hT], writes=[r_pA[j]], pe_acc=True)
                                        for c in range(nch):
                                            fw.op("tensor", lambda e, c=c, xk=xk, o=o, wb=wb, ch0=ch0, nch=nch: e.matmul(
                                                pX[xk][:], lhsT=wbr[wb][:, c, :], rhs=brT[:, ch0 + c, o:o + 512],
                                                start=(c == 0), stop=(c == nch - 1)),
                                                reads=[r_wbr[wb], r_br], writes=[r_pX[xk]], pe_acc=True)
                                        fw.op("scalar", lambda e, j=j, xk=xk: e.activation(out=sgt[xk][:], in_=pA[j][:], func=AF.Sigmoid),
                                              reads=[r_pA[j]], writes=[r_sgt[xk]])
                                        if br == 0:
                                            fw.op("vector", lambda e, xk=xk, o=o: e.tensor_tensor(
                                                out=mac[:, o:o + 512], in0=sgt[xk][:], in1=pX[xk][:], op=ALU.mult),
                                                reads=[r_sgt[xk], r_pX[xk]], writes=[r_mac])
                                        else:
                                            fw.op("vector", lambda e, xk=xk: e.tensor_tensor(
                                                out=sgt[xk][:], in0=sgt[xk][:], in1=pX[xk][:], op=ALU.mult),
                                                reads=[r_sgt[xk], r_pX[xk]], writes=[r_sgt[xk]])
                                            fw.op("vector", lambda e, xk=xk, o=o: e.tensor_tensor(
                                                out=mac[:, o:o + 512], in0=mac[:, o:o + 512], in1=sgt[xk][:], op=ALU.add),
                                                reads=[r_sgt[xk], r_mac], writes=[r_mac])
                                mi = jo % 2
                                fw.op("scalar", lambda e, mi=mi: e.activation(out=mbf[mi][:], in_=mac[:], func=AF.Copy),
                                      reads=[r_mac], writes=[r_mbf[mi]])
                                fw.dma("sync", lambda e, jo=jo, t0=t0, mi=mi: e.dma_start(out=MTs[jo, :, t0:t0 + ST], in_=mbf[mi][:]),
                                       reads=[r_mbf[mi]])
                    fw.barrier()
                    if stop_after in ("attn", "branches"):
                        continue
                    with ExitStack() as S3:
                        def sbu(name, shape, dt=F32):
                            return S3.enter_context(nc.sbuf_tensor(uname("b4_" + name), shape, dt))
                        fw.dma("sync", lambda e, t0=t0: e.dma_start(
                            out=hT[:], in_=MTs[:, :, t0:t0 + ST].rearrange("k p n -> p k n")), writes=[r_hT])
                        wo32 = [sbu("wo32_%d" % i, [128, 8, 256]) for i in range(2)]
                        r_wo32 = [Res("wo32_%d" % i) for i in range(2)]
                        wo = sbu("wo", [128, 8, D], BF16)
                        r_wo = Res("wo")
                        for pc in range(4):
                            a = pc % 2
                            fw.dma("sync", lambda e, pc=pc, a=a: e.dma_start(
                                out=wo32[a][:], in_=W["w_o"][l][:, pc * 256:(pc + 1) * 256].rearrange("(k p) n -> p k n", p=128)),
                                writes=[r_wo32[a]])
                            fw.op("gpsimd", lambda e, pc=pc, a=a: e.tensor_copy(out=wo[:, :, pc * 256:(pc + 1) * 256], in_=wo32[a][:]),
                                  reads=[r_wo32[a]], writes=[r_wo])
                        load_gb("ln1_g", "ln1_b", l)
                        xb = [sbu("xb%d" % i, [128, D]) for i in range(2)]
                        r_xb = [Res("xb%d" % i) for i in range(2)]
                        tb = [sbu("tb%d" % i, [128, D]) for i in range(2)]
                        r_tb = [Res("tb%d" % i) for i in range(2)]
                        hb16 = [sbu("hb16_%d" % i, [128, D], BF16) for i in range(2)]
                        r_hb16 = [Res("hb16_%d" % i) for i in range(2)]
                        st4 = [sbu("st4_%d" % i, [128, 8]) for i in range(2)]
                        r_st4 = [Res("st4_%d" % i) for i in range(2)]
                        mo = [sbu("mo%d" % i, [128, D]) for i in range(2)]
                        r_mo = [Res("mo%d" % i) for i in range(2)]
                        tkb = [sbu("tkb%d" % i, [128, 8, 128], BF16) for i in range(2)]
                        r_tkb = [Res("tkb%d" % i) for i in range(2)]
                        for blk in range(ST // 128):
                            i = blk % 2
                            r0 = t0 + blk * 128
                            fw.dma("sync", lambda e, i=i, r0=r0: e.dma_start(out=xb[i][:], in_=H0[r0:r0 + 128, :]), writes=[r_xb[i]])
                            for nh in range(2):
                                j = cnt["pA"] % 4
                                cnt["pA"] += 1
                                for k in range(8):
                                    fw.op("tensor", lambda e, k=k, j=j, nh=nh, blk=blk: e.matmul(
                                        pA[j][:], lhsT=hT[:, k, blk * 128:(blk + 1) * 128], rhs=wo[:, k, nh * 512:(nh + 1) * 512],
                                        start=(k == 0), stop=(k == 7)), reads=[r_hT, r_wo], writes=[r_pA[j]], pe_acc=True)
                                fw.op("scalar", lambda e, i=i, j=j, nh=nh: e.activation(
                                    out=mo[i][:, nh * 512:(nh + 1) * 512], in_=pA[j][:], func=AF.Copy),
                                    reads=[r_pA[j]], writes=[r_mo[i]])
                            if debug:
                                fw.dma("sync", lambda e, i=i, r0=r0: e.dma_start(out=dbg["mix"][r0:r0 + 128, :], in_=mo[i][:]),
                                       reads=[r_mo[i]])
                            fw.op("vector", lambda e, i=i: e.scalar_tensor_tensor(
                                out=xb[i][:], in0=xb[i][:], scalar=ALPHA, in1=mo[i][:], op0=ALU.mult, op1=ALU.add),
                                reads=[r_xb[i], r_mo[i]], writes=[r_xb[i]])
                            ln_rows(fw, xb[i], r_xb[i], tb[i], r_tb[i], st4[i], r_st4[i], gam, bet, r_gb, xb[i], r_xb[i])
                            if debug:
                                fw.dma("sync", lambda e, i=i, r0=r0: e.dma_start(out=dbg["st"][r0:r0 + 128, :], in_=st4[i][:]),
                                       reads=[r_st4[i]])
                            fw.dma("sync", lambda e, i=i, r0=r0: e.dma_start(out=H1[r0:r0 + 128, :], in_=xb[i][:]), reads=[r_xb[i]])
                            fw.op("scalar", lambda e, i=i: e.activation(out=hb16[i][:], in_=xb[i][:], func=AF.Copy),
                                  reads=[r_xb[i]], writes=[r_hb16[i]])
                            for k in range(8):
                                fw.op("tensor", lambda e, k=k, i=i: e.transpose(pT[i][:, k, :], hb16[i][:, k * 128:(k + 1) * 128], ident[:]),
                                      reads=[r_hb16[i], r_ident], writes=[r_pT[i]], pe_acc=True)
                            fw.op("vector", lambda e, i=i: e.tensor_copy(out=tkb[i][:], in_=pT[i][:]), reads=[r_pT[i]], writes=[r_tkb[i]])
                            fw.dma("sync", lambda e, i=i, r0=r0: e.dma_start(
                                out=HT[:, :, r0:r0 + 128].rearrange("k p n -> p k n"), in_=tkb[i][:]), reads=[r_tkb[i]])
                    fw.barrier()
                    if stop_after == "ln1":
                        continue
                    with ExitStack() as S4:
                        def sbv(name, shape, dt=F32):
                            return S4.enter_context(nc.sbuf_tensor(uname("b5_" + name), shape, dt))
                        fw.dma("sync", lambda e, t0=t0: e.dma_start(
                            out=hT[:], in_=HT[:, :, t0:t0 + ST].rearrange("k p n -> p k n")), writes=[r_hT])
                        wr32 = sbv("wr32", [128, 8, 20])
                        wr = sbv("wr", [128, 8, 20], BF16)
                        r_wr = Res("wr")
                        fw.dma("sync", lambda e: e.dma_start(out=wr32[:, :, 0:4], in_=W["router_group_w"][l].rearrange("(k p) n -> p k n", p=128),
                                                             allow_slow_non_contiguous=True), writes=[r_wr])
                        fw.dma("sync", lambda e: e.dma_start(out=wr32[:, :, 4:20], in_=W["router_expert_w"][l].rearrange("(k p) n -> p k n", p=128),
                                                             allow_slow_non_contiguous=True), writes=[r_wr])
                        fw.op("vector", lambda e: e.tensor_copy(out=wr[:], in_=wr32[:]), reads=[r_wr], writes=[r_wr])
                        rb = sbv("rb", [128, 20])
                        r_rb = Res("rb")
                        fw.dma("sync", lambda e: e.dma_start(out=rb[:, 0:4], in_=W["router_group_b"][l].partition_broadcast(128)), writes=[r_rb])
                        fw.dma("sync", lambda e: e.dma_start(out=rb[:, 4:20], in_=W["router_expert_b"][l].partition_broadcast(128)), writes=[r_rb])
                        comb = sbv("comb", [128, ST // 128, 16])
                        r_comb = Res("comb")
                        rt = sbv("rt", [128, 64])
                        r_rt = Res("rt")
                        for blk in range(ST // 128):
                            xk = blk % 2
                            for k in range(8):
                                fw.op("tensor", lambda e, k=k, xk=xk, blk=blk: e.matmul(
                                    pX[xk][:, 0:20], lhsT=hT[:, k, blk * 128:(blk + 1) * 128], rhs=wr[:, k, :],
                                    start=(k == 0), stop=(k == 7)), reads=[r_hT, r_wr], writes=[r_pX[xk]], pe_acc=True)
                            lg = rt[:, 0:20]

                            def V(fn, extra_r=()):
                                fw.op("vector", fn, reads=[r_rt] + list(extra_r), writes=[r_rt])
                            fw.op("vector", lambda e, xk=xk: e.tensor_tensor(out=rt[:, 0:20], in0=pX[xk][:, 0:20], in1=rb[:], op=ALU.add),
                                  reads=[r_pX[xk], r_rb], writes=[r_rt])
                            V(lambda e: e.reduce_max(out=rt[:, 20:21], in_=rt[:, 0:4], axis=AX.X))
                            V(lambda e: e.tensor_scalar(out=rt[:, 24:28], in0=rt[:, 0:4], scalar1=rt[:, 20:21], scalar2=None, op0=ALU.subtract))
                            fw.op("scalar", lambda e: e.activation(out=rt[:, 28:32], in_=rt[:, 24:28], func=AF.Exp), reads=[r_rt], writes=[r_rt])
                            V(lambda e: e.reduce_sum(out=rt[:, 21:22], in_=rt[:, 28:32], axis=AX.X))
                            V(lambda e: e.reciprocal(out=rt[:, 21:22], in_=rt[:, 21:22]))
                            V(lambda e: e.tensor_scalar(out=rt[:, 24:28], in0=rt[:, 24:28], scalar1=-1e30, scalar2=1.0, op0=ALU.mult, op1=ALU.min))
                            V(lambda e: e.tensor_scalar(out=rt[:, 24:28], in0=rt[:, 24:28], scalar1=-1.0, scalar2=1.0, op0=ALU.mult, op1=ALU.add))
                            V(lambda e: e.tensor_scalar(out=rt[:, 32:36], in0=rt[:, 4:8], scalar1=rt[:, 24:25], scalar2=None, op0=ALU.mult))
                            for gq in range(1, 4):
                                V(lambda e, gq=gq: e.scalar_tensor_tensor(out=rt[:, 32:36], in0=rt[:, 4 + 4 * gq:8 + 4 * gq],
                                                                          scalar=rt[:, 24 + gq:25 + gq], in1=rt[:, 32:36],
                                                                          op0=ALU.mult, op1=ALU.add))
                            V(lambda e: e.reduce_max(out=rt[:, 22:23], in_=rt[:, 32:36], axis=AX.X))
                            V(lambda e: e.tensor_scalar(out=rt[:, 36:40], in0=rt[:, 32:36], scalar1=rt[:, 22:23], scalar2=None, op0=ALU.subtract))
                            V(lambda e: e.tensor_scalar(out=rt[:, 36:40], in0=rt[:, 36:40], scalar1=-1e30, scalar2=1.0, op0=ALU.mult, op1=ALU.min))
                            V(lambda e: e.tensor_scalar(out=rt[:, 36:40], in0=rt[:, 36:40], scalar1=-1.0, scalar2=1.0, op0=ALU.mult, op1=ALU.add))
                            V(lambda e: e.scalar_tensor_tensor(out=rt[:, 40:44], in0=rt[:, 36:40], scalar=-1e4, in1=rt[:, 32:36],
                                                               op0=ALU.mult, op1=ALU.add))
                            V(lambda e: e.reduce_max(out=rt[:, 23:24], in_=rt[:, 40:44], axis=AX.X))
                            V(lambda e: e.tensor_scalar(out=rt[:, 44:48], in0=rt[:, 40:44], scalar1=rt[:, 23:24], scalar2=None, op0=ALU.subtract))
                            V(lambda e: e.tensor_scalar(out=rt[:, 44:48], in0=rt[:, 44:48], scalar1=-1e30, scalar2=1.0, op0=ALU.mult, op1=ALU.min))
                            V(lambda e: e.tensor_scalar(out=rt[:, 44:48], in0=rt[:, 44:48], scalar1=-1.0, scalar2=1.0, op0=ALU.mult, op1=ALU.add))
                            V(lambda e: e.tensor_tensor(out=rt[:, 48:49], in0=rt[:, 23:24], in1=rt[:, 22:23], op=ALU.subtract))
                            fw.op("scalar", lambda e: e.activation(out=rt[:, 49:50], in_=rt[:, 48:49], func=AF.Exp), reads=[r_rt], writes=[r_rt])
                            V(lambda e: e.tensor_scalar(out=rt[:, 50:51], in0=rt[:, 49:50], scalar1=1.0, scalar2=None, op0=ALU.add))
                            V(lambda e: e.reciprocal(out=rt[:, 50:51], in_=rt[:, 50:51]))
                            V(lambda e: e.tensor_tensor(out=rt[:, 51:52], in0=rt[:, 49:50], in1=rt[:, 50:51], op=ALU.mult))
                            V(lambda e: e.tensor_tensor(out=rt[:, 50:51], in0=rt[:, 50:51], in1=rt[:, 21:22], op=ALU.mult))
                            V(lambda e: e.tensor_tensor(out=rt[:, 51:52], in0=rt[:, 51:52], in1=rt[:, 21:22], op=ALU.mult))
                            V(lambda e: e.tensor_scalar(out=rt[:, 52:56], in0=rt[:, 36:40], scalar1=rt[:, 50:51], scalar2=None, op0=ALU.mult))
                            V(lambda e: e.scalar_tensor_tensor(out=rt[:, 52:56], in0=rt[:, 44:48], scalar=rt[:, 51:52], in1=rt[:, 52:56],
                                                               op0=ALU.mult, op1=ALU.add))
                            for gq in range(4):
                                fw.op("vector", lambda e, gq=gq, blk=blk: e.tensor_scalar(
                                    out=comb[:, blk, 4 * gq:4 * gq + 4], in0=rt[:, 52:56], scalar1=rt[:, 24 + gq:25 + gq], scalar2=None,
                                    op0=ALU.mult), reads=[r_rt], writes=[r_comb])
                        wg32 = sbv("wg32", [128, 8, 256])
                        wu32 = sbv("wu32", [128, 8, 256])
                        wgb = sbv("wgb", [128, 8, 256], BF16)
                        wub = sbv("wub", [128, 8, 256], BF16)
                        wd32 = sbv("wd32", [128, 2, D])
                        wdb = sbv("wdb", [128, 2, D], BF16)
                        r_wg32, r_wu32, r_wd32 = Res("wg32"), Res("wu32"), Res("wd32")
                        r_wgb, r_wub, r_wdb = Res("wgb"), Res("wub"), Res("wdb")
                        HB = ST // 128
                        macc = sbv("macc", [128, HB, D])
                        r_macc = Res("macc")
                        actT = sbv("actT", [128, 2, ST], BF16)
                        r_act = Res("actT")
                        sgl = [sbv("sgl%d" % i, [128, 512]) for i in range(2)]
                        r_sgl = [Res("sgl%d" % i) for i in range(2)]
                        xq = [sbv("xq%d" % i, [128, D]) for i in range(2)]
                        r_xq = [Res("xq%d" % i) for i in range(2)]
                        tq = [sbv("tq%d" % i, [128, D]) for i in range(2)]
                        r_tq = [Res("tq%d" % i) for i in range(2)]
                        sq4 = [sbv("sq4_%d" % i, [128, 8]) for i in range(2)]
                        r_sq4 = [Res("sq4_%d" % i) for i in range(2)]
                        load_gb("ln2_g", "ln2_b", l)
                        for hv in range(1):
                            c0 = 0
                            for ex in range(16):
                                fw.dma("sync", lambda e, ex=ex: e.dma_start(
                                    out=wg32[:], in_=W["expert_w_gate"][l, ex].rearrange("(k p) n -> p k n", p=128)), writes=[r_wg32])
                                fw.op("gpsimd", lambda e: e.tensor_copy(out=wgb[:], in_=wg32[:]), reads=[r_wg32], writes=[r_wgb])
                                fw.dma("sync", lambda e, ex=ex: e.dma_start(
                                    out=wu32[:], in_=W["expert_w_up"][l, ex].rearrange("(k p) n -> p k n", p=128)), writes=[r_wu32])
                                fw.op("gpsimd", lambda e: e.tensor_copy(out=wub[:], in_=wu32[:]), reads=[r_wu32], writes=[r_wub])
                                fw.dma("sync", lambda e, ex=ex: e.dma_start(
                                    out=wd32[:], in_=W["expert_w_down"][l, ex].rearrange("(c p) n -> p c n", p=128)), writes=[r_wd32])
                                fw.op("gpsimd", lambda e: e.tensor_copy(out=wdb[:], in_=wd32[:]), reads=[r_wd32], writes=[r_wdb])
                                for c in range(2):
                                    for ts in range(ST // 512):
                                        o = c0 + ts * 512
                                        j = cnt["pA"] % 4
                                        cnt["pA"] += 1
                                        j2 = cnt["pA"] % 4
                                        cnt["pA"] += 1
                                        for k in range(8):
                                            fw.op("tensor", lambda e, k=k, j=j, c=c, o=o: e.matmul(
                                                pA[j][:], lhsT=wgb[:, k, c * 128:(c + 1) * 128], rhs=hT[:, k, o:o + 512],
                                                start=(k == 0), stop=(k == 7)), reads=[r_wgb, r_hT], writes=[r_pA[j]], pe_acc=True)
                                        for k in range(8):
                                            fw.op("tensor", lambda e, k=k, j2=j2, c=c, o=o: e.matmul(
                                                pA[j2][:], lhsT=wub[:, k, c * 128:(c + 1) * 128], rhs=hT[:, k, o:o + 512],
                                                start=(k == 0), stop=(k == 7)), reads=[r_wub, r_hT], writes=[r_pA[j2]], pe_acc=True)
                                        si = (c + ts) % 2
                                        fw.op("scalar", lambda e, j=j, si=si: e.activation(out=sgl[si][:], in_=pA[j][:], func=AF.Silu),
                                              reads=[r_pA[j]], writes=[r_sgl[si]])
                                        fw.op("vector", lambda e, j2=j2, si=si, c=c, ts=ts: e.tensor_tensor(
                                            out=actT[:, c, ts * 512:(ts + 1) * 512], in0=sgl[si][:], in1=pA[j2][:], op=ALU.mult),
                                            reads=[r_sgl[si], r_pA[j2]], writes=[r_act])
                                for bl in range(HB):
                                    blk = hv * HB + bl
                                    for nh in range(2):
                                        xk = (bl * 2 + nh) % 2
                                        for c in range(2):
                                            fw.op("tensor", lambda e, c=c, xk=xk, bl=bl, nh=nh: e.matmul(
                                                pX[xk][:], lhsT=actT[:, c, bl * 128:(bl + 1) * 128], rhs=wdb[:, c, nh * 512:(nh + 1) * 512],
                                                start=(c == 0), stop=(c == 1)), reads=[r_act, r_wdb], writes=[r_pX[xk]], pe_acc=True)
                                        if ex == 0:
                                            fw.op("vector", lambda e, xk=xk, bl=bl, nh=nh, blk=blk, ex=ex: e.tensor_scalar(
                                                out=macc[:, bl, nh * 512:(nh + 1) * 512], in0=pX[xk][:], scalar1=comb[:, blk, ex:ex + 1],
                                                scalar2=None, op0=ALU.mult), reads=[r_pX[xk], r_comb], writes=[r_macc])
                                        else:
                                            fw.op("vector", lambda e, xk=xk, bl=bl, nh=nh, blk=blk, ex=ex: e.scalar_tensor_tensor(
                                                out=macc[:, bl, nh * 512:(nh + 1) * 512], in0=pX[xk][:], scalar=comb[:, blk, ex:ex + 1],
                                                in1=macc[:, bl, nh * 512:(nh + 1) * 512], op0=ALU.mult, op1=ALU.add),
                                                reads=[r_pX[xk], r_comb, r_macc], writes=[r_macc])
                            dst = y if last else H0
                            for bl in range(HB):
                                i = bl % 2
                                r0 = t0 + (hv * HB + bl) * 128
                                fw.dma("sync", lambda e, i=i, r0=r0: e.dma_start(out=xq[i][:], in_=H1[r0:r0 + 128, :]), writes=[r_xq[i]])
                                fw.op("vector", lambda e, i=i, bl=bl: e.scalar_tensor_tensor(
                                    out=xq[i][:], in0=xq[i][:], scalar=ALPHA, in1=macc[:, bl, :], op0=ALU.mult, op1=ALU.add),
                                    reads=[r_xq[i], r_macc], writes=[r_xq[i]])
                                ln_rows(fw, xq[i], r_xq[i], tq[i], r_tq[i], sq4[i], r_sq4[i], gam, bet, r_gb, xq[i], r_xq[i])
                                fw.dma("sync", lambda e, i=i, r0=r0, dst=dst: e.dma_start(out=dst[r0:r0 + 128, :], in_=xq[i][:]),
                                       reads=[r_xq[i]])
                    fw.barrier()
            fw.barrier()

        if debug:
            dbg["br"] = nc.dram_tensor("dbg_br", [10, 128, NTOK], BF16, kind="ExternalOutput").ap()
        for l in range(depth):
            phase_a(l)
            if stop_after in ("a", "ua"):
                break
            phase_s5(l)
            if stop_after == "s5":
                break
            phase_h()
            phase_b(l, l == depth - 1)
            if stop_after in ("attn", "branches", "ln1"):
                break
    return nc, fw


def ln_rows(fw, xin, r_xin, tmp, r_tmp, s, r_s, gam, bet, r_gb, out_tile, r_out):
    fw.op("vector", lambda e: e.reduce_sum(out=s[:, 0:1], in_=xin[:], axis=AX.X), reads=[r_xin], writes=[r_s])
    fw.op("scalar", lambda e: e.activation(out=tmp[:], in_=xin[:], func=AF.Square), reads=[r_xin, r_s], writes=[r_tmp])
    fw.op("vector", lambda e: e.reduce_sum(out=s[:, 1:2], in_=tmp[:], axis=AX.X), reads=[r_tmp], writes=[r_s])
    fw.op("vector", lambda e: e.tensor_scalar(out=s[:, 2:3], in0=s[:, 0:1], scalar1=1.0 / D, scalar2=None,
                                              op0=ALU.mult), reads=[r_s], writes=[r_s])
    fw.op("vector", lambda e: e.tensor_tensor(out=s[:, 3:4], in0=s[:, 2:3], in1=s[:, 2:3], op=ALU.mult),
          reads=[r_s], writes=[r_s])
    fw.op("vector", lambda e: e.scalar_tensor_tensor(out=s[:, 4:5], in0=s[:, 1:2], scalar=1.0 / D, in1=s[:, 3:4],
                                                     op0=ALU.mult, op1=ALU.subtract), reads=[r_s], writes=[r_s])
    fw.op("vector", lambda e: e.tensor_scalar(out=s[:, 4:5], in0=s[:, 4:5], scalar1=LN_EPS, scalar2=None,
                                              op0=ALU.add), reads=[r_s], writes=[r_s])
    fw.op("scalar", lambda e: e.activation(out=s[:, 5:6], in_=s[:, 4:5], func=AF.Sqrt), reads=[r_s], writes=[r_s])
    fw.op("vector", lambda e: e.reciprocal(out=s[:, 6:7], in_=s[:, 5:6]), reads=[r_s], writes=[r_s])
    fw.op("vector", lambda e: e.scalar_tensor_tensor(out=s[:, 7:8], in0=s[:, 2:3], scalar=-1.0, in1=s[:, 6:7],
                                                     op0=ALU.mult, op1=ALU.mult), reads=[r_s], writes=[r_s])
    fw.op("vector", lambda e: e.tensor_scalar(out=tmp[:], in0=xin[:], scalar1=s[:, 2:3], scalar2=s[:, 6:7],
                                              op0=ALU.subtract, op1=ALU.mult), reads=[r_xin, r_s], writes=[r_tmp])
    fw.op("vector", lambda e: e.tensor_tensor(out=tmp[:], in0=tmp[:], in1=gam[:], op=ALU.mult),
          reads=[r_tmp, r_gb], writes=[r_tmp])
    fw.op("vector", lambda e: e.tensor_tensor(out=out_tile[:], in0=tmp[:], in1=bet[:], op=ALU.add),
          reads=[r_tmp, r_gb], writes=[r_out])


_CACHE = {}


def kernel(**inputs):
    xp = np.ascontiguousarray(inputs["x_prompt"], dtype=np.float32)
    xs = np.ascontiguousarray(inputs["x_sample"], dtype=np.float32)
    slots = {0: [("s", 0)], 1: [("s", 1)], 2: [("p", 0), ("p", 1)], 3: [("p", 2), ("p", 3)],
             4: [("p", 4)], 5: [("p", 5)], 6: [("p", 6)], 7: [("p", 7)]}
    in_maps = []
    for c in range(8):
        xc = np.zeros((NTOK, D), np.float32)
        fl = np.zeros(5, np.float32)
        if slots[c][0][0] == "s":
            xc[:] = xs[slots[c][0][1]]
            fl[1:4] = 1.0
        else:
            for j, (_, pi) in enumerate(slots[c]):
                xc[j * SEG:(j + 1) * SEG] = xp[pi]
        m = {"x": xc, "flags": fl}
        for n in WNAMES:
            m[n] = np.ascontiguousarray(inputs[n], dtype=np.float32)
        in_maps.append(m)
    if "nc" not in _CACHE:
        nc, fw = build()
        fw.finish(_CACHE.get("final", []))
        _CACHE["nc"] = nc
    res = run_bass_kernel_spmd(_CACHE["nc"], in_maps, core_ids=list(range(8)))
    yp = np.zeros_like(xp)
    ys = np.zeros_like(xs)
    for c in range(8):
        yc = np.asarray(res.results[c]["y"], dtype=np.float32)
        if slots[c][0][0] == "s":
            ys[slots[c][0][1]] = yc
        else:
            for j, (_, pi) in enumerate(slots[c]):
                yp[pi] = yc[j * SEG:(j + 1) * SEG]
    return (yp, ys)
```

```python
import math
import numpy as np
from contextlib import ExitStack
import concourse.bass as bass
import concourse.mybir as mybir
from concourse.bass_utils import run_bass_kernel_spmd

F32 = mybir.dt.float32
BF16 = mybir.dt.bfloat16
AF = mybir.ActivationFunctionType
ALU = mybir.AluOpType
AX = mybir.AxisListType

D = 1024
NSEG = 4
SEG = 4096
NTOK = NSEG * SEG
PAD = 1024
SEGP = SEG + 2 * PAD
NTOKP = NSEG * SEGP
ST = 2048
NST = NTOK // ST
DEPTH = 2
ALPHA = (2 * DEPTH) ** 0.25
LN_EPS = 1e-5
IN_COLS = 7424
SLOPES = [2.0 ** (-8.0 * (i + 1) / 12) for i in range(12)]
DIL = [(128, 1), (512, 4), (2048, 16)]

WNAMES = ["ln_in_g", "ln_in_b", "w_in", "s5_a_re", "s5_a_im", "s5_log_dt", "s5_b_re", "s5_b_im", "s5_c_re",
          "s5_c_im", "s5_d", "s5_glu_w", "s5_glu_b", "conv_w", "conv_b", "swa_sink", "w_branch_a", "w_branch_b",
          "w_branch_c", "w_branch_d", "w_o", "ln1_g", "ln1_b", "router_group_w", "router_group_b",
          "router_expert_w", "router_expert_b", "expert_w_gate", "expert_w_up", "expert_w_down", "ln2_g", "ln2_b"]
WSHAPES = {
    "ln_in_g": [D], "ln_in_b": [D], "w_in": [2, D, IN_COLS], "s5_a_re": [2, 2, 24, 64], "s5_a_im": [2, 2, 24, 64],
    "s5_log_dt": [2, 2, 24], "s5_b_re": [2, 2, 24, 64, 16], "s5_b_im": [2, 2, 24, 64, 16],
    "s5_c_re": [2, 2, 24, 16, 64], "s5_c_im": [2, 2, 24, 16, 64], "s5_d": [2, 384], "s5_glu_w": [2, 384, 384],
    "s5_glu_b": [2, 384], "conv_w": [2, 3, 384], "conv_b": [2, 384], "swa_sink": [2, 6],
    "w_branch_a": [2, 384, D], "w_branch_b": [2, 384, D], "w_branch_c": [2, 128, D], "w_branch_d": [2, 384, D],
    "w_o": [2, D, D], "ln1_g": [2, D], "ln1_b": [2, D], "router_group_w": [2, D, 4], "router_group_b": [2, 4],
    "router_expert_w": [2, D, 16], "router_expert_b": [2, 16], "expert_w_gate": [2, 16, D, 256],
    "expert_w_up": [2, 16, D, 256], "expert_w_down": [2, 16, 256, D], "ln2_g": [2, D], "ln2_b": [2, D],
}


ENGS = ["sync", "scalar", "vector", "gpsimd", "tensor"]
SEM_ROLL = 30000


class Res:
    __slots__ = ("name", "w", "r")

    def __init__(self, name):
        self.name = name
        self.w = None
        self.r = []


class FW:
    def __init__(self, nc, es):
        self.nc = nc
        self.es = es
        self.q = {e: [] for e in ENGS}
        self.sems = {e: [es.enter_context(nc.semaphore("s_" + e + "0"))] for e in ENGS}
        self.cnt = {e: 0 for e in ENGS}
        self.seen = {e: {} for e in ENGS}
        self.dma_sems = [es.enter_context(nc.semaphore("d%d" % i)) for i in range(24)]
        self.dma_cnt = [0] * 24
        self.dma_i = 0
        self.n_ops = 0
        self.fence = []

    def barrier(self):
        f = []
        for e in ENGS:
            if self.cnt[e] > 0:
                f.append((self.sems[e][-1], self.cnt[e], e))
        for k in range(len(self.dma_sems)):
            if self.dma_cnt[k] > 0:
                f.append((self.dma_sems[k], self.dma_cnt[k], "dma"))
        self.fence = f

    def _ev_new(self, eng):
        if self.cnt[eng] >= SEM_ROLL:
            self.sems[eng].append(self.es.enter_context(self.nc.semaphore("s_%s%d" % (eng, len(self.sems[eng])))))
            self.cnt[eng] = 0
        self.cnt[eng] += 1
        return (self.sems[eng][-1], self.cnt[eng], eng)

    def _need(self, eng, ev, waits, pe_ok=False):
        if ev is None:
            return
        sem, val, src = ev
        if pe_ok and src == "tensor" and eng == "tensor":
            return
        key = id(sem)
        if self.seen[eng].get(key, 0) >= val:
            return
        if key not in waits or waits[key][1] < val:
            waits[key] = (sem, val)

    def op(self, eng, fn, reads=(), writes=(), pe_acc=False):
        waits = {}
        for ev in self.fence:
            self._need(eng, ev, waits)
        for r in reads:
            self._need(eng, r.w, waits)
        for w in writes:
            self._need(eng, w.w, waits, pe_ok=pe_acc)
            for ev in w.r:
                self._need(eng, ev, waits)
        for key, (sem, val) in waits.items():
            self.seen[eng][key] = val
        ev = self._ev_new(eng)
        self.q[eng].append((list(waits.values()), fn, (ev[0], 1)))
        for r in reads:
            r.r.append(ev)
        for w in writes:
            w.w = ev
            w.r = []
        self.n_ops += 1
        return ev

    def dma(self, eng, fn, reads=(), writes=()):
        if len(writes) == 0 and eng == "sync":
            eng = "scalar"
        waits = {}
        for ev in self.fence:
            self._need(eng, ev, waits)
        for r in reads:
            self._need(eng, r.w, waits)
        for w in writes:
            self._need(eng, w.w, waits)
            for ev in w.r:
                self._need(eng, ev, waits)
        k = self.dma_i % len(self.dma_sems)
        self.dma_i += 1
        sem = self.dma_sems[k]
        if self.dma_cnt[k] > 0:
            self._need(eng, (sem, self.dma_cnt[k], "dma"), waits)
        for key, (s, val) in waits.items():
            self.seen[eng][key] = val
        self.dma_cnt[k] += 16
        ev = (sem, self.dma_cnt[k], "dma")
        self.q[eng].append((list(waits.values()), fn, (sem, 16)))
        for r in reads:
            r.r.append(ev)
        for w in writes:
            w.w = ev
            w.r = []
        self.n_ops += 1
        return ev

    def finish(self, final_res):
        waits = {}
        for r in final_res:
            self._need("sync", r.w, waits)
        for k in range(len(self.dma_sems)):
            if self.dma_cnt[k] > 0:
                self._need("sync", (self.dma_sems[k], self.dma_cnt[k], "dma"), waits)
        tail = list(waits.values())
        q = self.q
        with self.nc.Block() as block:
            def replay(e, name):
                for ws, fn, inc in q[name]:
                    for sem, val in ws:
                        e.wait_ge(sem, val)
                    fn(e).then_inc(inc[0], inc[1])
                if name == "sync":
                    for sem, val in tail:
                        e.wait_ge(sem, val)

            @block.sync
            def _(e):
                replay(e, "sync")

            @block.scalar
            def _(e):
                replay(e, "scalar")

            @block.vector
            def _(e):
                replay(e, "vector")

            @block.gpsimd
            def _(e):
                replay(e, "gpsimd")

            @block.tensor
            def _(e):
                replay(e, "tensor")


def build(debug=False, stop_after=None, depth=DEPTH):
    nc = bass.Bass("TRN2", target_bir_lowering=False)
    x = nc.dram_tensor("x", [NTOK, D], F32, kind="ExternalInput").ap()
    flags_d = nc.dram_tensor("flags", [5], F32, kind="ExternalInput").ap()
    W = {n: nc.dram_tensor(n, WSHAPES[n], F32, kind="ExternalInput").ap() for n in WNAMES}
    y = nc.dram_tensor("y", [NTOK, D], F32, kind="ExternalOutput").ap()
    H0 = nc.dram_tensor("H0", [NTOK, D], F32).ap()
    H1 = nc.dram_tensor("H1", [NTOK, D], F32, kind=("ExternalOutput" if debug else "Internal")).ap()
    HT = nc.dram_tensor("HT", [8, 128, NTOK], BF16).ap()
    UAs = nc.dram_tensor("UAs", [3, 128, NTOK], F32).ap()
    CVs = nc.dram_tensor("CVs", [3, 128, NTOKP], BF16).ap()
    KTs = nc.dram_tensor("KTs", [4, 128, NTOKP], BF16).ap()
    VTs = nc.dram_tensor("VTs", [4, 128, NTOKP], BF16).ap()
    MTs = nc.dram_tensor("MTs", [8, 128, NTOK], BF16, kind=("ExternalOutput" if debug else "Internal")).ap()
    YA = nc.dram_tensor("YA", [2, 3, 128, NTOK], F32, kind=("ExternalOutput" if debug else "Internal")).ap()
    dbg = {}
    if debug:
        dbg["h0"] = nc.dram_tensor("dbg_h0", [NTOK, D], F32, kind="ExternalOutput").ap()
        dbg["ua"] = nc.dram_tensor("dbg_ua", [3, 128, NTOK], F32, kind="ExternalOutput").ap()
        dbg["mix"] = nc.dram_tensor("dbg_mix", [NTOK, D], F32, kind="ExternalOutput").ap()
        dbg["st"] = nc.dram_tensor("dbg_st", [NTOK, 8], F32, kind="ExternalOutput").ap()

    es = ExitStack()
    with es:
        fw = FW(nc, es)

        def sb(name, shape, dt=F32):
            return es.enter_context(nc.sbuf_tensor(name, shape, dt))

        def ps(name, shape, dt=F32):
            return es.enter_context(nc.psum_tensor(name, shape, dt))

        ident = sb("ident", [128, 128], BF16)
        r_ident = Res("ident")
        fw.op("gpsimd", lambda e: e.memset(ident[:], 0.0), writes=[r_ident])
        fw.op("gpsimd", lambda e: e.affine_select(out=ident[:], in_=ident[:], pattern=[[-1, 128]],
                                                  compare_op=ALU.not_equal, fill=1.0, base=0, channel_multiplier=1),
              reads=[r_ident], writes=[r_ident])
        flg = sb("flg", [128, 5])
        r_flg = Res("flg")
        fw.dma("sync", lambda e: e.dma_start(out=flg[:], in_=flags_d.partition_broadcast(128)), writes=[r_flg])
        gam = sb("gam", [128, D])
        bet = sb("bet", [128, D])
        r_gb = Res("gb")

        def load_gb(gname, bname, l):
            gsrc = W[gname] if l is None else W[gname][l]
            bsrc = W[bname] if l is None else W[bname][l]
            fw.dma("sync", lambda e: e.dma_start(out=gam[:], in_=gsrc.partition_broadcast(128)), writes=[r_gb])
            fw.dma("sync", lambda e: e.dma_start(out=bet[:], in_=bsrc.partition_broadcast(128)), writes=[r_gb])

        pT = [ps("pT%d" % i, [128, 8, 128], BF16) for i in range(2)]
        r_pT = [Res("pT%d" % i) for i in range(2)]
        pA = [ps("pA%d" % i, [128, 512]) for i in range(4)]
        r_pA = [Res("pA%d" % i) for i in range(4)]
        pX = [ps("pX%d" % i, [128, 512]) for i in range(2)]
        r_pX = [Res("pX%d" % i) for i in range(2)]
        cnt = {"w": 0, "pA": 0, "blk": 0, "uid": 0}

        def uname(n):
            cnt["uid"] += 1
            return "%s_%d" % (n, cnt["uid"])

        def padpos(t):
            return (t // SEG) * SEGP + PAD + (t % SEG)

        def phase_a(l):
            with ExitStack() as pes:
                def sb2(name, shape, dt=F32):
                    return pes.enter_context(nc.sbuf_tensor(uname("a_" + name), shape, dt))
                hT = sb2("hT", [128, 8, ST], BF16)
                r_hT = Res("hT")
                xb = [sb2("xb%d" % i, [128, D]) for i in range(2)]
                r_xb = [Res("xb%d" % i) for i in range(2)]
                tb = [sb2("tb%d" % i, [128, D]) for i in range(2)]
                r_tb = [Res("tb%d" % i) for i in range(2)]
                hb16 = [sb2("hb16_%d" % i, [128, D], BF16) for i in range(2)]
                r_hb16 = [Res("hb16_%d" % i) for i in range(2)]
                st4 = [sb2("st4_%d" % i, [128, 8]) for i in range(2)]
                r_st4 = [Res("st4_%d" % i) for i in range(2)]
                wst = [sb2("wst%d" % i, [128, 8, 128]) for i in range(2)]
                r_wst = [Res("wst%d" % i) for i in range(2)]
                wbf = [sb2("wbf%d" % i, [128, 8, 128], BF16) for i in range(2)]
                r_wbf = [Res("wbf%d" % i) for i in range(2)]
                zt0 = sb2("zt0", [128, ST], BF16)
                r_zt0 = Res("zt0")
                zf = [sb2("zf%d" % i, [128, 512]) for i in range(4)]
                r_zf = [Res("zf%d" % i) for i in range(4)]
                zb = [sb2("zb%d" % i, [128, 512], BF16) for i in range(4)]
                r_zb = [Res("zb%d" % i) for i in range(4)]

                def layer_norm_block(i):
                    ln_rows(fw, xb[i], r_xb[i], tb[i], r_tb[i], st4[i], r_st4[i], gam, bet, r_gb, xb[i], r_xb[i])

                def transpose_block(i, col0):
                    fw.op("scalar", lambda e: e.activation(out=hb16[i][:], in_=xb[i][:], func=AF.Copy),
                          reads=[r_xb[i]], writes=[r_hb16[i]])
                    for k in range(8):
                        fw.op("tensor", lambda e, k=k: e.transpose(pT[i][:, k, :], hb16[i][:, k * 128:(k + 1) * 128],
                                                                   ident[:]),
                              reads=[r_hb16[i], r_ident], writes=[r_pT[i]], pe_acc=True)
                    fw.op("vector", lambda e: e.tensor_copy(out=hT[:, :, col0:col0 + 128], in_=pT[i][:]),
                          reads=[r_pT[i]], writes=[r_hT])

                def load_w_chunk(src_ap):
                    j = cnt["w"] % 2
                    cnt["w"] += 1
                    fw.dma("sync", lambda e: e.dma_start(out=wst[j][:], in_=src_ap.rearrange("(k p) n -> p k n", p=128)),
                           writes=[r_wst[j]])
                    fw.op("gpsimd", lambda e: e.tensor_copy(out=wbf[j][:], in_=wst[j][:]),
                          reads=[r_wst[j]], writes=[r_wbf[j]])
                    return wbf[j], r_wbf[j]

                def proj_fm(wt, r_w, evac):
                    for ts in range(ST // 512):
                        j = cnt["pA"] % 4
                        cnt["pA"] += 1
                        for k in range(8):
                            fw.op("tensor", lambda e, k=k, j=j, ts=ts: e.matmul(
                                pA[j][:], lhsT=wt[:, k, :], rhs=hT[:, k, ts * 512:(ts + 1) * 512],
                                start=(k == 0), stop=(k == 7)),
                                reads=[r_w, r_hT], writes=[r_pA[j]], pe_acc=True)
                        evac(ts, j, pA[j], r_pA[j])

                if l == 0:
                    load_gb("ln_in_g", "ln_in_b", None)
                for st_i in range(NST):
                    t0 = st_i * ST
                    p0 = padpos(t0)
                    for b in range(ST // 128):
                        i = cnt["blk"] % 2
                        cnt["blk"] += 1
                        r0 = t0 + b * 128
                        if l == 0:
                            fw.dma("sync", lambda e, i=i, r0=r0: e.dma_start(out=xb[i][:], in_=x[r0:r0 + 128, :]),
                                   writes=[r_xb[i]])
                            layer_norm_block(i)
                            fw.dma("sync", lambda e, i=i, r0=r0: e.dma_start(out=H0[r0:r0 + 128, :], in_=xb[i][:]),
                                   reads=[r_xb[i]])
                            if debug:
                                fw.dma("sync", lambda e, i=i, r0=r0: e.dma_start(out=dbg["h0"][r0:r0 + 128, :],
                                                                                in_=xb[i][:]), reads=[r_xb[i]])
                        else:
                            fw.dma("sync", lambda e, i=i, r0=r0: e.dma_start(out=xb[i][:], in_=H0[r0:r0 + 128, :]),
                                   writes=[r_xb[i]])
                        transpose_block(i, b * 128)
                    fw.dma("sync", lambda e, t0=t0: e.dma_start(out=HT[:, :, t0:t0 + ST].rearrange("k p n -> p k n"),
                                                                in_=hT[:]), reads=[r_hT])

                    def store_plain(dst, c, t0=t0):
                        def ev(ts, j, pt, r_pt):
                            fw.op("scalar", lambda e: e.activation(out=zf[j][:], in_=pt[:], func=AF.Copy),
                                  reads=[r_pt], writes=[r_zf[j]])
                            fw.dma("sync", lambda e: e.dma_start(out=dst[c, :, t0 + ts * 512:t0 + (ts + 1) * 512],
                                                                 in_=zf[j][:]), reads=[r_zf[j]])
                            if debug and dst is UAs:
                                fw.dma("sync", lambda e: e.dma_start(
                                    out=dbg["ua"][c, :, t0 + ts * 512:t0 + (ts + 1) * 512], in_=zf[j][:]),
                                    reads=[r_zf[j]])
                        return ev

                    def store_pad(dst, c, p0=p0):
                        def ev(ts, j, pt, r_pt):
                            fw.op("scalar", lambda e: e.activation(out=zb[j][:], in_=pt[:], func=AF.Copy),
                                  reads=[r_pt], writes=[r_zb[j]])
                            fw.dma("sync", lambda e: e.dma_start(out=dst[c, :, p0 + ts * 512:p0 + (ts + 1) * 512],
                                                                 in_=zb[j][:]), reads=[r_zb[j]])
                        return ev

                    wi = W["w_in"][l]
                    for c in range(3):
                        wt, r_w = load_w_chunk(wi[:, c * 128:(c + 1) * 128])
                        proj_fm(wt, r_w, store_plain(UAs, c))
                    if stop_after == "ua":
                        continue
                    for c in range(3):
                        wt, r_w = load_w_chunk(wi[:, (15 + c) * 128:(16 + c) * 128])
                        proj_fm(wt, r_w, store_pad(KTs, c))
                    wt, r_w = load_w_chunk(wi[:, 24 * 128:25 * 128])
                    proj_fm(wt, r_w, store_pad(KTs, 3))
                    for c in range(3):
                        wt, r_w = load_w_chunk(wi[:, (18 + c) * 128:(19 + c) * 128])
                        proj_fm(wt, r_w, store_pad(VTs, c))
                    wt, r_w = load_w_chunk(wi[:, 25 * 128:26 * 128])
                    proj_fm(wt, r_w, store_pad(VTs, 3))
                    for c in range(3):
                        wt, r_w = load_w_chunk(wi[:, (3 + c) * 128:(4 + c) * 128])

                        def ev_vb(ts, j, pt, r_pt):
                            fw.op("scalar", lambda e: e.activation(out=zt0[:, ts * 512:(ts + 1) * 512], in_=pt[:],
                                                                   func=AF.Copy), reads=[r_pt], writes=[r_zt0])
                        proj_fm(wt, r_w, ev_vb)
                        wt, r_w = load_w_chunk(wi[:, (9 + c) * 128:(10 + c) * 128])

                        def ev_gc(ts, j, pt, r_pt, c=c, p0=p0):
                            fw.op("vector", lambda e: e.tensor_tensor(out=zb[j][:], in0=pt[:],
                                                                      in1=zt0[:, ts * 512:(ts + 1) * 512], op=ALU.mult),
                                  reads=[r_pt, r_zt0], writes=[r_zb[j]])
                            fw.dma("sync", lambda e: e.dma_start(out=CVs[c, :, p0 + ts * 512:p0 + (ts + 1) * 512],
                                                                 in_=zb[j][:]), reads=[r_zb[j]])
                        proj_fm(wt, r_w, ev_gc)
            fw.barrier()

        def phase_s5(l):
            with ExitStack() as pes:
                def sb2(name, shape, dt=F32):
                    return pes.enter_context(nc.sbuf_tensor(uname("s_" + name), shape, dt))
                r_p = Res("prm")
                names = ["are", "aim", "ldt", "dt", "rho", "th", "c", "s", "t1", "t2", "lr", "li", "nr", "den",
                         "numr", "numi", "kr", "ki", "nki"]
                P = {n: sb2(n, [128, 24]) for n in names}

                def tt(o, a, b, op):
                    fw.op("vector", lambda e: e.tensor_tensor(out=P[o][:], in0=P[a][:], in1=P[b][:], op=op),
                          reads=[r_p], writes=[r_p])

                def ts_(o, a, s1, op0, s2=None, op1=None):
                    if op1 is None:
                        fw.op("vector", lambda e: e.tensor_scalar(out=P[o][:], in0=P[a][:], scalar1=s1, scalar2=None,
                                                                  op0=op0), reads=[r_p], writes=[r_p])
                    else:
                        fw.op("vector", lambda e: e.tensor_scalar(out=P[o][:], in0=P[a][:], scalar1=s1, scalar2=s2,
                                                                  op0=op0, op1=op1), reads=[r_p], writes=[r_p])

                def act(o, a, func, scale=1.0):
                    fw.op("scalar", lambda e: e.activation(out=P[o][:], in_=P[a][:], func=func, scale=scale),
                          reads=[r_p], writes=[r_p])

                for d in range(2):
                    fw.dma("sync", lambda e, d=d: e.dma_start(
                        out=P["are"][:, d * 12:(d + 1) * 12],
                        in_=W["s5_a_re"][l, d].rearrange("(gp g2) p -> (g2 p) gp", g2=2),
                        allow_slow_non_contiguous=True), writes=[r_p])
                    fw.dma("sync", lambda e, d=d: e.dma_start(
                        out=P["aim"][:, d * 12:(d + 1) * 12],
                        in_=W["s5_a_im"][l, d].rearrange("(gp g2) p -> (g2 p) gp", g2=2),
                        allow_slow_non_contiguous=True), writes=[r_p])
                    for g2 in range(2):
                        fw.dma("sync", lambda e, d=d, g2=g2: e.dma_start(
                            out=P["ldt"][64 * g2:64 * g2 + 64, d * 12:(d + 1) * 12],
                            in_=W["s5_log_dt"][l, d].rearrange("(gp g2) -> g2 gp", g2=2)[g2].partition_broadcast(64),
                            allow_slow_non_contiguous=True), writes=[r_p])
                act("dt", "ldt", AF.Exp)
                tt("t1", "are", "dt", ALU.mult)
                act("rho", "t1", AF.Exp)
                tt("th", "aim", "dt", ALU.mult)
                act("t1", "th", AF.Sin, scale=1.0 / 128)
                tt("t2", "t1", "t1", ALU.mult)
                ts_("c", "t2", -2.0, ALU.mult, 1.0, ALU.add)
                act("s", "th", AF.Sin, scale=1.0 / 64)
                for _ in range(6):
                    tt("t1", "c", "c", ALU.mult)
                    tt("t2", "s", "s", ALU.mult)
                    fw.op("vector", lambda e: e.scalar_tensor_tensor(out=P["s"][:], in0=P["c"][:], scalar=2.0,
                                                                     in1=P["s"][:], op0=ALU.mult, op1=ALU.mult),
                          reads=[r_p], writes=[r_p])
                    tt("c", "t1", "t2", ALU.subtract)
                tt("lr", "rho", "c", ALU.mult)
                tt("li", "rho", "s", ALU.mult)
                ts_("nr", "lr", -1.0, ALU.add)
                tt("t1", "are", "are", ALU.mult)
                tt("t2", "aim", "aim", ALU.mult)
                tt("den", "t1", "t2", ALU.add)
                fw.op("vector", lambda e: e.reciprocal(out=P["den"][:], in_=P["den"][:]), reads=[r_p], writes=[r_p])
                tt("t1", "nr", "are", ALU.mult)
                tt("t2", "li", "aim", ALU.mult)
                tt("numr", "t1", "t2", ALU.add)
                tt("t1", "li", "are", ALU.mult)
                tt("t2", "nr", "aim", ALU.mult)
                tt("numi", "t1", "t2", ALU.subtract)
                tt("kr", "numr", "den", ALU.mult)
                tt("ki", "numi", "den", ALU.mult)
                ts_("nki", "ki", -1.0, ALU.mult)
                LRR = sb2("LRR", [128, 2, 24])
                LIS = sb2("LIS", [128, 2, 24])
                for hh in range(2):
                    fw.op("vector", lambda e, hh=hh: e.tensor_copy(out=LRR[:, hh, :], in_=P["lr"][:]),
                          reads=[r_p], writes=[r_p])
                fw.op("vector", lambda e: e.tensor_scalar(out=LIS[:, 0, :], in0=P["li"][:], scalar1=-1.0, scalar2=None,
                                                          op0=ALU.mult), reads=[r_p], writes=[r_p])
                fw.op("vector", lambda e: e.tensor_copy(out=LIS[:, 1, :], in_=P["li"][:]), reads=[r_p], writes=[r_p])

                for nm in ["l2r", "l2i"]:
                    P[nm] = sb2(nm, [128, 24])
                tt("t1", "lr", "lr", ALU.mult)
                tt("t2", "li", "li", ALU.mult)
                tt("l2r", "t1", "t2", ALU.subtract)
                fw.op("vector", lambda e: e.scalar_tensor_tensor(out=P["l2i"][:], in0=P["lr"][:], scalar=2.0,
                                                                 in1=P["li"][:], op0=ALU.mult, op1=ALU.mult),
                      reads=[r_p], writes=[r_p])
                L2RR = sb2("L2RR", [128, 2, 24])
                L2IS = sb2("L2IS", [128, 2, 24])
                for hh in range(2):
                    fw.op("vector", lambda e, hh=hh: e.tensor_copy(out=L2RR[:, hh, :], in_=P["l2r"][:]),
                          reads=[r_p], writes=[r_p])
                fw.op("vector", lambda e: e.tensor_scalar(out=L2IS[:, 0, :], in0=P["l2i"][:], scalar1=-1.0, scalar2=None,
                                                          op0=ALU.mult), reads=[r_p], writes=[r_p])
                fw.op("vector", lambda e: e.tensor_copy(out=L2IS[:, 1, :], in_=P["l2i"][:]), reads=[r_p], writes=[r_p])
                LRR64 = sb2("LRR64", [128, 2, 24, 64])
                LIS64 = sb2("LIS64", [128, 2, 24, 64])
                for (dst64, src3) in [(LRR64, LRR), (LIS64, LIS)]:
                    fw.op("vector", lambda e, dst64=dst64, src3=src3: e.tensor_copy(out=dst64[:, :, :, 0], in_=src3[:]),
                          reads=[r_p], writes=[r_p])
                    w_ = 1
                    while w_ < 64:
                        fw.op("vector", lambda e, dst64=dst64, w_=w_: e.tensor_copy(out=dst64[:, :, :, w_:2 * w_],
                                                                                   in_=dst64[:, :, :, 0:w_]),
                              reads=[r_p], writes=[r_p])
                        w_ *= 2
                for nm in ["l4r", "l4i"]:
                    P[nm] = sb2(nm, [128, 24])
                tt("t1", "l2r", "l2r", ALU.mult)
                tt("t2", "l2i", "l2i", ALU.mult)
                tt("l4r", "t1", "t2", ALU.subtract)
                fw.op("vector", lambda e: e.scalar_tensor_tensor(out=P["l4i"][:], in0=P["l2r"][:], scalar=2.0,
                                                                 in1=P["l2i"][:], op0=ALU.mult, op1=ALU.mult),
                      reads=[r_p], writes=[r_p])
                L4RR = sb2("L4RR", [128, 2, 24])
                L4IS = sb2("L4IS", [128, 2, 24])
                for hh in range(2):
                    fw.op("vector", lambda e, hh=hh: e.tensor_copy(out=L4RR[:, hh, :], in_=P["l4r"][:]),
                          reads=[r_p], writes=[r_p])
                fw.op("vector", lambda e: e.tensor_scalar(out=L4IS[:, 0, :], in0=P["l4i"][:], scalar1=-1.0, scalar2=None,
                                                          op0=ALU.mult), reads=[r_p], writes=[r_p])
                fw.op("vector", lambda e: e.tensor_copy(out=L4IS[:, 1, :], in_=P["l4i"][:]), reads=[r_p], writes=[r_p])
                L2R32 = sb2("L2R32", [128, 2, 24, 32])
                L2I32 = sb2("L2I32", [128, 2, 24, 32])
                for (dst32, src3) in [(L2R32, L2RR), (L2I32, L2IS)]:
                    fw.op("vector", lambda e, dst32=dst32, src3=src3: e.tensor_copy(out=dst32[:, :, :, 0], in_=src3[:]),
                          reads=[r_p], writes=[r_p])
                    w_ = 1
                    while w_ < 32:
                        fw.op("vector", lambda e, dst32=dst32, w_=w_: e.tensor_copy(out=dst32[:, :, :, w_:2 * w_],
                                                                                   in_=dst32[:, :, :, 0:w_]),
                              reads=[r_p], writes=[r_p])
                        w_ *= 2
                T1 = sb2("T1", [128, 2, 24, 64])
                T2 = sb2("T2", [128, 2, 24, 64])
                CB2 = T2[:, :, :, 32:64]
                CB = sb2("CB", [128, 2, 24, 64])
                r_t12 = Res("T12")
                r_cb = Res("CB")
                Bw = sb2("Bw", [128, 48, 128])
                Cw = sb2("Cw", [128, 48, 128])
                r_bw = Res("Bw")
                r_cw = Res("Cw")
                fw.op("gpsimd", lambda e: e.memset(Bw[:], 0.0), writes=[r_bw])
                fw.op("gpsimd", lambda e: e.memset(Cw[:], 0.0), writes=[r_cw])

                def widx(d, gp, ri):
                    return (d * 12 + gp) * 2 + ri
                for d in range(2):
                    for gp in range(12):
                        for g2 in range(2):
                            g = 2 * gp + g2
                            r0 = 16 * (g % 8)
                            for ri, (bn, cn) in enumerate([("s5_b_re", "s5_c_re"), ("s5_b_im", "s5_c_im")]):
                                fw.dma("sync", lambda e, d=d, gp=gp, g2=g2, g=g, r0=r0, ri=ri, bn=bn: e.dma_start(
                                    out=Bw[r0:r0 + 16, widx(d, gp, ri), 64 * g2:64 * g2 + 64],
                                    in_=W[bn][l, d, g].rearrange("p h -> h p"),
                                    allow_slow_non_contiguous=True), writes=[r_bw])
                                fw.dma("sync", lambda e, d=d, gp=gp, g2=g2, g=g, r0=r0, ri=ri, cn=cn: e.dma_start(
                                    out=Cw[64 * g2:64 * g2 + 64, widx(d, gp, ri), r0:r0 + 16],
                                    in_=W[cn][l, d, g].rearrange("h p -> p h"),
                                    allow_slow_non_contiguous=True), writes=[r_cw])
                Cw4 = Cw[:].rearrange("p (a r) n -> p a r n", r=2)
                fw.op("vector", lambda e: e.tensor_scalar(out=Cw4[:, :, 1, :], in0=Cw4[:, :, 1, :], scalar1=-1.0,
                                                          scalar2=None, op0=ALU.mult), reads=[r_cw], writes=[r_cw])

                XS = sb2("XS", [128, 2, 24, 129])
                BU = sb2("BU", [128, 2, 24, 128])
                PQ = sb2("PQ", [128, 2, 2, 24])
                r_xs = Res("XS")
                r_bu = Res("BU")
                r_pq = Res("PQ")
                r_pq1 = Res("PQ1")
                tmpb = [sb2("tmpb%d" % i, [128, 2, 128]) for i in range(2)]
                r_tmpb = [Res("tmpb%d" % i) for i in range(2)]
                ua = [[sb2("ua%d_%d" % (i, d), [128, 3, 128]) for d in range(2)] for i in range(2)]
                r_ua = [[Res("ua%d_%d" % (i, d)) for d in range(2)] for i in range(2)]
                yo = [sb2("yo%d" % i, [128, 128]) for i in range(2)]
                r_yo = [Res("yo%d" % i) for i in range(2)]
                fw.op("vector", lambda e: e.memset(XS[:], 0.0), writes=[r_xs])
                NT = NTOK // 128
                kcount = 0
                for i in range(NT):
                    tiles = [i, NT - 1 - i]
                    bi = i % 2
                    for d in range(2):
                        tk = tiles[d] * 128
                        fw.dma("sync", lambda e, d=d, tk=tk, bi=bi: e.dma_start(
                            out=ua[bi][d][:], in_=UAs[:, :, tk:tk + 128].rearrange("c p n -> p c n")),
                            writes=[r_ua[bi][d]])
                    for d in range(2):
                        for gp in range(12):
                            col = d * 12 + gp
                            c3 = gp // 4
                            pp = 2 * (col % 2)
                            for ri in range(2):
                                fw.op("tensor", lambda e, d=d, gp=gp, ri=ri, pp=pp, c3=c3, bi=bi: e.matmul(
                                    pA[pp + ri][:, 0:128], lhsT=Bw[:, widx(d, gp, ri), :], rhs=ua[bi][d][:, c3, :],
                                    start=True, stop=True),
                                    reads=[r_bw, r_ua[bi][d]], writes=[r_pA[pp + ri]])
                            tbk = tmpb[col % 2]
                            r_tbk = r_tmpb[col % 2]
                            if d == 0:
                                bre, bim = BU[:, 0, col, :], BU[:, 1, col, :]
                            else:
                                bre, bim = BU[:, 0, col, ::-1], BU[:, 1, col, ::-1]
                            fw.op("scalar", lambda e, tbk=tbk, pp=pp, col=col: e.activation(
                                out=tbk[:, 0, :], in_=pA[pp][:, 0:128], func=AF.Identity, scale=P["kr"][:, col:col + 1]),
                                reads=[r_pA[pp], r_p], writes=[r_tbk])
                            fw.op("vector", lambda e, tbk=tbk, pp=pp, col=col, bre=bre: e.scalar_tensor_tensor(
                                out=bre, in0=pA[pp + 1][:, 0:128], scalar=P["nki"][:, col:col + 1], in1=tbk[:, 0, :],
                                op0=ALU.mult, op1=ALU.add), reads=[r_pA[pp + 1], r_p, r_tbk], writes=[r_bu])
                            fw.op("scalar", lambda e, tbk=tbk, pp=pp, col=col: e.activation(
                                out=tbk[:, 1, :], in_=pA[pp + 1][:, 0:128], func=AF.Identity, scale=P["kr"][:, col:col + 1]),
                                reads=[r_pA[pp + 1], r_p], writes=[r_tbk])
                            fw.op("vector", lambda e, tbk=tbk, pp=pp, col=col, bim=bim: e.scalar_tensor_tensor(
                                out=bim, in0=pA[pp][:, 0:128], scalar=P["ki"][:, col:col + 1], in1=tbk[:, 1, :],
                                op0=ALU.mult, op1=ALU.add), reads=[r_pA[pp], r_p, r_tbk], writes=[r_bu])
                    BUe = BU[:, :, :, 0:128:2]
                    BUo = BU[:, :, :, 1:128:2]
                    BUes = BU[:, ::-1, :, 0:128:2]
                    fw.op("vector", lambda e, BUe=BUe: e.tensor_tensor(out=T1[:], in0=LRR64[:], in1=BUe, op=ALU.mult),
                          reads=[r_bu, r_p], writes=[r_t12])
                    fw.op("vector", lambda e, BUes=BUes: e.tensor_tensor(out=T2[:], in0=LIS64[:], in1=BUes, op=ALU.mult),
                          reads=[r_bu, r_p], writes=[r_t12])
                    fw.op("vector", lambda e: e.tensor_tensor(out=T1[:], in0=T1[:], in1=T2[:], op=ALU.add),
                          reads=[r_t12], writes=[r_t12])
                    fw.op("vector", lambda e, BUo=BUo: e.tensor_tensor(out=CB[:], in0=T1[:], in1=BUo, op=ALU.add),
                          reads=[r_t12, r_bu], writes=[r_cb])
                    C1e = CB[:, :, :, 0:64:2]
                    C1o = CB[:, :, :, 1:64:2]
                    C1es = CB[:, ::-1, :, 0:64:2]
                    fw.op("vector", lambda e, C1e=C1e: e.tensor_tensor(out=T1[:, :, :, 0:32], in0=L2R32[:], in1=C1e, op=ALU.mult),
                          reads=[r_cb, r_p], writes=[r_t12])
                    fw.op("vector", lambda e, C1es=C1es: e.tensor_tensor(out=T2[:, :, :, 0:32], in0=L2I32[:], in1=C1es, op=ALU.mult),
                          reads=[r_cb, r_p], writes=[r_t12])
                    fw.op("vector", lambda e: e.tensor_tensor(out=T1[:, :, :, 0:32], in0=T1[:, :, :, 0:32], in1=T2[:, :, :, 0:32],
                                                              op=ALU.add), reads=[r_t12], writes=[r_t12])
                    fw.op("vector", lambda e, C1o=C1o: e.tensor_tensor(out=CB2, in0=T1[:, :, :, 0:32], in1=C1o, op=ALU.add),
                          reads=[r_t12, r_cb], writes=[r_t12])
                    for n_ in range(32):
                        j = 4 * n_
                        fw.op("vector", lambda e, j=j: e.tensor_tensor(out=PQ[:, 0], in0=L4RR[:], in1=XS[:, :, :, j],
                                                                       op=ALU.mult),
                              reads=[r_xs, r_p], writes=[r_pq])
                        fw.op("vector", lambda e, j=j: e.tensor_tensor(out=PQ[:, 1], in0=L4IS[:], in1=XS[:, ::-1, :, j],
                                                                       op=ALU.mult),
                              reads=[r_xs, r_p], writes=[r_pq1])
                        fw.op("vector", lambda e: e.tensor_tensor(out=PQ[:, 0], in0=PQ[:, 0], in1=PQ[:, 1], op=ALU.add),
                              reads=[r_pq, r_pq1], writes=[r_pq])
                        fw.op("vector", lambda e, j=j, n_=n_: e.tensor_tensor(out=XS[:, :, :, j + 4], in0=PQ[:, 0],
                                                                              in1=T2[:, :, :, 32 + n_], op=ALU.add),
                              reads=[r_pq, r_t12], writes=[r_xs])
                    X4 = XS[:, :, :, 0:128:4]
                    X4s = XS[:, ::-1, :, 0:128:4]
                    X42 = XS[:, :, :, 2:129:4]
                    fw.op("vector", lambda e, X4=X4: e.tensor_tensor(out=T1[:, :, :, 0:32], in0=L2R32[:], in1=X4, op=ALU.mult),
                          reads=[r_xs, r_p], writes=[r_t12])
                    fw.op("vector", lambda e, X4s=X4s: e.tensor_tensor(out=T2[:, :, :, 0:32], in0=L2I32[:], in1=X4s, op=ALU.mult),
                          reads=[r_xs, r_p], writes=[r_t12])
                    fw.op("vector", lambda e: e.tensor_tensor(out=T1[:, :, :, 0:32], in0=T1[:, :, :, 0:32], in1=T2[:, :, :, 0:32],
                                                              op=ALU.add), reads=[r_t12], writes=[r_t12])
                    fw.op("vector", lambda e, X42=X42, C1e=C1e: e.tensor_tensor(out=X42, in0=T1[:, :, :, 0:32], in1=C1e, op=ALU.add),
                          reads=[r_t12, r_cb], writes=[r_xs])
                    XSe = XS[:, :, :, 0:128:2]
                    XSes = XS[:, ::-1, :, 0:128:2]
                    XSo = XS[:, :, :, 1:129:2]
                    fw.op("vector", lambda e, XSe=XSe: e.tensor_tensor(out=T1[:], in0=LRR64[:], in1=XSe, op=ALU.mult),
                          reads=[r_xs, r_p], writes=[r_t12])
                    fw.op("vector", lambda e, XSes=XSes: e.tensor_tensor(out=T2[:], in0=LIS64[:], in1=XSes, op=ALU.mult),
                          reads=[r_xs, r_p], writes=[r_t12])
                    fw.op("vector", lambda e: e.tensor_tensor(out=T1[:], in0=T1[:], in1=T2[:], op=ALU.add),
                          reads=[r_t12], writes=[r_t12])
                    fw.op("vector", lambda e, XSo=XSo, BUe=BUe: e.tensor_tensor(out=XSo, in0=T1[:], in1=BUe, op=ALU.add),
                          reads=[r_t12, r_bu], writes=[r_xs])
                    for d in range(2):
                        tk = tiles[d] * 128
                        for c3 in range(3):
                            pj = kcount % 2
                            kcount += 1
                            n = 0
                            for gq in range(4):
                                gp = c3 * 4 + gq
                                col = d * 12 + gp
                                for ri in range(2):
                                    fw.op("tensor", lambda e, d=d, gp=gp, ri=ri, col=col, pj=pj, n=n: e.matmul(
                                        pX[pj][:, 0:128], lhsT=Cw[:, widx(d, gp, ri), :], rhs=XS[:, ri, col, 1:129],
                                        start=(n == 0), stop=(n == 7)),
                                        reads=[r_cw, r_xs], writes=[r_pX[pj]], pe_acc=True)
                                    n += 1
                            ov = yo[pj][:, :] if d == 0 else yo[pj][:, ::-1]
                            fw.op("scalar", lambda e, pj=pj, ov=ov: e.activation(out=ov, in_=pX[pj][:, 0:128],
                                                                                 func=AF.Copy),
                                  reads=[r_pX[pj]], writes=[r_yo[pj]])
                            fw.dma("sync", lambda e, d=d, c3=c3, tk=tk, pj=pj: e.dma_start(
                                out=YA[d, c3, :, tk:tk + 128], in_=yo[pj][:]), reads=[r_yo[pj]])
                    fw.op("vector", lambda e: e.tensor_copy(out=XS[:, :, :, 0], in_=XS[:, :, :, 128]),
                          reads=[r_xs], writes=[r_xs])
                    if (i + 1) % 32 == 0 and i + 1 < NT:
                        sgn = (i + 1) // 32
                        fw.op("vector", lambda e, sgn=sgn: e.tensor_scalar(
                            out=XS[:, :, 0:12, 0], in0=XS[:, :, 0:12, 0], scalar1=flg[:, sgn:sgn + 1], scalar2=None,
                            op0=ALU.mult), reads=[r_xs, r_flg], writes=[r_xs])
                        fw.op("vector", lambda e, sgn=sgn: e.tensor_scalar(
                            out=XS[:, :, 12:24, 0], in0=XS[:, :, 12:24, 0], scalar1=flg[:, 4 - sgn:5 - sgn],
                            scalar2=None, op0=ALU.mult), reads=[r_xs, r_flg], writes=[r_xs])
            fw.barrier()

        def phase_h():
            with ExitStack() as pes:
                hb = [pes.enter_context(nc.sbuf_tensor(uname("h_hb%d" % i), [128, 4, PAD], BF16)) for i in range(2)]
                r_hb = [Res("hb%d" % i) for i in range(2)]
                k = 0
                for (T, nch) in [(KTs, 4), (VTs, 4), (CVs, 3)]:
                    for sg in range(NSEG):
                        jobs = []
                        src = ((sg - 1) * SEGP + SEG) if sg > 0 else (sg * SEGP + PAD)
                        jobs.append((src, sg * SEGP, sg))
                        src = ((sg + 1) * SEGP + PAD) if sg < NSEG - 1 else (sg * SEGP + SEG)
                        jobs.append((src, sg * SEGP + PAD + SEG, sg + 1))
                        for (src, dst, fc) in jobs:
                            b = k % 2
                            k += 1
                            fw.dma("sync", lambda e, T=T, nch=nch, src=src, b=b: e.dma_start(
                                out=hb[b][:, 0:nch, :], in_=T[0:nch, :, src:src + PAD].rearrange("c p n -> p c n")),
                                writes=[r_hb[b]])
                            fw.op("vector", lambda e, nch=nch, b=b, fc=fc: e.tensor_scalar(
                                out=hb[b][:, 0:nch, :], in0=hb[b][:, 0:nch, :], scalar1=flg[:, fc:fc + 1], scalar2=None,
                                op0=ALU.mult), reads=[r_hb[b], r_flg], writes=[r_hb[b]])
                            fw.dma("sync", lambda e, T=T, nch=nch, dst=dst, b=b: e.dma_start(
                                out=T[0:nch, :, dst:dst + PAD].rearrange("c p n -> p c n"), in_=hb[b][:, 0:nch, :]),
                                reads=[r_hb[b]])
            fw.barrier()

        maskD = sb("maskD", [128, 6, 256])
        maskS = sb("maskS", [128, 6, 384])
        r_mask = Res("mask")
        ones_col = sb("ones_col", [128, 1])
        fw.op("vector", lambda e: e.memset(ones_col[:], 1.0), writes=[r_mask])
        with ExitStack() as mes:
            ii = mes.enter_context(nc.sbuf_tensor("m_ii", [128, 128], mybir.dt.int32))
            fi = mes.enter_context(nc.sbuf_tensor("m_fi", [128, 128], F32))
            ta = mes.enter_context(nc.sbuf_tensor("m_ta", [128, 128], F32))
            tv = mes.enter_context(nc.sbuf_tensor("m_tv", [128, 128], F32))
            r_m = Res("m")
            fw.op("gpsimd", lambda e: e.iota(ii[:], pattern=[[-1, 128]], base=0, channel_multiplier=1), writes=[r_m])
            fw.op("vector", lambda e: e.tensor_copy(out=fi[:], in_=ii[:]), reads=[r_m], writes=[r_m])

            def mk_mask(dst, off, half, coef):
                fw.op("vector", lambda e: e.tensor_scalar(out=ta[:], in0=fi[:], scalar1=float(off), scalar2=None,
                                                          op0=ALU.add), reads=[r_m], writes=[r_m])
                fw.op("vector", lambda e: e.tensor_scalar(out=tv[:], in0=ta[:], scalar1=-1.0, scalar2=None,
                                                          op0=ALU.mult), reads=[r_m], writes=[r_m])
                fw.op("vector", lambda e: e.tensor_tensor(out=ta[:], in0=ta[:], in1=tv[:], op=ALU.max),
                      reads=[r_m], writes=[r_m])
                fw.op("vector", lambda e: e.tensor_scalar(out=tv[:], in0=ta[:], scalar1=-1.0, scalar2=float(half) + 0.5,
                                                          op0=ALU.mult, op1=ALU.add), reads=[r_m], writes=[r_m])
                fw.op("vector", lambda e: e.tensor_scalar(out=tv[:], in0=tv[:], scalar1=0.0, scalar2=0.5,
                                                          op0=ALU.max, op1=ALU.min), reads=[r_m], writes=[r_m])
                fw.op("scalar", lambda e: e.activation(out=ta[:], in_=ta[:], func=AF.Exp, scale=-float(coef)),
                      reads=[r_m], writes=[r_m])
                fw.op("vector", lambda e: e.scalar_tensor_tensor(out=dst, in0=ta[:], scalar=2.0, in1=tv[:],
                                                                 op0=ALU.mult, op1=ALU.mult),
                      reads=[r_m], writes=[r_m, r_mask])
            for gi, (win, dil) in enumerate(DIL):
                for h in range(2):
                    sl = SLOPES[6 + 2 * gi + h]
                    for kt in range(2):
                        mk_mask(maskD[:, 2 * gi + h, kt * 128:(kt + 1) * 128], -64 + 128 * kt, 64, sl * dil)
            for h in range(6):
                for kt in range(3):
                    mk_mask(maskS[:, h, kt * 128:(kt + 1) * 128], 128 * (kt - 1), 128, SLOPES[h])
        fw.barrier()

        def phase_b(l, last):
            with ExitStack() as L0:
                def sb0(name, shape, dt=F32):
                    return L0.enter_context(nc.sbuf_tensor(uname("b_" + name), shape, dt))
                hT = sb0("hT", [128, 8, ST], BF16)
                r_hT = Res("hT")
                vcol = sb0("vcol", [128, 2])
                r_vcol = Res("vcol")
                sexp = sb0("sexp", [128, 6])
                r_sexp = Res("sexp")
                fw.dma("sync", lambda e: e.dma_start(out=sexp[:], in_=W["swa_sink"][l].partition_broadcast(128)),
                       writes=[r_sexp])
                fw.op("scalar", lambda e: e.activation(out=sexp[:], in_=sexp[:], func=AF.Exp),
                      reads=[r_sexp], writes=[r_sexp])
                wi = W["w_in"][l]
                for st_i in range(NST):
                    t0 = st_i * ST
                    p0 = padpos(t0)
                    sg = st_i // 2
                    hf = st_i % 2
                    fw.dma("sync", lambda e, t0=t0: e.dma_start(
                        out=hT[:], in_=HT[:, :, t0:t0 + ST].rearrange("k p n -> p k n")), writes=[r_hT])
                    fw.op("vector", lambda e: e.memset(vcol[:], 1.0), writes=[r_vcol])
                    fw.op("vector", lambda e, sg=sg: e.tensor_copy(out=vcol[0:64, 0:1], in_=flg[0:64, sg:sg + 1]),
                          reads=[r_flg], writes=[r_vcol])
                    fw.op("vector", lambda e, sg=sg: e.tensor_copy(out=vcol[64:128, 1:2], in_=flg[64:128, sg + 1:sg + 2]),
                          reads=[r_flg], writes=[r_vcol])
                    with ExitStack() as L1:
                        def sb1(name, shape, dt=F32):
                            return L1.enter_context(nc.sbuf_tensor(uname("b1_" + name), shape, dt))
                        brT = sb1("brT", [128, 10, ST], BF16)
                        r_br = Res("brT")
                        wst = [sb1("wst%d" % i, [128, 8, 128]) for i in range(2)]
                        r_wst = [Res("wst%d" % i) for i in range(2)]
                        wbf = [sb1("wbf%d" % i, [128, 8, 128], BF16) for i in range(2)]
                        r_wbf = [Res("wbf%d" % i) for i in range(2)]

                        def load_w_chunk(parts):
                            j = cnt["w"] % 2
                            cnt["w"] += 1
                            for (src_ap, c0, n) in parts:
                                fw.dma("sync", lambda e, src_ap=src_ap, c0=c0, n=n, j=j: e.dma_start(
                                    out=wst[j][:, :, c0:c0 + n], in_=src_ap.rearrange("(k p) n -> p k n", p=128)),
                                    writes=[r_wst[j]])
                            fw.op("gpsimd", lambda e, j=j: e.tensor_copy(out=wbf[j][:], in_=wst[j][:]),
                                  reads=[r_wst[j]], writes=[r_wbf[j]])
                            return wbf[j], r_wbf[j]

                        def proj_fm(wt, r_w, evac):
                            for ts in range(ST // 512):
                                j = cnt["pA"] % 4
                                cnt["pA"] += 1
                                for k in range(8):
                                    fw.op("tensor", lambda e, k=k, j=j, ts=ts: e.matmul(
                                        pA[j][:], lhsT=wt[:, k, :], rhs=hT[:, k, ts * 512:(ts + 1) * 512],
                                        start=(k == 0), stop=(k == 7)),
                                        reads=[r_w, r_hT], writes=[r_pA[j]], pe_acc=True)
                                evac(ts, j, pA[j], r_pA[j])

                        with ExitStack() as S1:
                            def sbs(name, shape, dt=F32):
                                return S1.enter_context(nc.sbuf_tensor(uname("b2_" + name), shape, dt))
                            KT1 = sbs("KT1", [128, 2 * ST], BF16)
                            VT1 = sbs("VT1", [128, 2 * ST], BF16)
                            r_kv = Res("kv")
                            QT1 = sbs("QT1", [128, ST], BF16)
                            r_q = Res("q")
                            UACC = sbs("UACC", [128, 2, ST])
                            r_ua = Res("uacc")
                            RC = sbs("RC", [128, ST])
                            r_rc = Res("rc")
                            Et = [sbs("E%d" % i, [128, 384]) for i in range(2)]
                            r_E = [Res("E%d" % i) for i in range(2)]
                            Pt = [sbs("P%d" % i, [128, 384], BF16) for i in range(2)]
                            r_P = [Res("P%d" % i) for i in range(2)]
                            VE = [sbs("VE%d" % i, [128, 3, 2, 192], BF16) for i in range(2)]
                            r_VE = [Res("VE%d" % i) for i in range(2)]
                            sm = [sbs("sm%d" % i, [128, 128]) for i in range(2)]
                            r_sm = [Res("sm%d" % i) for i in range(2)]
                            for i in range(2):
                                fw.op("vector", lambda e, i=i: e.memset(VE[i][:], 1.0), writes=[r_VE[i]])
                            ac = {"u": 0}

                            def q_evac(ts, j, pt, r_pt):
                                fw.op("scalar", lambda e: e.activation(out=QT1[:, ts * 512:(ts + 1) * 512], in_=pt[:],
                                                                       func=AF.Copy), reads=[r_pt], writes=[r_q])

                            def load_kv(c, p0=p0):
                                fw.dma("sync", lambda e, c=c, p0=p0: e.dma_start(
                                    out=KT1[:], in_=KTs[c, :, p0 - PAD:p0 - PAD + 2 * ST]), writes=[r_kv])
                                fw.dma("sync", lambda e, c=c, p0=p0: e.dma_start(
                                    out=VT1[:], in_=VTs[c, :, p0 - PAD:p0 - PAD + 2 * ST]), writes=[r_kv])

                            def unit(nkt, kcols, qcols, heads, mask_of, valid_of, lhs_of, sink_dst):
                                u = ac["u"] % 2
                                ac["u"] += 1
                                for kt in range(nkt):
                                    fw.op("tensor", lambda e, kt=kt, u=u: e.transpose(
                                        pT[u][:, kt, :], VT1[:, kcols(kt)], ident[:]),
                                        reads=[r_kv, r_ident], writes=[r_pT[u]], pe_acc=True)
                                fw.op("vector", lambda e, u=u: e.tensor_copy(
                                    out=VE[u][:, 0:nkt, :, 64:128],
                                    in_=pT[u][:, 0:nkt, :].rearrange("p k (h d) -> p k h d", h=2)),
                                    reads=[r_pT[u]], writes=[r_VE[u]])
                                for (hrow, vslot, tag) in heads:
                                    j = cnt["pA"] % 4
                                    cnt["pA"] += 1
                                    j2 = cnt["pA"] % 4
                                    cnt["pA"] += 1
                                    ei = ac["u"] % 2
                                    for kt in range(nkt):
                                        fw.op("tensor", lambda e, kt=kt, j=j, hrow=hrow: e.matmul(
                                            pA[j][:, kt * 128:(kt + 1) * 128], lhsT=KT1[hrow:hrow + 64, kcols(kt)],
                                            rhs=QT1[hrow:hrow + 64, qcols], start=True, stop=True),
                                            reads=[r_kv, r_q], writes=[r_pA[j]], pe_acc=True)
                                    fw.op("scalar", lambda e, j=j, ei=ei: e.activation(
                                        out=Et[ei][:, 0:nkt * 128], in_=pA[j][:, 0:nkt * 128], func=AF.Exp, scale=0.125),
                                        reads=[r_pA[j]], writes=[r_E[ei]])
                                    for kt in range(nkt):
                                        vc = valid_of(kt)
                                        fw.op("vector", lambda e, kt=kt, ei=ei, vc=vc, tag=tag: e.scalar_tensor_tensor(
                                            out=Pt[ei][:, kt * 128:(kt + 1) * 128], in0=Et[ei][:, kt * 128:(kt + 1) * 128],
                                            scalar=vc, in1=mask_of(tag)[:, kt * 128:(kt + 1) * 128],
                                            op0=ALU.mult, op1=ALU.mult),
                                            reads=[r_E[ei], r_mask, r_vcol, r_flg], writes=[r_P[ei]])
                                    for kt in range(nkt):
                                        fw.op("tensor", lambda e, kt=kt, j2=j2, ei=ei, u=u, vslot=vslot, tag=tag: e.matmul(
                                            pA[j2][:, 0:128], lhsT=lhs_of(VE[u], kt, vslot, tag),
                                            rhs=Pt[ei][:, kt * 128:(kt + 1) * 128], start=(kt == 0), stop=(kt == nkt - 1)),
                                            reads=[r_VE[u], r_P[ei]], writes=[r_pA[j2]], pe_acc=True)
                                    sink_dst(tag, pA[j2], r_pA[j2])

                            for gi, (win, dil) in enumerate(DIL):
                                wt, r_w = load_w_chunk([(wi[:, (12 + gi) * 128:(13 + gi) * 128], 0, 128)])
                                proj_fm(wt, r_w, q_evac)
                                load_kv(gi)
                                nsub = ST // dil
                                for r in range(dil):
                                    for qb in range(nsub // 128):
                                        q0 = qb * 128
                                        c_lo = r + dil * q0

                                        def kcols(kt, c_lo=c_lo, dil=dil):
                                            b = PAD + c_lo + dil * (-64 + 128 * kt)
                                            return slice(b, b + 127 * dil + 1, dil)
                                        qcols = slice(c_lo, c_lo + 127 * dil + 1, dil)

                                        def valid_of(kt, qb=qb, nsub=nsub):
                                            if hf == 0 and qb == 0 and kt == 0:
                                                return vcol[:, 0:1]
                                            if hf == 1 and qb == nsub // 128 - 1 and kt == 1:
                                                return vcol[:, 1:2]
                                            return ones_col[:, 0:1]

                                        def mask_of(tag, gi=gi):
                                            return maskD[:, 2 * gi + tag, :]

                                        def lhs_of(ve, kt, vslot, tag):
                                            return ve[:, kt, tag, 64:192] if tag == 0 else ve[:, kt, tag, 0:128]

                                        def sink_dst(tag, pu, r_pu, gi=gi, qcols=qcols):
                                            dstv = UACC[:, tag, qcols]
                                            if gi == 0:
                                                fw.op("vector", lambda e: e.tensor_copy(out=dstv, in_=pu[:, 0:128]),
                                                      reads=[r_pu], writes=[r_ua])
                                            else:
                                                fw.op("vector", lambda e: e.tensor_tensor(out=dstv, in0=dstv,
                                                                                          in1=pu[:, 0:128], op=ALU.add),
                                                      reads=[r_pu, r_ua], writes=[r_ua])
                                        unit(2, kcols, qcols, [(0, 0, 0), (64, 1, 1)], mask_of, valid_of, lhs_of, sink_dst)
                            fw.op("vector", lambda e: e.reciprocal(out=RC[0:64, :], in_=UACC[64:128, 0, :]),
                                  reads=[r_ua], writes=[r_rc])
                            fw.op("vector", lambda e: e.reciprocal(out=RC[64:128, :], in_=UACC[0:64, 1, :]),
                                  reads=[r_ua], writes=[r_rc])
                            fw.op("vector", lambda e: e.tensor_tensor(out=brT[0:64, 6, :], in0=UACC[0:64, 0, :],
                                                                      in1=RC[0:64, :], op=ALU.mult),
                                  reads=[r_ua, r_rc], writes=[r_br])
                            fw.op("vector", lambda e: e.tensor_tensor(out=brT[64:128, 6, :], in0=UACC[64:128, 1, :],
                                                                      in1=RC[64:128, :], op=ALU.mult),
                                  reads=[r_ua, r_rc], writes=[r_br])
                            load_kv(3)
                            for jq in range(3):
                                wt, r_w = load_w_chunk([(wi[:, 2688 + 64 * jq:2688 + 64 * jq + 64], 0, 64),
                                                        (wi[:, 2688 + 64 * (jq + 3):2688 + 64 * (jq + 3) + 64], 64, 64)])
                                proj_fm(wt, r_w, q_evac)
                                for qb in range(ST // 128):
                                    q0 = qb * 128

                                    def kcols(kt, q0=q0):
                                        b = PAD + q0 - 128 + 128 * kt
                                        return slice(b, b + 128)
                                    qcols = slice(q0, q0 + 128)

                                    def valid_of(kt, qb=qb):
                                        if hf == 0 and qb == 0 and kt == 0:
                                            return flg[:, sg:sg + 1]
                                        if hf == 1 and qb == ST // 128 - 1 and kt == 2:
                                            return flg[:, sg + 1:sg + 2]
                                        return ones_col[:, 0:1]

                                    def mask_of(tag):
                                        return maskS[:, tag, :]

                                    def lhs_of(ve, kt, vslot, tag):
                                        return ve[:, kt, vslot, 64:192] if tag % 2 == 0 else ve[:, kt, vslot, 0:128]

                                    def sink_dst(tag, pu, r_pu, qcols=qcols):
                                        h = tag
                                        ch, half = 7 + h // 2, h % 2
                                        si = ac["u"] % 2
                                        if half == 0:
                                            urows, drows = slice(0, 64), slice(64, 128)
                                        else:
                                            urows, drows = slice(64, 128), slice(0, 64)
                                        fw.op("vector", lambda e: e.tensor_scalar(
                                            out=sm[si][drows, :], in0=pu[drows, 0:128], scalar1=sexp[drows, h:h + 1],
                                            scalar2=None, op0=ALU.add), reads=[r_pu, r_sexp], writes=[r_sm[si]])
                                        fw.op("vector", lambda e: e.reciprocal(out=sm[si][drows, :], in_=sm[si][drows, :]),
                                              reads=[r_sm[si]], writes=[r_sm[si]])
                                        fw.op("vector", lambda e: e.tensor_tensor(
                                            out=brT[urows, ch, qcols], in0=pu[urows, 0:128], in1=sm[si][drows, :],
                                            op=ALU.mult), reads=[r_pu, r_sm[si]], writes=[r_br])
                                    unit(3, kcols, qcols, [(0, 0, jq), (64, 1, jq + 3)], mask_of, valid_of, lhs_of, sink_dst)
                        fw.barrier()
                        if stop_after == "attn":
                            fw.dma("sync", lambda e, t0=t0: e.dma_start(
                                out=dbg["br"][:, :, t0:t0 + ST].rearrange("c p n -> p c n"), in_=brT[:]), reads=[r_br])
                            fw.barrier()
                            continue
                        with ExitStack() as S2:
                            def sbt(name, shape, dt=F32):
                                return S2.enter_context(nc.sbuf_tensor(uname("b3_" + name), shape, dt))
                            dvec = sbt("dvec", [128, 3])
                            glub = sbt("glub", [128, 3])
                            cwt = sbt("cwt", [128, 3, 3])
                            cbt = sbt("cbt", [128, 3])
                            r_sv = Res("sv")
                            fw.dma("sync", lambda e: e.dma_start(out=dvec[:], in_=W["s5_d"][l].rearrange("(c p) -> p c", p=128),
                                                                 allow_slow_non_contiguous=True), writes=[r_sv])
                            fw.dma("sync", lambda e: e.dma_start(out=glub[:], in_=W["s5_glu_b"][l].rearrange("(c p) -> p c", p=128),
                                                                 allow_slow_non_contiguous=True), writes=[r_sv])
                            fw.dma("sync", lambda e: e.dma_start(out=cwt[:], in_=W["conv_w"][l].rearrange("t (c p) -> p t c", p=128),
                                                                 allow_slow_non_contiguous=True), writes=[r_sv])
                            fw.dma("sync", lambda e: e.dma_start(out=cbt[:], in_=W["conv_b"][l].rearrange("(c p) -> p c", p=128),
                                                                 allow_slow_non_contiguous=True), writes=[r_sv])
                            gluw32 = sbt("gluw32", [128, 3, 384])
                            gluw = sbt("gluw", [128, 3, 384], BF16)
                            r_gluw = Res("gluw")
                            fw.dma("sync", lambda e: e.dma_start(out=gluw32[:], in_=W["s5_glu_w"][l].rearrange("(c p) n -> p c n", p=128)),
                                   writes=[r_gluw])
                            fw.op("gpsimd", lambda e: e.tensor_copy(out=gluw[:], in_=gluw32[:]), reads=[r_gluw], writes=[r_gluw])
                            yf = [sbt("yf%d" % i, [128, 512]) for i in range(2)]
                            yb_ = [sbt("yb%d" % i, [128, 512]) for i in range(2)]
                            uu = [sbt("uu%d" % i, [128, 512]) for i in range(2)]
                            r_y3 = [Res("y3_%d" % i) for i in range(2)]
                            zf32 = sbt("zf32", [128, 3, 512])
                            zb16 = sbt("zb16", [128, 3, 512], BF16)
                            r_z = Res("z")
                            gt = [sbt("gt%d" % i, [128, 512]) for i in range(2)]
                            r_gt = [Res("gt%d" % i) for i in range(2)]
                            kk = 0
                            for ts in range(ST // 512):
                                tk = t0 + ts * 512
                                for c in range(3):
                                    b = kk % 2
                                    kk += 1
                                    fw.dma("sync", lambda e, c=c, tk=tk, b=b: e.dma_start(out=yf[b][:], in_=YA[0, c, :, tk:tk + 512]),
                                           writes=[r_y3[b]])
                                    fw.dma("sync", lambda e, c=c, tk=tk, b=b: e.dma_start(out=yb_[b][:], in_=YA[1, c, :, tk:tk + 512]),
                                           writes=[r_y3[b]])
                                    fw.dma("sync", lambda e, c=c, tk=tk, b=b: e.dma_start(out=uu[b][:], in_=UAs[c, :, tk:tk + 512]),
                                           writes=[r_y3[b]])
                                    fw.op("vector", lambda e, b=b: e.tensor_tensor(out=yf[b][:], in0=yf[b][:], in1=yb_[b][:], op=ALU.add),
                                          reads=[r_y3[b]], writes=[r_y3[b]])
                                    fw.op("vector", lambda e, b=b, c=c: e.scalar_tensor_tensor(
                                        out=yf[b][:], in0=uu[b][:], scalar=dvec[:, c:c + 1], in1=yf[b][:], op0=ALU.mult, op1=ALU.add),
                                        reads=[r_y3[b], r_sv], writes=[r_y3[b]])
                                    fw.op("scalar", lambda e, b=b, c=c: e.activation(out=zf32[:, c, :], in_=yf[b][:], func=AF.Gelu),
                                          reads=[r_y3[b]], writes=[r_z])
                                    fw.op("vector", lambda e, c=c: e.tensor_copy(out=zb16[:, c, :], in_=zf32[:, c, :]),
                                          reads=[r_z], writes=[r_z])
                                for co in range(3):
                                    j = cnt["pA"] % 4
                                    cnt["pA"] += 1
                                    for ci in range(3):
                                        fw.op("tensor", lambda e, ci=ci, co=co, j=j: e.matmul(
                                            pA[j][:], lhsT=gluw[:, ci, co * 128:(co + 1) * 128], rhs=zb16[:, ci, :],
                                            start=(ci == 0), stop=(ci == 2)), reads=[r_gluw, r_z], writes=[r_pA[j]], pe_acc=True)
                                    g2 = co % 2
                                    fw.op("vector", lambda e, j=j, g2=g2, co=co: e.tensor_scalar(
                                        out=gt[g2][:], in0=pA[j][:], scalar1=glub[:, co:co + 1], scalar2=None, op0=ALU.add),
                                        reads=[r_pA[j], r_sv], writes=[r_gt[g2]])
                                    fw.op("scalar", lambda e, g2=g2: e.activation(out=gt[g2][:], in_=gt[g2][:], func=AF.Sigmoid),
                                          reads=[r_gt[g2]], writes=[r_gt[g2]])
                                    fw.op("vector", lambda e, g2=g2, co=co, ts=ts: e.tensor_tensor(
                                        out=brT[:, co, ts * 512:(ts + 1) * 512], in0=zf32[:, co, :], in1=gt[g2][:], op=ALU.mult),
                                        reads=[r_z, r_gt[g2]], writes=[r_br])
                            cvt = sbt("cvt", [128, ST + 2], BF16)
                            r_cvt = Res("cvt")
                            accf = [sbt("accf%d" % i, [128, 512]) for i in range(2)]
                            r_accf = [Res("accf%d" % i) for i in range(2)]
                            for c in range(3):
                                wt, r_w = load_w_chunk([(wi[:, (6 + c) * 128:(7 + c) * 128], 0, 128)])
                                fw.dma("sync", lambda e, c=c, p0=p0: e.dma_start(out=cvt[:], in_=CVs[c, :, p0 - 1:p0 + ST + 1]),
                                       writes=[r_cvt])

                                def ev_gb(ts, j, pt, r_pt, c=c):
                                    a = j % 2
                                    o = ts * 512
                                    fw.op("vector", lambda e: e.tensor_scalar(out=accf[a][:], in0=cvt[:, o:o + 512],
                                                                              scalar1=cwt[:, 0, c:c + 1], scalar2=None, op0=ALU.mult),
                                          reads=[r_cvt, r_sv], writes=[r_accf[a]])
                                    for tap in (1, 2):
                                        fw.op("vector", lambda e, tap=tap: e.scalar_tensor_tensor(
                                            out=accf[a][:], in0=cvt[:, o + tap:o + tap + 512], scalar=cwt[:, tap, c:c + 1],
                                            in1=accf[a][:], op0=ALU.mult, op1=ALU.add),
                                            reads=[r_cvt, r_sv, r_accf[a]], writes=[r_accf[a]])
                                    fw.op("vector", lambda e: e.scalar_tensor_tensor(
                                        out=brT[:, 3 + c, o:o + 512], in0=accf[a][:], scalar=cbt[:, c:c + 1], in1=pt[:],
                                        op0=ALU.add, op1=ALU.mult), reads=[r_accf[a], r_sv, r_pt], writes=[r_br])
                                proj_fm(wt, r_w, ev_gb)
                            if stop_after == "branches":
                                fw.dma("sync", lambda e, t0=t0: e.dma_start(
                                    out=dbg["br"][:, :, t0:t0 + ST].rearrange("c p n -> p c n"), in_=brT[:]), reads=[r_br])
                                fw.barrier()
                                continue
                            wbr32 = [sbt("wbr32_%d" % i, [128, 3, 128]) for i in range(2)]
                            wbr = [sbt("wbr%d" % i, [128, 3, 128], BF16) for i in range(2)]
                            r_wbr = [Res("wbr%d" % i) for i in range(2)]
                            mac = sbt("mac", [128, ST])
                            r_mac = Res("mac")
                            mbf = [sbt("mbf%d" % i, [128, ST], BF16) for i in range(2)]
                            r_mbf = [Res("mbf%d" % i) for i in range(2)]
                            sgt = [sbt("sgt%d" % i, [128, 512]) for i in range(2)]
                            r_sgt = [Res("sgt%d" % i) for i in range(2)]
                            BRS = [("w_branch_a", 3, 0), ("w_branch_b", 3, 3), ("w_branch_c", 1, 6), ("w_branch_d", 3, 7)]
                            q = 0
                            for jo in range(8):
                                for br, (wn, nch, ch0) in enumerate(BRS):
                                    wg, r_wg = load_w_chunk([(wi[:, 3328 + br * 1024 + jo * 128:3328 + br * 1024 + (jo + 1) * 128], 0, 128)])
                                    wb = q % 2
                                    q += 1
                                    fw.dma("sync", lambda e, wn=wn, nch=nch, jo=jo, wb=wb: e.dma_start(
                                        out=wbr32[wb][:, 0:nch, :],
                                        in_=W[wn][l][:, jo * 128:(jo + 1) * 128].rearrange("(c p) n -> p c n", p=128)),
                                        writes=[r_wbr[wb]])
                                    fw.op("gpsimd", lambda e, nch=nch, wb=wb: e.tensor_copy(out=wbr[wb][:, 0:nch, :],
                                                                                          in_=wbr32[wb][:, 0:nch, :]),
                                          reads=[r_wbr[wb]], writes=[r_wbr[wb]])
                                    for ts in range(ST // 512):
                                        j = cnt["pA"] % 4
                                        cnt["pA"] += 1
                                        xk = (q + ts) % 2
                                        o = ts * 512
                                        for k in range(8):
                                            fw.op("tensor", lambda e, k=k, j=j, o=o, wg=wg: e.matmul(
                                                pA[j][:], lhsT=wg[:, k, :], rhs=hT[:, k, o:o + 512], start=(k == 0), stop=(k == 7)),
                                                reads=[r_wg, r_hT], writes=[r_pA[j]], pe_acc=True)
                                        for c in range(nch):
                                            fw.op("tensor", lambda e, c=c, xk=xk, o=o, wb=wb, ch0=ch0, nch=nch: e.matmul(
                                                pX[xk][:], lhsT=wbr[wb][:, c, :], rhs=brT[:, ch0 + c, o:o + 512],
                                                start=(c == 0), stop=(c == nch - 1)),
                                                reads=[r_wbr[wb], r_br], writes=[r_pX[xk]], pe_acc=True)
                                        fw.op("scalar", lambda e, j=j, xk=xk: e.activation(out=sgt[xk][:], in_=pA[j][:], func=AF.Sigmoid),
                                              reads=[r_pA[j]], writes=[r_sgt[xk]])
                                        if br == 0:
                                            fw.op("vector", lambda e, xk=xk, o=o: e.tensor_tensor(
                                                out=mac[:, o:o + 512], in0=sgt[xk][:], in1=pX[xk][:], op=ALU.mult),
                                                reads=[r_sgt[xk], r_pX[xk]], writes=[r_mac])
                                        else:
                                            fw.op("vector", lambda e, xk=xk: e.tensor_tensor(
                                                out=sgt[xk][:], in0=sgt[xk][:], in1=pX[xk][:], op=ALU.mult),
                                                reads=[r_sgt[xk], r_pX[xk]], writes=[r_sgt[xk]])
                                            fw.op("vector", lambda e, xk=xk, o=o: e.tensor_tensor(
                                                out=mac[:, o:o + 512], in0=mac[:, o:o + 512], in1=sgt[xk][:], op=ALU.add),
                                                reads=[r_sgt[xk], r_mac], writes=[r_mac])
                                mi = jo % 2
                                fw.op("scalar", lambda e, mi=mi: e.activation(out=mbf[mi][:], in_=mac[:], func=AF.Copy),
                                      reads=[r_mac], writes=[r_mbf[mi]])
                                fw.dma("sync", lambda e, jo=jo, t0=t0, mi=mi: e.dma_start(out=MTs[jo, :, t0:t0 + ST], in_=mbf[mi][:]),
                                       reads=[r_mbf[mi]])
                    fw.barrier()
                    if stop_after in ("attn", "branches"):
                        continue
                    with ExitStack() as S3:
                        def sbu(name, shape, dt=F32):
                            return S3.enter_context(nc.sbuf_tensor(uname("b4_" + name), shape, dt))
                        fw.dma("sync", lambda e, t0=t0: e.dma_start(
                            out=hT[:], in_=MTs[:, :, t0:t0 + ST].rearrange("k p n -> p k n")), writes=[r_hT])
                        wo32 = [sbu("wo32_%d" % i, [128, 8, 256]) for i in range(2)]
                        r_wo32 = [Res("wo32_%d" % i) for i in range(2)]
                        wo = sbu("wo", [128, 8, D], BF16)
                        r_wo = Res("wo")
                        for pc in range(4):
                            a = pc % 2
                            fw.dma("sync", lambda e, pc=pc, a=a: e.dma_start(
                                out=wo32[a][:], in_=W["w_o"][l][:, pc * 256:(pc + 1) * 256].rearrange("(k p) n -> p k n", p=128)),
                                writes=[r_wo32[a]])
                            fw.op("gpsimd", lambda e, pc=pc, a=a: e.tensor_copy(out=wo[:, :, pc * 256:(pc + 1) * 256], in_=wo32[a][:]),
                                  reads=[r_wo32[a]], writes=[r_wo])
                        load_gb("ln1_g", "ln1_b", l)
                        xb = [sbu("xb%d" % i, [128, D]) for i in range(2)]
                        r_xb = [Res("xb%d" % i) for i in range(2)]
                        tb = [sbu("tb%d" % i, [128, D]) for i in range(2)]
                        r_tb = [Res("tb%d" % i) for i in range(2)]
                        hb16 = [sbu("hb16_%d" % i, [128, D], BF16) for i in range(2)]
                        r_hb16 = [Res("hb16_%d" % i) for i in range(2)]
                        st4 = [sbu("st4_%d" % i, [128, 8]) for i in range(2)]
                        r_st4 = [Res("st4_%d" % i) for i in range(2)]
                        mo = [sbu("mo%d" % i, [128, D]) for i in range(2)]
                        r_mo = [Res("mo%d" % i) for i in range(2)]
                        tkb = [sbu("tkb%d" % i, [128, 8, 128], BF16) for i in range(2)]
                        r_tkb = [Res("tkb%d" % i) for i in range(2)]
                        for blk in range(ST // 128):
                            i = blk % 2
                            r0 = t0 + blk * 128
                            fw.dma("sync", lambda e, i=i, r0=r0: e.dma_start(out=xb[i][:], in_=H0[r0:r0 + 128, :]), writes=[r_xb[i]])
                            for nh in range(2):
                                j = cnt["pA"] % 4
                                cnt["pA"] += 1
                                for k in range(8):
                                    fw.op("tensor", lambda e, k=k, j=j, nh=nh, blk=blk: e.matmul(
                                        pA[j][:], lhsT=hT[:, k, blk * 128:(blk + 1) * 128], rhs=wo[:, k, nh * 512:(nh + 1) * 512],
                                        start=(k == 0), stop=(k == 7)), reads=[r_hT, r_wo], writes=[r_pA[j]], pe_acc=True)
                                fw.op("scalar", lambda e, i=i, j=j, nh=nh: e.activation(
                                    out=mo[i][:, nh * 512:(nh + 1) * 512], in_=pA[j][:], func=AF.Copy),
                                    reads=[r_pA[j]], writes=[r_mo[i]])
                            if debug:
                                fw.dma("sync", lambda e, i=i, r0=r0: e.dma_start(out=dbg["mix"][r0:r0 + 128, :], in_=mo[i][:]),
                                       reads=[r_mo[i]])
                            fw.op("vector", lambda e, i=i: e.scalar_tensor_tensor(
                                out=xb[i][:], in0=xb[i][:], scalar=ALPHA, in1=mo[i][:], op0=ALU.mult, op1=ALU.add),
                                reads=[r_xb[i], r_mo[i]], writes=[r_xb[i]])
                            ln_rows(fw, xb[i], r_xb[i], tb[i], r_tb[i], st4[i], r_st4[i], gam, bet, r_gb, xb[i], r_xb[i])
                            if debug:
                                fw.dma("sync", lambda e, i=i, r0=r0: e.dma_start(out=dbg["st"][r0:r0 + 128, :], in_=st4[i][:]),
                                       reads=[r_st4[i]])
                            fw.dma("sync", lambda e, i=i, r0=r0: e.dma_start(out=H1[r0:r0 + 128, :], in_=xb[i][:]), reads=[r_xb[i]])
                            fw.op("scalar", lambda e, i=i: e.activation(out=hb16[i][:], in_=xb[i][:], func=AF.Copy),
                                  reads=[r_xb[i]], writes=[r_hb16[i]])
                            for k in range(8):
                                fw.op("tensor", lambda e, k=k, i=i: e.transpose(pT[i][:, k, :], hb16[i][:, k * 128:(k + 1) * 128], ident[:]),
                                      reads=[r_hb16[i], r_ident], writes=[r_pT[i]], pe_acc=True)
                            fw.op("vector", lambda e, i=i: e.tensor_copy(out=tkb[i][:], in_=pT[i][:]), reads=[r_pT[i]], writes=[r_tkb[i]])
                            fw.dma("sync", lambda e, i=i, r0=r0: e.dma_start(
                                out=HT[:, :, r0:r0 + 128].rearrange("k p n -> p k n"), in_=tkb[i][:]), reads=[r_tkb[i]])
                    fw.barrier()
                    if stop_after == "ln1":
                        continue
                    with ExitStack() as S4:
                        def sbv(name, shape, dt=F32):
                            return S4.enter_context(nc.sbuf_tensor(uname("b5_" + name), shape, dt))
                        fw.dma("sync", lambda e, t0=t0: e.dma_start(
                            out=hT[:], in_=HT[:, :, t0:t0 + ST].rearrange("k p n -> p k n")), writes=[r_hT])
                        wr32 = sbv("wr32", [128, 8, 20])
                        wr = sbv("wr", [128, 8, 20], BF16)
                        r_wr = Res("wr")
                        fw.dma("sync", lambda e: e.dma_start(out=wr32[:, :, 0:4], in_=W["router_group_w"][l].rearrange("(k p) n -> p k n", p=128),
                                                             allow_slow_non_contiguous=True), writes=[r_wr])
                        fw.dma("sync", lambda e: e.dma_start(out=wr32[:, :, 4:20], in_=W["router_expert_w"][l].rearrange("(k p) n -> p k n", p=128),
                                                             allow_slow_non_contiguous=True), writes=[r_wr])
                        fw.op("vector", lambda e: e.tensor_copy(out=wr[:], in_=wr32[:]), reads=[r_wr], writes=[r_wr])
                        rb = sbv("rb", [128, 20])
                        r_rb = Res("rb")
                        fw.dma("sync", lambda e: e.dma_start(out=rb[:, 0:4], in_=W["router_group_b"][l].partition_broadcast(128)), writes=[r_rb])
                        fw.dma("sync", lambda e: e.dma_start(out=rb[:, 4:20], in_=W["router_expert_b"][l].partition_broadcast(128)), writes=[r_rb])
                        comb = sbv("comb", [128, ST // 128, 16])
                        r_comb = Res("comb")
                        rt = sbv("rt", [128, 64])
                        r_rt = Res("rt")
                        for blk in range(ST // 128):
                            xk = blk % 2
                            for k in range(8):
                                fw.op("tensor", lambda e, k=k, xk=xk, blk=blk: e.matmul(
                                    pX[xk][:, 0:20], lhsT=hT[:, k, blk * 128:(blk + 1) * 128], rhs=wr[:, k, :],
                                    start=(k == 0), stop=(k == 7)), reads=[r_hT, r_wr], writes=[r_pX[xk]], pe_acc=True)
                            lg = rt[:, 0:20]

                            def V(fn, extra_r=()):
                                fw.op("vector", fn, reads=[r_rt] + list(extra_r), writes=[r_rt])
                            fw.op("vector", lambda e, xk=xk: e.tensor_tensor(out=rt[:, 0:20], in0=pX[xk][:, 0:20], in1=rb[:], op=ALU.add),
                                  reads=[r_pX[xk], r_rb], writes=[r_rt])
                            V(lambda e: e.reduce_max(out=rt[:, 20:21], in_=rt[:, 0:4], axis=AX.X))
                            V(lambda e: e.tensor_scalar(out=rt[:, 24:28], in0=rt[:, 0:4], scalar1=rt[:, 20:21], scalar2=None, op0=ALU.subtract))
                            fw.op("scalar", lambda e: e.activation(out=rt[:, 28:32], in_=rt[:, 24:28], func=AF.Exp), reads=[r_rt], writes=[r_rt])
                            V(lambda e: e.reduce_sum(out=rt[:, 21:22], in_=rt[:, 28:32], axis=AX.X))
                            V(lambda e: e.reciprocal(out=rt[:, 21:22], in_=rt[:, 21:22]))
                            V(lambda e: e.tensor_scalar(out=rt[:, 24:28], in0=rt[:, 24:28], scalar1=-1e30, scalar2=1.0, op0=ALU.mult, op1=ALU.min))
                            V(lambda e: e.tensor_scalar(out=rt[:, 24:28], in0=rt[:, 24:28], scalar1=-1.0, scalar2=1.0, op0=ALU.mult, op1=ALU.add))
                            V(lambda e: e.tensor_scalar(out=rt[:, 32:36], in0=rt[:, 4:8], scalar1=rt[:, 24:25], scalar2=None, op0=ALU.mult))
                            for gq in range(1, 4):
                                V(lambda e, gq=gq: e.scalar_tensor_tensor(out=rt[:, 32:36], in0=rt[:, 4 + 4 * gq:8 + 4 * gq],
                                                                          scalar=rt[:, 24 + gq:25 + gq], in1=rt[:, 32:36],
                                                                          op0=ALU.mult, op1=ALU.add))
                            V(lambda e: e.reduce_max(out=rt[:, 22:23], in_=rt[:, 32:36], axis=AX.X))
                            V(lambda e: e.tensor_scalar(out=rt[:, 36:40], in0=rt[:, 32:36], scalar1=rt[:, 22:23], scalar2=None, op0=ALU.subtract))
                            V(lambda e: e.tensor_scalar(out=rt[:, 36:40], in0=rt[:, 36:40], scalar1=-1e30, scalar2=1.0, op0=ALU.mult, op1=ALU.min))
                            V(lambda e: e.tensor_scalar(out=rt[:, 36:40], in0=rt[:, 36:40], scalar1=-1.0, scalar2=1.0, op0=ALU.mult, op1=ALU.add))
                            V(lambda e: e.scalar_tensor_tensor(out=rt[:, 40:44], in0=rt[:, 36:40], scalar=-1e4, in1=rt[:, 32:36],
                                                               op0=ALU.mult, op1=ALU.add))
                            V(lambda e: e.reduce_max(out=rt[:, 23:24], in_=rt[:, 40:44], axis=AX.X))
                            V(lambda e: e.tensor_scalar(out=rt[:, 44:48], in0=rt[:, 40:44], scalar1=rt[:, 23:24], scalar2=None, op0=ALU.subtract))
                            V(lambda e: e.tensor_scalar(out=rt[:, 44:48], in0=rt[:, 44:48], scalar1=-1e30, scalar2=1.0, op0=ALU.mult, op1=ALU.min))
                            V(lambda e: e.tensor_scalar(out=rt[:, 44:48], in0=rt[:, 44:48], scalar1=-1.0, scalar2=1.0, op0=ALU.mult, op1=ALU.add))
                            V(lambda e: e.tensor_tensor(out=rt[:, 48:49], in0=rt[:, 23:24], in1=rt[:, 22:23], op=ALU.subtract))
                            fw.op("scalar", lambda e: e.activation(out=rt[:, 49:50], in_=rt[:, 48:49], func=AF.Exp), reads=[r_rt], writes=[r_rt])
                            V(lambda e: e.tensor_scalar(out=rt[:, 50:51], in0=rt[:, 49:50], scalar1=1.0, scalar2=None, op0=ALU.add))
                            V(lambda e: e.reciprocal(out=rt[:, 50:51], in_=rt[:, 50:51]))
                            V(lambda e: e.tensor_tensor(out=rt[:, 51:52], in0=rt[:, 49:50], in1=rt[:, 50:51], op=ALU.mult))
                            V(lambda e: e.tensor_tensor(out=rt[:, 50:51], in0=rt[:, 50:51], in1=rt[:, 21:22], op=ALU.mult))
                            V(lambda e: e.tensor_tensor(out=rt[:, 51:52], in0=rt[:, 51:52], in1=rt[:, 21:22], op=ALU.mult))
                            V(lambda e: e.tensor_scalar(out=rt[:, 52:56], in0=rt[:, 36:40], scalar1=rt[:, 50:51], scalar2=None, op0=ALU.mult))
                            V(lambda e: e.scalar_tensor_tensor(out=rt[:, 52:56], in0=rt[:, 44:48], scalar=rt[:, 51:52], in1=rt[:, 52:56],
                                                               op0=ALU.mult, op1=ALU.add))
                            for gq in range(4):
                                fw.op("vector", lambda e, gq=gq, blk=blk: e.tensor_scalar(
                                    out=comb[:, blk, 4 * gq:4 * gq + 4], in0=rt[:, 52:56], scalar1=rt[:, 24 + gq:25 + gq], scalar2=None,
                                    op0=ALU.mult), reads=[r_rt], writes=[r_comb])
                        wg32 = sbv("wg32", [128, 8, 256])
                        wu32 = sbv("wu32", [128, 8, 256])
                        wgb = [sbv("wgb%d" % i, [128, 8, 256], BF16) for i in range(2)]
                        wub = [sbv("wub%d" % i, [128, 8, 256], BF16) for i in range(2)]
                        wd32 = sbv("wd32", [128, 2, D])
                        wdb = [sbv("wdb%d" % i, [128, 2, D], BF16) for i in range(2)]
                        r_wg32, r_wu32, r_wd32 = Res("wg32"), Res("wu32"), Res("wd32")
                        r_wgb = [Res("wgb%d" % i) for i in range(2)]
                        r_wub = [Res("wub%d" % i) for i in range(2)]
                        r_wdb = [Res("wdb%d" % i) for i in range(2)]
                        HB = ST // 128
                        macc = sbv("macc", [128, HB, D])
                        r_macc = Res("macc")
                        actT = sbv("actT", [128, 2, ST], BF16)
                        r_act = Res("actT")
                        sgl = [sbv("sgl%d" % i, [128, 512]) for i in range(2)]
                        r_sgl = [Res("sgl%d" % i) for i in range(2)]
                        xq = [sbv("xq%d" % i, [128, D]) for i in range(2)]
                        r_xq = [Res("xq%d" % i) for i in range(2)]
                        tq = [sbv("tq%d" % i, [128, D]) for i in range(2)]
                        r_tq = [Res("tq%d" % i) for i in range(2)]
                        sq4 = [sbv("sq4_%d" % i, [128, 8]) for i in range(2)]
                        r_sq4 = [Res("sq4_%d" % i) for i in range(2)]
                        load_gb("ln2_g", "ln2_b", l)
                        for hv in range(1):
                            c0 = 0
                            for ex in range(16):
                                eb = ex % 2
                                fw.dma("sync", lambda e, ex=ex: e.dma_start(
                                    out=wg32[:], in_=W["expert_w_gate"][l, ex].rearrange("(k p) n -> p k n", p=128)), writes=[r_wg32])
                                fw.op("gpsimd", lambda e, eb=eb: e.tensor_copy(out=wgb[eb][:], in_=wg32[:]), reads=[r_wg32], writes=[r_wgb[eb]])
                                fw.dma("sync", lambda e, ex=ex: e.dma_start(
                                    out=wu32[:], in_=W["expert_w_up"][l, ex].rearrange("(k p) n -> p k n", p=128)), writes=[r_wu32])
                                fw.op("gpsimd", lambda e, eb=eb: e.tensor_copy(out=wub[eb][:], in_=wu32[:]), reads=[r_wu32], writes=[r_wub[eb]])
                                fw.dma("sync", lambda e, ex=ex: e.dma_start(
                                    out=wd32[:], in_=W["expert_w_down"][l, ex].rearrange("(c p) n -> p c n", p=128)), writes=[r_wd32])
                                fw.op("gpsimd", lambda e, eb=eb: e.tensor_copy(out=wdb[eb][:], in_=wd32[:]), reads=[r_wd32], writes=[r_wdb[eb]])
                                for c in range(2):
                                    for ts in range(ST // 512):
                                        o = c0 + ts * 512
                                        j = cnt["pA"] % 4
                                        cnt["pA"] += 1
                                        j2 = cnt["pA"] % 4
                                        cnt["pA"] += 1
                                        for k in range(8):
                                            fw.op("tensor", lambda e, k=k, j=j, c=c, o=o, eb=eb: e.matmul(
                                                pA[j][:], lhsT=wgb[eb][:, k, c * 128:(c + 1) * 128], rhs=hT[:, k, o:o + 512],
                                                start=(k == 0), stop=(k == 7)), reads=[r_wgb[eb], r_hT], writes=[r_pA[j]], pe_acc=True)
                                        for k in range(8):
                                            fw.op("tensor", lambda e, k=k, j2=j2, c=c, o=o, eb=eb: e.matmul(
                                                pA[j2][:], lhsT=wub[eb][:, k, c * 128:(c + 1) * 128], rhs=hT[:, k, o:o + 512],
                                                start=(k == 0), stop=(k == 7)), reads=[r_wub[eb], r_hT], writes=[r_pA[j2]], pe_acc=True)
                                        si = (c + ts) % 2
                                        fw.op("scalar", lambda e, j=j, si=si: e.activation(out=sgl[si][:], in_=pA[j][:], func=AF.Silu),
                                              reads=[r_pA[j]], writes=[r_sgl[si]])
                                        fw.op("vector", lambda e, j2=j2, si=si, c=c, ts=ts: e.tensor_tensor(
                                            out=actT[:, c, ts * 512:(ts + 1) * 512], in0=sgl[si][:], in1=pA[j2][:], op=ALU.mult),
                                            reads=[r_sgl[si], r_pA[j2]], writes=[r_act])
                                for bl in range(HB):
                                    blk = hv * HB + bl
                                    for nh in range(2):
                                        xk = (bl * 2 + nh) % 2
                                        for c in range(2):
                                            fw.op("tensor", lambda e, c=c, xk=xk, bl=bl, nh=nh, eb=eb: e.matmul(
                                                pX[xk][:], lhsT=actT[:, c, bl * 128:(bl + 1) * 128], rhs=wdb[eb][:, c, nh * 512:(nh + 1) * 512],
                                                start=(c == 0), stop=(c == 1)), reads=[r_act, r_wdb[eb]], writes=[r_pX[xk]], pe_acc=True)
                                        if ex == 0:
                                            fw.op("vector", lambda e, xk=xk, bl=bl, nh=nh, blk=blk, ex=ex: e.tensor_scalar(
                                                out=macc[:, bl, nh * 512:(nh + 1) * 512], in0=pX[xk][:], scalar1=comb[:, blk, ex:ex + 1],
                                                scalar2=None, op0=ALU.mult), reads=[r_pX[xk], r_comb], writes=[r_macc])
                                        else:
                                            fw.op("vector", lambda e, xk=xk, bl=bl, nh=nh, blk=blk, ex=ex: e.scalar_tensor_tensor(
                                                out=macc[:, bl, nh * 512:(nh + 1) * 512], in0=pX[xk][:], scalar=comb[:, blk, ex:ex + 1],
                                                in1=macc[:, bl, nh * 512:(nh + 1) * 512], op0=ALU.mult, op1=ALU.add),
                                                reads=[r_pX[xk], r_comb, r_macc], writes=[r_macc])
                            dst = y if last else H0
                            for bl in range(HB):
                                i = bl % 2
                                r0 = t0 + (hv * HB + bl) * 128
                                fw.dma("sync", lambda e, i=i, r0=r0: e.dma_start(out=xq[i][:], in_=H1[r0:r0 + 128, :]), writes=[r_xq[i]])
                                fw.op("vector", lambda e, i=i, bl=bl: e.scalar_tensor_tensor(
                                    out=xq[i][:], in0=xq[i][:], scalar=ALPHA, in1=macc[:, bl, :], op0=ALU.mult, op1=ALU.add),
                                    reads=[r_xq[i], r_macc], writes=[r_xq[i]])
                                ln_rows(fw, xq[i], r_xq[i], tq[i], r_tq[i], sq4[i], r_sq4[i], gam, bet, r_gb, xq[i], r_xq[i])
                                fw.dma("sync", lambda e, i=i, r0=r0, dst=dst: e.dma_start(out=dst[r0:r0 + 128, :], in_=xq[i][:]),
                                       reads=[r_xq[i]])
                    fw.barrier()
            fw.barrier()

        if debug:
            dbg["br"] = nc.dram_tensor("dbg_br", [10, 128, NTOK], BF16, kind="ExternalOutput").ap()
        for l in range(depth):
            phase_a(l)
            if stop_after in ("a", "ua"):
                break
            phase_s5(l)
            if stop_after == "s5":
                break
            phase_h()
            phase_b(l, l == depth - 1)
            if stop_after in ("attn", "branches", "ln1"):
                break
    return nc, fw


def ln_rows(fw, xin, r_xin, tmp, r_tmp, s, r_s, gam, bet, r_gb, out_tile, r_out):
    fw.op("vector", lambda e: e.reduce_sum(out=s[:, 0:1], in_=xin[:], axis=AX.X), reads=[r_xin], writes=[r_s])
    fw.op("scalar", lambda e: e.activation(out=tmp[:], in_=xin[:], func=AF.Square), reads=[r_xin, r_s], writes=[r_tmp])
    fw.op("vector", lambda e: e.reduce_sum(out=s[:, 1:2], in_=tmp[:], axis=AX.X), reads=[r_tmp], writes=[r_s])
    fw.op("vector", lambda e: e.tensor_scalar(out=s[:, 2:3], in0=s[:, 0:1], scalar1=1.0 / D, scalar2=None,
                                              op0=ALU.mult), reads=[r_s], writes=[r_s])
    fw.op("vector", lambda e: e.tensor_tensor(out=s[:, 3:4], in0=s[:, 2:3], in1=s[:, 2:3], op=ALU.mult),
          reads=[r_s], writes=[r_s])
    fw.op("vector", lambda e: e.scalar_tensor_tensor(out=s[:, 4:5], in0=s[:, 1:2], scalar=1.0 / D, in1=s[:, 3:4],
                                                     op0=ALU.mult, op1=ALU.subtract), reads=[r_s], writes=[r_s])
    fw.op("vector", lambda e: e.tensor_scalar(out=s[:, 4:5], in0=s[:, 4:5], scalar1=LN_EPS, scalar2=None,
                                              op0=ALU.add), reads=[r_s], writes=[r_s])
    fw.op("scalar", lambda e: e.activation(out=s[:, 5:6], in_=s[:, 4:5], func=AF.Sqrt), reads=[r_s], writes=[r_s])
    fw.op("vector", lambda e: e.reciprocal(out=s[:, 6:7], in_=s[:, 5:6]), reads=[r_s], writes=[r_s])
    fw.op("vector", lambda e: e.scalar_tensor_tensor(out=s[:, 7:8], in0=s[:, 2:3], scalar=-1.0, in1=s[:, 6:7],
                                                     op0=ALU.mult, op1=ALU.mult), reads=[r_s], writes=[r_s])
    fw.op("vector", lambda e: e.tensor_scalar(out=tmp[:], in0=xin[:], scalar1=s[:, 2:3], scalar2=s[:, 6:7],
                                              op0=ALU.subtract, op1=ALU.mult), reads=[r_xin, r_s], writes=[r_tmp])
    fw.op("vector", lambda e: e.tensor_tensor(out=tmp[:], in0=tmp[:], in1=gam[:], op=ALU.mult),
          reads=[r_tmp, r_gb], writes=[r_tmp])
    fw.op("vector", lambda e: e.tensor_tensor(out=out_tile[:], in0=tmp[:], in1=bet[:], op=ALU.add),
          reads=[r_tmp, r_gb], writes=[r_out])


_CACHE = {}


def kernel(**inputs):
    xp = np.ascontiguousarray(inputs["x_prompt"], dtype=np.float32)
    xs = np.ascontiguousarray(inputs["x_sample"], dtype=np.float32)
    slots = {0: [("s", 0)], 1: [("s", 1)], 2: [("p", 0), ("p", 1)], 3: [("p", 2), ("p", 3)],
             4: [("p", 4)], 5: [("p", 5)], 6: [("p", 6)], 7: [("p", 7)]}
    in_maps = []
    for c in range(8):
        xc = np.zeros((NTOK, D), np.float32)
        fl = np.zeros(5, np.float32)
        if slots[c][0][0] == "s":
            xc[:] = xs[slots[c][0][1]]
            fl[1:4] = 1.0
        else:
            for j, (_, pi) in enumerate(slots[c]):
                xc[j * SEG:(j + 1) * SEG] = xp[pi]
        m = {"x": xc, "flags": fl}
        for n in WNAMES:
            m[n] = np.ascontiguousarray(inputs[n], dtype=np.float32)
        in_maps.append(m)
    if "nc" not in _CACHE:
        nc, fw = build()
        fw.finish(_CACHE.get("final", []))
        _CACHE["nc"] = nc
    res = run_bass_kernel_spmd(_CACHE["nc"], in_maps, core_ids=list(range(8)))
    yp = np.zeros_like(xp)
    ys = np.zeros_like(xs)
    for c in range(8):
        yc = np.asarray(res.results[c]["y"], dtype=np.float32)
        if slots[c][0][0] == "s":
            ys[slots[c][0][1]] = yc
        else:
            for j, (_, pi) in enumerate(slots[c]):
                yp[pi] = yc[j * SEG:(j + 1) * SEG]
    return (yp, ys)
```
